# Optimizing a Trainium2 kernel written in Bass

```python
import math
import jax, jax.numpy as jnp
from jax import lax
import numpy as np

D_MODEL = 1024
BATCH = 4
SEQ = 4096
DEPTH = 1

DN_HEADS = 8
DN_HEAD_DIM = 128
DN_WIDTH = DN_HEADS * DN_HEAD_DIM
CONV_K = 5
CHUNK = 64
MLA_HEADS = 8
Q_LORA = 512
KV_LORA = 256
NOPE_DIM = 128
ROPE_DIM = 64
V_DIM = 128
QK_DIM = NOPE_DIM + ROPE_DIM
MLA_WIDTH = MLA_HEADS * V_DIM
ROPE_THETA = 10000.0
Q_BLOCK = 128
N_EXPERTS = 16
CAPACITY_FACTOR = 2
D_EXPERT = 1024
N_BRANCHES = 2
N_MOD = 6
EPS = 1e-6

IN_SPLITS = (3 * DN_WIDTH,
             DN_WIDTH,
             2 * DN_HEADS,
             2 * DN_HEADS,
             Q_LORA,
             KV_LORA,
             ROPE_DIM,
             N_BRANCHES * D_MODEL)
N_IN = sum(IN_SPLITS)

kernel_name = 'hybrid_gdn_mla_ec_block'


def rms_norm(x, g):
    xf = x.astype(jnp.float32)
    y = xf * lax.rsqrt(jnp.mean(xf * xf, axis=-1, keepdims=True) + EPS)
    return (y * g.astype(jnp.float32)).astype(x.dtype)


def l2_norm(x):
    return x * lax.rsqrt(jnp.sum(x * x, axis=-1, keepdims=True) + EPS)


def apply_rope(x, cos, sin):
    x1, x2 = jnp.split(x.astype(jnp.float32), 2, axis=-1)
    return jnp.concatenate([x1 * cos - x2 * sin, x2 * cos + x1 * sin], axis=-1).astype(x.dtype)


def centred_depthwise_conv(x, w):
    ch = x.shape[-1]
    pad = (CONV_K - 1) // 2
    return lax.conv_general_dilated(x, w[:, None, :].astype(x.dtype), window_strides=(1,),
                                    padding=[(pad, pad)], dimension_numbers=('NWC', 'WIO', 'NWC'),
                                    feature_group_count=ch)


def chunk_gated_delta_rule(q, k, v, g, beta):
    b, h, s, dk = k.shape
    dv = v.shape[-1]
    nc = s // CHUNK
    q = q * (dk ** -0.5)
    qc = q.reshape(b, h, nc, CHUNK, dk)
    kc = k.reshape(b, h, nc, CHUNK, dk)
    vc = v.reshape(b, h, nc, CHUNK, dv)
    bc = beta.reshape(b, h, nc, CHUNK)
    gc = jnp.cumsum(g.reshape(b, h, nc, CHUNK), axis=-1)
    tril = jnp.tril(jnp.ones((CHUNK, CHUNK), dtype=bool))
    strict = jnp.tril(jnp.ones((CHUNK, CHUNK), dtype=bool), -1)
    diff = gc[..., :, None] - gc[..., None, :]
    decay = jnp.where(tril, jnp.exp(jnp.where(tril, diff, 0.0)), 0.0)
    k_beta = kc * bc[..., None]
    v_beta = vc * bc[..., None]
    lower = jnp.where(strict, jnp.einsum('bhnik,bhnjk->bhnij', k_beta, kc) * decay, 0.0)
    eye = jnp.eye(CHUNK, dtype=jnp.float32)
    t_inv = lax.linalg.triangular_solve(eye + lower, jnp.broadcast_to(eye, lower.shape),
                                        left_side=True, lower=True, unit_diagonal=True)
    u = t_inv @ v_beta
    w = t_inv @ (k_beta * jnp.exp(gc)[..., None])
    intra = jnp.where(tril, jnp.einsum('bhnik,bhnjk->bhnij', qc, kc) * decay, 0.0)
    q_dec = qc * jnp.exp(gc)[..., None]
    g_last = gc[..., -1]
    k_state = kc * jnp.exp(g_last[..., None] - gc)[..., None]

    def step(state, xs):
        w_i, u_i, intra_i, qd_i, ks_i, gl_i = xs
        v_new = u_i - w_i @ state
        o_i = qd_i @ state + intra_i @ v_new
        state = state * jnp.exp(gl_i)[..., None, None] + jnp.swapaxes(ks_i, -1, -2) @ v_new
        return state, o_i

    xs = tuple(jnp.moveaxis(t, 2, 0) for t in (w, u, intra, q_dec, k_state, g_last))
    state0 = jnp.zeros((b, h, dk, dv), jnp.float32)
    _, o = lax.scan(step, state0, xs)
    return jnp.moveaxis(o, 0, 2).reshape(b, h, s, dv)


def gated_deltanet_branch(qkv, z, b_in, a_in, conv_w, a_log, dt_bias, o_gain):
    bsz, s, _ = qkv.shape
    qkv = jax.nn.silu(centred_depthwise_conv(qkv, conv_w))
    q, k, v = jnp.split(qkv, 3, axis=-1)

    def heads(t):
        return t.reshape(bsz, s, DN_HEADS, DN_HEAD_DIM).transpose(0, 2, 1, 3).astype(jnp.float32)

    q, k, v = l2_norm(heads(q)), l2_norm(heads(k)), heads(v)
    beta = jax.nn.sigmoid(b_in.astype(jnp.float32)).reshape(bsz, s, 2, DN_HEADS).transpose(2, 0, 3, 1)
    a = a_in.astype(jnp.float32).reshape(bsz, s, 2, DN_HEADS)
    g = -jnp.exp(a_log.astype(jnp.float32)) * jax.nn.softplus(a + dt_bias.astype(jnp.float32))
    g = g.transpose(2, 0, 3, 1)
    o_fwd = chunk_gated_delta_rule(q, k, v, g[0], beta[0])
    flip = lambda t: jnp.flip(t, axis=2)
    o_bwd = flip(chunk_gated_delta_rule(flip(q), flip(k), flip(v), flip(g[1]), flip(beta[1])))
    o = (o_fwd + o_bwd).transpose(0, 2, 1, 3)
    zf = z.astype(jnp.float32).reshape(bsz, s, DN_HEADS, DN_HEAD_DIM)
    o = rms_norm(o, o_gain) * jax.nn.silu(zf)
    return o.reshape(bsz, s, DN_WIDTH).astype(qkv.dtype)


def mla_branch(c_q, c_kv, k_r, q_gain, w_uq, kv_gain, w_ukv, cos, sin):
    bsz, s, _ = c_q.shape
    q = (rms_norm(c_q, q_gain) @ w_uq).reshape(bsz, s, MLA_HEADS, QK_DIM)
    q = jnp.concatenate([q[..., :NOPE_DIM],
                         apply_rope(q[..., NOPE_DIM:], cos[:, :, None], sin[:, :, None])], axis=-1)
    kv = (rms_norm(c_kv, kv_gain) @ w_ukv).reshape(bsz, s, MLA_HEADS, NOPE_DIM + V_DIM)
    k_nope, v = kv[..., :NOPE_DIM], kv[..., NOPE_DIM:]
    k_rope = apply_rope(k_r, cos, sin)
    k = jnp.concatenate([k_nope, jnp.broadcast_to(k_rope[:, :, None], (bsz, s, MLA_HEADS, ROPE_DIM))], axis=-1)
    qb = (q * (QK_DIM ** -0.5)).reshape(bsz, s // Q_BLOCK, Q_BLOCK, MLA_HEADS, QK_DIM).transpose(1, 0, 2, 3, 4)

    def attend(q_blk):
        scores = jnp.einsum('bqhd,bkhd->bhqk', q_blk, k, preferred_element_type=jnp.float32)
        p = jax.nn.softmax(scores, axis=-1)
        return jnp.einsum('bhqk,bkhd->bqhd', p.astype(v.dtype), v)

    o = lax.map(attend, qb)
    return o.transpose(1, 0, 2, 3, 4).reshape(bsz, s, MLA_WIDTH)


def expert_choice_ffn(h, w_router, w_gate, w_up, w_down):
    bsz, s, d = h.shape
    cap = CAPACITY_FACTOR * s // N_EXPERTS
    aff = jax.nn.softmax((h @ w_router).astype(jnp.float32), axis=-1)
    gate, idx = lax.top_k(aff.transpose(0, 2, 1), cap)
    idx_flat = idx.reshape(bsz, N_EXPERTS * cap)
    xe = jnp.take_along_axis(h, idx_flat[..., None], axis=1).reshape(bsz, N_EXPERTS, cap, d)
    hid = jax.nn.silu(jnp.einsum('becd,edf->becf', xe, w_gate)) * jnp.einsum('becd,edf->becf', xe, w_up)
    ye = jnp.einsum('becf,efd->becd', hid, w_down) * gate[..., None].astype(h.dtype)
    out = jnp.zeros_like(h).at[jnp.arange(bsz)[:, None], idx_flat].add(ye.reshape(bsz, N_EXPERTS * cap, d))
    return out


def setup_inputs(seed: int = 0) -> dict:
    key = jax.random.key(seed)
    ks = jax.random.split(key, 24)
    f32 = jnp.float32

    def nrm(k, shape, fan_in):
        return jax.random.normal(k, shape, f32) * (fan_in ** -0.5)

    def gain(k, shape):
        return 1.0 + 0.05 * jax.random.normal(k, shape, f32)

    x = jax.random.normal(ks[0], (BATCH, SEQ, D_MODEL), f32)
    c = jax.random.normal(ks[1], (BATCH, D_MODEL), f32)
    positions = (jnp.cumsum(jax.random.randint(ks[2], (BATCH, SEQ), 1, 3), axis=1) - 1).astype(jnp.int32)
    w_mod = 0.5 * nrm(ks[3], (DEPTH, D_MODEL, N_MOD * D_MODEL), D_MODEL)
    b_mod = 0.02 * jax.random.normal(ks[4], (DEPTH, N_MOD * D_MODEL), f32)
    g_mix = gain(ks[5], (DEPTH, D_MODEL))
    w_in = nrm(ks[6], (DEPTH, D_MODEL, N_IN), D_MODEL)
    conv_w = nrm(ks[7], (DEPTH, CONV_K, 3 * DN_WIDTH), CONV_K)
    a_log = jnp.log(jax.random.uniform(ks[8], (DEPTH, 2, DN_HEADS), f32, 1.0, 16.0))
    dt = jnp.exp(jax.random.uniform(ks[9], (DEPTH, 2, DN_HEADS), f32, math.log(1e-3), math.log(1e-1)))
    dt_bias = dt + jnp.log(-jnp.expm1(-dt))
    dn_o_gain = gain(ks[10], (DEPTH, DN_HEAD_DIM))
    q_gain = gain(ks[11], (DEPTH, Q_LORA))
    w_uq = nrm(ks[12], (DEPTH, Q_LORA, MLA_HEADS * QK_DIM), Q_LORA)
    kv_gain = gain(ks[13], (DEPTH, KV_LORA))
    w_ukv = nrm(ks[14], (DEPTH, KV_LORA, MLA_HEADS * (NOPE_DIM + V_DIM)), KV_LORA)
    w_o_dn = nrm(ks[15], (DEPTH, DN_WIDTH, D_MODEL), DN_WIDTH)
    w_o_mla = nrm(ks[16], (DEPTH, MLA_WIDTH, D_MODEL), MLA_WIDTH)
    w_out = nrm(ks[17], (DEPTH, D_MODEL, D_MODEL), D_MODEL)
    g_ffn = gain(ks[18], (DEPTH, D_MODEL))
    w_router = nrm(ks[19], (DEPTH, D_MODEL, N_EXPERTS), D_MODEL)
    w_gate = nrm(ks[20], (DEPTH, N_EXPERTS, D_MODEL, D_EXPERT), D_MODEL)
    w_up = nrm(ks[21], (DEPTH, N_EXPERTS, D_MODEL, D_EXPERT), D_MODEL)
    w_down = nrm(ks[22], (DEPTH, N_EXPERTS, D_EXPERT, D_MODEL), D_EXPERT)
    g_final = gain(ks[23], (D_MODEL,))
    return {'x': x, 'c': c, 'positions': positions, 'w_mod': w_mod, 'b_mod': b_mod, 'g_mix': g_mix,
            'w_in': w_in, 'conv_w': conv_w, 'a_log': a_log, 'dt_bias': dt_bias, 'dn_o_gain': dn_o_gain,
            'q_gain': q_gain, 'w_uq': w_uq, 'kv_gain': kv_gain, 'w_ukv': w_ukv, 'w_o_dn': w_o_dn,
            'w_o_mla': w_o_mla, 'w_out': w_out, 'g_ffn': g_ffn, 'w_router': w_router, 'w_gate': w_gate,
            'w_up': w_up, 'w_down': w_down, 'g_final': g_final}


def reference(x, c, positions, w_mod, b_mod, g_mix, w_in, conv_w, a_log, dt_bias, dn_o_gain,
              q_gain, w_uq, kv_gain, w_ukv, w_o_dn, w_o_mla, w_out, g_ffn, w_router, w_gate,
              w_up, w_down, g_final):
    half = ROPE_DIM // 2
    inv_freq = ROPE_THETA ** (-jnp.arange(half, dtype=jnp.float32) / half)
    ang = positions.astype(jnp.float32)[..., None] * inv_freq
    cos, sin = jnp.cos(ang), jnp.sin(ang)
    split_at = np.cumsum(IN_SPLITS)[:-1].tolist()
    for l in range(DEPTH):
        mod = jax.nn.silu(c) @ w_mod[l] + b_mod[l]
        sh1, sc1, gt1, sh2, sc2, gt2 = jnp.split(mod[:, None, :], N_MOD, axis=-1)
        h = rms_norm(x, g_mix[l]) * (1.0 + sc1) + sh1
        proj = h @ w_in[l]
        dn_qkv, dn_z, dn_b, dn_a, c_q, c_kv, k_r, gates = jnp.split(proj, split_at, axis=-1)
        y_dn = gated_deltanet_branch(dn_qkv, dn_z, dn_b, dn_a, conv_w[l], a_log[l], dt_bias[l],
                                     dn_o_gain[l]) @ w_o_dn[l]
        y_mla = mla_branch(c_q, c_kv, k_r, q_gain[l], w_uq[l], kv_gain[l], w_ukv[l], cos, sin) @ w_o_mla[l]
        g_dn, g_mla = jnp.split(jax.nn.sigmoid(gates), N_BRANCHES, axis=-1)
        merged = g_dn * y_dn + g_mla * y_mla
        x = x + gt1 * (merged @ w_out[l])
        h = rms_norm(x, g_ffn[l]) * (1.0 + sc2) + sh2
        x = x + gt2 * expert_choice_ffn(h, w_router[l], w_gate[l], w_up[l], w_down[l])
    return rms_norm(x, g_final)
```

```python
import numpy as np
import ml_dtypes
import concourse.bass as bass
import concourse.mybir as mybir
from concourse.bass_utils import run_bass_kernel_spmd
from contextlib import ExitStack

F32 = mybir.dt.float32
BF16 = mybir.dt.bfloat16
I32 = mybir.dt.int32
AF = mybir.ActivationFunctionType
ALU = mybir.AluOpType
AX = mybir.AxisListType

import os
DBGN = int(os.environ.get('DBGN', '99'))
S = 4096
D = 1024
NT = S // 128
EPS = 1e-6


class Prog:
    def __init__(self, nc, ndma=16):
        self.nc = nc
        self.eng = {'pe': nc.tensor, 'act': nc.scalar, 'dve': nc.vector,
                    'pool': nc.gpsimd, 'sp': nc.sync}
        self.sem = {k: nc.alloc_semaphore(name=f"s_{k}") for k in self.eng}
        self.cnt = {k: 0 for k in self.eng}
        self.waited = {k: {} for k in self.eng}
        self.dsem = {q: [nc.alloc_semaphore(name=f"s_dma_{q}{i}") for i in range(ndma)] for q in ('sp', 'pool')}
        self.dcnt = {q: [0] * ndma for q in ('sp', 'pool')}
        self.nd = {'sp': 0, 'pool': 0}
        self.lastw = {}
        self.readers = {}
        self.n = 0

    def _wait(self, e, tok):
        if tok is None:
            return
        sem, val, key = tok
        w = self.waited[e]
        if w.get(key, 0) >= val:
            return
        w[key] = val
        self.eng[e].wait_ge(sem, val)

    def op(self, e, fn, r=(), w=(), dma=False):
        w = list(w) + [t for t in r if isinstance(t, str) and t.startswith('ps') and t not in w]
        toks = []
        for t in r:
            toks.append(self.lastw.get(t))
        for t in w:
            toks.append(self.lastw.get(t))
            toks.extend(self.readers.get(t, ()))
        for tok in toks:
            self._wait(e, tok)
        if dma:
            i = self.nd[e] % len(self.dsem[e])
            if self.dcnt[e][i] > 0:
                self._wait(e, (self.dsem[e][i], self.dcnt[e][i], f"dma_{e}{i}"))
        ins = fn(self.eng[e])
        self.n += 1
        if dma:
            i = self.nd[e] % len(self.dsem[e])
            self.nd[e] += 1
            self.dcnt[e][i] += 16
            ins.then_inc(self.dsem[e][i], 16)
            tok = (self.dsem[e][i], self.dcnt[e][i], f"dma_{e}{i}")
        else:
            self.cnt[e] += 1
            ins.then_inc(self.sem[e], 1)
            tok = (self.sem[e], self.cnt[e], e)
        for t in r:
            self.readers.setdefault(t, []).append(tok)
        for t in w:
            self.lastw[t] = tok
            self.readers[t] = []
        return tok

    def barrier(self):
        toks = [(self.sem[k], self.cnt[k], k) for k in self.eng if self.cnt[k] > 0]
        toks += [(self.dsem[q][i], self.dcnt[q][i], f"dma_{q}{i}") for q in self.dsem for i in range(len(self.dsem[q])) if self.dcnt[q][i] > 0]
        for e in self.eng:
            for tok in toks:
                self._wait(e, tok)

    def finish(self, toks):
        for tok in toks:
            self._wait('sp', tok)


class _Stop(Exception):
    pass


def build(debug=None, stages=('A', 'MLA', 'DN', 'MG'), ext_in=(), dn_heads=range(8), dn_stop=0):
    nc = bass.Bass("TRN2", target_bir_lowering=False)
    P = Prog(nc)

    def din(name, shape, dt=F32):
        return nc.dram_tensor(name, list(shape), dt, kind="ExternalInput").ap()

    def dscr(name, shape, dt):
        kind = "ExternalOutput" if (debug and name in debug) else ("ExternalInput" if name in ext_in else "Internal")
        return nc.dram_tensor(name, list(shape), dt, kind=kind).ap()

    x = din("x", [S, D])
    cT = din("cT", [128, 8])
    w_mod = din("w_mod", [D, 6 * D])
    b_mod = din("b_mod", [1, 6 * D])
    g_mix = din("g_mix", [128, D])
    w_qkv = din("w_qkv", [D, 3072])
    w_z = din("w_z", [D, 1024])
    w_g = din("w_g", [D, 2048])
    w_ba = din("w_ba", [D, 32])
    w_cq = din("w_cq", [D, 512])
    w_ckv = din("w_ckv", [D, 256])
    w_kr2 = din("w_kr2", [D, 128])
    q_gain = din("q_gain", [128, 512])
    kv_gain = din("kv_gain", [128, 256])
    identf = din("identf", [128, 128])
    posr = din("posr", [64, S], I32)
    invf2 = din("invf2", [64, 1])
    sgn2 = din("sgn2", [64, 1])
    sel64 = din("sel64", [128, 65])
    w_uqh = din("w_uqh", [8, 512, 256])
    w_ukvh = din("w_ukvh", [8, 256, 256])
    conv_wT = din("conv_wT", [3072, 5])
    dn_sc = din("dn_sc", [8, 4])
    dn_gain = din("dn_gain", [128, 128])
    dmasks = din("dmasks", [128, 4, 128])
    dn_lmask = din("dn_lmask", [128, 5, 512])
    w_o_dn = din("w_o_dn", [D, D])
    w_o_mla = din("w_o_mla", [D, D])
    w_out = din("w_out", [D, D])
    g_ffn = din("g_ffn", [128, D])
    g_final = din("g_final", [128, D])
    w_router = din("w_router", [D, 16])
    w_gate = din("w_gate", [16, D, D])
    w_up = din("w_up", [16, D, D])
    w_down = din("w_down", [16, D, D])
    iota512 = din("iota512", [128, 512])
    tri_in = din("tri_in", [128, 128])
    out = nc.dram_tensor("out", [S // 2, D], F32, kind="ExternalOutput").ap()

    qkvT = dscr("qkvT", [24, 128, S], BF16)
    zs = dscr("zs", [S, 1024], BF16)
    gs = dscr("gs", [S, 2048], BF16)
    baT = dscr("baT", [32, S], F32)
    cqnT = dscr("cqnT", [4, 128, S], BF16)
    ckvnT = dscr("ckvnT", [2, 128, S], BF16)
    krT = dscr("krT", [2, 64, S], BF16)
    modrow = dscr("modrow", [1, 6 * D], F32)
    oT_mla = dscr("oT_mla", [8, 128, S], BF16)
    oT_dn = dscr("oT_dn", [8, 128, S], BF16)
    x1s = dscr("x1s", [S, D], F32)
    ye_all = dscr("ye_all", [16, 512, D], BF16)

    sb = lambda n, s, d: nc.alloc_sbuf_tensor(n, list(s), d)
    ps_ = lambda n, s, d=F32: nc.alloc_psum_tensor(n, list(s), d)

    ident = sb("ident", [128, 128], F32)
    identb = sb("identb", [128, 128], BF16)
    ones_f = sb("ones_f", [128, 128], F32)
    P.op('sp', lambda e: e.dma_start(out=ident[:], in_=identf[:, :]), w=['ident'], dma=True)
    P.op('dve', lambda e: e.tensor_copy(out=identb[:], in_=ident[:]), r=['ident'], w=['identb'])
    P.op('dve', lambda e: e.memset(ones_f[:], 1.0), w=['ones_f'])
    epsb = sb("epsb", [128, 1], F32)
    P.op('dve', lambda e: e.memset(epsb[:], EPS), w=['epsb'])

    psA = [ps_(f"psA{i}", [128, 512]) for i in range(4)]
    psT = [ps_(f"psT{i}", [128, 512], BF16) for i in range(2)]
    psS = [ps_(f"psS{i}", [128, 512]) for i in range(2)]
    pa_i = [0]

    def next_psA():
        i = pa_i[0] % 4
        pa_i[0] += 1
        return psA[i], f"psA{i}"

    sbs = lambda es, n, s_, d: es.enter_context(nc.sbuf_tensor(n, list(s_), d))
    fin = []

    def phase_A():
        cT_sb = sb("cT_sb", [128, 8], F32)
        scT = sb("scT", [128, 8], F32)
        P.op('sp', lambda e: e.dma_start(out=cT_sb[:], in_=cT[:, :]), w=['cT'], dma=True)
        P.op('act', lambda e: e.activation(out=scT[:], in_=cT_sb[:], func=AF.Silu), r=['cT'], w=['scT'])
        esA = ExitStack()
        modbc = sbs(esA, "modbc", [128, 6, D], F32)
        gmx = sbs(esA, "gmx", [128, D], F32)
        A1 = sbs(esA, "A1", [128, D], F32)
        es0 = ExitStack()
        mod_sb = sbs(es0, "mod_sb", [1, 6 * D], F32)
        bm_sb = sbs(es0, "bm_sb", [1, 6 * D], F32)
        P.op('sp', lambda e: e.dma_start(out=bm_sb[:], in_=b_mod[:, :]), w=['bm'], dma=True)
        wm = [sbs(es0, f"wm{i}", [128, 8, 512], F32) for i in range(2)]
        w_mod_v = w_mod.rearrange("(k p) n -> p k n", p=128)
        for j in range(12):
            wt, wk = wm[j % 2], f"wm{j % 2}"
            P.op('sp', lambda e: e.dma_start(out=wt[:], in_=w_mod_v[:, :, j * 512:(j + 1) * 512]), w=[wk], dma=True)
            pt, pk = next_psA()
            for k in range(8):
                P.op('pe', lambda e: e.matmul(pt[0:1, :], lhsT=scT[:, k:k + 1], rhs=wt[:, k, :], start=(k == 0), stop=(k == 7)),
                     r=[wk, 'scT'], w=[pk])
            P.op('dve', lambda e: e.tensor_tensor(out=mod_sb[:, j * 512:(j + 1) * 512], in0=pt[0:1, :], in1=bm_sb[:, j * 512:(j + 1) * 512], op=ALU.add),
                 r=[pk, 'bm'], w=['mod'])
        for j in range(12):
            pt, pk = next_psA()
            P.op('pe', lambda e: e.matmul(pt[:], lhsT=ones_f[0:1, :], rhs=mod_sb[:, j * 512:(j + 1) * 512], start=True, stop=True),
                 r=['ones_f', 'mod'], w=[pk])
            P.op('act', lambda e: e.copy(out=modbc[:, j // 2, (j % 2) * 512:(j % 2 + 1) * 512], in_=pt[:]), r=[pk], w=['modbc'])
        P.op('sp', lambda e: e.dma_start(out=gmx[:], in_=g_mix[:, :]), w=['gmx'], dma=True)
        P.op('dve', lambda e: e.scalar_tensor_tensor(out=A1[:], in0=modbc[:, 1, :], scalar=1.0, in1=gmx[:], op0=ALU.add, op1=ALU.mult),
             r=['modbc', 'gmx'], w=['A1'])

        fin.append(P.op('sp', lambda e: e.dma_start(out=modrow[:, :], in_=mod_sb[:]), r=['mod'], dma=True))
        P.barrier()
        es0.close()
        hT = sbs(esA, "hT", [128, 8, S], BF16)
        xt = [sbs(esA, f"xt{i}", [128, D], F32) for i in range(2)]
        xn = [sbs(esA, f"xn{i}", [128, D], F32) for i in range(2)]
        hb = [sbs(esA, f"hb{i}", [128, D], BF16) for i in range(2)]
        st = [sbs(esA, f"st{i}", [128, 4], F32) for i in range(2)]
        junk = sbs(esA, "junk", [128, D], F32)
        for t in range(NT):
            i = t % 2
            P.op('sp', lambda e: e.dma_start(out=xt[i][:], in_=x[t * 128:(t + 1) * 128, :]), w=[f'xt{i}'], dma=True)
            P.op('dve', lambda e: e.memset(st[i][:], 0.0), w=[f'st{i}'])
            P.op('act', lambda e: e.activation(out=junk[:], in_=xt[i][:], func=AF.Square, accum_out=st[i][:, 0:1]),
                 r=[f'xt{i}'], w=['junk', f'st{i}'])
            P.op('act', lambda e: e.activation(out=st[i][:, 1:2], in_=st[i][:, 0:1], func=AF.Sqrt, scale=1.0 / D, bias=epsb[:]),
                 r=[f'st{i}', 'epsb'], w=[f'st{i}'])
            P.op('dve', lambda e: e.reciprocal(out=st[i][:, 2:3], in_=st[i][:, 1:2]), r=[f'st{i}'], w=[f'st{i}'])
            P.op('dve', lambda e: e.scalar_tensor_tensor(out=xn[i][:], in0=xt[i][:], scalar=st[i][:, 2:3], in1=A1[:], op0=ALU.mult, op1=ALU.mult),
                 r=[f'xt{i}', f'st{i}', 'A1'], w=[f'xn{i}'])
            P.op('pool', lambda e: e.tensor_tensor(out=hb[i][:], in0=xn[i][:], in1=modbc[:, 0, :], op=ALU.add),
                 r=[f'xn{i}', 'modbc'], w=[f'hb{i}'])
            for half in range(2):
                pt, pk = psT[half], f'psT{half}'
                for k4 in range(4):
                    k = half * 4 + k4
                    P.op('pe', lambda e: e.transpose(out=pt[:, k4 * 128:(k4 + 1) * 128], in_=hb[i][:, k * 128:(k + 1) * 128], identity=identb[:]),
                         r=[f'hb{i}', 'identb'], w=[pk])
                eng = 'act' if half == 0 else 'dve'
                if eng == 'act':
                    P.op('act', lambda e: e.copy(out=hT[:, half * 4:(half + 1) * 4, t * 128:(t + 1) * 128],
                                                 in_=pt[:].rearrange("p (k n) -> p k n", k=4)), r=[pk], w=[('hT', t)])
                else:
                    P.op('dve', lambda e: e.tensor_copy(out=hT[:, half * 4:(half + 1) * 4, t * 128:(t + 1) * 128],
                                                        in_=pt[:].rearrange("p (k n) -> p k n", k=4)), r=[pk], w=[('hT', t)])
        hT_all = [('hT', t) for t in range(NT)]

        wb = [sbs(esA, f"wb{i}", [128, 8, 512], BF16) for i in range(2)]
        wb_i = [0]

        def load_w(src, c0, ncols):
            i = wb_i[0] % 2
            wb_i[0] += 1
            v = src.rearrange("(k p) n -> p k n", p=128)
            P.op('pool', lambda e: e.dma_start(out=wb[i][:, :, 0:ncols], in_=v[:, :, c0:c0 + ncols]), w=[f'wb{i}'], dma=True)
            return wb[i], f'wb{i}'

        stg = [sbs(esA, f"stg{i}", [128, S], BF16) for i in range(2)]
        stg_i = [0]

        def chan_major(src, ncols_total, dst_fn, M, dt_out=BF16, stgs=stg):
            nblk = (ncols_total + 511) // 512
            for blk in range(nblk):
                nc_ = min(512, ncols_total - blk * 512)
                wt, wk = load_w(src, blk * 512, nc_)
                for c in range(nc_ // M):
                    si = stg_i[0] % 2
                    stg_i[0] += 1
                    sg, sk = stgs[si], f'{stgs[si].name}'
                    for g in range(8):
                        pt, pk = next_psA()
                        for k in range(8):
                            P.op('pe', lambda e: e.matmul(pt[0:M, :], lhsT=wt[:, k, c * M:(c + 1) * M], rhs=hT[:, k, g * 512:(g + 1) * 512],
                                                          start=(k == 0), stop=(k == 7)),
                                 r=[wk] + hT_all[g * 4:(g + 1) * 4], w=[pk])
                        if g % 2 == 0:
                            P.op('act', lambda e: e.copy(out=sg[0:M, g * 512:(g + 1) * 512], in_=pt[0:M, :]), r=[pk], w=[sk])
                        else:
                            P.op('dve', lambda e: e.tensor_copy(out=sg[0:M, g * 512:(g + 1) * 512], in_=pt[0:M, :]), r=[pk], w=[sk])
                    fin.append(P.op('sp', lambda e: e.dma_start(out=dst_fn(blk * (512 // M) + c), in_=sg[0:M, :]), r=[sk], w=[('scr', dst_fn.__name__)], dma=True))

        def dst_qkv(c):
            return qkvT[c, :, :]
        chan_major(w_qkv, 3072, dst_qkv, 128)

        def dst_kr(c):
            return krT[c, :, :]
        chan_major(w_kr2, 128, dst_kr, 64)
        stgf0 = sbs(esA, "stgf0", [32, S], F32)
        stgf = [stgf0, stgf0]

        def dst_ba(c):
            return baT[:, :]
        chan_major(w_ba, 32, dst_ba, 32, F32, stgf)

        tst = [sbs(esA, f"tst{i}", [128, 512], BF16) for i in range(4)]
        tst_i = [0]

        def tok_major_act(src, ncols_total, dst, func):
            for blk in range(ncols_total // 512):
                wt, wk = load_w(src, blk * 512, 512)
                for t in range(NT):
                    pt, pk = next_psA()
                    for k in range(8):
                        P.op('pe', lambda e: e.matmul(pt[:], lhsT=hT[:, k, t * 128:(t + 1) * 128], rhs=wt[:, k, :], start=(k == 0), stop=(k == 7)),
                             r=[wk, ('hT', t)], w=[pk])
                    si = tst_i[0] % 4
                    tst_i[0] += 1
                    P.op('act', lambda e: e.activation(out=tst[si][:], in_=pt[:], func=func), r=[pk], w=[f'tst{si}'])
                    fin.append(P.op('sp', lambda e: e.dma_start(out=dst[t * 128:(t + 1) * 128, blk * 512:(blk + 1) * 512], in_=tst[si][:]),
                                    r=[f'tst{si}'], w=[('scr', dst.name, t, blk)], dma=True))
        tok_major_act(w_z, 1024, zs, AF.Silu)
        tok_major_act(w_g, 2048, gs, AF.Sigmoid)

        def latent(src, ncols, gain_in, dstT, nm):
            gsb = sbs(esA, f"gain_{nm}", [128, ncols], F32)
            P.op('sp', lambda e: e.dma_start(out=gsb[:], in_=gain_in[:, :]), w=[f'gain_{nm}'], dma=True)
            lst = [sbs(esA, f"lst_{nm}{i_}", [128, ncols // 128, 128], BF16) for i_ in range(2)]
            wt, wk = load_w(src, 0, ncols)
            for t in range(NT):
                i = t % 2
                pt, pk = next_psA()
                for k in range(8):
                    P.op('pe', lambda e: e.matmul(pt[:, 0:ncols], lhsT=hT[:, k, t * 128:(t + 1) * 128], rhs=wt[:, k, 0:ncols], start=(k == 0), stop=(k == 7)),
                         r=[wk, ('hT', t)], w=[pk])
                P.op('dve', lambda e: e.memset(st[i][:], 0.0), w=[f'st{i}'])
                P.op('act', lambda e: e.activation(out=junk[:, 0:ncols], in_=pt[:, 0:ncols], func=AF.Square, accum_out=st[i][:, 0:1]),
                     r=[pk], w=['junk', f'st{i}'])
                P.op('act', lambda e: e.activation(out=st[i][:, 1:2], in_=st[i][:, 0:1], func=AF.Sqrt, scale=1.0 / ncols, bias=epsb[:]),
                     r=[f'st{i}', 'epsb'], w=[f'st{i}'])
                P.op('dve', lambda e: e.reciprocal(out=st[i][:, 2:3], in_=st[i][:, 1:2]), r=[f'st{i}'], w=[f'st{i}'])
                P.op('dve', lambda e: e.scalar_tensor_tensor(out=hb[i][:, 0:ncols], in0=pt[:, 0:ncols], scalar=st[i][:, 2:3], in1=gsb[:], op0=ALU.mult, op1=ALU.mult),
                     r=[pk, f'st{i}', f'gain_{nm}'], w=[f'hb{i}'])
                tp, tk = psT[i], f'psT{i}'
                for c in range(ncols // 128):
                    P.op('pe', lambda e: e.transpose(out=tp[:, c * 128:(c + 1) * 128], in_=hb[i][:, c * 128:(c + 1) * 128], identity=identb[:]),
                         r=[f'hb{i}', 'identb'], w=[tk])
                P.op('act', lambda e: e.copy(out=lst[i][:], in_=tp[:, 0:ncols].rearrange("p (k n) -> p k n", k=ncols // 128)),
                     r=[tk], w=[f'lst_{nm}{i}'])
                fin.append(P.op('sp', lambda e: e.dma_start(out=dstT[:, :, t * 128:(t + 1) * 128].rearrange("c p n -> p c n"), in_=lst[i][:]),
                                r=[f'lst_{nm}{i}'], w=[('scr', nm, t)], dma=True))
        latent(w_cq, 512, q_gain, cqnT, 'cq')
        latent(w_ckv, 256, kv_gain, ckvnT, 'ckv')


        P.barrier()
        esA.close()

    def phase_MLA():
        esM = ExitStack()
        TWO_PI = float(2 * np.pi)
        SCL = float(192 ** -0.5)
        cos2 = sbs(esM, "cos2", [64, S], F32)
        sin2 = sbs(esM, "sin2", [64, S], F32)
        krA = sbs(esM, "krA", [65, S], BF16)
        QrA = sbs(esM, "QrA", [65, S], BF16)
        onesb = sbs(esM, "onesb", [128, 128], BF16)
        sel_b = sbs(esM, "sel_b", [128, 65], BF16)
        if True:
            es1 = ExitStack()
            posi = sbs(es1, "posi", [64, S], I32)
            ang = sbs(es1, "ang", [64, S], F32)
            ti = sbs(es1, "ti", [64, S], I32)
            tf = sbs(es1, "tf", [64, S], F32)
            tg = sbs(es1, "tg", [64, S], F32)
            ivf = sbs(es1, "ivf", [64, 2], F32)
            kr0 = sbs(es1, "kr0", [64, S], BF16)
            kr1 = sbs(es1, "kr1", [64, S], BF16)
            self_f = sbs(es1, "self_f", [128, 65], F32)
            P.op('sp', lambda e: e.dma_start(out=posi[:], in_=posr[:, :]), w=['posi'], dma=True)
            P.op('sp', lambda e: e.dma_start(out=ivf[:, 0:1], in_=invf2[:, :]), w=['ivf'], dma=True)
            P.op('sp', lambda e: e.dma_start(out=ivf[:, 1:2], in_=sgn2[:, :]), w=['ivf'], dma=True)
            P.op('sp', lambda e: e.dma_start(out=self_f[:], in_=sel64[:, :]), w=['self_f'], dma=True)
            P.op('dve', lambda e: e.tensor_copy(out=sel_b[:], in_=self_f[:]), r=['self_f'], w=['sel_b'])
            P.op('dve', lambda e: e.memset(onesb[:], 1.0), w=['onesb'])
            P.op('dve', lambda e: e.tensor_copy(out=ang[:], in_=posi[:]), r=['posi'], w=['ang'])
            P.op('dve', lambda e: e.tensor_scalar(out=ang[:], in0=ang[:], scalar1=ivf[:, 0:1], scalar2=float(1.0 / TWO_PI), op0=ALU.mult, op1=ALU.mult),
                 r=['ang', 'ivf'], w=['ang'])
            for which, dst in ((0, sin2), (1, cos2)):
                dk_ = 'sin2' if which == 0 else 'cos2'
                P.op('dve', lambda e: e.tensor_scalar(out=tg[:], in0=ang[:], scalar1=0.25 * which, scalar2=None, op0=ALU.add), r=['ang'], w=['tg'])
                P.op('dve', lambda e: e.tensor_copy(out=ti[:], in_=tg[:]), r=['tg'], w=['ti'])
                P.op('dve', lambda e: e.tensor_copy(out=tf[:], in_=ti[:]), r=['ti'], w=['tf'])
                P.op('dve', lambda e: e.tensor_tensor(out=tg[:], in0=tg[:], in1=tf[:], op=ALU.subtract), r=['tg', 'tf'], w=['tg'])
                P.op('dve', lambda e: e.tensor_scalar(out=tf[:], in0=tg[:], scalar1=0.5, scalar2=None, op0=ALU.is_gt), r=['tg'], w=['tf'])
                P.op('dve', lambda e: e.tensor_tensor(out=tg[:], in0=tg[:], in1=tf[:], op=ALU.subtract), r=['tg', 'tf'], w=['tg'])
                P.op('dve', lambda e: e.tensor_scalar(out=tf[:], in0=tg[:], scalar1=-0.5, scalar2=None, op0=ALU.is_lt), r=['tg'], w=['tf'])
                P.op('dve', lambda e: e.tensor_tensor(out=tg[:], in0=tg[:], in1=tf[:], op=ALU.add), r=['tg', 'tf'], w=['tg'])
                P.op('act', lambda e: e.activation(out=dst[:], in_=tg[:], func=AF.Sin, scale=TWO_PI), r=['tg'], w=[dk_])
            P.op('dve', lambda e: e.tensor_scalar(out=sin2[:], in0=sin2[:], scalar1=ivf[:, 1:2], scalar2=None, op0=ALU.mult), r=['sin2', 'ivf'], w=['sin2'])
            P.op('sp', lambda e: e.dma_start(out=kr0[:], in_=krT[0, :, :]), r=[('scr', 'dst_kr')], w=['kr0'], dma=True)
            P.op('sp', lambda e: e.dma_start(out=kr1[:], in_=krT[1, :, :]), r=[('scr', 'dst_kr')], w=['kr1'], dma=True)
            P.op('dve', lambda e: e.tensor_tensor(out=tg[:], in0=kr0[:], in1=cos2[:], op=ALU.mult), r=['kr0', 'cos2'], w=['tg'])
            P.op('dve', lambda e: e.tensor_tensor(out=tf[:], in0=kr1[:], in1=sin2[:], op=ALU.mult), r=['kr1', 'sin2'], w=['tf'])
            P.op('dve', lambda e: e.memset(krA[:], 1.0), w=['krA'])
            P.op('dve', lambda e: e.tensor_tensor(out=krA[0:64, :], in0=tg[:], in1=tf[:], op=ALU.add), r=['tg', 'tf'], w=['krA'])
            P.op('dve', lambda e: e.memset(QrA[:], 0.0), w=['QrA'])
            P.barrier()
            es1.close()
        cqn = sbs(esM, "cqn", [128, 4, S], BF16)
        ckvn = sbs(esM, "ckvn", [128, 2, S], BF16)
        QnT = sbs(esM, "QnT", [128, S], BF16)
        KnT = sbs(esM, "KnT", [128, S], BF16)
        Vt = sbs(esM, "Vt", [128, NT, 128], BF16)
        oTs = sbs(esM, "oTs", [128, S], BF16)
        kmx = sbs(esM, "kmx", [65, 16], F32)
        wuq = sbs(esM, "wuq", [128, 4, 256], BF16)
        wukv = sbs(esM, "wukv", [128, 2, 256], BF16)
        sq = sbs(esM, "sq", [128, 512], BF16)
        t1 = sbs(esM, "t1", [64, 512], F32)
        t2 = sbs(esM, "t2", [64, 512], F32)
        rowt = sbs(esM, "rowt", [65, 512], F32)
        pT = [sbs(esM, f"pT{i}", [128, 512], BF16) for i in range(3)]
        rden = sbs(esM, "rden", [128, 512], F32)
        dacc = sbs(esM, "dacc", [128, 512], F32)
        for c in range(4):
            P.op('sp', lambda e: e.dma_start(out=cqn[:, c, :], in_=cqnT[c, :, :]), r=[('scr', 'cq', t_) for t_ in range(NT)], w=['cqn'], dma=True)
        for c in range(2):
            P.op('sp', lambda e: e.dma_start(out=ckvn[:, c, :], in_=ckvnT[c, :, :]), r=[('scr', 'ckv', t_) for t_ in range(NT)], w=['ckvn'], dma=True)
        for h in range(8):
            P.op('pool', lambda e: e.dma_start(out=wuq[:], in_=w_uqh[h].rearrange("(k p) n -> p k n", p=128)), w=['wuq'], dma=True)
            P.op('pool', lambda e: e.dma_start(out=wukv[:], in_=w_ukvh[h].rearrange("(k p) n -> p k n", p=128)), w=['wukv'], dma=True)
            for g in range(8):
                gs_ = slice(g * 512, (g + 1) * 512)
                pt, pk = psA[2], 'psA2'
                for k in range(4):
                    P.op('pe', lambda e: e.matmul(pt[:], lhsT=wuq[:, k, 0:128], rhs=cqn[:, k, gs_], start=(k == 0), stop=(k == 3)), r=['wuq', 'cqn'], w=[pk])
                P.op('act', lambda e: e.copy(out=QnT[:, gs_], in_=pt[:]), r=[pk], w=[('QnT', g)])
                pt, pk = psA[3], 'psA3'
                for k in range(4):
                    P.op('pe', lambda e: e.matmul(pt[0:64, :], lhsT=wuq[:, k, 128:192], rhs=cqn[:, k, gs_], start=(k == 0), stop=(k == 3)), r=['wuq', 'cqn'], w=[pk])
                P.op('dve', lambda e: e.tensor_tensor(out=t1[:], in0=pt[0:64, :], in1=cos2[:, gs_], op=ALU.mult), r=[pk, 'cos2'], w=['t1'])
                for k in range(4):
                    P.op('pe', lambda e: e.matmul(pt[0:64, :], lhsT=wuq[:, k, 192:256], rhs=cqn[:, k, gs_], start=(k == 0), stop=(k == 3)), r=['wuq', 'cqn'], w=[pk])
                P.op('dve', lambda e: e.tensor_tensor(out=t2[:], in0=pt[0:64, :], in1=sin2[:, gs_], op=ALU.mult), r=[pk, 'sin2'], w=['t2'])
                P.op('dve', lambda e: e.tensor_tensor(out=QrA[0:64, gs_], in0=t1[:], in1=t2[:], op=ALU.add), r=['t1', 't2'], w=[('QrA', g)])
                pt, pk = psA[2], 'psA2'
                for k in range(2):
                    P.op('pe', lambda e: e.matmul(pt[:], lhsT=wukv[:, k, 0:128], rhs=ckvn[:, k, gs_], start=(k == 0), stop=(k == 1)), r=['wukv', 'ckvn'], w=[pk])
                P.op('act', lambda e: e.copy(out=KnT[:, gs_], in_=pt[:]), r=[pk], w=[('KnT', g)])
                pt, pk = psA[3], 'psA3'
                for j in range(4):
                    t_ = g * 4 + j
                    for k in range(2):
                        P.op('pe', lambda e: e.matmul(pt[:, j * 128:(j + 1) * 128], lhsT=ckvn[:, k, t_ * 128:(t_ + 1) * 128], rhs=wukv[:, k, 128:256], start=(k == 0), stop=(k == 1)),
                             r=['wukv', 'ckvn'], w=[pk])
                P.op('dve', lambda e: e.tensor_copy(out=Vt[:, g * 4:(g + 1) * 4, :], in_=pt[:].rearrange("p (j n) -> p j n", j=4)), r=[pk], w=[('Vt', g)])
                pt, pk = psA[2], 'psA2'
                P.op('act', lambda e: e.activation(out=sq[:], in_=KnT[:, gs_], func=AF.Square), r=[('KnT', g)], w=['sq'])
                P.op('pe', lambda e: e.matmul(pt[0:65, :], lhsT=sel_b[:, :], rhs=sq[:], start=True, stop=False), r=['sq', 'sel_b'], w=[pk])
                P.op('act', lambda e: e.activation(out=sq[0:64, :], in_=krA[0:64, gs_], func=AF.Square), r=['krA'], w=['sq'])
                P.op('pe', lambda e: e.matmul(pt[0:65, :], lhsT=sel_b[0:64, :], rhs=sq[0:64, :], start=False, stop=True), r=['sq', 'sel_b'], w=[pk])
                P.op('dve', lambda e: e.tensor_reduce(out=kmx[64:65, g:g + 1], in_=pt[64:65, :], axis=AX.X, op=ALU.max), r=[pk], w=['kmx'])
            P.op('dve', lambda e: e.tensor_reduce(out=kmx[64:65, 8:9], in_=kmx[64:65, 0:8], axis=AX.X, op=ALU.max), r=['kmx'], w=['kmx'])
            for g in range(8):
                gs_ = slice(g * 512, (g + 1) * 512)
                pt, pk = psA[2], 'psA2'
                P.op('act', lambda e: e.activation(out=sq[:], in_=QnT[:, gs_], func=AF.Square), r=[('QnT', g)], w=['sq'])
                P.op('pe', lambda e: e.matmul(pt[0:65, :], lhsT=sel_b[:, :], rhs=sq[:], start=True, stop=False), r=['sq', 'sel_b'], w=[pk])
                P.op('act', lambda e: e.activation(out=sq[0:64, :], in_=QrA[0:64, gs_], func=AF.Square), r=[('QrA', g)], w=['sq'])
                P.op('pe', lambda e: e.matmul(pt[0:65, :], lhsT=sel_b[0:64, :], rhs=sq[0:64, :], start=False, stop=True), r=['sq', 'sel_b'], w=[pk])
                P.op('act', lambda e: e.activation(out=rowt[64:65, :], in_=pt[64:65, :], func=AF.Sqrt, scale=kmx[64:65, 8:9]), r=[pk, 'kmx'], w=['rowt'])
                P.op('dve', lambda e: e.tensor_scalar(out=QrA[64:65, gs_], in0=rowt[64:65, :], scalar1=-1.0, scalar2=None, op0=ALU.mult), r=['rowt'], w=[('QrA', g)])
            for g in range(8):
                gs_ = slice(g * 512, (g + 1) * 512)
                po, pd = psA[0], psA[1]

                def scores(kt):
                    ks_ = slice(kt * 128, (kt + 1) * 128)
                    sc_, sck = psS[kt % 2], f'psS{kt % 2}'
                    P.op('pe', lambda e: e.matmul(sc_[:], lhsT=KnT[:, ks_], rhs=QnT[:, gs_], start=True, stop=False),
                         r=[('KnT', kt // 4), ('QnT', g)], w=[sck])
                    P.op('pe', lambda e: e.matmul(sc_[:], lhsT=krA[:, ks_], rhs=QrA[:, gs_], start=False, stop=True),
                         r=['krA', ('QrA', g)], w=[sck])
                scores(0)
                for kt in range(NT):
                    sc_, sck = psS[kt % 2], f'psS{kt % 2}'
                    pi = kt % 3
                    P.op('act', lambda e: e.activation(out=pT[pi][:], in_=sc_[:], func=AF.Exp, scale=SCL), r=[sck], w=[f'pT{pi}'])
                    if kt + 1 < NT:
                        scores(kt + 1)
                    P.op('pe', lambda e: e.matmul(po[:], lhsT=Vt[:, kt, :], rhs=pT[pi][:], start=(kt == 0), stop=(kt == NT - 1)),
                         r=[('Vt', kt // 4), f'pT{pi}'], w=['psA0'])
                    if kt == 0:
                        P.op('dve', lambda e: e.tensor_copy(out=dacc[:], in_=pT[pi][:]), r=[f'pT{pi}'], w=['dacc'])
                    else:
                        P.op('dve', lambda e: e.tensor_tensor(out=dacc[:], in0=pT[pi][:], in1=dacc[:], op=ALU.add), r=[f'pT{pi}', 'dacc'], w=['dacc'])
                P.op('pe', lambda e: e.matmul(pd[:], lhsT=ones_f[:], rhs=dacc[:], start=True, stop=True), r=['ones_f', 'dacc'], w=['psA1'])
                P.op('dve', lambda e: e.reciprocal(out=rden[:], in_=pd[:]), r=['psA1'], w=['rden'])
                P.op('dve', lambda e: e.tensor_tensor(out=oTs[:, gs_], in0=po[:], in1=rden[:], op=ALU.mult), r=['psA0', 'rden'], w=['oTs'])
            fin.append(P.op('sp', lambda e: e.dma_start(out=oT_mla[h, :, :], in_=oTs[:]), r=['oTs'], w=[('scr', 'oT_mla', h)], dma=True))
        P.barrier()
        esM.close()


    def phase_DN():
        def stop(k):
            if dn_stop == k:
                raise _Stop()
        esD = ExitStack()
        pM, pG, pX0, pX1, pZT, pU = psA[0], psA[1], psA[2], psA[3], psS[0], psS[1]
        kM, kG, kX0, kX1, kZT, kU = 'psA0', 'psA1', 'psA2', 'psA3', 'psS0', 'psS1'
        onesb = sbs(esD, "d_onesb", [128, 128], BF16)
        P.op('dve', lambda e: e.memset(onesb[:], 1.0), w=['d_onesb'])
        msk = sbs(esD, "d_msk", [128, 4, 128], F32)
        P.op('sp', lambda e: e.dma_start(out=msk[:], in_=dmasks[:, :, :]), w=['d_msk'], dma=True)
        gain = sbs(esD, "d_gain", [128, 128], F32)
        P.op('sp', lambda e: e.dma_start(out=gain[:], in_=dn_gain[:, :]), w=['d_gain'], dma=True)
        tokS = sbs(esD, "tokS", [128, NT, 48], F32)
        one1 = sbs(esD, "one1", [128, 1], F32)
        P.op('dve', lambda e: e.memset(one1[:], 1.0), w=['one1'])
        eps_l2 = sbs(esD, "eps_l2", [128, 1], F32)
        P.op('dve', lambda e: e.memset(eps_l2[:], EPS), w=['eps_l2'])
        es1 = ExitStack()
        sc = sbs(es1, "d_sc", [8, 4], F32)
        nA = sbs(es1, "d_nA", [8, 2], F32)
        P.op('sp', lambda e: e.dma_start(out=sc[:], in_=dn_sc[:, :]), w=['d_sc'], dma=True)
        P.op('act', lambda e: e.activation(out=nA[:], in_=sc[:, 0:2], func=AF.Exp), r=['d_sc'], w=['d_nA'])
        P.op('dve', lambda e: e.tensor_scalar(out=nA[:], in0=nA[:], scalar1=-1.0, scalar2=None, op0=ALU.mult), r=['d_nA'], w=['d_nA'])
        rows = {}
        ra = sbs(es1, "d_ra", [8, S], F32)
        rb = sbs(es1, "d_rb", [8, S], F32)
        rc = sbs(es1, "d_rc", [8, S], F32)
        for d in range(2):
            beta = sbs(es1, f"d_beta{d}", [8, S], F32)
            nbeta = sbs(es1, f"d_nbeta{d}", [8, S], F32)
            gc = sbs(es1, f"d_gc{d}", [8, S], F32)
            rows[d] = (beta, nbeta, gc)
            P.op('sp', lambda e: e.dma_start(out=ra[:], in_=baT[d * 8:(d + 1) * 8, :]), r=[('scr', 'dst_ba')], w=['d_ra'], dma=True)
            P.op('act', lambda e: e.activation(out=beta[:], in_=ra[:], func=AF.Sigmoid), r=['d_ra'], w=[f'd_beta{d}'])
            P.op('dve', lambda e: e.tensor_scalar(out=nbeta[:], in0=beta[:], scalar1=-1.0, scalar2=None, op0=ALU.mult), r=[f'd_beta{d}'], w=[f'd_nbeta{d}'])
            P.op('sp', lambda e: e.dma_start(out=ra[:], in_=baT[16 + d * 8:16 + (d + 1) * 8, :]), r=[('scr', 'dst_ba')], w=['d_ra'], dma=True)
            P.op('dve', lambda e: e.tensor_scalar(out=ra[:], in0=ra[:], scalar1=sc[:, 2 + d:3 + d], scalar2=None, op0=ALU.add), r=['d_ra', 'd_sc'], w=['d_ra'])
            P.op('act', lambda e: e.activation(out=rb[:], in_=ra[:], func=AF.Abs), r=['d_ra'], w=['d_rb'])
            P.op('act', lambda e: e.activation(out=rb[:], in_=rb[:], func=AF.Exp, scale=-1.0), r=['d_rb'], w=['d_rb'])
            P.op('act', lambda e: e.activation(out=rb[:], in_=rb[:], func=AF.Ln, bias=one1[0:8, :], scale=1.0), r=['d_rb', 'one1'], w=['d_rb'])
            P.op('dve', lambda e: e.scalar_tensor_tensor(out=rc[:], in0=ra[:], scalar=0.0, in1=rb[:], op0=ALU.max, op1=ALU.add), r=['d_ra', 'd_rb'], w=['d_rc'])
            P.op('dve', lambda e: e.tensor_scalar(out=rc[:], in0=rc[:], scalar1=nA[:, d:d + 1], scalar2=None, op0=ALU.mult), r=['d_rc', 'd_nA'], w=['d_rc'])
            cur, curk, nxt, nxtk = rc, 'd_rc', gc, f'd_gc{d}'
            for sft in (1, 2, 4, 8, 16, 32, 64):
                c3 = cur[:].rearrange("p (t n) -> p t n", n=128)
                n3 = nxt[:].rearrange("p (t n) -> p t n", n=128)
                P.op('act', lambda e: e.copy(out=nxt[:], in_=cur[:]), r=[curk], w=[nxtk])
                if d == 0:
                    P.op('dve', lambda e: e.tensor_tensor(out=n3[:, :, sft:], in0=c3[:, :, sft:], in1=c3[:, :, :128 - sft], op=ALU.add), r=[curk], w=[nxtk])
                else:
                    P.op('dve', lambda e: e.tensor_tensor(out=n3[:, :, :128 - sft], in0=c3[:, :, :128 - sft], in1=c3[:, :, sft:], op=ALU.add), r=[curk], w=[nxtk])
                cur, curk, nxt, nxtk = nxt, nxtk, cur, curk
            if cur is not gc:
                P.op('act', lambda e: e.copy(out=gc[:], in_=cur[:]), r=[curk], w=[f'd_gc{d}'])
        for t in range(NT):
            for d in range(2):
                for j in range(3):
                    src = rows[d][j]
                    col = d * 24 + j * 8
                    P.op('pe', lambda e: e.transpose(out=pG[:, col:col + 8], in_=src[:, t * 128:(t + 1) * 128], identity=ident[0:8, 0:8]),
                         r=[f'd_beta{d}', f'd_nbeta{d}', f'd_gc{d}', 'ident'], w=[kG])
            P.op('act', lambda e: e.copy(out=tokS[:, t, :], in_=pG[:, 0:48]), r=[kG], w=['tokS'])
        P.barrier()
        stop(1)
        es1.close()
        lm = sbs(esD, "d_lm", [128, 5, 4 * 128], F32)
        for j5 in range(5):
            P.op('sp', lambda e: e.dma_start(out=lm[:, j5, :], in_=dn_lmask[:, j5, :]), w=['d_lm'], dma=True)
        QKV = [sbs(esD, f"d_qkv{i}", [128, S], BF16) for i in range(3)]
        Ust = sbs(esD, "d_U", [128, NT, 2, 128], BF16)
        WTst = sbs(esD, "d_WT", [128, NT, 2, 128], BF16)
        ITst = sbs(esD, "d_IT", [128, NT, 2, 128], BF16)
        QDst = sbs(esD, "d_QD", [128, NT, 2, 128], BF16)
        KSst = sbs(esD, "d_KS", [128, NT, 2, 128], BF16)
        egl = sbs(esD, "d_egl", [128, NT, 2], F32)
        Oacc = sbs(esD, "d_Oacc", [128, NT, 128], F32)
        zsh = sbs(esD, "d_zsh", [128, NT, 128], BF16)
        oTd = sbs(esD, "d_oTd", [128, S], BF16)
        S32 = [sbs(esD, f"d_S32{d}", [128, 128], F32) for d in range(2)]
        Sbf = [sbs(esD, f"d_Sbf{d}", [128, 128], BF16) for d in range(2)]
        Vn = [sbs(esD, f"d_Vn{d}", [128, 128], BF16) for d in range(2)]
        ost = sbs(esD, "d_ost", [128, 8], F32)
        on = sbs(esD, "d_on", [128, 128], F32)
        onb = sbs(esD, "d_onb", [128, 128], BF16)
        G = 4
        QSC = float(128 ** -0.5)
        for h in dn_heads:
            esC = ExitStack()
            xpad = sbs(esC, f"d_xpad_{h}", [128, S + 4], F32)
            acc = sbs(esC, f"d_acc_{h}", [128, S], F32)
            cw = sbs(esC, f"d_cw_{h}", [128, 5], F32)
            rst = sbs(esC, f"d_rst_{h}", [128, 512], F32)
            sqb = sbs(esC, f"d_sqb_{h}", [128, 512], BF16)
            P.op('dve', lambda e: e.memset(xpad[:, 0:2], 0.0), w=['d_xpad'])
            P.op('dve', lambda e: e.memset(xpad[:, S + 2:S + 4], 0.0), w=['d_xpad'])
            for ci in range(3):
                ch = ci * 8 + h
                P.op('sp', lambda e: e.dma_start(out=cw[:], in_=conv_wT[ch * 128:(ch + 1) * 128, :]), w=['d_cw'], dma=True)
                P.op('pool', lambda e: e.dma_start(out=xpad[:, 2:S + 2], in_=qkvT[ch, :, :]), r=[('scr', 'dst_qkv')], w=['d_xpad'], dma=True)
                eng = 'dve'
                P.op(eng, lambda e: e.tensor_scalar(out=acc[:], in0=xpad[:, 0:S], scalar1=cw[:, 0:1], scalar2=None, op0=ALU.mult), r=['d_xpad', 'd_cw'], w=['d_acc'])
                for j in range(1, 5):
                    P.op(eng, lambda e: e.scalar_tensor_tensor(out=acc[:], in0=xpad[:, j:j + S], scalar=cw[:, j:j + 1], in1=acc[:], op0=ALU.mult, op1=ALU.add),
                         r=['d_xpad', 'd_cw', 'd_acc'], w=['d_acc'])
                P.op('act', lambda e: e.activation(out=acc[:], in_=acc[:], func=AF.Silu), r=['d_acc'], w=['d_acc'])
                if ci == 2:
                    P.op('dve', lambda e: e.tensor_copy(out=QKV[2][:], in_=acc[:]), r=['d_acc'], w=['d_qkv2'])
                else:
                    for g in range(8):
                        gs_ = slice(g * 512, (g + 1) * 512)
                        P.op('act', lambda e: e.activation(out=sqb[:], in_=acc[:, gs_], func=AF.Square), r=['d_acc'], w=['d_sqb'])
                        P.op('pe', lambda e: e.matmul(pM[:], lhsT=onesb[:], rhs=sqb[:], start=True, stop=True), r=['d_onesb', 'd_sqb'], w=[kM])
                        P.op('act', lambda e: e.activation(out=rst[:], in_=pM[:], func=AF.Sqrt, bias=eps_l2[:], scale=1.0), r=[kM, 'eps_l2'], w=['d_rst'])
                        P.op('dve', lambda e: e.reciprocal(out=rst[:], in_=rst[:]), r=['d_rst'], w=['d_rst'])
                        P.op('dve', lambda e: e.scalar_tensor_tensor(out=QKV[ci][:, gs_], in0=acc[:, gs_], scalar=(QSC if ci == 0 else 1.0), in1=rst[:], op0=ALU.mult, op1=ALU.mult),
                             r=['d_acc', 'd_rst'], w=[f'd_qkv{ci}'])
            Qt, Kt, Vch = QKV
            stop(2)
            P.barrier()
            esC.close()
            esW = ExitStack()
            Ktok = sbs(esW, f"d_Ktok_{h}", [128, 2, 128], BF16)
            Vtok = sbs(esW, f"d_Vtok_{h}", [128, 2, 128], BF16)
            Dg = sbs(esW, f"d_Dg_{h}", [128, G, 128], F32)
            tA = sbs(esW, f"d_tA_{h}", [128, G, 128], F32)
            tI = sbs(esW, f"d_tI_{h}", [128, G, 128], F32)
            EGB = sbs(esW, f"d_EGB_{h}", [128, G, 128], F32)
            A32 = sbs(esW, f"d_A32_{h}", [128, G, 128], F32)
            AT32 = sbs(esW, f"d_AT32_{h}", [128, G, 128], F32)
            ZY = sbs(esW, f"d_ZY_{h}", [128, G, 2, 128], F32)
            ZTYT = sbs(esW, f"d_ZTYT_{h}", [128, G, 2, 128], F32)
            Lb = sbs(esW, f"d_Lb_{h}", [128, G, 128], BF16)
            LTb = sbs(esW, f"d_LTb_{h}", [128, G, 128], BF16)
            Qb = sbs(esW, f"d_Qb_{h}", [128, G, 128], BF16)
            Rb = sbs(esW, f"d_Rb_{h}", [128, G, 128], BF16)
            TT = sbs(esW, f"d_TT_{h}", [128, G, 128], BF16)
            Tb = sbs(esW, f"d_Tb_{h}", [128, G, 128], BF16)
            ZYb = sbs(esW, f"d_ZYb_{h}", [128, G, 2, 128], BF16)
            ZTYTb = sbs(esW, f"d_ZTYTb_{h}", [128, G, 2, 128], BF16)
            Kbe = sbs(esW, f"d_Kbe_{h}", [128, G, 128], BF16)
            Vb = sbs(esW, f"d_Vb_{h}", [128, G, 128], BF16)
            egc = sbs(esW, f"d_egc_{h}", [128, G], F32)
            ebh = sbs(esW, f"d_ebh_{h}", [128, NT, 2], F32)
            for d_ in range(2):
                P.op('act', lambda e: e.activation(out=ebh[:, :, d_], in_=tokS[:, :, d_ * 24 + 16 + h], func=AF.Exp), r=['tokS'], w=['d_ebh'])
                P.op('dve', lambda e: e.tensor_tensor(out=ebh[:, :, d_], in0=ebh[:, :, d_], in1=tokS[:, :, d_ * 24 + h], op=ALU.mult), r=['d_ebh', 'tokS'], w=['d_ebh'])
            zs_v = zs[:, h * 128:(h + 1) * 128].rearrange("(t p) c -> p t c", p=128)
            for q4 in range(8):
                P.op('sp', lambda e: e.dma_start(out=zsh[:, q4 * 4:(q4 + 1) * 4, :], in_=zs_v[:, q4 * 4:(q4 + 1) * 4, :]),
                     r=[('scr', 'zs', t_, b_) for t_ in range(q4 * 4, q4 * 4 + 4) for b_ in range(2)], w=['d_zsh'], dma=True)
            for t0 in range(0, NT, 2):
                units = [(ti, d) for ti in range(2) for d in range(2)]
                for ti in range(2):
                    ts_ = slice((t0 + ti) * 128, (t0 + ti + 1) * 128)
                    P.op('pe', lambda e: e.transpose(out=psT[0][:, ti * 256:ti * 256 + 128], in_=Kt[:, ts_], identity=identb[:]), r=['d_qkv1', 'identb'], w=['psT0'])
                    P.op('pe', lambda e: e.transpose(out=psT[0][:, ti * 256 + 128:ti * 256 + 256], in_=Vch[:, ts_], identity=identb[:]), r=['d_qkv2', 'identb'], w=['psT0'])
                    P.op('pe', lambda e: e.matmul(pM[:, ti * 256:ti * 256 + 128], lhsT=Kt[:, ts_], rhs=Kt[:, ts_], start=True, stop=True), r=['d_qkv1'], w=[kM])
                    P.op('pe', lambda e: e.matmul(pM[:, ti * 256 + 128:ti * 256 + 256], lhsT=Kt[:, ts_], rhs=Qt[:, ts_], start=True, stop=True), r=['d_qkv1', 'd_qkv0'], w=[kM])
                pT4 = psT[0][:].rearrange("p (a b n) -> p a b n", a=2, b=2)
                P.op('act', lambda e: e.copy(out=Ktok[:], in_=pT4[:, :, 0, :]), r=['psT0'], w=['d_Ktok'])
                P.op('act', lambda e: e.copy(out=Vtok[:], in_=pT4[:, :, 1, :]), r=['psT0'], w=['d_Vtok'])
                stop(31)
                for u, (ti, d) in enumerate(units):
                    t = t0 + ti
                    gcol = tokS[:, t, d * 24 + 16 + h:d * 24 + 17 + h]
                    P.op('dve', lambda e: e.tensor_scalar(out=Dg[:, u, :], in0=ident[:], scalar1=gcol, scalar2=None, op0=ALU.mult), r=['ident', 'tokS'], w=[('d_Dg', u)])
                    P.op('pe', lambda e: e.matmul(pG[:, u * 128:(u + 1) * 128], lhsT=ones_f[:], rhs=Dg[:, u, :], start=True, stop=True), r=['ones_f', ('d_Dg', u)], w=[kG])
                stop(32)
                P.op('act', lambda e: e.copy(out=EGB[:], in_=pG[:].rearrange("p (u n) -> p u n", u=G)), r=[kG], w=['d_GBs'])
                for u, (ti, d) in enumerate(units):
                    t = t0 + ti
                    gcol = tokS[:, t, d * 24 + 16 + h:d * 24 + 17 + h]
                    P.op('dve', lambda e: e.scalar_tensor_tensor(out=tA[:, u, :], in0=EGB[:, u, :], scalar=gcol, in1=msk[:, 2 * d, :], op0=ALU.subtract, op1=ALU.max),
                         r=['d_GBs', 'tokS', 'd_msk'], w=[('d_tA', u)])
                    P.op('dve', lambda e: e.scalar_tensor_tensor(out=tI[:, u, :], in0=EGB[:, u, :], scalar=gcol, in1=msk[:, 2 * d + 1, :], op0=ALU.subtract, op1=ALU.min),
                         r=['d_GBs', 'tokS', 'd_msk'], w=[('d_tI', u)])
                kTA = [('d_tA', u) for u in range(G)]
                kTI = [('d_tI', u) for u in range(G)]
                P.op('act', lambda e: e.activation(out=tA[:], in_=tA[:], func=AF.Exp, scale=-1.0), r=kTA, w=kTA)
                P.op('act', lambda e: e.activation(out=tI[:], in_=tI[:], func=AF.Exp), r=kTI, w=kTI)
                P.op('act', lambda e: e.activation(out=EGB[:], in_=EGB[:], func=AF.Exp), r=['d_GBs'] + kTA + kTI, w=['d_GBs'])
                stop(33)
                for u, (ti, d) in enumerate(units):
                    t = t0 + ti
                    ts_ = slice(t * 128, (t + 1) * 128)
                    bcol = tokS[:, t, d * 24 + h:d * 24 + h + 1]
                    lastc = 127 if d == 0 else 0
                    P.op('dve', lambda e: e.scalar_tensor_tensor(out=A32[:, u, :], in0=pM[:, ti * 256:ti * 256 + 128], scalar=bcol, in1=tA[:, u, :], op0=ALU.mult, op1=ALU.mult),
                         r=[kM, 'tokS', ('d_tA', u)], w=[('d_A32', u)])
                    P.op('pe', lambda e: e.transpose(out=pX0[:, u * 128:(u + 1) * 128], in_=A32[:, u, :], identity=ident[:]), r=[('d_A32', u), 'ident'], w=[kX0])
                for u, (ti, d) in enumerate(units):
                    t = t0 + ti
                    ts_ = slice(t * 128, (t + 1) * 128)
                    bcol = tokS[:, t, d * 24 + h:d * 24 + h + 1]
                    lastc = 127 if d == 0 else 0
                    P.op('dve', lambda e: e.tensor_tensor(out=ITst[:, t, d, :], in0=pM[:, ti * 256 + 128:ti * 256 + 256], in1=tI[:, u, :], op=ALU.mult),
                         r=[kM, ('d_tI', u)], w=[('d_IT', t, d)])
                    P.op('dve', lambda e: e.tensor_tensor(out=QDst[:, t, d, :], in0=Qt[:, ts_], in1=EGB[:, u, :], op=ALU.mult), r=['d_qkv0', 'd_GBs'], w=[('d_QD', t, d)])
                    P.op('act', lambda e: e.copy(out=egl[:, t, d:d + 1], in_=EGB[:, u, lastc:lastc + 1]), r=['d_GBs'], w=[('d_egl', t, d)])
                    P.op('act', lambda e: e.activation(out=KSst[:, t, d, :], in_=Ktok[:, ti, :], func=AF.Copy, scale=tI[:, u, lastc:lastc + 1]),
                         r=['d_Ktok', ('d_tI', u)], w=[('d_KS', t, d)])
                    P.op('act', lambda e: e.activation(out=Kbe[:, u, :], in_=Ktok[:, ti, :], func=AF.Copy, scale=ebh[:, t, d:d + 1]),
                         r=['d_Ktok', 'd_ebh'], w=[('d_Kbe', u)])
                    P.op('act', lambda e: e.activation(out=Vb[:, u, :], in_=Vtok[:, ti, :], func=AF.Copy, scale=bcol), r=['d_Vtok', 'tokS'], w=[('d_Vb', u)])
                stop(34)
                kA = [('d_A32', u) for u in range(G)]
                v3 = lambda p_: p_[:].rearrange("p (u n) -> p u n", u=G)
                lmv = lambda j_: lm[:, j_, :].rearrange("p (u n) -> p u n", u=G)
                P.op('act', lambda e: e.copy(out=AT32[:], in_=v3(pX0)), r=[kX0], w=['d_AT32'])
                P.op('dve', lambda e: e.scalar_tensor_tensor(out=ZY[:, :, 0, :], in0=A32[:], scalar=-1.0, in1=lmv(0), op0=ALU.mult, op1=ALU.mult), r=kA + ['d_lm'], w=['d_ZY'])
                P.op('dve', lambda e: e.scalar_tensor_tensor(out=ZTYT[:, :, 0, :], in0=AT32[:], scalar=-1.0, in1=lmv(0), op0=ALU.mult, op1=ALU.mult), r=['d_AT32', 'd_lm'], w=['d_ZTYT'])
                P.op('pool', lambda e: e.tensor_tensor(out=ZY[:, :, 1, :], in0=ZY[:, :, 0, :], in1=lmv(4), op=ALU.add), r=['d_ZY', 'd_lm'], w=['d_ZY'])
                P.op('dve', lambda e: e.tensor_tensor(out=ZTYT[:, :, 1, :], in0=ZTYT[:, :, 0, :], in1=lmv(4), op=ALU.add), r=['d_ZTYT', 'd_lm'], w=['d_ZTYT'])
                stop(35)
                P.op('act', lambda e: e.copy(out=ZYb[:], in_=ZY[:]), r=['d_ZY'], w=['d_ZYb'])
                P.op('dve', lambda e: e.tensor_copy(out=ZTYTb[:], in_=ZTYT[:]), r=['d_ZTYT'], w=['d_ZTYTb'])
                for u in range(G):
                    us_ = slice(u * 128, (u + 1) * 128)
                    P.op('pe', lambda e: e.matmul(pX0[:, us_], lhsT=ZTYTb[:, u, 0, :], rhs=ZYb[:, u, 0, :], start=True, stop=True), r=['d_ZYb', 'd_ZTYTb'], w=[kX0])
                    P.op('pe', lambda e: e.matmul(pX1[:, us_], lhsT=ZYb[:, u, 0, :], rhs=ZTYTb[:, u, 0, :], start=True, stop=True), r=['d_ZYb', 'd_ZTYTb'], w=[kX1])
                P.op('act', lambda e: e.copy(out=ZYb[:, :, 0, :], in_=v3(pX0)), r=[kX0], w=['d_ZYb'])
                P.op('dve', lambda e: e.tensor_copy(out=ZTYTb[:, :, 0, :], in_=v3(pX1)), r=[kX1], w=['d_ZTYTb'])
                for lvl in (1, 2):
                    for u in range(G):
                        px, kx = (pX0, kX0) if u < 2 else (pX1, kX1)
                        pz, kz = (pZT, kZT) if u < 2 else (pU, kU)
                        cs_ = slice((u % 2) * 256, (u % 2) * 256 + 256)
                        P.op('pe', lambda e: e.matmul(px[:, cs_], lhsT=ZTYTb[:, u, 0, :], rhs=ZYb[:, u, :, :].rearrange("p c n -> p (c n)"), start=True, stop=True), r=['d_ZYb', 'd_ZTYTb'], w=[kx])
                        P.op('pe', lambda e: e.matmul(pz[:, cs_], lhsT=ZYb[:, u, 0, :], rhs=ZTYTb[:, u, :, :].rearrange("p c n -> p (c n)"), start=True, stop=True), r=['d_ZYb', 'd_ZTYTb'], w=[kz])
                    for hf, (px, kx, pz, kz) in enumerate(((pX0, kX0, pZT, kZT), (pX1, kX1, pU, kU))):
                        p4 = px[:].rearrange("p (u c n) -> p u c n", u=2, c=2)
                        z4 = pz[:].rearrange("p (u c n) -> p u c n", u=2, c=2)
                        hs2 = slice(hf * 2, hf * 2 + 2)
                        P.op('act', lambda e: e.copy(out=ZYb[:, hs2, 0, :], in_=p4[:, :, 0, :]), r=[kx], w=['d_ZYb'])
                        P.op('dve', lambda e: e.tensor_tensor(out=ZY[:, hs2, 1, :], in0=p4[:, :, 1, :], in1=ZY[:, hs2, 1, :], op=ALU.add), r=[kx, 'd_ZY'], w=['d_ZY'])
                        P.op('act', lambda e: e.copy(out=ZTYTb[:, hs2, 0, :], in_=z4[:, :, 0, :]), r=[kz], w=['d_ZTYTb'])
                        P.op('dve', lambda e: e.tensor_tensor(out=ZTYT[:, hs2, 1, :], in0=z4[:, :, 1, :], in1=ZTYT[:, hs2, 1, :], op=ALU.add), r=[kz, 'd_ZTYT'], w=['d_ZTYT'])
                    P.op('act', lambda e: e.copy(out=ZYb[:, :, 1, :], in_=ZY[:, :, 1, :]), r=['d_ZY'], w=['d_ZYb'])
                    P.op('dve', lambda e: e.tensor_copy(out=ZTYTb[:, :, 1, :], in_=ZTYT[:, :, 1, :]), r=['d_ZTYT'], w=['d_ZTYTb'])
                for u in range(G):
                    us_ = slice(u * 128, (u + 1) * 128)
                    P.op('pe', lambda e: e.matmul(pX0[:, us_], lhsT=ZTYTb[:, u, 0, :], rhs=ZYb[:, u, 1, :], start=True, stop=True), r=['d_ZYb', 'd_ZTYTb'], w=[kX0])
                    P.op('pe', lambda e: e.matmul(pX1[:, us_], lhsT=ZYb[:, u, 0, :], rhs=ZTYTb[:, u, 1, :], start=True, stop=True), r=['d_ZYb', 'd_ZTYTb'], w=[kX1])
                P.op('dve', lambda e: e.tensor_tensor(out=ZY[:, :, 1, :], in0=v3(pX0), in1=ZY[:, :, 1, :], op=ALU.add), r=[kX0, 'd_ZY'], w=['d_ZY'])
                P.op('dve', lambda e: e.tensor_tensor(out=ZTYT[:, :, 1, :], in0=v3(pX1), in1=ZTYT[:, :, 1, :], op=ALU.add), r=[kX1, 'd_ZTYT'], w=['d_ZTYT'])
                P.op('act', lambda e: e.copy(out=TT[:], in_=ZTYT[:, :, 1, :]), r=['d_ZTYT'], w=['d_TT'])
                P.op('dve', lambda e: e.tensor_copy(out=Tb[:], in_=ZY[:, :, 1, :]), r=['d_ZY'], w=['d_Tb'])
                for li in range(3):
                    last = (li == 2)
                    P.op('pool', lambda e: e.tensor_tensor(out=Lb[:], in0=A32[:], in1=lmv(1 + li), op=ALU.mult), r=kA + ['d_lm'], w=['d_Lb'])
                    if not last:
                        P.op('dve', lambda e: e.tensor_tensor(out=LTb[:], in0=AT32[:], in1=lmv(1 + li), op=ALU.mult), r=['d_AT32', 'd_lm'], w=['d_LTb'])
                    for u in range(G):
                        us_ = slice(u * 128, (u + 1) * 128)
                        P.op('pe', lambda e: e.matmul(pX1[:, us_], lhsT=Lb[:, u, :], rhs=TT[:, u, :], start=True, stop=True), r=['d_Lb', 'd_TT'], w=[kX1])
                        if not last:
                            P.op('pe', lambda e: e.matmul(pX0[:, us_], lhsT=LTb[:, u, :], rhs=Tb[:, u, :], start=True, stop=True), r=['d_LTb', 'd_Tb'], w=[kX0])
                    P.op('act', lambda e: e.copy(out=Rb[:], in_=v3(pX1)), r=[kX1], w=['d_Rb'])
                    if not last:
                        P.op('dve', lambda e: e.tensor_copy(out=Qb[:], in_=v3(pX0)), r=[kX0], w=['d_Qb'])
                    for u in range(G):
                        us_ = slice(u * 128, (u + 1) * 128)
                        P.op('pe', lambda e: e.matmul(pU[:, us_], lhsT=Tb[:, u, :], rhs=Rb[:, u, :], start=True, stop=True), r=['d_Tb', 'd_Rb'], w=[kU])
                        if not last:
                            P.op('pe', lambda e: e.matmul(pZT[:, us_], lhsT=TT[:, u, :], rhs=Qb[:, u, :], start=True, stop=True), r=['d_TT', 'd_Qb'], w=[kZT])
                    if not last:
                        P.op('dve', lambda e: e.tensor_tensor(out=ZTYT[:, :, 1, :], in0=ZTYT[:, :, 1, :], in1=v3(pU), op=ALU.subtract), r=[kU, 'd_ZTYT'], w=['d_ZTYT'])
                        P.op('dve', lambda e: e.tensor_tensor(out=ZY[:, :, 1, :], in0=ZY[:, :, 1, :], in1=v3(pZT), op=ALU.subtract), r=[kZT, 'd_ZY'], w=['d_ZY'])
                        P.op('act', lambda e: e.copy(out=TT[:], in_=ZTYT[:, :, 1, :]), r=['d_ZTYT'], w=['d_TT'])
                        P.op('act', lambda e: e.copy(out=Tb[:], in_=ZY[:, :, 1, :]), r=['d_ZY'], w=['d_Tb'])
                    else:
                        P.op('dve', lambda e: e.tensor_tensor(out=TT[:], in0=ZTYT[:, :, 1, :], in1=v3(pU), op=ALU.subtract), r=[kU, 'd_ZTYT'], w=['d_TT'])
                stop(36)
                for u, (ti, d) in enumerate(units):
                    P.op('pe', lambda e: e.matmul(pU[:, u * 128:(u + 1) * 128], lhsT=TT[:, u, :], rhs=Vb[:, u, :], start=True, stop=True), r=['d_TT', ('d_Vb', u)], w=[kU])
                    P.op('pe', lambda e: e.matmul(pG[:, u * 128:(u + 1) * 128], lhsT=Kbe[:, u, :], rhs=TT[:, u, :], start=True, stop=True), r=['d_TT', ('d_Kbe', u)], w=[kG])
                P.op('act', lambda e: e.copy(out=Ust[:, t0:t0 + 2, :, :].rearrange("p a b n -> p (a b) n"), in_=pU[:].rearrange("p (u n) -> p u n", u=G)), r=[kU], w=[('d_U', t0)])
                P.op('dve', lambda e: e.tensor_copy(out=WTst[:, t0:t0 + 2, :, :].rearrange("p a b n -> p (a b) n"), in_=pG[:].rearrange("p (u n) -> p u n", u=G)), r=[kG], w=[('d_WT', t0)])
                stop(3)
            P.barrier()
            stop(4)
            for d in range(2):
                P.op('dve', lambda e: e.memset(S32[d][:], 0.0), w=[f'd_S32{d}'])
                P.op('dve', lambda e: e.memset(Sbf[d][:], 0.0), w=[f'd_Sbf{d}'])
            for step in range(NT):
                tt = [step, NT - 1 - step]
                bank = [((pM, kM), (pX0, kX0), (pZT, kZT)), ((pG, kG), (pX1, kX1), (pU, kU))]
                for d in range(2):
                    t = tt[d]; t0 = (t // 2) * 2
                    (pW, kW) = bank[d][0]
                    P.op('pe', lambda e: e.matmul(pW[:, 0:128], lhsT=WTst[:, t, d, :], rhs=Sbf[d][:], start=True, stop=True), r=[('d_WT', t0), f'd_Sbf{d}'], w=[kW])
                for d in range(2):
                    t = tt[d]; t0 = (t // 2) * 2
                    (pW, kW), (pO, kO) = bank[d][0], bank[d][1]
                    P.op('dve', lambda e: e.tensor_tensor(out=Vn[d][:], in0=Ust[:, t, d, :], in1=pW[:, 0:128], op=ALU.subtract), r=[('d_U', t0), kW], w=[f'd_Vn{d}'])
                    P.op('pe', lambda e: e.matmul(pO[:, 0:128], lhsT=QDst[:, t, d, :], rhs=Sbf[d][:], start=True, stop=False), r=[('d_QD', t, d), f'd_Sbf{d}'], w=[kO])
                for d in range(2):
                    t = tt[d]
                    (pO, kO), (pD, kD) = bank[d][1], bank[d][2]
                    P.op('pe', lambda e: e.matmul(pO[:, 0:128], lhsT=ITst[:, t, d, :], rhs=Vn[d][:], start=False, stop=True), r=[('d_IT', t, d), f'd_Vn{d}'], w=[kO])
                    P.op('pe', lambda e: e.matmul(pD[:, 0:128], lhsT=KSst[:, t, d, :], rhs=Vn[d][:], start=True, stop=True), r=[('d_KS', t, d), f'd_Vn{d}'], w=[kD])
                for d in range(2):
                    t = tt[d]
                    (pO, kO), (pD, kD) = bank[d][1], bank[d][2]
                    P.op('dve', lambda e: e.scalar_tensor_tensor(out=Sbf[d][:], in0=S32[d][:], scalar=egl[:, t, d:d + 1], in1=pD[:, 0:128], op0=ALU.mult, op1=ALU.add),
                         r=[f'd_S32{d}', ('d_egl', t, d), kD], w=[f'd_Sbf{d}'])
                    P.op('dve', lambda e: e.scalar_tensor_tensor(out=S32[d][:], in0=S32[d][:], scalar=egl[:, t, d:d + 1], in1=pD[:, 0:128], op0=ALU.mult, op1=ALU.add),
                         r=[f'd_S32{d}', ('d_egl', t, d), kD], w=[f'd_S32{d}'])
                    if step < NT // 2:
                        P.op('act', lambda e: e.copy(out=Oacc[:, t, :], in_=pO[:, 0:128]), r=[kO], w=[('d_Oacc', t)])
                    else:
                        P.op('pool', lambda e: e.tensor_tensor(out=Oacc[:, t, :], in0=Oacc[:, t, :], in1=Oacc[:, t, :], op=ALU.add), r=[('d_Oacc', t)], w=[('d_Oacc', t)]) if False else \
                            P.op('dve', lambda e: e.tensor_tensor(out=Oacc[:, t, :], in0=pO[:, 0:128], in1=Oacc[:, t, :], op=ALU.add), r=[kO, ('d_Oacc', t)], w=[('d_Oacc', t)])
            stop(5)
            osq = [sbs(esW, f"d_osq{i_}_{h}", [128, 128], F32) for i_ in range(2)]
            ors = sbs(esW, f"d_ors_{h}", [128, 2, NT], F32)
            ont = [sbs(esW, f"d_ont{i_}_{h}", [128, 128], F32) for i_ in range(4)]
            onbt = [sbs(esW, f"d_onbt{i_}_{h}", [128, 128], BF16) for i_ in range(4)]
            kO_all = [('d_Oacc', t_) for t_ in range(NT)]
            P.op('dve', lambda e: e.memset(ors[:], 0.0), w=['d_ors'])
            for t in range(NT):
                P.op('act', lambda e: e.activation(out=osq[t % 2][:], in_=Oacc[:, t, :], func=AF.Square, accum_out=ors[:, 0, t:t + 1]), r=[('d_Oacc', t), 'd_ors'], w=[f'd_osq{t % 2}', ('d_ors_acc', t)])
            P.op('act', lambda e: e.activation(out=ors[:, 1, :], in_=ors[:, 0, :], func=AF.Sqrt, scale=1.0 / 128, bias=eps_l2[:]), r=['d_ors', 'eps_l2'] + [('d_ors_acc', t_) for t_ in range(NT)], w=['d_ors'])
            P.op('dve', lambda e: e.reciprocal(out=ors[:, 1, :], in_=ors[:, 1, :]), r=['d_ors'], w=['d_ors'])
            for t4 in range(0, NT, 4):
                for j in range(4):
                    t = t4 + j
                    P.op('dve', lambda e: e.scalar_tensor_tensor(out=ont[j][:], in0=Oacc[:, t, :], scalar=ors[:, 1, t:t + 1], in1=gain[:], op0=ALU.mult, op1=ALU.mult),
                         r=[('d_Oacc', t), 'd_ors', 'd_gain'], w=[f'd_ont{j}'])
                    P.op('pool', lambda e: e.tensor_tensor(out=onbt[j][:], in0=ont[j][:], in1=zsh[:, t, :], op=ALU.mult), r=[f'd_ont{j}', 'd_zsh'], w=[f'd_onbt{j}'])
                    P.op('pe', lambda e: e.transpose(out=psT[1][:, j * 128:(j + 1) * 128], in_=onbt[j][:], identity=identb[:]), r=[f'd_onbt{j}', 'identb'], w=['psT1'])
                P.op('act', lambda e: e.copy(out=oTd[:, t4 * 128:(t4 + 4) * 128], in_=psT[1][:]), r=['psT1'], w=['d_oTd'])
            fin.append(P.op('sp', lambda e: e.dma_start(out=oT_dn[h, :, :], in_=oTd[:]), r=['d_oTd'], w=[('scr', 'oT_dn', h)], dma=True))
            P.barrier()
            esW.close()
        P.barrier()
        esD.close()

    def phase_MG_MOE():
        esP = ExitStack()
        affTM = sbs(esP, "affTM", [128, NT, 16], F32)
        posm = sbs(esP, "posm", [128, NT, 16], F32)
        iot = sbs(esP, "iot", [128, 512], F32)
        bcs = sbs(esP, "bcs", [128, 4, D], F32)
        gfin = sbs(esP, "gfin", [128, D], F32)
        onesb = sbs(esP, "m_onesb", [128, 128], BF16)
        trib = sbs(esP, "trib", [128, 128], BF16)
        st2 = sbs(esP, "st2", [128, 8], F32)
        P.op('dve', lambda e: e.memset(onesb[:], 1.0), w=['m_onesb'])
        P.op('sp', lambda e: e.dma_start(out=iot[:], in_=iota512[:, :]), w=['iot'], dma=True)
        P.op('sp', lambda e: e.dma_start(out=gfin[:], in_=g_final[:, :]), w=['gfin'], dma=True)
        esH = ExitStack()
        h2b = sbs(esH, "h2b", [128, NT, D], BF16)
        esT = ExitStack()
        affT = sbs(esT, "affT", [16, S], F32)
        esG = ExitStack()
        es1 = ExitStack()
        mrow = sbs(es1, "g_mrow", [1, 4 * D], F32)
        trif = sbs(es1, "g_trif", [128, 128], F32)
        gff = sbs(es1, "g_gff", [128, D], F32)
        P.op('sp', lambda e: e.dma_start(out=mrow[:], in_=modrow[:, 2 * D:6 * D]), r=['mod'], w=['g_mrow'], dma=True)
        P.op('sp', lambda e: e.dma_start(out=trif[:], in_=tri_in[:, :]), w=['g_trif'], dma=True)
        P.op('dve', lambda e: e.tensor_copy(out=trib[:], in_=trif[:]), r=['g_trif'], w=['trib'])
        P.op('sp', lambda e: e.dma_start(out=gff[:], in_=g_ffn[:, :]), w=['g_gff'], dma=True)
        for j in range(8):
            src = j // 2
            dsti = {0: 0, 1: 2, 2: 1, 3: 3}[src]
            pt, pk = next_psA()
            P.op('pe', lambda e: e.matmul(pt[:], lhsT=ones_f[0:1, :], rhs=mrow[:, j * 512:(j + 1) * 512], start=True, stop=True), r=['ones_f', 'g_mrow'], w=[pk])
            P.op('act', lambda e: e.copy(out=bcs[:, dsti, (j % 2) * 512:(j % 2 + 1) * 512], in_=pt[:]), r=[pk], w=['bcs'])
        P.op('dve', lambda e: e.scalar_tensor_tensor(out=bcs[:, 1, :], in0=bcs[:, 1, :], scalar=1.0, in1=gff[:], op0=ALU.add, op1=ALU.mult), r=['bcs', 'g_gff'], w=['bcs'])
        P.barrier()
        es1.close()
        wod = sbs(esG, "g_wod", [128, 8, D], BF16)
        wom = sbs(esG, "g_wom", [128, 8, D], BF16)
        wou = sbs(esG, "g_wou", [128, 8, D], BF16)
        wr = sbs(esG, "g_wr", [128, 8, 16], F32)
        for wt_, src_, k_ in ((wod, w_o_dn, 'g_wod'), (wom, w_o_mla, 'g_wom'), (wou, w_out, 'g_wou')):
            v = src_.rearrange("(k p) n -> p k n", p=128)
            for kk in range(0, 8, 2):
                P.op('pool', lambda e: e.dma_start(out=wt_[:, kk:kk + 2, :], in_=v[:, kk:kk + 2, :]), w=[k_], dma=True)
        P.op('sp', lambda e: e.dma_start(out=wr[:], in_=w_router.rearrange("(k p) n -> p k n", p=128)), w=['g_wr'], dma=True)
        odn = [sbs(esG, f"g_odn{i}", [128, 8, 128], BF16) for i in range(2)]
        oml = [sbs(esG, f"g_oml{i}", [128, 8, 128], BF16) for i in range(2)]
        gst = [sbs(esG, f"g_gst{i}", [128, 2 * D], BF16) for i in range(2)]
        xt0 = sbs(esG, "g_xt0", [128, D], F32)
        xt = [xt0, xt0]
        m1 = sbs(esG, "g_m1", [128, D], F32)
        m2 = sbs(esG, "g_m2", [128, D], F32)
        mb = sbs(esG, "g_mb", [128, D], BF16)
        mT = sbs(esG, "g_mT", [128, 8, 128], BF16)
        x1t = [sbs(esG, f"g_x1t{i}", [128, D], F32) for i in range(2)]
        h2f = sbs(esG, "g_h2f", [128, D], F32)
        h2T = sbs(esG, "g_h2T", [128, 8, 128], F32)
        lg = sbs(esG, "g_lg", [128, 16], F32)
        for t in range(NT):
            i = t % 2
            ts_ = slice(t * 128, (t + 1) * 128)
            P.op('sp', lambda e: e.dma_start(out=odn[i][:], in_=oT_dn[:, :, ts_].rearrange("h p n -> p h n")), r=[('scr', 'oT_dn', h_) for h_ in range(8)], w=[f'g_odn{i}'], dma=True)
            P.op('sp', lambda e: e.dma_start(out=oml[i][:], in_=oT_mla[:, :, ts_].rearrange("h p n -> p h n")), r=[('scr', 'oT_mla', h_) for h_ in range(8)], w=[f'g_oml{i}'], dma=True)
            P.op('sp', lambda e: e.dma_start(out=gst[i][:], in_=gs[ts_, :]), r=[('scr', 'gs', t, b_) for b_ in range(4)], w=[f'g_gst{i}'], dma=True)
            P.op('sp', lambda e: e.dma_start(out=xt[i][:], in_=x[ts_, :]), w=['g_xt0'], dma=True)
            for br, (o_, ok_, w_, wk_) in enumerate(((odn[i], f'g_odn{i}', wod, 'g_wod'), (oml[i], f'g_oml{i}', wom, 'g_wom'))):
                for hc in range(2):
                    pt, pk = psA[br * 2 + hc], f'psA{br * 2 + hc}'
                    for k in range(8):
                        P.op('pe', lambda e: e.matmul(pt[:], lhsT=o_[:, k, :], rhs=w_[:, k, hc * 512:(hc + 1) * 512], start=(k == 0), stop=(k == 7)), r=[ok_, wk_], w=[pk])
            for hc in range(2):
                hs_ = slice(hc * 512, (hc + 1) * 512)
                P.op('dve', lambda e: e.tensor_tensor(out=m1[:, hs_], in0=psA[hc][:], in1=gst[i][:, hs_], op=ALU.mult), r=[f'psA{hc}', f'g_gst{i}'], w=['g_m1'])
                P.op('dve', lambda e: e.tensor_tensor(out=m2[:, hs_], in0=psA[2 + hc][:], in1=gst[i][:, D + hc * 512:D + (hc + 1) * 512], op=ALU.mult), r=[f'psA{2 + hc}', f'g_gst{i}'], w=['g_m2'])
            P.op('pool', lambda e: e.tensor_tensor(out=mb[:], in0=m1[:], in1=m2[:], op=ALU.add), r=['g_m1', 'g_m2'], w=['g_mb'])
            for half in range(2):
                for k4 in range(4):
                    k = half * 4 + k4
                    P.op('pe', lambda e: e.transpose(out=psT[half][:, k4 * 128:(k4 + 1) * 128], in_=mb[:, k * 128:(k + 1) * 128], identity=identb[:]), r=['g_mb', 'identb'], w=[f'psT{half}'])
                P.op('act', lambda e: e.copy(out=mT[:, half * 4:(half + 1) * 4, :], in_=psT[half][:].rearrange("p (k n) -> p k n", k=4)), r=[f'psT{half}'], w=['g_mT'])
            for hc in range(2):
                hs_ = slice(hc * 512, (hc + 1) * 512)
                for k in range(8):
                    P.op('pe', lambda e: e.matmul(psS[hc][:], lhsT=mT[:, k, :], rhs=wou[:, k, hs_], start=(k == 0), stop=(k == 7)), r=['g_mT', 'g_wou'], w=[f'psS{hc}'])
                P.op('dve', lambda e: e.tensor_tensor(out=x1t[i][:, hs_], in0=psS[hc][:], in1=bcs[:, 0, hs_], op=ALU.mult), r=[f'psS{hc}', 'bcs'], w=[f'g_x1t{i}'])
            P.op('pool', lambda e: e.tensor_tensor(out=x1t[i][:], in0=x1t[i][:], in1=xt[i][:], op=ALU.add), r=[f'g_x1t{i}', 'g_xt0'], w=[f'g_x1t{i}'])
            fin.append(P.op('sp', lambda e: e.dma_start(out=x1s[ts_, :], in_=x1t[i][:]), r=[f'g_x1t{i}'], w=[('scr', 'x1s', t)], dma=True))
            P.op('dve', lambda e: e.memset(st2[:], 0.0), w=['st2'])
            P.op('act', lambda e: e.activation(out=m1[:], in_=x1t[i][:], func=AF.Square, accum_out=st2[:, 0:1]), r=[f'g_x1t{i}'], w=['g_m1', 'st2'])
            P.op('act', lambda e: e.activation(out=st2[:, 1:2], in_=st2[:, 0:1], func=AF.Sqrt, scale=1.0 / D, bias=epsb[:]), r=['st2', 'epsb'], w=['st2'])
            P.op('dve', lambda e: e.reciprocal(out=st2[:, 2:3], in_=st2[:, 1:2]), r=['st2'], w=['st2'])
            P.op('dve', lambda e: e.scalar_tensor_tensor(out=h2f[:], in0=x1t[i][:], scalar=st2[:, 2:3], in1=bcs[:, 1, :], op0=ALU.mult, op1=ALU.mult), r=[f'g_x1t{i}', 'st2', 'bcs'], w=['g_h2f'])
            P.op('pool', lambda e: e.tensor_tensor(out=h2f[:], in0=h2f[:], in1=bcs[:, 2, :], op=ALU.add), r=['g_h2f', 'bcs'], w=['g_h2f'])
            P.op('act', lambda e: e.copy(out=h2b[:, t, :], in_=h2f[:]), r=['g_h2f'], w=[('h2b', t)])
            for half in range(2):
                for k4 in range(4):
                    k = half * 4 + k4
                    P.op('pe', lambda e: e.transpose(out=psA[half][:, k4 * 128:(k4 + 1) * 128], in_=h2f[:, k * 128:(k + 1) * 128], identity=ident[:]), r=['g_h2f', 'ident'], w=[f'psA{half}'])
                P.op('act', lambda e: e.copy(out=h2T[:, half * 4:(half + 1) * 4, :], in_=psA[half][:].rearrange("p (k n) -> p k n", k=4)), r=[f'psA{half}'], w=['g_h2T'])
            for k in range(8):
                P.op('pe', lambda e: e.matmul(psA[2][:, 0:16], lhsT=h2T[:, k, :], rhs=wr[:, k, :], start=(k == 0), stop=(k == 7)), r=['g_h2T', 'g_wr'], w=['psA2'])
            P.op('dve', lambda e: e.tensor_reduce(out=st2[:, 3:4], in_=psA[2][:, 0:16], axis=AX.X, op=ALU.max), r=['psA2'], w=['st2'])
            P.op('dve', lambda e: e.tensor_scalar(out=st2[:, 4:5], in0=st2[:, 3:4], scalar1=-1.0, scalar2=None, op0=ALU.mult), r=['st2'], w=['st2'])
            P.op('dve', lambda e: e.memset(st2[:, 5:6], 0.0), r=['st2'], w=['st2'])
            P.op('act', lambda e: e.activation(out=lg[:], in_=psA[2][:, 0:16], func=AF.Exp, bias=st2[:, 4:5], scale=1.0, accum_out=st2[:, 5:6]), r=['psA2', 'st2'], w=['g_lg', 'st2'])
            P.op('dve', lambda e: e.reciprocal(out=st2[:, 6:7], in_=st2[:, 5:6]), r=['st2'], w=['st2'])
            P.op('dve', lambda e: e.tensor_scalar(out=affTM[:, t, :], in0=lg[:], scalar1=st2[:, 6:7], scalar2=None, op0=ALU.mult), r=['g_lg', 'st2'], w=[('affTM', t)])
            P.op('pe', lambda e: e.transpose(out=psA[3][0:16, 0:128], in_=affTM[:, t, :], identity=ident[:]), r=[('affTM', t), 'ident'], w=['psA3'])
            P.op('act', lambda e: e.copy(out=affT[:, ts_], in_=psA[3][0:16, 0:128]), r=['psA3'], w=['affT'])
        P.barrier()
        esG.close()
        esE = ExitStack()
        es1 = ExitStack()
        cmpb = sbs(es1, "e_cmp", [16, S], F32)
        bis = sbs(es1, "e_bis", [16, 8], F32)
        maskT = sbs(es1, "e_maskT", [16, S], F32)
        P.op('dve', lambda e: e.memset(bis[:], 0.0), w=['e_bis'])
        P.op('dve', lambda e: e.memset(bis[:, 1:2], 1.0), r=['e_bis'], w=['e_bis'])
        for it in range(32):
            P.op('dve', lambda e: e.tensor_tensor(out=bis[:, 2:3], in0=bis[:, 0:1], in1=bis[:, 1:2], op=ALU.add), r=['e_bis'], w=['e_bis'])
            P.op('dve', lambda e: e.tensor_scalar(out=bis[:, 2:3], in0=bis[:, 2:3], scalar1=0.5, scalar2=None, op0=ALU.mult), r=['e_bis'], w=['e_bis'])
            P.op('dve', lambda e: e.tensor_scalar(out=cmpb[:], in0=affT[:], scalar1=bis[:, 2:3], scalar2=None, op0=ALU.is_ge), r=['affT', 'e_bis'], w=['e_cmp'])
            P.op('dve', lambda e: e.tensor_reduce(out=bis[:, 3:4], in_=cmpb[:], axis=AX.X, op=ALU.add), r=['e_cmp'], w=['e_bis'])
            P.op('dve', lambda e: e.tensor_scalar(out=bis[:, 4:5], in0=bis[:, 3:4], scalar1=511.5, scalar2=None, op0=ALU.is_ge), r=['e_bis'], w=['e_bis'])
            P.op('dve', lambda e: e.tensor_tensor(out=bis[:, 5:6], in0=bis[:, 2:3], in1=bis[:, 0:1], op=ALU.subtract), r=['e_bis'], w=['e_bis'])
            P.op('dve', lambda e: e.tensor_tensor(out=bis[:, 6:7], in0=bis[:, 1:2], in1=bis[:, 2:3], op=ALU.subtract), r=['e_bis'], w=['e_bis'])
            P.op('dve', lambda e: e.scalar_tensor_tensor(out=bis[:, 0:1], in0=bis[:, 5:6], scalar=bis[:, 4:5], in1=bis[:, 0:1], op0=ALU.mult, op1=ALU.add), r=['e_bis'], w=['e_bis'])
            P.op('dve', lambda e: e.scalar_tensor_tensor(out=bis[:, 1:2], in0=bis[:, 6:7], scalar=bis[:, 4:5], in1=bis[:, 2:3], op0=ALU.mult, op1=ALU.add), r=['e_bis'], w=['e_bis'])
        P.op('dve', lambda e: e.tensor_scalar(out=maskT[:], in0=affT[:], scalar1=bis[:, 0:1], scalar2=None, op0=ALU.is_ge), r=['affT', 'e_bis'], w=['e_maskT'])
        mk32 = sbs(es1, "e_mk32", [128, 16], F32)
        mkb = sbs(es1, "e_mkb", [128, 16], BF16)
        base = sbs(es1, "e_base", [128, 16], F32)
        ptmp = sbs(es1, "e_ptmp", [128, 16], F32)
        P.op('dve', lambda e: e.memset(base[:], 0.0), w=['e_base'])
        for t in range(NT):
            ts_ = slice(t * 128, (t + 1) * 128)
            P.op('pe', lambda e: e.transpose(out=psA[0][:, 0:16], in_=maskT[:, ts_], identity=ident[0:16, 0:16]), r=['e_maskT', 'ident'], w=['psA0'])
            P.op('act', lambda e: e.copy(out=mk32[:], in_=psA[0][:, 0:16]), r=['psA0'], w=['e_mk32'])
            P.op('dve', lambda e: e.tensor_copy(out=mkb[:], in_=mk32[:]), r=['e_mk32'], w=['e_mkb'])
            P.op('pe', lambda e: e.matmul(psA[1][:, 0:16], lhsT=trib[:], rhs=mkb[:], start=True, stop=True), r=['trib', 'e_mkb'], w=['psA1'])
            P.op('pe', lambda e: e.matmul(psA[2][:, 0:16], lhsT=onesb[:], rhs=mkb[:], start=True, stop=True), r=['m_onesb', 'e_mkb'], w=['psA2'])
            P.op('dve', lambda e: e.tensor_tensor(out=ptmp[:], in0=psA[1][:, 0:16], in1=base[:], op=ALU.add), r=['psA1', 'e_base'], w=['e_ptmp'])
            P.op('dve', lambda e: e.scalar_tensor_tensor(out=ptmp[:], in0=ptmp[:], scalar=1.0, in1=mk32[:], op0=ALU.add, op1=ALU.mult), r=['e_ptmp', 'e_mk32'], w=['e_ptmp'])
            P.op('dve', lambda e: e.tensor_scalar(out=posm[:, t, :], in0=ptmp[:], scalar1=-1.0, scalar2=None, op0=ALU.add), r=['e_ptmp'], w=['posm'])
            P.op('dve', lambda e: e.tensor_tensor(out=base[:], in0=psA[2][:, 0:16], in1=base[:], op=ALU.add), r=['psA2', 'e_base'], w=['e_base'])
        P.barrier()
        es1.close()
        esT.close()
        wg = sbs(esE, "e_wg", [128, 8, D], BF16)
        wu = sbs(esE, "e_wu", [128, 8, D], BF16)
        wd = sbs(esE, "e_wd", [128, 8, D], BF16)
        Sel = sbs(esE, "e_Sel", [128, NT, 512], BF16)
        xeT = sbs(esE, "e_xeT", [128, 8, 512], BF16)
        hid = sbs(esE, "e_hid", [128, 8, 512], BF16)
        sg0 = sbs(esE, "e_sg0", [128, 512], F32)
        sg = [sg0, sg0]
        yeb = sbs(esE, "e_yeb", [128, 4, D], BF16)
        stgw = [sbs(esE, f"e_stg{i}", [128, D], F32) for i in range(2)]
        stg_n = [0]
        for ex in range(16):
            for wt_, src_, k_ in ((wg, w_gate, 'e_wg'), (wu, w_up, 'e_wu'), (wd, w_down, 'e_wd')):
                v = src_[ex].rearrange("(k p) n -> p k n", p=128)
                for kk in range(8):
                    if kk % 2 == 0:
                        P.op('pool', lambda e: e.dma_start(out=wt_[:, kk, :], in_=v[:, kk, :]), w=[k_], dma=True)
                    else:
                        j = stg_n[0] % 2
                        stg_n[0] += 1
                        P.op('sp', lambda e: e.dma_start(out=stgw[j][:], in_=v[:, kk, :]), w=[f'e_stg{j}'], dma=True)
                        P.op('act', lambda e: e.copy(out=wt_[:, kk, :], in_=stgw[j][:]), r=[f'e_stg{j}'], w=[k_])
            for t in range(NT):
                P.op('dve', lambda e: e.tensor_scalar(out=Sel[:, t, :], in0=iot[:], scalar1=posm[:, t, ex:ex + 1], scalar2=None, op0=ALU.is_equal), r=['iot', 'posm'], w=[('e_Sel', t)])
            for k in range(8):
                pt, pk = psA[k % 4], f'psA{k % 4}'
                for t in range(NT):
                    P.op('pe', lambda e: e.matmul(pt[:], lhsT=h2b[:, t, k * 128:(k + 1) * 128], rhs=Sel[:, t, :], start=(t == 0), stop=(t == NT - 1)), r=[('h2b', t), ('e_Sel', t)], w=[pk])
                if k % 2 == 0:
                    P.op('act', lambda e: e.copy(out=xeT[:, k, :], in_=pt[:]), r=[pk], w=['e_xeT'])
                else:
                    P.op('dve', lambda e: e.tensor_copy(out=xeT[:, k, :], in_=pt[:]), r=[pk], w=['e_xeT'])
            for f in range(8):
                j = f % 2
                pg, pgk = psA[j * 2], f'psA{j * 2}'
                pu, puk = psA[j * 2 + 1], f'psA{j * 2 + 1}'
                for k in range(8):
                    P.op('pe', lambda e: e.matmul(pg[:], lhsT=wg[:, k, f * 128:(f + 1) * 128], rhs=xeT[:, k, :], start=(k == 0), stop=(k == 7)), r=['e_wg', 'e_xeT'], w=[pgk])
                for k in range(8):
                    P.op('pe', lambda e: e.matmul(pu[:], lhsT=wu[:, k, f * 128:(f + 1) * 128], rhs=xeT[:, k, :], start=(k == 0), stop=(k == 7)), r=['e_wu', 'e_xeT'], w=[puk])
                P.op('act', lambda e: e.activation(out=sg[j][:], in_=pg[:], func=AF.Silu), r=[pgk], w=['e_sg0'])
                P.op('dve', lambda e: e.tensor_tensor(out=hid[:, f, :], in0=pu[:], in1=sg[j][:], op=ALU.mult), r=[puk, 'e_sg0'], w=['e_hid'])
            for q in range(4):
                for hc in range(2):
                    ps_y, pyk = psS[hc], f'psS{hc}'
                    for f in range(8):
                        P.op('pe', lambda e: e.matmul(ps_y[:], lhsT=hid[:, f, q * 128:(q + 1) * 128], rhs=wd[:, f, hc * 512:(hc + 1) * 512], start=(f == 0), stop=(f == 7)), r=['e_hid', 'e_wd'], w=[pyk])
                    if hc == 0:
                        P.op('act', lambda e: e.copy(out=yeb[:, q, 0:512], in_=ps_y[:]), r=[pyk], w=['e_yeb'])
                    else:
                        P.op('dve', lambda e: e.tensor_copy(out=yeb[:, q, 512:1024], in_=ps_y[:]), r=[pyk], w=['e_yeb'])
            fin.append(P.op('sp', lambda e: e.dma_start(out=ye_all[ex].rearrange("(q p) d -> p q d", p=128), in_=yeb[:]), r=['e_yeb'], w=[('scr', 'ye', ex)], dma=True))
        P.barrier()
        esE.close()
        esH.close()
        esF = ExitStack()
        yeA = sbs(esF, "f_yeA", [128, 16, 4, D], BF16)
        for ex in range(16):
            P.op('sp', lambda e: e.dma_start(out=yeA[:, ex, :, :], in_=ye_all[ex].rearrange("(q p) d -> p q d", p=128)), r=[('scr', 'ye', ex)], w=['f_yeA'], dma=True)
        Sg = [sbs(esF, f"f_Sg{i}", [128, 512], BF16) for i in range(2)]
        SgT = sbs(esF, "f_SgT", [128, 16, 4, 128], BF16)
        x1l = [sbs(esF, f"f_x1l{i}", [128, D], F32) for i in range(2)]
        ft = sbs(esF, "f_ft", [128, D], F32)
        ot = [sbs(esF, f"f_ot{i}", [128, D], F32) for i in range(2)]
        junk2 = sbs(esF, "f_junk", [128, D], F32)
        for t in range(NT // 2):
            i = t % 2
            ts_ = slice(t * 128, (t + 1) * 128)
            P.op('sp', lambda e: e.dma_start(out=x1l[i][:], in_=x1s[ts_, :]), r=[('scr', 'x1s', t)], w=[f'f_x1l{i}'], dma=True)
            for ex in range(16):
                j = ex % 2
                P.op('dve', lambda e: e.tensor_scalar(out=Sg[j][:], in0=iot[:], scalar1=posm[:, t, ex:ex + 1], scalar2=affTM[:, t, ex:ex + 1], op0=ALU.is_equal, op1=ALU.mult),
                     r=['iot', 'posm', ('affTM', t)], w=[f'f_Sg{j}'])
                for q in range(4):
                    P.op('pe', lambda e: e.transpose(out=psT[j][:, q * 128:(q + 1) * 128], in_=Sg[j][:, q * 128:(q + 1) * 128], identity=identb[:]), r=[f'f_Sg{j}', 'identb'], w=[f'psT{j}'])
                if j == 0:
                    P.op('act', lambda e: e.copy(out=SgT[:, ex, :, :], in_=psT[j][:].rearrange("p (q n) -> p q n", q=4)), r=[f'psT{j}'], w=[('f_SgT', ex)])
                else:
                    P.op('pool', lambda e: e.tensor_copy(out=SgT[:, ex, :, :], in_=psT[j][:].rearrange("p (q n) -> p q n", q=4)), r=[f'psT{j}'], w=[('f_SgT', ex)]) if False else \
                        P.op('act', lambda e: e.copy(out=SgT[:, ex, :, :], in_=psT[j][:].rearrange("p (q n) -> p q n", q=4)), r=[f'psT{j}'], w=[('f_SgT', ex)])
            for hc in range(2):
                hs_ = slice(hc * 512, (hc + 1) * 512)
                pt, pk = psA[(t % 2) * 2 + hc], f'psA{(t % 2) * 2 + hc}'
                n = 0
                for ex in range(16):
                    for q in range(4):
                        P.op('pe', lambda e: e.matmul(pt[:], lhsT=SgT[:, ex, q, :], rhs=yeA[:, ex, q, hs_], start=(n == 0), stop=(n == 63)), r=[('f_SgT', ex), 'f_yeA'], w=[pk])
                        n += 1
                P.op('dve', lambda e: e.tensor_tensor(out=ft[:, hs_], in0=pt[:], in1=bcs[:, 3, hs_], op=ALU.mult), r=[pk, 'bcs'], w=['f_ft'])
            P.op('pool', lambda e: e.tensor_tensor(out=ft[:], in0=ft[:], in1=x1l[i][:], op=ALU.add), r=['f_ft', f'f_x1l{i}'], w=['f_ft'])
            P.op('dve', lambda e: e.memset(st2[:], 0.0), w=['st2'])
            P.op('act', lambda e: e.activation(out=junk2[:], in_=ft[:], func=AF.Square, accum_out=st2[:, 0:1]), r=['f_ft'], w=['f_junk', 'st2'])
            P.op('act', lambda e: e.activation(out=st2[:, 1:2], in_=st2[:, 0:1], func=AF.Sqrt, scale=1.0 / D, bias=epsb[:]), r=['st2', 'epsb'], w=['st2'])
            P.op('dve', lambda e: e.reciprocal(out=st2[:, 2:3], in_=st2[:, 1:2]), r=['st2'], w=['st2'])
            P.op('dve', lambda e: e.scalar_tensor_tensor(out=ot[i][:], in0=ft[:], scalar=st2[:, 2:3], in1=gfin[:], op0=ALU.mult, op1=ALU.mult), r=['f_ft', 'st2', 'gfin'], w=[f'f_ot{i}'])
            fin.append(P.op('sp', lambda e: e.dma_start(out=out[ts_, :], in_=ot[i][:]), r=[f'f_ot{i}'], dma=True))
        P.barrier()
        esF.close()
        esP.close()

    if 'A' in stages:
        phase_A()
    if 'MLA' in stages:
        phase_MLA()
    if 'DN' in stages:
        try:
            phase_DN()
        except _Stop:
            P.barrier()
    if 'MG' in stages:
        phase_MG_MOE()
    P.finish(fin)
    print("instructions:", P.n)
    return nc


_invf = (10000.0 ** (-np.arange(32, dtype=np.float32) / np.float32(32))).astype(np.float32)
INVF2 = np.concatenate([_invf, _invf])[:, None].astype(np.float32)
SGN2 = np.concatenate([-np.ones(32), np.ones(32)])[:, None].astype(np.float32)
SEL64 = np.zeros((128, 65), np.float32)
SEL64[:, 64] = 1.0
_xi = np.arange(128)[:, None]
_yi = np.arange(128)[None, :]
BIG = 1.0e4
DMASKS = np.stack([np.where(_xi > _yi, 0.0, BIG), np.where(_yi >= _xi, 0.0, -BIG),
                   np.where(_xi < _yi, 0.0, BIG), np.where(_yi <= _xi, 0.0, -BIG)], axis=1).astype(np.float32)
def _bd(s_):
    return ((_xi // s_) == (_yi // s_)).astype(np.float32)
DN_LMASK = np.ascontiguousarray(np.stack([np.tile(m_, (1, 4)) for m_ in
                                          (_bd(16), _bd(32) - _bd(16), _bd(64) - _bd(32), 1.0 - _bd(64), np.eye(128, dtype=np.float32))], axis=1))
IOTA512 = np.ascontiguousarray(np.broadcast_to(np.arange(512, dtype=np.float32)[None, :], (128, 512)))
TRI = (_xi < _yi).astype(np.float32)
IN_SPL = np.cumsum([3072, 1024, 16, 16, 512, 256, 64, 2048])


def prep_inputs(inputs, core):
    b, half = core // 2, core % 2
    f = lambda a: np.ascontiguousarray(a, dtype=np.float32)
    w_in = inputs['w_in'][0]
    qkv, z, bb, aa, cq, ckv, kr, g = np.split(w_in, IN_SPL[:-1], axis=1)
    xb = inputs['x'][b]
    posb = inputs['positions'][b]
    conv_w = inputs['conv_w'][0]
    a_log, dt_bias = inputs['a_log'][0], inputs['dt_bias'][0]
    if half == 1:
        xb = xb[::-1]
        posb = posb[::-1]
        conv_w = conv_w[::-1]
        bb = np.concatenate([bb[:, 8:16], bb[:, 0:8]], axis=1)
        aa = np.concatenate([aa[:, 8:16], aa[:, 0:8]], axis=1)
        a_log, dt_bias = a_log[::-1], dt_bias[::-1]
    swap = np.concatenate([np.arange(32, 64), np.arange(0, 32)])
    wuq_ = inputs['w_uq'][0].reshape(512, 8, 192)
    wukv_ = inputs['w_ukv'][0].reshape(256, 8, 256)
    m = {
        'x': f(xb),
        'cT': f(inputs['c'][b].reshape(8, 128).T),
        'w_mod': f(inputs['w_mod'][0]),
        'b_mod': f(inputs['b_mod'][0][None, :]),
        'g_mix': f(np.broadcast_to(inputs['g_mix'][0][None, :], (128, D))),
        'w_qkv': f(qkv), 'w_z': f(z), 'w_g': f(g),
        'w_ba': f(np.concatenate([bb, aa], axis=1)),
        'w_cq': f(cq), 'w_ckv': f(ckv),
        'w_kr2': f(np.concatenate([kr, kr[:, swap]], axis=1)),
        'q_gain': f(np.broadcast_to(inputs['q_gain'][0][None, :], (128, 512))),
        'kv_gain': f(np.broadcast_to(inputs['kv_gain'][0][None, :], (128, 256))),
        'identf': np.eye(128, dtype=np.float32),
        'posr': np.ascontiguousarray(np.broadcast_to(posb[None, :], (64, S)).astype(np.int32)),
        'invf2': INVF2, 'sgn2': SGN2, 'sel64': SEL64,
        'w_uqh': f(np.stack([np.concatenate([wuq_[:, h, 0:128], wuq_[:, h, 128:192], wuq_[:, h, 128:192][:, swap]], axis=1) for h in range(8)])),
        'w_ukvh': f(np.stack([wukv_[:, h, :] for h in range(8)])),
        'conv_wT': f(conv_w.T),
        'dn_sc': f(np.stack([a_log[0], a_log[1], dt_bias[0], dt_bias[1]], axis=1)),
        'dn_gain': f(np.broadcast_to(inputs['dn_o_gain'][0][None, :], (128, 128))),
        'dmasks': DMASKS, 'dn_lmask': DN_LMASK,
        'w_o_dn': f(inputs['w_o_dn'][0]), 'w_o_mla': f(inputs['w_o_mla'][0]), 'w_out': f(inputs['w_out'][0]),
        'g_ffn': f(np.broadcast_to(inputs['g_ffn'][0][None, :], (128, D))),
        'g_final': f(np.broadcast_to(inputs['g_final'][None, :], (128, D))),
        'w_router': f(inputs['w_router'][0]),
        'w_gate': f(inputs['w_gate'][0]), 'w_up': f(inputs['w_up'][0]), 'w_down': f(inputs['w_down'][0]),
        'iota512': IOTA512, 'tri_in': TRI,
    }
    return m


def kernel(**inputs):
    inputs = {k: np.asarray(v) for k, v in inputs.items()}
    nc = build()
    in_maps = [prep_inputs(inputs, c) for c in range(8)]
    res = run_bass_kernel_spmd(nc, in_maps, core_ids=list(range(8)))
    outp = np.zeros((4, S, D), np.float32)
    for c in range(8):
        b, half = c // 2, c % 2
        o_ = res.results[c]["out"]
        if half == 0:
            outp[b, 0:2048] = o_
        else:
            outp[b, 2048:4096] = o_[::-1]
    return outp
```

```python
import numpy as np
import ml_dtypes
import concourse.bass as bass
import concourse.mybir as mybir
from concourse.bass_utils import run_bass_kernel_spmd
from contextlib import ExitStack

F32 = mybir.dt.float32
BF16 = mybir.dt.bfloat16
I32 = mybir.dt.int32
AF = mybir.ActivationFunctionType
ALU = mybir.AluOpType
AX = mybir.AxisListType

import os
DBGN = int(os.environ.get('DBGN', '99'))
S = 4096
D = 1024
NT = S // 128
EPS = 1e-6


class Prog:
    def __init__(self, nc, ndma=16):
        self.nc = nc
        self.eng = {'pe': nc.tensor, 'act': nc.scalar, 'dve': nc.vector,
                    'pool': nc.gpsimd, 'sp': nc.sync}
        self.sem = {k: nc.alloc_semaphore(name=f"s_{k}") for k in self.eng}
        self.cnt = {k: 0 for k in self.eng}
        self.waited = {k: {} for k in self.eng}
        self.dsem = {q: [nc.alloc_semaphore(name=f"s_dma_{q}{i}") for i in range(ndma)] for q in ('sp', 'pool')}
        self.dcnt = {q: [0] * ndma for q in ('sp', 'pool')}
        self.nd = {'sp': 0, 'pool': 0}
        self.lastw = {}
        self.readers = {}
        self.n = 0

    def _wait(self, e, tok):
        if tok is None:
            return
        sem, val, key = tok
        w = self.waited[e]
        if w.get(key, 0) >= val:
            return
        w[key] = val
        self.eng[e].wait_ge(sem, val)

    def op(self, e, fn, r=(), w=(), dma=False):
        w = list(w) + [t for t in r if isinstance(t, str) and t.startswith('ps') and t not in w]
        toks = []
        for t in r:
            toks.append(self.lastw.get(t))
        for t in w:
            toks.append(self.lastw.get(t))
            toks.extend(self.readers.get(t, ()))
        for tok in toks:
            self._wait(e, tok)
        if dma:
            i = self.nd[e] % len(self.dsem[e])
            if self.dcnt[e][i] > 0:
                self._wait(e, (self.dsem[e][i], self.dcnt[e][i], f"dma_{e}{i}"))
        ins = fn(self.eng[e])
        self.n += 1
        if dma:
            i = self.nd[e] % len(self.dsem[e])
            self.nd[e] += 1
            self.dcnt[e][i] += 16
            ins.then_inc(self.dsem[e][i], 16)
            tok = (self.dsem[e][i], self.dcnt[e][i], f"dma_{e}{i}")
        else:
            self.cnt[e] += 1
            ins.then_inc(self.sem[e], 1)
            tok = (self.sem[e], self.cnt[e], e)
        for t in r:
            self.readers.setdefault(t, []).append(tok)
        for t in w:
            self.lastw[t] = tok
            self.readers[t] = []
        return tok

    def barrier(self):
        toks = [(self.sem[k], self.cnt[k], k) for k in self.eng if self.cnt[k] > 0]
        toks += [(self.dsem[q][i], self.dcnt[q][i], f"dma_{q}{i}") for q in self.dsem for i in range(len(self.dsem[q])) if self.dcnt[q][i] > 0]
        for e in self.eng:
            for tok in toks:
                self._wait(e, tok)

    def finish(self, toks):
        for tok in toks:
            self._wait('sp', tok)


class _Stop(Exception):
    pass


def build(debug=None, stages=('A', 'MLA', 'DN', 'MG'), ext_in=(), dn_heads=range(8), dn_stop=0):
    nc = bass.Bass("TRN2", target_bir_lowering=False)
    P = Prog(nc)

    def din(name, shape, dt=F32):
        return nc.dram_tensor(name, list(shape), dt, kind="ExternalInput").ap()

    def dscr(name, shape, dt):
        kind = "ExternalOutput" if (debug and name in debug) else ("ExternalInput" if name in ext_in else "Internal")
        return nc.dram_tensor(name, list(shape), dt, kind=kind).ap()

    x = din("x", [S, D])
    cT = din("cT", [128, 8])
    w_mod = din("w_mod", [D, 6 * D])
    b_mod = din("b_mod", [1, 6 * D])
    g_mix = din("g_mix", [128, D])
    w_qkv = din("w_qkv", [D, 3072])
    w_z = din("w_z", [D, 1024])
    w_g = din("w_g", [D, 2048])
    w_ba = din("w_ba", [D, 32])
    w_cq = din("w_cq", [D, 512])
    w_ckv = din("w_ckv", [D, 256])
    w_kr2 = din("w_kr2", [D, 128])
    q_gain = din("q_gain", [128, 512])
    kv_gain = din("kv_gain", [128, 256])
    identf = din("identf", [128, 128])
    posr = din("posr", [64, S], I32)
    invf2 = din("invf2", [64, 1])
    sgn2 = din("sgn2", [64, 1])
    sel64 = din("sel64", [128, 65])
    w_uqh = din("w_uqh", [8, 512, 256])
    w_ukvh = din("w_ukvh", [8, 256, 256])
    conv_wT = din("conv_wT", [3072, 5])
    dn_sc = din("dn_sc", [8, 4])
    dn_gain = din("dn_gain", [128, 128])
    dmasks = din("dmasks", [128, 4, 128])
    dn_lmask = din("dn_lmask", [128, 5, 512])
    w_o_dn = din("w_o_dn", [D, D])
    w_o_mla = din("w_o_mla", [D, D])
    w_out = din("w_out", [D, D])
    g_ffn = din("g_ffn", [128, D])
    g_final = din("g_final", [128, D])
    w_router = din("w_router", [D, 16])
    w_gate = din("w_gate", [16, D, D])
    w_up = din("w_up", [16, D, D])
    w_down = din("w_down", [16, D, D])
    iota512 = din("iota512", [128, 512])
    tri_in = din("tri_in", [128, 128])
    out = nc.dram_tensor("out", [S // 2, D], F32, kind="ExternalOutput").ap()

    qkvT = dscr("qkvT", [24, 128, S], BF16)
    zs = dscr("zs", [S, 1024], BF16)
    gs = dscr("gs", [S, 2048], BF16)
    baT = dscr("baT", [32, S], F32)
    cqnT = dscr("cqnT", [4, 128, S], BF16)
    ckvnT = dscr("ckvnT", [2, 128, S], BF16)
    krT = dscr("krT", [2, 64, S], BF16)
    modrow = dscr("modrow", [1, 6 * D], F32)
    oT_mla = dscr("oT_mla", [8, 128, S], BF16)
    oT_dn = dscr("oT_dn", [8, 128, S], BF16)
    x1s = dscr("x1s", [S, D], F32)
    ye_all = dscr("ye_all", [16, 512, D], BF16)

    sb = lambda n, s, d: nc.alloc_sbuf_tensor(n, list(s), d)
    ps_ = lambda n, s, d=F32: nc.alloc_psum_tensor(n, list(s), d)

    ident = sb("ident", [128, 128], F32)
    identb = sb("identb", [128, 128], BF16)
    ones_f = sb("ones_f", [128, 128], F32)
    P.op('sp', lambda e: e.dma_start(out=ident[:], in_=identf[:, :]), w=['ident'], dma=True)
    P.op('dve', lambda e: e.tensor_copy(out=identb[:], in_=ident[:]), r=['ident'], w=['identb'])
    P.op('dve', lambda e: e.memset(ones_f[:], 1.0), w=['ones_f'])
    epsb = sb("epsb", [128, 1], F32)
    P.op('dve', lambda e: e.memset(epsb[:], EPS), w=['epsb'])

    psA = [ps_(f"psA{i}", [128, 512]) for i in range(4)]
    psT = [ps_(f"psT{i}", [128, 512], BF16) for i in range(2)]
    psS = [ps_(f"psS{i}", [128, 512]) for i in range(2)]
    pa_i = [0]

    def next_psA():
        i = pa_i[0] % 4
        pa_i[0] += 1
        return psA[i], f"psA{i}"

    sbs = lambda es, n, s_, d: es.enter_context(nc.sbuf_tensor(n, list(s_), d))
    fin = []

    def phase_A():
        cT_sb = sb("cT_sb", [128, 8], F32)
        scT = sb("scT", [128, 8], F32)
        P.op('sp', lambda e: e.dma_start(out=cT_sb[:], in_=cT[:, :]), w=['cT'], dma=True)
        P.op('act', lambda e: e.activation(out=scT[:], in_=cT_sb[:], func=AF.Silu), r=['cT'], w=['scT'])
        esA = ExitStack()
        modbc = sbs(esA, "modbc", [128, 6, D], F32)
        gmx = sbs(esA, "gmx", [128, D], F32)
        A1 = sbs(esA, "A1", [128, D], F32)
        es0 = ExitStack()
        mod_sb = sbs(es0, "mod_sb", [1, 6 * D], F32)
        bm_sb = sbs(es0, "bm_sb", [1, 6 * D], F32)
        P.op('sp', lambda e: e.dma_start(out=bm_sb[:], in_=b_mod[:, :]), w=['bm'], dma=True)
        wm = [sbs(es0, f"wm{i}", [128, 8, 512], F32) for i in range(2)]
        w_mod_v = w_mod.rearrange("(k p) n -> p k n", p=128)
        for j in range(12):
            wt, wk = wm[j % 2], f"wm{j % 2}"
            P.op('sp', lambda e: e.dma_start(out=wt[:], in_=w_mod_v[:, :, j * 512:(j + 1) * 512]), w=[wk], dma=True)
            pt, pk = next_psA()
            for k in range(8):
                P.op('pe', lambda e: e.matmul(pt[0:1, :], lhsT=scT[:, k:k + 1], rhs=wt[:, k, :], start=(k == 0), stop=(k == 7)),
                     r=[wk, 'scT'], w=[pk])
            P.op('dve', lambda e: e.tensor_tensor(out=mod_sb[:, j * 512:(j + 1) * 512], in0=pt[0:1, :], in1=bm_sb[:, j * 512:(j + 1) * 512], op=ALU.add),
                 r=[pk, 'bm'], w=['mod'])
        for j in range(12):
            pt, pk = next_psA()
            P.op('pe', lambda e: e.matmul(pt[:], lhsT=ones_f[0:1, :], rhs=mod_sb[:, j * 512:(j + 1) * 512], start=True, stop=True),
                 r=['ones_f', 'mod'], w=[pk])
            P.op('act', lambda e: e.copy(out=modbc[:, j // 2, (j % 2) * 512:(j % 2 + 1) * 512], in_=pt[:]), r=[pk], w=['modbc'])
        P.op('sp', lambda e: e.dma_start(out=gmx[:], in_=g_mix[:, :]), w=['gmx'], dma=True)
        P.op('dve', lambda e: e.scalar_tensor_tensor(out=A1[:], in0=modbc[:, 1, :], scalar=1.0, in1=gmx[:], op0=ALU.add, op1=ALU.mult),
             r=['modbc', 'gmx'], w=['A1'])

        fin.append(P.op('sp', lambda e: e.dma_start(out=modrow[:, :], in_=mod_sb[:]), r=['mod'], dma=True))
        P.barrier()
        es0.close()
        hT = sbs(esA, "hT", [128, 8, S], BF16)
        xt = [sbs(esA, f"xt{i}", [128, D], F32) for i in range(2)]
        xn = [sbs(esA, f"xn{i}", [128, D], F32) for i in range(2)]
        hb = [sbs(esA, f"hb{i}", [128, D], BF16) for i in range(2)]
        st = [sbs(esA, f"st{i}", [128, 4], F32) for i in range(2)]
        junk = sbs(esA, "junk", [128, D], F32)
        for t in range(NT):
            i = t % 2
            P.op('sp', lambda e: e.dma_start(out=xt[i][:], in_=x[t * 128:(t + 1) * 128, :]), w=[f'xt{i}'], dma=True)
            P.op('dve', lambda e: e.memset(st[i][:], 0.0), w=[f'st{i}'])
            P.op('act', lambda e: e.activation(out=junk[:], in_=xt[i][:], func=AF.Square, accum_out=st[i][:, 0:1]),
                 r=[f'xt{i}'], w=['junk', f'st{i}'])
            P.op('act', lambda e: e.activation(out=st[i][:, 1:2], in_=st[i][:, 0:1], func=AF.Sqrt, scale=1.0 / D, bias=epsb[:]),
                 r=[f'st{i}', 'epsb'], w=[f'st{i}'])
            P.op('dve', lambda e: e.reciprocal(out=st[i][:, 2:3], in_=st[i][:, 1:2]), r=[f'st{i}'], w=[f'st{i}'])
            P.op('dve', lambda e: e.scalar_tensor_tensor(out=xn[i][:], in0=xt[i][:], scalar=st[i][:, 2:3], in1=A1[:], op0=ALU.mult, op1=ALU.mult),
                 r=[f'xt{i}', f'st{i}', 'A1'], w=[f'xn{i}'])
            P.op('pool', lambda e: e.tensor_tensor(out=hb[i][:], in0=xn[i][:], in1=modbc[:, 0, :], op=ALU.add),
                 r=[f'xn{i}', 'modbc'], w=[f'hb{i}'])
            for half in range(2):
                pt, pk = psT[half], f'psT{half}'
                for k4 in range(4):
                    k = half * 4 + k4
                    P.op('pe', lambda e: e.transpose(out=pt[:, k4 * 128:(k4 + 1) * 128], in_=hb[i][:, k * 128:(k + 1) * 128], identity=identb[:]),
                         r=[f'hb{i}', 'identb'], w=[pk])
                eng = 'act' if half == 0 else 'dve'
                if eng == 'act':
                    P.op('act', lambda e: e.copy(out=hT[:, half * 4:(half + 1) * 4, t * 128:(t + 1) * 128],
                                                 in_=pt[:].rearrange("p (k n) -> p k n", k=4)), r=[pk], w=[('hT', t)])
                else:
                    P.op('dve', lambda e: e.tensor_copy(out=hT[:, half * 4:(half + 1) * 4, t * 128:(t + 1) * 128],
                                                        in_=pt[:].rearrange("p (k n) -> p k n", k=4)), r=[pk], w=[('hT', t)])
        hT_all = [('hT', t) for t in range(NT)]

        wb = [sbs(esA, f"wb{i}", [128, 8, 512], BF16) for i in range(2)]
        wb_i = [0]

        def load_w(src, c0, ncols):
            i = wb_i[0] % 2
            wb_i[0] += 1
            v = src.rearrange("(k p) n -> p k n", p=128)
            P.op('pool', lambda e: e.dma_start(out=wb[i][:, :, 0:ncols], in_=v[:, :, c0:c0 + ncols]), w=[f'wb{i}'], dma=True)
            return wb[i], f'wb{i}'

        stg = [sbs(esA, f"stg{i}", [128, S], BF16) for i in range(2)]
        stg_i = [0]

        def chan_major(src, ncols_total, dst_fn, M, dt_out=BF16, stgs=stg):
            nblk = (ncols_total + 511) // 512
            for blk in range(nblk):
                nc_ = min(512, ncols_total - blk * 512)
                wt, wk = load_w(src, blk * 512, nc_)
                for c in range(nc_ // M):
                    si = stg_i[0] % 2
                    stg_i[0] += 1
                    sg, sk = stgs[si], f'{stgs[si].name}'
                    for g in range(8):
                        pt, pk = next_psA()
                        for k in range(8):
                            P.op('pe', lambda e: e.matmul(pt[0:M, :], lhsT=wt[:, k, c * M:(c + 1) * M], rhs=hT[:, k, g * 512:(g + 1) * 512],
                                                          start=(k == 0), stop=(k == 7)),
                                 r=[wk] + hT_all[g * 4:(g + 1) * 4], w=[pk])
                        if g % 2 == 0:
                            P.op('act', lambda e: e.copy(out=sg[0:M, g * 512:(g + 1) * 512], in_=pt[0:M, :]), r=[pk], w=[sk])
                        else:
                            P.op('dve', lambda e: e.tensor_copy(out=sg[0:M, g * 512:(g + 1) * 512], in_=pt[0:M, :]), r=[pk], w=[sk])
                    fin.append(P.op('sp', lambda e: e.dma_start(out=dst_fn(blk * (512 // M) + c), in_=sg[0:M, :]), r=[sk], w=[('scr', dst_fn.__name__)], dma=True))

        def dst_qkv(c):
            return qkvT[c, :, :]
        chan_major(w_qkv, 3072, dst_qkv, 128)

        def dst_kr(c):
            return krT[c, :, :]
        chan_major(w_kr2, 128, dst_kr, 64)
        stgf0 = sbs(esA, "stgf0", [32, S], F32)
        stgf = [stgf0, stgf0]

        def dst_ba(c):
            return baT[:, :]
        chan_major(w_ba, 32, dst_ba, 32, F32, stgf)

        tst = [sbs(esA, f"tst{i}", [128, 512], BF16) for i in range(4)]
        tst_i = [0]

        def tok_major_act(src, ncols_total, dst, func):
            for blk in range(ncols_total // 512):
                wt, wk = load_w(src, blk * 512, 512)
                for t in range(NT):
                    pt, pk = next_psA()
                    for k in range(8):
                        P.op('pe', lambda e: e.matmul(pt[:], lhsT=hT[:, k, t * 128:(t + 1) * 128], rhs=wt[:, k, :], start=(k == 0), stop=(k == 7)),
                             r=[wk, ('hT', t)], w=[pk])
                    si = tst_i[0] % 4
                    tst_i[0] += 1
                    P.op('act', lambda e: e.activation(out=tst[si][:], in_=pt[:], func=func), r=[pk], w=[f'tst{si}'])
                    fin.append(P.op('sp', lambda e: e.dma_start(out=dst[t * 128:(t + 1) * 128, blk * 512:(blk + 1) * 512], in_=tst[si][:]),
                                    r=[f'tst{si}'], w=[('scr', dst.name, t, blk)], dma=True))
        tok_major_act(w_z, 1024, zs, AF.Silu)
        tok_major_act(w_g, 2048, gs, AF.Sigmoid)

        def latent(src, ncols, gain_in, dstT, nm):
            gsb = sbs(esA, f"gain_{nm}", [128, ncols], F32)
            P.op('sp', lambda e: e.dma_start(out=gsb[:], in_=gain_in[:, :]), w=[f'gain_{nm}'], dma=True)
            lst = [sbs(esA, f"lst_{nm}{i_}", [128, ncols // 128, 128], BF16) for i_ in range(2)]
            wt, wk = load_w(src, 0, ncols)
            for t in range(NT):
                i = t % 2
                pt, pk = next_psA()
                for k in range(8):
                    P.op('pe', lambda e: e.matmul(pt[:, 0:ncols], lhsT=hT[:, k, t * 128:(t + 1) * 128], rhs=wt[:, k, 0:ncols], start=(k == 0), stop=(k == 7)),
                         r=[wk, ('hT', t)], w=[pk])
                P.op('dve', lambda e: e.memset(st[i][:], 0.0), w=[f'st{i}'])
                P.op('act', lambda e: e.activation(out=junk[:, 0:ncols], in_=pt[:, 0:ncols], func=AF.Square, accum_out=st[i][:, 0:1]),
                     r=[pk], w=['junk', f'st{i}'])
                P.op('act', lambda e: e.activation(out=st[i][:, 1:2], in_=st[i][:, 0:1], func=AF.Sqrt, scale=1.0 / ncols, bias=epsb[:]),
                     r=[f'st{i}', 'epsb'], w=[f'st{i}'])
                P.op('dve', lambda e: e.reciprocal(out=st[i][:, 2:3], in_=st[i][:, 1:2]), r=[f'st{i}'], w=[f'st{i}'])
                P.op('dve', lambda e: e.scalar_tensor_tensor(out=hb[i][:, 0:ncols], in0=pt[:, 0:ncols], scalar=st[i][:, 2:3], in1=gsb[:], op0=ALU.mult, op1=ALU.mult),
                     r=[pk, f'st{i}', f'gain_{nm}'], w=[f'hb{i}'])
                tp, tk = psT[i], f'psT{i}'
                for c in range(ncols // 128):
                    P.op('pe', lambda e: e.transpose(out=tp[:, c * 128:(c + 1) * 128], in_=hb[i][:, c * 128:(c + 1) * 128], identity=identb[:]),
                         r=[f'hb{i}', 'identb'], w=[tk])
                P.op('act', lambda e: e.copy(out=lst[i][:], in_=tp[:, 0:ncols].rearrange("p (k n) -> p k n", k=ncols // 128)),
                     r=[tk], w=[f'lst_{nm}{i}'])
                fin.append(P.op('sp', lambda e: e.dma_start(out=dstT[:, :, t * 128:(t + 1) * 128].rearrange("c p n -> p c n"), in_=lst[i][:]),
                                r=[f'lst_{nm}{i}'], w=[('scr', nm, t)], dma=True))
        latent(w_cq, 512, q_gain, cqnT, 'cq')
        latent(w_ckv, 256, kv_gain, ckvnT, 'ckv')


        P.barrier()
        esA.close()

    def phase_MLA():
        esM = ExitStack()
        TWO_PI = float(2 * np.pi)
        SCL = float(192 ** -0.5)
        cos2 = sbs(esM, "cos2", [64, S], F32)
        sin2 = sbs(esM, "sin2", [64, S], F32)
        krA = sbs(esM, "krA", [65, S], BF16)
        QrA = sbs(esM, "QrA", [65, S], BF16)
        onesb = sbs(esM, "onesb", [128, 128], BF16)
        sel_b = sbs(esM, "sel_b", [128, 65], BF16)
        if True:
            es1 = ExitStack()
            posi = sbs(es1, "posi", [64, S], I32)
            ang = sbs(es1, "ang", [64, S], F32)
            ti = sbs(es1, "ti", [64, S], I32)
            tf = sbs(es1, "tf", [64, S], F32)
            tg = sbs(es1, "tg", [64, S], F32)
            ivf = sbs(es1, "ivf", [64, 2], F32)
            kr0 = sbs(es1, "kr0", [64, S], BF16)
            kr1 = sbs(es1, "kr1", [64, S], BF16)
            self_f = sbs(es1, "self_f", [128, 65], F32)
            P.op('sp', lambda e: e.dma_start(out=posi[:], in_=posr[:, :]), w=['posi'], dma=True)
            P.op('sp', lambda e: e.dma_start(out=ivf[:, 0:1], in_=invf2[:, :]), w=['ivf'], dma=True)
            P.op('sp', lambda e: e.dma_start(out=ivf[:, 1:2], in_=sgn2[:, :]), w=['ivf'], dma=True)
            P.op('sp', lambda e: e.dma_start(out=self_f[:], in_=sel64[:, :]), w=['self_f'], dma=True)
            P.op('dve', lambda e: e.tensor_copy(out=sel_b[:], in_=self_f[:]), r=['self_f'], w=['sel_b'])
            P.op('dve', lambda e: e.memset(onesb[:], 1.0), w=['onesb'])
            P.op('dve', lambda e: e.tensor_copy(out=ang[:], in_=posi[:]), r=['posi'], w=['ang'])
            P.op('dve', lambda e: e.tensor_scalar(out=ang[:], in0=ang[:], scalar1=ivf[:, 0:1], scalar2=float(1.0 / TWO_PI), op0=ALU.mult, op1=ALU.mult),
                 r=['ang', 'ivf'], w=['ang'])
            for which, dst in ((0, sin2), (1, cos2)):
                dk_ = 'sin2' if which == 0 else 'cos2'
                P.op('dve', lambda e: e.tensor_scalar(out=tg[:], in0=ang[:], scalar1=0.25 * which, scalar2=None, op0=ALU.add), r=['ang'], w=['tg'])
                P.op('dve', lambda e: e.tensor_copy(out=ti[:], in_=tg[:]), r=['tg'], w=['ti'])
                P.op('dve', lambda e: e.tensor_copy(out=tf[:], in_=ti[:]), r=['ti'], w=['tf'])
                P.op('dve', lambda e: e.tensor_tensor(out=tg[:], in0=tg[:], in1=tf[:], op=ALU.subtract), r=['tg', 'tf'], w=['tg'])
                P.op('dve', lambda e: e.tensor_scalar(out=tf[:], in0=tg[:], scalar1=0.5, scalar2=None, op0=ALU.is_gt), r=['tg'], w=['tf'])
                P.op('dve', lambda e: e.tensor_tensor(out=tg[:], in0=tg[:], in1=tf[:], op=ALU.subtract), r=['tg', 'tf'], w=['tg'])
                P.op('dve', lambda e: e.tensor_scalar(out=tf[:], in0=tg[:], scalar1=-0.5, scalar2=None, op0=ALU.is_lt), r=['tg'], w=['tf'])
                P.op('dve', lambda e: e.tensor_tensor(out=tg[:], in0=tg[:], in1=tf[:], op=ALU.add), r=['tg', 'tf'], w=['tg'])
                P.op('act', lambda e: e.activation(out=dst[:], in_=tg[:], func=AF.Sin, scale=TWO_PI), r=['tg'], w=[dk_])
            P.op('dve', lambda e: e.tensor_scalar(out=sin2[:], in0=sin2[:], scalar1=ivf[:, 1:2], scalar2=None, op0=ALU.mult), r=['sin2', 'ivf'], w=['sin2'])
            P.op('sp', lambda e: e.dma_start(out=kr0[:], in_=krT[0, :, :]), r=[('scr', 'dst_kr')], w=['kr0'], dma=True)
            P.op('sp', lambda e: e.dma_start(out=kr1[:], in_=krT[1, :, :]), r=[('scr', 'dst_kr')], w=['kr1'], dma=True)
            P.op('dve', lambda e: e.tensor_tensor(out=tg[:], in0=kr0[:], in1=cos2[:], op=ALU.mult), r=['kr0', 'cos2'], w=['tg'])
            P.op('dve', lambda e: e.tensor_tensor(out=tf[:], in0=kr1[:], in1=sin2[:], op=ALU.mult), r=['kr1', 'sin2'], w=['tf'])
            P.op('dve', lambda e: e.memset(krA[:], 1.0), w=['krA'])
            P.op('dve', lambda e: e.tensor_tensor(out=krA[0:64, :], in0=tg[:], in1=tf[:], op=ALU.add), r=['tg', 'tf'], w=['krA'])
            P.op('dve', lambda e: e.memset(QrA[:], 0.0), w=['QrA'])
            P.barrier()
            es1.close()
        cqn = sbs(esM, "cqn", [128, 4, S], BF16)
        ckvn = sbs(esM, "ckvn", [128, 2, S], BF16)
        QnT = sbs(esM, "QnT", [128, S], BF16)
        KnT = sbs(esM, "KnT", [128, S], BF16)
        Vt = sbs(esM, "Vt", [128, NT, 128], BF16)
        oTs = sbs(esM, "oTs", [128, S], BF16)
        kmx = sbs(esM, "kmx", [65, 16], F32)
        wuq = sbs(esM, "wuq", [128, 4, 256], BF16)
        wukv = sbs(esM, "wukv", [128, 2, 256], BF16)
        sq = sbs(esM, "sq", [128, 512], BF16)
        t1 = sbs(esM, "t1", [64, 512], F32)
        t2 = sbs(esM, "t2", [64, 512], F32)
        rowt = sbs(esM, "rowt", [65, 512], F32)
        pT = [sbs(esM, f"pT{i}", [128, 512], BF16) for i in range(3)]
        rden = sbs(esM, "rden", [128, 512], F32)
        for c in range(4):
            P.op('sp', lambda e: e.dma_start(out=cqn[:, c, :], in_=cqnT[c, :, :]), r=[('scr', 'cq', t_) for t_ in range(NT)], w=['cqn'], dma=True)
        for c in range(2):
            P.op('sp', lambda e: e.dma_start(out=ckvn[:, c, :], in_=ckvnT[c, :, :]), r=[('scr', 'ckv', t_) for t_ in range(NT)], w=['ckvn'], dma=True)
        for h in range(8):
            P.op('pool', lambda e: e.dma_start(out=wuq[:], in_=w_uqh[h].rearrange("(k p) n -> p k n", p=128)), w=['wuq'], dma=True)
            P.op('pool', lambda e: e.dma_start(out=wukv[:], in_=w_ukvh[h].rearrange("(k p) n -> p k n", p=128)), w=['wukv'], dma=True)
            for g in range(8):
                gs_ = slice(g * 512, (g + 1) * 512)
                pt, pk = psA[2], 'psA2'
                for k in range(4):
                    P.op('pe', lambda e: e.matmul(pt[:], lhsT=wuq[:, k, 0:128], rhs=cqn[:, k, gs_], start=(k == 0), stop=(k == 3)), r=['wuq', 'cqn'], w=[pk])
                P.op('act', lambda e: e.copy(out=QnT[:, gs_], in_=pt[:]), r=[pk], w=[('QnT', g)])
                pt, pk = psA[3], 'psA3'
                for k in range(4):
                    P.op('pe', lambda e: e.matmul(pt[0:64, :], lhsT=wuq[:, k, 128:192], rhs=cqn[:, k, gs_], start=(k == 0), stop=(k == 3)), r=['wuq', 'cqn'], w=[pk])
                P.op('dve', lambda e: e.tensor_tensor(out=t1[:], in0=pt[0:64, :], in1=cos2[:, gs_], op=ALU.mult), r=[pk, 'cos2'], w=['t1'])
                for k in range(4):
                    P.op('pe', lambda e: e.matmul(pt[0:64, :], lhsT=wuq[:, k, 192:256], rhs=cqn[:, k, gs_], start=(k == 0), stop=(k == 3)), r=['wuq', 'cqn'], w=[pk])
                P.op('dve', lambda e: e.tensor_tensor(out=t2[:], in0=pt[0:64, :], in1=sin2[:, gs_], op=ALU.mult), r=[pk, 'sin2'], w=['t2'])
                P.op('dve', lambda e: e.tensor_tensor(out=QrA[0:64, gs_], in0=t1[:], in1=t2[:], op=ALU.add), r=['t1', 't2'], w=[('QrA', g)])
                pt, pk = psA[2], 'psA2'
                for k in range(2):
                    P.op('pe', lambda e: e.matmul(pt[:], lhsT=wukv[:, k, 0:128], rhs=ckvn[:, k, gs_], start=(k == 0), stop=(k == 1)), r=['wukv', 'ckvn'], w=[pk])
                P.op('act', lambda e: e.copy(out=KnT[:, gs_], in_=pt[:]), r=[pk], w=[('KnT', g)])
                pt, pk = psA[3], 'psA3'
                for j in range(4):
                    t_ = g * 4 + j
                    for k in range(2):
                        P.op('pe', lambda e: e.matmul(pt[:, j * 128:(j + 1) * 128], lhsT=ckvn[:, k, t_ * 128:(t_ + 1) * 128], rhs=wukv[:, k, 128:256], start=(k == 0), stop=(k == 1)),
                             r=['wukv', 'ckvn'], w=[pk])
                P.op('dve', lambda e: e.tensor_copy(out=Vt[:, g * 4:(g + 1) * 4, :], in_=pt[:].rearrange("p (j n) -> p j n", j=4)), r=[pk], w=[('Vt', g)])
                pt, pk = psA[2], 'psA2'
                P.op('act', lambda e: e.activation(out=sq[:], in_=KnT[:, gs_], func=AF.Square), r=[('KnT', g)], w=['sq'])
                P.op('pe', lambda e: e.matmul(pt[0:65, :], lhsT=sel_b[:, :], rhs=sq[:], start=True, stop=False), r=['sq', 'sel_b'], w=[pk])
                P.op('act', lambda e: e.activation(out=sq[0:64, :], in_=krA[0:64, gs_], func=AF.Square), r=['krA'], w=['sq'])
                P.op('pe', lambda e: e.matmul(pt[0:65, :], lhsT=sel_b[0:64, :], rhs=sq[0:64, :], start=False, stop=True), r=['sq', 'sel_b'], w=[pk])
                P.op('dve', lambda e: e.tensor_reduce(out=kmx[64:65, g:g + 1], in_=pt[64:65, :], axis=AX.X, op=ALU.max), r=[pk], w=['kmx'])
            P.op('dve', lambda e: e.tensor_reduce(out=kmx[64:65, 8:9], in_=kmx[64:65, 0:8], axis=AX.X, op=ALU.max), r=['kmx'], w=['kmx'])
            for g in range(8):
                gs_ = slice(g * 512, (g + 1) * 512)
                pt, pk = psA[2], 'psA2'
                P.op('act', lambda e: e.activation(out=sq[:], in_=QnT[:, gs_], func=AF.Square), r=[('QnT', g)], w=['sq'])
                P.op('pe', lambda e: e.matmul(pt[0:65, :], lhsT=sel_b[:, :], rhs=sq[:], start=True, stop=False), r=['sq', 'sel_b'], w=[pk])
                P.op('act', lambda e: e.activation(out=sq[0:64, :], in_=QrA[0:64, gs_], func=AF.Square), r=[('QrA', g)], w=['sq'])
                P.op('pe', lambda e: e.matmul(pt[0:65, :], lhsT=sel_b[0:64, :], rhs=sq[0:64, :], start=False, stop=True), r=['sq', 'sel_b'], w=[pk])
                P.op('act', lambda e: e.activation(out=rowt[64:65, :], in_=pt[64:65, :], func=AF.Sqrt, scale=kmx[64:65, 8:9]), r=[pk, 'kmx'], w=['rowt'])
                P.op('dve', lambda e: e.tensor_scalar(out=QrA[64:65, gs_], in0=rowt[64:65, :], scalar1=-1.0, scalar2=None, op0=ALU.mult), r=['rowt'], w=[('QrA', g)])
            for g in range(8):
                gs_ = slice(g * 512, (g + 1) * 512)
                po, pd = psA[(g % 2) * 2], psA[(g % 2) * 2 + 1]
                kpo, kpd = f'psA{(g % 2) * 2}', f'psA{(g % 2) * 2 + 1}'

                def scores(kt):
                    ks_ = slice(kt * 128, (kt + 1) * 128)
                    sc_, sck = psS[kt % 2], f'psS{kt % 2}'
                    P.op('pe', lambda e: e.matmul(sc_[:], lhsT=KnT[:, ks_], rhs=QnT[:, gs_], start=True, stop=False),
                         r=[('KnT', kt // 4), ('QnT', g)], w=[sck])
                    P.op('pe', lambda e: e.matmul(sc_[:], lhsT=krA[:, ks_], rhs=QrA[:, gs_], start=False, stop=True),
                         r=['krA', ('QrA', g)], w=[sck])
                scores(0)
                for kt in range(NT):
                    sc_, sck = psS[kt % 2], f'psS{kt % 2}'
                    pi = kt % 3
                    P.op('act', lambda e: e.activation(out=pT[pi][:], in_=sc_[:], func=AF.Exp, scale=SCL), r=[sck], w=[f'pT{pi}'])
                    if kt + 1 < NT:
                        scores(kt + 1)
                    P.op('pe', lambda e: e.matmul(po[:], lhsT=Vt[:, kt, :], rhs=pT[pi][:], start=(kt == 0), stop=(kt == NT - 1)),
                         r=[('Vt', kt // 4), f'pT{pi}'], w=[kpo])
                    P.op('pe', lambda e: e.matmul(pd[:], lhsT=onesb[:], rhs=pT[pi][:], start=(kt == 0), stop=(kt == NT - 1)),
                         r=['onesb', f'pT{pi}'], w=[kpd])
                P.op('dve', lambda e: e.reciprocal(out=rden[:], in_=pd[:]), r=[kpd], w=['rden'])
                P.op('dve', lambda e: e.tensor_tensor(out=oTs[:, gs_], in0=po[:], in1=rden[:], op=ALU.mult), r=[kpo, 'rden'], w=['oTs'])
            fin.append(P.op('sp', lambda e: e.dma_start(out=oT_mla[h, :, :], in_=oTs[:]), r=['oTs'], w=[('scr', 'oT_mla', h)], dma=True))
        P.barrier()
        esM.close()


    def phase_DN():
        def stop(k):
            if dn_stop == k:
                raise _Stop()
        esD = ExitStack()
        pM, pG, pX0, pX1, pZT, pU = psA[0], psA[1], psA[2], psA[3], psS[0], psS[1]
        kM, kG, kX0, kX1, kZT, kU = 'psA0', 'psA1', 'psA2', 'psA3', 'psS0', 'psS1'
        onesb = sbs(esD, "d_onesb", [128, 128], BF16)
        P.op('dve', lambda e: e.memset(onesb[:], 1.0), w=['d_onesb'])
        msk = sbs(esD, "d_msk", [128, 4, 128], F32)
        P.op('sp', lambda e: e.dma_start(out=msk[:], in_=dmasks[:, :, :]), w=['d_msk'], dma=True)
        gain = sbs(esD, "d_gain", [128, 128], F32)
        P.op('sp', lambda e: e.dma_start(out=gain[:], in_=dn_gain[:, :]), w=['d_gain'], dma=True)
        tokS = sbs(esD, "tokS", [128, NT, 48], F32)
        one1 = sbs(esD, "one1", [128, 1], F32)
        P.op('dve', lambda e: e.memset(one1[:], 1.0), w=['one1'])
        eps_l2 = sbs(esD, "eps_l2", [128, 1], F32)
        P.op('dve', lambda e: e.memset(eps_l2[:], EPS), w=['eps_l2'])
        es1 = ExitStack()
        sc = sbs(es1, "d_sc", [8, 4], F32)
        nA = sbs(es1, "d_nA", [8, 2], F32)
        P.op('sp', lambda e: e.dma_start(out=sc[:], in_=dn_sc[:, :]), w=['d_sc'], dma=True)
        P.op('act', lambda e: e.activation(out=nA[:], in_=sc[:, 0:2], func=AF.Exp), r=['d_sc'], w=['d_nA'])
        P.op('dve', lambda e: e.tensor_scalar(out=nA[:], in0=nA[:], scalar1=-1.0, scalar2=None, op0=ALU.mult), r=['d_nA'], w=['d_nA'])
        rows = {}
        ra = sbs(es1, "d_ra", [8, S], F32)
        rb = sbs(es1, "d_rb", [8, S], F32)
        rc = sbs(es1, "d_rc", [8, S], F32)
        for d in range(2):
            beta = sbs(es1, f"d_beta{d}", [8, S], F32)
            nbeta = sbs(es1, f"d_nbeta{d}", [8, S], F32)
            gc = sbs(es1, f"d_gc{d}", [8, S], F32)
            rows[d] = (beta, nbeta, gc)
            P.op('sp', lambda e: e.dma_start(out=ra[:], in_=baT[d * 8:(d + 1) * 8, :]), r=[('scr', 'dst_ba')], w=['d_ra'], dma=True)
            P.op('act', lambda e: e.activation(out=beta[:], in_=ra[:], func=AF.Sigmoid), r=['d_ra'], w=[f'd_beta{d}'])
            P.op('dve', lambda e: e.tensor_scalar(out=nbeta[:], in0=beta[:], scalar1=-1.0, scalar2=None, op0=ALU.mult), r=[f'd_beta{d}'], w=[f'd_nbeta{d}'])
            P.op('sp', lambda e: e.dma_start(out=ra[:], in_=baT[16 + d * 8:16 + (d + 1) * 8, :]), r=[('scr', 'dst_ba')], w=['d_ra'], dma=True)
            P.op('dve', lambda e: e.tensor_scalar(out=ra[:], in0=ra[:], scalar1=sc[:, 2 + d:3 + d], scalar2=None, op0=ALU.add), r=['d_ra', 'd_sc'], w=['d_ra'])
            P.op('act', lambda e: e.activation(out=rb[:], in_=ra[:], func=AF.Abs), r=['d_ra'], w=['d_rb'])
            P.op('act', lambda e: e.activation(out=rb[:], in_=rb[:], func=AF.Exp, scale=-1.0), r=['d_rb'], w=['d_rb'])
            P.op('act', lambda e: e.activation(out=rb[:], in_=rb[:], func=AF.Ln, bias=one1[0:8, :], scale=1.0), r=['d_rb', 'one1'], w=['d_rb'])
            P.op('dve', lambda e: e.scalar_tensor_tensor(out=rc[:], in0=ra[:], scalar=0.0, in1=rb[:], op0=ALU.max, op1=ALU.add), r=['d_ra', 'd_rb'], w=['d_rc'])
            P.op('dve', lambda e: e.tensor_scalar(out=rc[:], in0=rc[:], scalar1=nA[:, d:d + 1], scalar2=None, op0=ALU.mult), r=['d_rc', 'd_nA'], w=['d_rc'])
            cur, curk, nxt, nxtk = rc, 'd_rc', gc, f'd_gc{d}'
            for sft in (1, 2, 4, 8, 16, 32, 64):
                c3 = cur[:].rearrange("p (t n) -> p t n", n=128)
                n3 = nxt[:].rearrange("p (t n) -> p t n", n=128)
                P.op('act', lambda e: e.copy(out=nxt[:], in_=cur[:]), r=[curk], w=[nxtk])
                if d == 0:
                    P.op('dve', lambda e: e.tensor_tensor(out=n3[:, :, sft:], in0=c3[:, :, sft:], in1=c3[:, :, :128 - sft], op=ALU.add), r=[curk], w=[nxtk])
                else:
                    P.op('dve', lambda e: e.tensor_tensor(out=n3[:, :, :128 - sft], in0=c3[:, :, :128 - sft], in1=c3[:, :, sft:], op=ALU.add), r=[curk], w=[nxtk])
                cur, curk, nxt, nxtk = nxt, nxtk, cur, curk
            if cur is not gc:
                P.op('act', lambda e: e.copy(out=gc[:], in_=cur[:]), r=[curk], w=[f'd_gc{d}'])
        for t in range(NT):
            for d in range(2):
                for j in range(3):
                    src = rows[d][j]
                    col = d * 24 + j * 8
                    P.op('pe', lambda e: e.transpose(out=pG[:, col:col + 8], in_=src[:, t * 128:(t + 1) * 128], identity=ident[0:8, 0:8]),
                         r=[f'd_beta{d}', f'd_nbeta{d}', f'd_gc{d}', 'ident'], w=[kG])
            P.op('act', lambda e: e.copy(out=tokS[:, t, :], in_=pG[:, 0:48]), r=[kG], w=['tokS'])
        P.barrier()
        stop(1)
        es1.close()
        lm = sbs(esD, "d_lm", [128, 5, 4 * 128], F32)
        for j5 in range(5):
            P.op('sp', lambda e: e.dma_start(out=lm[:, j5, :], in_=dn_lmask[:, j5, :]), w=['d_lm'], dma=True)
        QKV = [sbs(esD, f"d_qkv{i}", [128, S], BF16) for i in range(3)]
        Ust = sbs(esD, "d_U", [128, NT, 2, 128], BF16)
        WTst = sbs(esD, "d_WT", [128, NT, 2, 128], BF16)
        ITst = sbs(esD, "d_IT", [128, NT, 2, 128], BF16)
        QDst = sbs(esD, "d_QD", [128, NT, 2, 128], BF16)
        KSst = sbs(esD, "d_KS", [128, NT, 2, 128], BF16)
        egl = sbs(esD, "d_egl", [128, NT, 2], F32)
        Oacc = sbs(esD, "d_Oacc", [128, NT, 128], F32)
        zsh = sbs(esD, "d_zsh", [128, NT, 128], BF16)
        oTd = sbs(esD, "d_oTd", [128, S], BF16)
        S32 = [sbs(esD, f"d_S32{d}", [128, 128], F32) for d in range(2)]
        Sbf = [sbs(esD, f"d_Sbf{d}", [128, 128], BF16) for d in range(2)]
        Vn = [sbs(esD, f"d_Vn{d}", [128, 128], BF16) for d in range(2)]
        ost = sbs(esD, "d_ost", [128, 8], F32)
        on = sbs(esD, "d_on", [128, 128], F32)
        onb = sbs(esD, "d_onb", [128, 128], BF16)
        G = 4
        QSC = float(128 ** -0.5)
        for h in dn_heads:
            esC = ExitStack()
            xpad = sbs(esC, f"d_xpad_{h}", [128, S + 4], F32)
            acc = sbs(esC, f"d_acc_{h}", [128, S], F32)
            cw = sbs(esC, f"d_cw_{h}", [128, 5], F32)
            rst = sbs(esC, f"d_rst_{h}", [128, 512], F32)
            sqb = sbs(esC, f"d_sqb_{h}", [128, 512], BF16)
            P.op('dve', lambda e: e.memset(xpad[:, 0:2], 0.0), w=['d_xpad'])
            P.op('dve', lambda e: e.memset(xpad[:, S + 2:S + 4], 0.0), w=['d_xpad'])
            for ci in range(3):
                ch = ci * 8 + h
                P.op('sp', lambda e: e.dma_start(out=cw[:], in_=conv_wT[ch * 128:(ch + 1) * 128, :]), w=['d_cw'], dma=True)
                P.op('pool', lambda e: e.dma_start(out=xpad[:, 2:S + 2], in_=qkvT[ch, :, :]), r=[('scr', 'dst_qkv')], w=['d_xpad'], dma=True)
                eng = 'dve'
                P.op(eng, lambda e: e.tensor_scalar(out=acc[:], in0=xpad[:, 0:S], scalar1=cw[:, 0:1], scalar2=None, op0=ALU.mult), r=['d_xpad', 'd_cw'], w=['d_acc'])
                for j in range(1, 5):
                    P.op(eng, lambda e: e.scalar_tensor_tensor(out=acc[:], in0=xpad[:, j:j + S], scalar=cw[:, j:j + 1], in1=acc[:], op0=ALU.mult, op1=ALU.add),
                         r=['d_xpad', 'd_cw', 'd_acc'], w=['d_acc'])
                P.op('act', lambda e: e.activation(out=acc[:], in_=acc[:], func=AF.Silu), r=['d_acc'], w=['d_acc'])
                if ci == 2:
                    P.op('dve', lambda e: e.tensor_copy(out=QKV[2][:], in_=acc[:]), r=['d_acc'], w=['d_qkv2'])
                else:
                    for g in range(8):
                        gs_ = slice(g * 512, (g + 1) * 512)
                        P.op('act', lambda e: e.activation(out=sqb[:], in_=acc[:, gs_], func=AF.Square), r=['d_acc'], w=['d_sqb'])
                        P.op('pe', lambda e: e.matmul(pM[:], lhsT=onesb[:], rhs=sqb[:], start=True, stop=True), r=['d_onesb', 'd_sqb'], w=[kM])
                        P.op('act', lambda e: e.activation(out=rst[:], in_=pM[:], func=AF.Sqrt, bias=eps_l2[:], scale=1.0), r=[kM, 'eps_l2'], w=['d_rst'])
                        P.op('dve', lambda e: e.reciprocal(out=rst[:], in_=rst[:]), r=['d_rst'], w=['d_rst'])
                        P.op('dve', lambda e: e.scalar_tensor_tensor(out=QKV[ci][:, gs_], in0=acc[:, gs_], scalar=(QSC if ci == 0 else 1.0), in1=rst[:], op0=ALU.mult, op1=ALU.mult),
                             r=['d_acc', 'd_rst'], w=[f'd_qkv{ci}'])
            Qt, Kt, Vch = QKV
            stop(2)
            P.barrier()
            esC.close()
            esW = ExitStack()
            Ktok = sbs(esW, f"d_Ktok_{h}", [128, 2, 128], BF16)
            Vtok = sbs(esW, f"d_Vtok_{h}", [128, 2, 128], BF16)
            Dg = sbs(esW, f"d_Dg_{h}", [128, G, 128], F32)
            tA = sbs(esW, f"d_tA_{h}", [128, G, 128], F32)
            tI = sbs(esW, f"d_tI_{h}", [128, G, 128], F32)
            EGB = sbs(esW, f"d_EGB_{h}", [128, G, 128], F32)
            A32 = sbs(esW, f"d_A32_{h}", [128, G, 128], F32)
            AT32 = sbs(esW, f"d_AT32_{h}", [128, G, 128], F32)
            ZY = sbs(esW, f"d_ZY_{h}", [128, G, 2, 128], F32)
            ZTYT = sbs(esW, f"d_ZTYT_{h}", [128, G, 2, 128], F32)
            Lb = sbs(esW, f"d_Lb_{h}", [128, G, 128], BF16)
            LTb = sbs(esW, f"d_LTb_{h}", [128, G, 128], BF16)
            Qb = sbs(esW, f"d_Qb_{h}", [128, G, 128], BF16)
            Rb = sbs(esW, f"d_Rb_{h}", [128, G, 128], BF16)
            TT = sbs(esW, f"d_TT_{h}", [128, G, 128], BF16)
            Tb = sbs(esW, f"d_Tb_{h}", [128, G, 128], BF16)
            ZYb = sbs(esW, f"d_ZYb_{h}", [128, G, 2, 128], BF16)
            ZTYTb = sbs(esW, f"d_ZTYTb_{h}", [128, G, 2, 128], BF16)
            Kbe = sbs(esW, f"d_Kbe_{h}", [128, G, 128], BF16)
            Vb = sbs(esW, f"d_Vb_{h}", [128, G, 128], BF16)
            egc = sbs(esW, f"d_egc_{h}", [128, G], F32)
            ebh = sbs(esW, f"d_ebh_{h}", [128, NT, 2], F32)
            for d_ in range(2):
                P.op('act', lambda e: e.activation(out=ebh[:, :, d_], in_=tokS[:, :, d_ * 24 + 16 + h], func=AF.Exp), r=['tokS'], w=['d_ebh'])
                P.op('dve', lambda e: e.tensor_tensor(out=ebh[:, :, d_], in0=ebh[:, :, d_], in1=tokS[:, :, d_ * 24 + h], op=ALU.mult), r=['d_ebh', 'tokS'], w=['d_ebh'])
            zs_v = zs[:, h * 128:(h + 1) * 128].rearrange("(t p) c -> p t c", p=128)
            for q4 in range(8):
                P.op('sp', lambda e: e.dma_start(out=zsh[:, q4 * 4:(q4 + 1) * 4, :], in_=zs_v[:, q4 * 4:(q4 + 1) * 4, :]),
                     r=[('scr', 'zs', t_, b_) for t_ in range(q4 * 4, q4 * 4 + 4) for b_ in range(2)], w=['d_zsh'], dma=True)
            for t0 in range(0, NT, 2):
                units = [(ti, d) for ti in range(2) for d in range(2)]
                for ti in range(2):
                    ts_ = slice((t0 + ti) * 128, (t0 + ti + 1) * 128)
                    P.op('pe', lambda e: e.transpose(out=psT[0][:, ti * 256:ti * 256 + 128], in_=Kt[:, ts_], identity=identb[:]), r=['d_qkv1', 'identb'], w=['psT0'])
                    P.op('pe', lambda e: e.transpose(out=psT[0][:, ti * 256 + 128:ti * 256 + 256], in_=Vch[:, ts_], identity=identb[:]), r=['d_qkv2', 'identb'], w=['psT0'])
                    P.op('pe', lambda e: e.matmul(pM[:, ti * 256:ti * 256 + 128], lhsT=Kt[:, ts_], rhs=Kt[:, ts_], start=True, stop=True), r=['d_qkv1'], w=[kM])
                    P.op('pe', lambda e: e.matmul(pM[:, ti * 256 + 128:ti * 256 + 256], lhsT=Kt[:, ts_], rhs=Qt[:, ts_], start=True, stop=True), r=['d_qkv1', 'd_qkv0'], w=[kM])
                pT4 = psT[0][:].rearrange("p (a b n) -> p a b n", a=2, b=2)
                P.op('act', lambda e: e.copy(out=Ktok[:], in_=pT4[:, :, 0, :]), r=['psT0'], w=['d_Ktok'])
                P.op('act', lambda e: e.copy(out=Vtok[:], in_=pT4[:, :, 1, :]), r=['psT0'], w=['d_Vtok'])
                stop(31)
                for u, (ti, d) in enumerate(units):
                    t = t0 + ti
                    gcol = tokS[:, t, d * 24 + 16 + h:d * 24 + 17 + h]
                    P.op('dve', lambda e: e.tensor_scalar(out=Dg[:, u, :], in0=ident[:], scalar1=gcol, scalar2=None, op0=ALU.mult), r=['ident', 'tokS'], w=[('d_Dg', u)])
                    P.op('pe', lambda e: e.matmul(pG[:, u * 128:(u + 1) * 128], lhsT=ones_f[:], rhs=Dg[:, u, :], start=True, stop=True), r=['ones_f', ('d_Dg', u)], w=[kG])
                stop(32)
                P.op('act', lambda e: e.copy(out=EGB[:], in_=pG[:].rearrange("p (u n) -> p u n", u=G)), r=[kG], w=['d_GBs'])
                for u, (ti, d) in enumerate(units):
                    t = t0 + ti
                    gcol = tokS[:, t, d * 24 + 16 + h:d * 24 + 17 + h]
                    P.op('dve', lambda e: e.scalar_tensor_tensor(out=tA[:, u, :], in0=EGB[:, u, :], scalar=gcol, in1=msk[:, 2 * d, :], op0=ALU.subtract, op1=ALU.max),
                         r=['d_GBs', 'tokS', 'd_msk'], w=[('d_tA', u)])
                    P.op('dve', lambda e: e.scalar_tensor_tensor(out=tI[:, u, :], in0=EGB[:, u, :], scalar=gcol, in1=msk[:, 2 * d + 1, :], op0=ALU.subtract, op1=ALU.min),
                         r=['d_GBs', 'tokS', 'd_msk'], w=[('d_tI', u)])
                kTA = [('d_tA', u) for u in range(G)]
                kTI = [('d_tI', u) for u in range(G)]
                P.op('act', lambda e: e.activation(out=tA[:], in_=tA[:], func=AF.Exp, scale=-1.0), r=kTA, w=kTA)
                P.op('act', lambda e: e.activation(out=tI[:], in_=tI[:], func=AF.Exp), r=kTI, w=kTI)
                P.op('act', lambda e: e.activation(out=EGB[:], in_=EGB[:], func=AF.Exp), r=['d_GBs'] + kTA + kTI, w=['d_GBs'])
                stop(33)
                for u, (ti, d) in enumerate(units):
                    t = t0 + ti
                    ts_ = slice(t * 128, (t + 1) * 128)
                    bcol = tokS[:, t, d * 24 + h:d * 24 + h + 1]
                    lastc = 127 if d == 0 else 0
                    P.op('dve', lambda e: e.scalar_tensor_tensor(out=A32[:, u, :], in0=pM[:, ti * 256:ti * 256 + 128], scalar=bcol, in1=tA[:, u, :], op0=ALU.mult, op1=ALU.mult),
                         r=[kM, 'tokS', ('d_tA', u)], w=[('d_A32', u)])
                    P.op('pe', lambda e: e.transpose(out=pX0[:, u * 128:(u + 1) * 128], in_=A32[:, u, :], identity=ident[:]), r=[('d_A32', u), 'ident'], w=[kX0])
                for u, (ti, d) in enumerate(units):
                    t = t0 + ti
                    ts_ = slice(t * 128, (t + 1) * 128)
                    bcol = tokS[:, t, d * 24 + h:d * 24 + h + 1]
                    lastc = 127 if d == 0 else 0
                    P.op('dve', lambda e: e.tensor_tensor(out=ITst[:, t, d, :], in0=pM[:, ti * 256 + 128:ti * 256 + 256], in1=tI[:, u, :], op=ALU.mult),
                         r=[kM, ('d_tI', u)], w=[('d_IT', t, d)])
                    P.op('dve', lambda e: e.tensor_tensor(out=QDst[:, t, d, :], in0=Qt[:, ts_], in1=EGB[:, u, :], op=ALU.mult), r=['d_qkv0', 'd_GBs'], w=[('d_QD', t, d)])
                    P.op('act', lambda e: e.copy(out=egl[:, t, d:d + 1], in_=EGB[:, u, lastc:lastc + 1]), r=['d_GBs'], w=[('d_egl', t, d)])
                    P.op('act', lambda e: e.activation(out=KSst[:, t, d, :], in_=Ktok[:, ti, :], func=AF.Copy, scale=tI[:, u, lastc:lastc + 1]),
                         r=['d_Ktok', ('d_tI', u)], w=[('d_KS', t, d)])
                    P.op('act', lambda e: e.activation(out=Kbe[:, u, :], in_=Ktok[:, ti, :], func=AF.Copy, scale=ebh[:, t, d:d + 1]),
                         r=['d_Ktok', 'd_ebh'], w=[('d_Kbe', u)])
                    P.op('act', lambda e: e.activation(out=Vb[:, u, :], in_=Vtok[:, ti, :], func=AF.Copy, scale=bcol), r=['d_Vtok', 'tokS'], w=[('d_Vb', u)])
                stop(34)
                kA = [('d_A32', u) for u in range(G)]
                v3 = lambda p_: p_[:].rearrange("p (u n) -> p u n", u=G)
                lmv = lambda j_: lm[:, j_, :].rearrange("p (u n) -> p u n", u=G)
                P.op('act', lambda e: e.copy(out=AT32[:], in_=v3(pX0)), r=[kX0], w=['d_AT32'])
                P.op('dve', lambda e: e.scalar_tensor_tensor(out=ZY[:, :, 0, :], in0=A32[:], scalar=-1.0, in1=lmv(0), op0=ALU.mult, op1=ALU.mult), r=kA + ['d_lm'], w=['d_ZY'])
                P.op('dve', lambda e: e.scalar_tensor_tensor(out=ZTYT[:, :, 0, :], in0=AT32[:], scalar=-1.0, in1=lmv(0), op0=ALU.mult, op1=ALU.mult), r=['d_AT32', 'd_lm'], w=['d_ZTYT'])
                P.op('pool', lambda e: e.tensor_tensor(out=ZY[:, :, 1, :], in0=ZY[:, :, 0, :], in1=lmv(4), op=ALU.add), r=['d_ZY', 'd_lm'], w=['d_ZY'])
                P.op('dve', lambda e: e.tensor_tensor(out=ZTYT[:, :, 1, :], in0=ZTYT[:, :, 0, :], in1=lmv(4), op=ALU.add), r=['d_ZTYT', 'd_lm'], w=['d_ZTYT'])
                stop(35)
                P.op('act', lambda e: e.copy(out=ZYb[:], in_=ZY[:]), r=['d_ZY'], w=['d_ZYb'])
                P.op('dve', lambda e: e.tensor_copy(out=ZTYTb[:], in_=ZTYT[:]), r=['d_ZTYT'], w=['d_ZTYTb'])
                for u in range(G):
                    us_ = slice(u * 128, (u + 1) * 128)
                    P.op('pe', lambda e: e.matmul(pX0[:, us_], lhsT=ZTYTb[:, u, 0, :], rhs=ZYb[:, u, 0, :], start=True, stop=True), r=['d_ZYb', 'd_ZTYTb'], w=[kX0])
                    P.op('pe', lambda e: e.matmul(pX1[:, us_], lhsT=ZYb[:, u, 0, :], rhs=ZTYTb[:, u, 0, :], start=True, stop=True), r=['d_ZYb', 'd_ZTYTb'], w=[kX1])
                P.op('act', lambda e: e.copy(out=ZYb[:, :, 0, :], in_=v3(pX0)), r=[kX0], w=['d_ZYb'])
                P.op('dve', lambda e: e.tensor_copy(out=ZTYTb[:, :, 0, :], in_=v3(pX1)), r=[kX1], w=['d_ZTYTb'])
                for lvl in (1, 2):
                    for u in range(G):
                        px, kx = (pX0, kX0) if u < 2 else (pX1, kX1)
                        pz, kz = (pZT, kZT) if u < 2 else (pU, kU)
                        cs_ = slice((u % 2) * 256, (u % 2) * 256 + 256)
                        P.op('pe', lambda e: e.matmul(px[:, cs_], lhsT=ZTYTb[:, u, 0, :], rhs=ZYb[:, u, :, :].rearrange("p c n -> p (c n)"), start=True, stop=True), r=['d_ZYb', 'd_ZTYTb'], w=[kx])
                        P.op('pe', lambda e: e.matmul(pz[:, cs_], lhsT=ZYb[:, u, 0, :], rhs=ZTYTb[:, u, :, :].rearrange("p c n -> p (c n)"), start=True, stop=True), r=['d_ZYb', 'd_ZTYTb'], w=[kz])
                    for hf, (px, kx, pz, kz) in enumerate(((pX0, kX0, pZT, kZT), (pX1, kX1, pU, kU))):
                        p4 = px[:].rearrange("p (u c n) -> p u c n", u=2, c=2)
                        z4 = pz[:].rearrange("p (u c n) -> p u c n", u=2, c=2)
                        hs2 = slice(hf * 2, hf * 2 + 2)
                        P.op('act', lambda e: e.copy(out=ZYb[:, hs2, 0, :], in_=p4[:, :, 0, :]), r=[kx], w=['d_ZYb'])
                        P.op('dve', lambda e: e.tensor_tensor(out=ZY[:, hs2, 1, :], in0=p4[:, :, 1, :], in1=ZY[:, hs2, 1, :], op=ALU.add), r=[kx, 'd_ZY'], w=['d_ZY'])
                        P.op('act', lambda e: e.copy(out=ZTYTb[:, hs2, 0, :], in_=z4[:, :, 0, :]), r=[kz], w=['d_ZTYTb'])
                        P.op('dve', lambda e: e.tensor_tensor(out=ZTYT[:, hs2, 1, :], in0=z4[:, :, 1, :], in1=ZTYT[:, hs2, 1, :], op=ALU.add), r=[kz, 'd_ZTYT'], w=['d_ZTYT'])
                    P.op('act', lambda e: e.copy(out=ZYb[:, :, 1, :], in_=ZY[:, :, 1, :]), r=['d_ZY'], w=['d_ZYb'])
                    P.op('dve', lambda e: e.tensor_copy(out=ZTYTb[:, :, 1, :], in_=ZTYT[:, :, 1, :]), r=['d_ZTYT'], w=['d_ZTYTb'])
                for u in range(G):
                    us_ = slice(u * 128, (u + 1) * 128)
                    P.op('pe', lambda e: e.matmul(pX0[:, us_], lhsT=ZTYTb[:, u, 0, :], rhs=ZYb[:, u, 1, :], start=True, stop=True), r=['d_ZYb', 'd_ZTYTb'], w=[kX0])
                    P.op('pe', lambda e: e.matmul(pX1[:, us_], lhsT=ZYb[:, u, 0, :], rhs=ZTYTb[:, u, 1, :], start=True, stop=True), r=['d_ZYb', 'd_ZTYTb'], w=[kX1])
                P.op('dve', lambda e: e.tensor_tensor(out=ZY[:, :, 1, :], in0=v3(pX0), in1=ZY[:, :, 1, :], op=ALU.add), r=[kX0, 'd_ZY'], w=['d_ZY'])
                P.op('dve', lambda e: e.tensor_tensor(out=ZTYT[:, :, 1, :], in0=v3(pX1), in1=ZTYT[:, :, 1, :], op=ALU.add), r=[kX1, 'd_ZTYT'], w=['d_ZTYT'])
                P.op('act', lambda e: e.copy(out=TT[:], in_=ZTYT[:, :, 1, :]), r=['d_ZTYT'], w=['d_TT'])
                P.op('dve', lambda e: e.tensor_copy(out=Tb[:], in_=ZY[:, :, 1, :]), r=['d_ZY'], w=['d_Tb'])
                for li in range(3):
                    last = (li == 2)
                    P.op('pool', lambda e: e.tensor_tensor(out=Lb[:], in0=A32[:], in1=lmv(1 + li), op=ALU.mult), r=kA + ['d_lm'], w=['d_Lb'])
                    if not last:
                        P.op('dve', lambda e: e.tensor_tensor(out=LTb[:], in0=AT32[:], in1=lmv(1 + li), op=ALU.mult), r=['d_AT32', 'd_lm'], w=['d_LTb'])
                    for u in range(G):
                        us_ = slice(u * 128, (u + 1) * 128)
                        P.op('pe', lambda e: e.matmul(pX1[:, us_], lhsT=Lb[:, u, :], rhs=TT[:, u, :], start=True, stop=True), r=['d_Lb', 'd_TT'], w=[kX1])
                        if not last:
                            P.op('pe', lambda e: e.matmul(pX0[:, us_], lhsT=LTb[:, u, :], rhs=Tb[:, u, :], start=True, stop=True), r=['d_LTb', 'd_Tb'], w=[kX0])
                    P.op('act', lambda e: e.copy(out=Rb[:], in_=v3(pX1)), r=[kX1], w=['d_Rb'])
                    if not last:
                        P.op('dve', lambda e: e.tensor_copy(out=Qb[:], in_=v3(pX0)), r=[kX0], w=['d_Qb'])
                    for u in range(G):
                        us_ = slice(u * 128, (u + 1) * 128)
                        P.op('pe', lambda e: e.matmul(pU[:, us_], lhsT=Tb[:, u, :], rhs=Rb[:, u, :], start=True, stop=True), r=['d_Tb', 'd_Rb'], w=[kU])
                        if not last:
                            P.op('pe', lambda e: e.matmul(pZT[:, us_], lhsT=TT[:, u, :], rhs=Qb[:, u, :], start=True, stop=True), r=['d_TT', 'd_Qb'], w=[kZT])
                    if not last:
                        P.op('dve', lambda e: e.tensor_tensor(out=ZTYT[:, :, 1, :], in0=ZTYT[:, :, 1, :], in1=v3(pU), op=ALU.subtract), r=[kU, 'd_ZTYT'], w=['d_ZTYT'])
                        P.op('dve', lambda e: e.tensor_tensor(out=ZY[:, :, 1, :], in0=ZY[:, :, 1, :], in1=v3(pZT), op=ALU.subtract), r=[kZT, 'd_ZY'], w=['d_ZY'])
                        P.op('act', lambda e: e.copy(out=TT[:], in_=ZTYT[:, :, 1, :]), r=['d_ZTYT'], w=['d_TT'])
                        P.op('act', lambda e: e.copy(out=Tb[:], in_=ZY[:, :, 1, :]), r=['d_ZY'], w=['d_Tb'])
                    else:
                        P.op('dve', lambda e: e.tensor_tensor(out=TT[:], in0=ZTYT[:, :, 1, :], in1=v3(pU), op=ALU.subtract), r=[kU, 'd_ZTYT'], w=['d_TT'])
                stop(36)
                for u, (ti, d) in enumerate(units):
                    P.op('pe', lambda e: e.matmul(pU[:, u * 128:(u + 1) * 128], lhsT=TT[:, u, :], rhs=Vb[:, u, :], start=True, stop=True), r=['d_TT', ('d_Vb', u)], w=[kU])
                    P.op('pe', lambda e: e.matmul(pG[:, u * 128:(u + 1) * 128], lhsT=Kbe[:, u, :], rhs=TT[:, u, :], start=True, stop=True), r=['d_TT', ('d_Kbe', u)], w=[kG])
                P.op('act', lambda e: e.copy(out=Ust[:, t0:t0 + 2, :, :].rearrange("p a b n -> p (a b) n"), in_=pU[:].rearrange("p (u n) -> p u n", u=G)), r=[kU], w=[('d_U', t0)])
                P.op('dve', lambda e: e.tensor_copy(out=WTst[:, t0:t0 + 2, :, :].rearrange("p a b n -> p (a b) n"), in_=pG[:].rearrange("p (u n) -> p u n", u=G)), r=[kG], w=[('d_WT', t0)])
                stop(3)
            P.barrier()
            stop(4)
            for d in range(2):
                P.op('dve', lambda e: e.memset(S32[d][:], 0.0), w=[f'd_S32{d}'])
                P.op('dve', lambda e: e.memset(Sbf[d][:], 0.0), w=[f'd_Sbf{d}'])
            for step in range(NT):
                tt = [step, NT - 1 - step]
                bank = [((pM, kM), (pX0, kX0), (pZT, kZT)), ((pG, kG), (pX1, kX1), (pU, kU))]
                for d in range(2):
                    t = tt[d]; t0 = (t // 2) * 2
                    (pW, kW) = bank[d][0]
                    P.op('pe', lambda e: e.matmul(pW[:, 0:128], lhsT=WTst[:, t, d, :], rhs=Sbf[d][:], start=True, stop=True), r=[('d_WT', t0), f'd_Sbf{d}'], w=[kW])
                for d in range(2):
                    t = tt[d]; t0 = (t // 2) * 2
                    (pW, kW), (pO, kO) = bank[d][0], bank[d][1]
                    P.op('dve', lambda e: e.tensor_tensor(out=Vn[d][:], in0=Ust[:, t, d, :], in1=pW[:, 0:128], op=ALU.subtract), r=[('d_U', t0), kW], w=[f'd_Vn{d}'])
                    P.op('pe', lambda e: e.matmul(pO[:, 0:128], lhsT=QDst[:, t, d, :], rhs=Sbf[d][:], start=True, stop=False), r=[('d_QD', t, d), f'd_Sbf{d}'], w=[kO])
                for d in range(2):
                    t = tt[d]
                    (pO, kO), (pD, kD) = bank[d][1], bank[d][2]
                    P.op('pe', lambda e: e.matmul(pO[:, 0:128], lhsT=ITst[:, t, d, :], rhs=Vn[d][:], start=False, stop=True), r=[('d_IT', t, d), f'd_Vn{d}'], w=[kO])
                    P.op('pe', lambda e: e.matmul(pD[:, 0:128], lhsT=KSst[:, t, d, :], rhs=Vn[d][:], start=True, stop=True), r=[('d_KS', t, d), f'd_Vn{d}'], w=[kD])
                for d in range(2):
                    t = tt[d]
                    (pO, kO), (pD, kD) = bank[d][1], bank[d][2]
                    P.op('dve', lambda e: e.scalar_tensor_tensor(out=Sbf[d][:], in0=S32[d][:], scalar=egl[:, t, d:d + 1], in1=pD[:, 0:128], op0=ALU.mult, op1=ALU.add),
                         r=[f'd_S32{d}', ('d_egl', t, d), kD], w=[f'd_Sbf{d}'])
                    P.op('dve', lambda e: e.scalar_tensor_tensor(out=S32[d][:], in0=S32[d][:], scalar=egl[:, t, d:d + 1], in1=pD[:, 0:128], op0=ALU.mult, op1=ALU.add),
                         r=[f'd_S32{d}', ('d_egl', t, d), kD], w=[f'd_S32{d}'])
                    if step < NT // 2:
                        P.op('act', lambda e: e.copy(out=Oacc[:, t, :], in_=pO[:, 0:128]), r=[kO], w=[('d_Oacc', t)])
                    else:
                        P.op('pool', lambda e: e.tensor_tensor(out=Oacc[:, t, :], in0=Oacc[:, t, :], in1=Oacc[:, t, :], op=ALU.add), r=[('d_Oacc', t)], w=[('d_Oacc', t)]) if False else \
                            P.op('dve', lambda e: e.tensor_tensor(out=Oacc[:, t, :], in0=pO[:, 0:128], in1=Oacc[:, t, :], op=ALU.add), r=[kO, ('d_Oacc', t)], w=[('d_Oacc', t)])
            stop(5)
            osq = [sbs(esW, f"d_osq{i_}_{h}", [128, 128], F32) for i_ in range(2)]
            ors = sbs(esW, f"d_ors_{h}", [128, 2, NT], F32)
            ont = [sbs(esW, f"d_ont{i_}_{h}", [128, 128], F32) for i_ in range(4)]
            onbt = [sbs(esW, f"d_onbt{i_}_{h}", [128, 128], BF16) for i_ in range(4)]
            kO_all = [('d_Oacc', t_) for t_ in range(NT)]
            P.op('dve', lambda e: e.memset(ors[:], 0.0), w=['d_ors'])
            for t in range(NT):
                P.op('act', lambda e: e.activation(out=osq[t % 2][:], in_=Oacc[:, t, :], func=AF.Square, accum_out=ors[:, 0, t:t + 1]), r=[('d_Oacc', t), 'd_ors'], w=[f'd_osq{t % 2}', ('d_ors_acc', t)])
            P.op('act', lambda e: e.activation(out=ors[:, 1, :], in_=ors[:, 0, :], func=AF.Sqrt, scale=1.0 / 128, bias=eps_l2[:]), r=['d_ors', 'eps_l2'] + [('d_ors_acc', t_) for t_ in range(NT)], w=['d_ors'])
            P.op('dve', lambda e: e.reciprocal(out=ors[:, 1, :], in_=ors[:, 1, :]), r=['d_ors'], w=['d_ors'])
            for t4 in range(0, NT, 4):
                for j in range(4):
                    t = t4 + j
                    P.op('dve', lambda e: e.scalar_tensor_tensor(out=ont[j][:], in0=Oacc[:, t, :], scalar=ors[:, 1, t:t + 1], in1=gain[:], op0=ALU.mult, op1=ALU.mult),
                         r=[('d_Oacc', t), 'd_ors', 'd_gain'], w=[f'd_ont{j}'])
                    P.op('pool', lambda e: e.tensor_tensor(out=onbt[j][:], in0=ont[j][:], in1=zsh[:, t, :], op=ALU.mult), r=[f'd_ont{j}', 'd_zsh'], w=[f'd_onbt{j}'])
                    P.op('pe', lambda e: e.transpose(out=psT[1][:, j * 128:(j + 1) * 128], in_=onbt[j][:], identity=identb[:]), r=[f'd_onbt{j}', 'identb'], w=['psT1'])
                P.op('act', lambda e: e.copy(out=oTd[:, t4 * 128:(t4 + 4) * 128], in_=psT[1][:]), r=['psT1'], w=['d_oTd'])
            fin.append(P.op('sp', lambda e: e.dma_start(out=oT_dn[h, :, :], in_=oTd[:]), r=['d_oTd'], w=[('scr', 'oT_dn', h)], dma=True))
            P.barrier()
            esW.close()
        P.barrier()
        esD.close()

    def phase_MG_MOE():
        esP = ExitStack()
        affTM = sbs(esP, "affTM", [128, NT, 16], F32)
        posm = sbs(esP, "posm", [128, NT, 16], F32)
        iot = sbs(esP, "iot", [128, 512], F32)
        bcs = sbs(esP, "bcs", [128, 4, D], F32)
        gfin = sbs(esP, "gfin", [128, D], F32)
        onesb = sbs(esP, "m_onesb", [128, 128], BF16)
        trib = sbs(esP, "trib", [128, 128], BF16)
        st2 = sbs(esP, "st2", [128, 8], F32)
        P.op('dve', lambda e: e.memset(onesb[:], 1.0), w=['m_onesb'])
        P.op('sp', lambda e: e.dma_start(out=iot[:], in_=iota512[:, :]), w=['iot'], dma=True)
        P.op('sp', lambda e: e.dma_start(out=gfin[:], in_=g_final[:, :]), w=['gfin'], dma=True)
        esH = ExitStack()
        h2b = sbs(esH, "h2b", [128, NT, D], BF16)
        esT = ExitStack()
        affT = sbs(esT, "affT", [16, S], F32)
        esG = ExitStack()
        es1 = ExitStack()
        mrow = sbs(es1, "g_mrow", [1, 4 * D], F32)
        trif = sbs(es1, "g_trif", [128, 128], F32)
        gff = sbs(es1, "g_gff", [128, D], F32)
        P.op('sp', lambda e: e.dma_start(out=mrow[:], in_=modrow[:, 2 * D:6 * D]), r=['mod'], w=['g_mrow'], dma=True)
        P.op('sp', lambda e: e.dma_start(out=trif[:], in_=tri_in[:, :]), w=['g_trif'], dma=True)
        P.op('dve', lambda e: e.tensor_copy(out=trib[:], in_=trif[:]), r=['g_trif'], w=['trib'])
        P.op('sp', lambda e: e.dma_start(out=gff[:], in_=g_ffn[:, :]), w=['g_gff'], dma=True)
        for j in range(8):
            src = j // 2
            dsti = {0: 0, 1: 2, 2: 1, 3: 3}[src]
            pt, pk = next_psA()
            P.op('pe', lambda e: e.matmul(pt[:], lhsT=ones_f[0:1, :], rhs=mrow[:, j * 512:(j + 1) * 512], start=True, stop=True), r=['ones_f', 'g_mrow'], w=[pk])
            P.op('act', lambda e: e.copy(out=bcs[:, dsti, (j % 2) * 512:(j % 2 + 1) * 512], in_=pt[:]), r=[pk], w=['bcs'])
        P.op('dve', lambda e: e.scalar_tensor_tensor(out=bcs[:, 1, :], in0=bcs[:, 1, :], scalar=1.0, in1=gff[:], op0=ALU.add, op1=ALU.mult), r=['bcs', 'g_gff'], w=['bcs'])
        P.barrier()
        es1.close()
        wod = sbs(esG, "g_wod", [128, 8, D], BF16)
        wom = sbs(esG, "g_wom", [128, 8, D], BF16)
        wou = sbs(esG, "g_wou", [128, 8, D], BF16)
        wr = sbs(esG, "g_wr", [128, 8, 16], F32)
        for wt_, src_, k_ in ((wod, w_o_dn, 'g_wod'), (wom, w_o_mla, 'g_wom'), (wou, w_out, 'g_wou')):
            v = src_.rearrange("(k p) n -> p k n", p=128)
            for kk in range(0, 8, 2):
                P.op('pool', lambda e: e.dma_start(out=wt_[:, kk:kk + 2, :], in_=v[:, kk:kk + 2, :]), w=[k_], dma=True)
        P.op('sp', lambda e: e.dma_start(out=wr[:], in_=w_router.rearrange("(k p) n -> p k n", p=128)), w=['g_wr'], dma=True)
        odn = [sbs(esG, f"g_odn{i}", [128, 8, 128], BF16) for i in range(2)]
        oml = [sbs(esG, f"g_oml{i}", [128, 8, 128], BF16) for i in range(2)]
        gst = [sbs(esG, f"g_gst{i}", [128, 2 * D], BF16) for i in range(2)]
        xt0 = sbs(esG, "g_xt0", [128, D], F32)
        xt = [xt0, xt0]
        m1 = sbs(esG, "g_m1", [128, D], F32)
        m2 = sbs(esG, "g_m2", [128, D], F32)
        mb = sbs(esG, "g_mb", [128, D], BF16)
        mT = sbs(esG, "g_mT", [128, 8, 128], BF16)
        x1t = [sbs(esG, f"g_x1t{i}", [128, D], F32) for i in range(2)]
        h2f = sbs(esG, "g_h2f", [128, D], F32)
        h2T = sbs(esG, "g_h2T", [128, 8, 128], F32)
        lg = sbs(esG, "g_lg", [128, 16], F32)
        for t in range(NT):
            i = t % 2
            ts_ = slice(t * 128, (t + 1) * 128)
            P.op('sp', lambda e: e.dma_start(out=odn[i][:], in_=oT_dn[:, :, ts_].rearrange("h p n -> p h n")), r=[('scr', 'oT_dn', h_) for h_ in range(8)], w=[f'g_odn{i}'], dma=True)
            P.op('sp', lambda e: e.dma_start(out=oml[i][:], in_=oT_mla[:, :, ts_].rearrange("h p n -> p h n")), r=[('scr', 'oT_mla', h_) for h_ in range(8)], w=[f'g_oml{i}'], dma=True)
            P.op('sp', lambda e: e.dma_start(out=gst[i][:], in_=gs[ts_, :]), r=[('scr', 'gs', t, b_) for b_ in range(4)], w=[f'g_gst{i}'], dma=True)
            P.op('sp', lambda e: e.dma_start(out=xt[i][:], in_=x[ts_, :]), w=['g_xt0'], dma=True)
            for br, (o_, ok_, w_, wk_) in enumerate(((odn[i], f'g_odn{i}', wod, 'g_wod'), (oml[i], f'g_oml{i}', wom, 'g_wom'))):
                for hc in range(2):
                    pt, pk = psA[br * 2 + hc], f'psA{br * 2 + hc}'
                    for k in range(8):
                        P.op('pe', lambda e: e.matmul(pt[:], lhsT=o_[:, k, :], rhs=w_[:, k, hc * 512:(hc + 1) * 512], start=(k == 0), stop=(k == 7)), r=[ok_, wk_], w=[pk])
            for hc in range(2):
                hs_ = slice(hc * 512, (hc + 1) * 512)
                P.op('dve', lambda e: e.tensor_tensor(out=m1[:, hs_], in0=psA[hc][:], in1=gst[i][:, hs_], op=ALU.mult), r=[f'psA{hc}', f'g_gst{i}'], w=['g_m1'])
                P.op('dve', lambda e: e.tensor_tensor(out=m2[:, hs_], in0=psA[2 + hc][:], in1=gst[i][:, D + hc * 512:D + (hc + 1) * 512], op=ALU.mult), r=[f'psA{2 + hc}', f'g_gst{i}'], w=['g_m2'])
            P.op('pool', lambda e: e.tensor_tensor(out=mb[:], in0=m1[:], in1=m2[:], op=ALU.add), r=['g_m1', 'g_m2'], w=['g_mb'])
            for half in range(2):
                for k4 in range(4):
                    k = half * 4 + k4
                    P.op('pe', lambda e: e.transpose(out=psT[half][:, k4 * 128:(k4 + 1) * 128], in_=mb[:, k * 128:(k + 1) * 128], identity=identb[:]), r=['g_mb', 'identb'], w=[f'psT{half}'])
                P.op('act', lambda e: e.copy(out=mT[:, half * 4:(half + 1) * 4, :], in_=psT[half][:].rearrange("p (k n) -> p k n", k=4)), r=[f'psT{half}'], w=['g_mT'])
            for hc in range(2):
                hs_ = slice(hc * 512, (hc + 1) * 512)
                for k in range(8):
                    P.op('pe', lambda e: e.matmul(psS[hc][:], lhsT=mT[:, k, :], rhs=wou[:, k, hs_], start=(k == 0), stop=(k == 7)), r=['g_mT', 'g_wou'], w=[f'psS{hc}'])
                P.op('dve', lambda e: e.tensor_tensor(out=x1t[i][:, hs_], in0=psS[hc][:], in1=bcs[:, 0, hs_], op=ALU.mult), r=[f'psS{hc}', 'bcs'], w=[f'g_x1t{i}'])
            P.op('pool', lambda e: e.tensor_tensor(out=x1t[i][:], in0=x1t[i][:], in1=xt[i][:], op=ALU.add), r=[f'g_x1t{i}', 'g_xt0'], w=[f'g_x1t{i}'])
            fin.append(P.op('sp', lambda e: e.dma_start(out=x1s[ts_, :], in_=x1t[i][:]), r=[f'g_x1t{i}'], w=[('scr', 'x1s', t)], dma=True))
            P.op('dve', lambda e: e.memset(st2[:], 0.0), w=['st2'])
            P.op('act', lambda e: e.activation(out=m1[:], in_=x1t[i][:], func=AF.Square, accum_out=st2[:, 0:1]), r=[f'g_x1t{i}'], w=['g_m1', 'st2'])
            P.op('act', lambda e: e.activation(out=st2[:, 1:2], in_=st2[:, 0:1], func=AF.Sqrt, scale=1.0 / D, bias=epsb[:]), r=['st2', 'epsb'], w=['st2'])
            P.op('dve', lambda e: e.reciprocal(out=st2[:, 2:3], in_=st2[:, 1:2]), r=['st2'], w=['st2'])
            P.op('dve', lambda e: e.scalar_tensor_tensor(out=h2f[:], in0=x1t[i][:], scalar=st2[:, 2:3], in1=bcs[:, 1, :], op0=ALU.mult, op1=ALU.mult), r=[f'g_x1t{i}', 'st2', 'bcs'], w=['g_h2f'])
            P.op('pool', lambda e: e.tensor_tensor(out=h2f[:], in0=h2f[:], in1=bcs[:, 2, :], op=ALU.add), r=['g_h2f', 'bcs'], w=['g_h2f'])
            P.op('act', lambda e: e.copy(out=h2b[:, t, :], in_=h2f[:]), r=['g_h2f'], w=[('h2b', t)])
            for half in range(2):
                for k4 in range(4):
                    k = half * 4 + k4
                    P.op('pe', lambda e: e.transpose(out=psA[half][:, k4 * 128:(k4 + 1) * 128], in_=h2f[:, k * 128:(k + 1) * 128], identity=ident[:]), r=['g_h2f', 'ident'], w=[f'psA{half}'])
                P.op('act', lambda e: e.copy(out=h2T[:, half * 4:(half + 1) * 4, :], in_=psA[half][:].rearrange("p (k n) -> p k n", k=4)), r=[f'psA{half}'], w=['g_h2T'])
            for k in range(8):
                P.op('pe', lambda e: e.matmul(psA[2][:, 0:16], lhsT=h2T[:, k, :], rhs=wr[:, k, :], start=(k == 0), stop=(k == 7)), r=['g_h2T', 'g_wr'], w=['psA2'])
            P.op('dve', lambda e: e.tensor_reduce(out=st2[:, 3:4], in_=psA[2][:, 0:16], axis=AX.X, op=ALU.max), r=['psA2'], w=['st2'])
            P.op('dve', lambda e: e.tensor_scalar(out=st2[:, 4:5], in0=st2[:, 3:4], scalar1=-1.0, scalar2=None, op0=ALU.mult), r=['st2'], w=['st2'])
            P.op('dve', lambda e: e.memset(st2[:, 5:6], 0.0), r=['st2'], w=['st2'])
            P.op('act', lambda e: e.activation(out=lg[:], in_=psA[2][:, 0:16], func=AF.Exp, bias=st2[:, 4:5], scale=1.0, accum_out=st2[:, 5:6]), r=['psA2', 'st2'], w=['g_lg', 'st2'])
            P.op('dve', lambda e: e.reciprocal(out=st2[:, 6:7], in_=st2[:, 5:6]), r=['st2'], w=['st2'])
            P.op('dve', lambda e: e.tensor_scalar(out=affTM[:, t, :], in0=lg[:], scalar1=st2[:, 6:7], scalar2=None, op0=ALU.mult), r=['g_lg', 'st2'], w=[('affTM', t)])
            P.op('pe', lambda e: e.transpose(out=psA[3][0:16, 0:128], in_=affTM[:, t, :], identity=ident[:]), r=[('affTM', t), 'ident'], w=['psA3'])
            P.op('act', lambda e: e.copy(out=affT[:, ts_], in_=psA[3][0:16, 0:128]), r=['psA3'], w=['affT'])
        P.barrier()
        esG.close()
        esE = ExitStack()
        es1 = ExitStack()
        cmpb = sbs(es1, "e_cmp", [16, S], F32)
        bis = sbs(es1, "e_bis", [16, 8], F32)
        maskT = sbs(es1, "e_maskT", [16, S], F32)
        P.op('dve', lambda e: e.memset(bis[:], 0.0), w=['e_bis'])
        P.op('dve', lambda e: e.memset(bis[:, 1:2], 1.0), r=['e_bis'], w=['e_bis'])
        for it in range(32):
            P.op('dve', lambda e: e.tensor_tensor(out=bis[:, 2:3], in0=bis[:, 0:1], in1=bis[:, 1:2], op=ALU.add), r=['e_bis'], w=['e_bis'])
            P.op('dve', lambda e: e.tensor_scalar(out=bis[:, 2:3], in0=bis[:, 2:3], scalar1=0.5, scalar2=None, op0=ALU.mult), r=['e_bis'], w=['e_bis'])
            P.op('dve', lambda e: e.tensor_scalar(out=cmpb[:], in0=affT[:], scalar1=bis[:, 2:3], scalar2=None, op0=ALU.is_ge), r=['affT', 'e_bis'], w=['e_cmp'])
            P.op('dve', lambda e: e.tensor_reduce(out=bis[:, 3:4], in_=cmpb[:], axis=AX.X, op=ALU.add), r=['e_cmp'], w=['e_bis'])
            P.op('dve', lambda e: e.tensor_scalar(out=bis[:, 4:5], in0=bis[:, 3:4], scalar1=511.5, scalar2=None, op0=ALU.is_ge), r=['e_bis'], w=['e_bis'])
            P.op('dve', lambda e: e.tensor_tensor(out=bis[:, 5:6], in0=bis[:, 2:3], in1=bis[:, 0:1], op=ALU.subtract), r=['e_bis'], w=['e_bis'])
            P.op('dve', lambda e: e.tensor_tensor(out=bis[:, 6:7], in0=bis[:, 1:2], in1=bis[:, 2:3], op=ALU.subtract), r=['e_bis'], w=['e_bis'])
            P.op('dve', lambda e: e.scalar_tensor_tensor(out=bis[:, 0:1], in0=bis[:, 5:6], scalar=bis[:, 4:5], in1=bis[:, 0:1], op0=ALU.mult, op1=ALU.add), r=['e_bis'], w=['e_bis'])
            P.op('dve', lambda e: e.scalar_tensor_tensor(out=bis[:, 1:2], in0=bis[:, 6:7], scalar=bis[:, 4:5], in1=bis[:, 2:3], op0=ALU.mult, op1=ALU.add), r=['e_bis'], w=['e_bis'])
        P.op('dve', lambda e: e.tensor_scalar(out=maskT[:], in0=affT[:], scalar1=bis[:, 0:1], scalar2=None, op0=ALU.is_ge), r=['affT', 'e_bis'], w=['e_maskT'])
        mk32 = sbs(es1, "e_mk32", [128, 16], F32)
        mkb = sbs(es1, "e_mkb", [128, 16], BF16)
        base = sbs(es1, "e_base", [128, 16], F32)
        ptmp = sbs(es1, "e_ptmp", [128, 16], F32)
        P.op('dve', lambda e: e.memset(base[:], 0.0), w=['e_base'])
        for t in range(NT):
            ts_ = slice(t * 128, (t + 1) * 128)
            P.op('pe', lambda e: e.transpose(out=psA[0][:, 0:16], in_=maskT[:, ts_], identity=ident[0:16, 0:16]), r=['e_maskT', 'ident'], w=['psA0'])
            P.op('act', lambda e: e.copy(out=mk32[:], in_=psA[0][:, 0:16]), r=['psA0'], w=['e_mk32'])
            P.op('dve', lambda e: e.tensor_copy(out=mkb[:], in_=mk32[:]), r=['e_mk32'], w=['e_mkb'])
            P.op('pe', lambda e: e.matmul(psA[1][:, 0:16], lhsT=trib[:], rhs=mkb[:], start=True, stop=True), r=['trib', 'e_mkb'], w=['psA1'])
            P.op('pe', lambda e: e.matmul(psA[2][:, 0:16], lhsT=onesb[:], rhs=mkb[:], start=True, stop=True), r=['m_onesb', 'e_mkb'], w=['psA2'])
            P.op('dve', lambda e: e.tensor_tensor(out=ptmp[:], in0=psA[1][:, 0:16], in1=base[:], op=ALU.add), r=['psA1', 'e_base'], w=['e_ptmp'])
            P.op('dve', lambda e: e.scalar_tensor_tensor(out=ptmp[:], in0=ptmp[:], scalar=1.0, in1=mk32[:], op0=ALU.add, op1=ALU.mult), r=['e_ptmp', 'e_mk32'], w=['e_ptmp'])
            P.op('dve', lambda e: e.tensor_scalar(out=posm[:, t, :], in0=ptmp[:], scalar1=-1.0, scalar2=None, op0=ALU.add), r=['e_ptmp'], w=['posm'])
            P.op('dve', lambda e: e.tensor_tensor(out=base[:], in0=psA[2][:, 0:16], in1=base[:], op=ALU.add), r=['psA2', 'e_base'], w=['e_base'])
        P.barrier()
        es1.close()
        esT.close()
        wg = sbs(esE, "e_wg", [128, 8, D], BF16)
        wu = sbs(esE, "e_wu", [128, 8, D], BF16)
        wd = sbs(esE, "e_wd", [128, 8, D], BF16)
        Sel = sbs(esE, "e_Sel", [128, NT, 512], BF16)
        xeT = sbs(esE, "e_xeT", [128, 8, 512], BF16)
        hid = sbs(esE, "e_hid", [128, 8, 512], BF16)
        sg0 = sbs(esE, "e_sg0", [128, 512], F32)
        sg = [sg0, sg0]
        yeb = sbs(esE, "e_yeb", [128, 4, D], BF16)
        stgw = [sbs(esE, f"e_stg{i}", [128, D], F32) for i in range(2)]
        stg_n = [0]
        for ex in range(16):
            for wt_, src_, k_ in ((wg, w_gate, 'e_wg'), (wu, w_up, 'e_wu'), (wd, w_down, 'e_wd')):
                v = src_[ex].rearrange("(k p) n -> p k n", p=128)
                for kk in range(8):
                    if kk % 2 == 0:
                        P.op('pool', lambda e: e.dma_start(out=wt_[:, kk, :], in_=v[:, kk, :]), w=[k_], dma=True)
                    else:
                        j = stg_n[0] % 2
                        stg_n[0] += 1
                        P.op('sp', lambda e: e.dma_start(out=stgw[j][:], in_=v[:, kk, :]), w=[f'e_stg{j}'], dma=True)
                        P.op('act', lambda e: e.copy(out=wt_[:, kk, :], in_=stgw[j][:]), r=[f'e_stg{j}'], w=[k_])
            for t in range(NT):
                P.op('dve', lambda e: e.tensor_scalar(out=Sel[:, t, :], in0=iot[:], scalar1=posm[:, t, ex:ex + 1], scalar2=None, op0=ALU.is_equal), r=['iot', 'posm'], w=[('e_Sel', t)])
            for k in range(8):
                pt, pk = psA[k % 4], f'psA{k % 4}'
                for t in range(NT):
                    P.op('pe', lambda e: e.matmul(pt[:], lhsT=h2b[:, t, k * 128:(k + 1) * 128], rhs=Sel[:, t, :], start=(t == 0), stop=(t == NT - 1)), r=[('h2b', t), ('e_Sel', t)], w=[pk])
                if k % 2 == 0:
                    P.op('act', lambda e: e.copy(out=xeT[:, k, :], in_=pt[:]), r=[pk], w=['e_xeT'])
                else:
                    P.op('dve', lambda e: e.tensor_copy(out=xeT[:, k, :], in_=pt[:]), r=[pk], w=['e_xeT'])
            for f in range(8):
                j = f % 2
                pg, pgk = psA[j * 2], f'psA{j * 2}'
                pu, puk = psA[j * 2 + 1], f'psA{j * 2 + 1}'
                for k in range(8):
                    P.op('pe', lambda e: e.matmul(pg[:], lhsT=wg[:, k, f * 128:(f + 1) * 128], rhs=xeT[:, k, :], start=(k == 0), stop=(k == 7)), r=['e_wg', 'e_xeT'], w=[pgk])
                for k in range(8):
                    P.op('pe', lambda e: e.matmul(pu[:], lhsT=wu[:, k, f * 128:(f + 1) * 128], rhs=xeT[:, k, :], start=(k == 0), stop=(k == 7)), r=['e_wu', 'e_xeT'], w=[puk])
                P.op('act', lambda e: e.activation(out=sg[j][:], in_=pg[:], func=AF.Silu), r=[pgk], w=['e_sg0'])
                P.op('dve', lambda e: e.tensor_tensor(out=hid[:, f, :], in0=pu[:], in1=sg[j][:], op=ALU.mult), r=[puk, 'e_sg0'], w=['e_hid'])
            for q in range(4):
                for hc in range(2):
                    ps_y, pyk = psS[hc], f'psS{hc}'
                    for f in range(8):
                        P.op('pe', lambda e: e.matmul(ps_y[:], lhsT=hid[:, f, q * 128:(q + 1) * 128], rhs=wd[:, f, hc * 512:(hc + 1) * 512], start=(f == 0), stop=(f == 7)), r=['e_hid', 'e_wd'], w=[pyk])
                    if hc == 0:
                        P.op('act', lambda e: e.copy(out=yeb[:, q, 0:512], in_=ps_y[:]), r=[pyk], w=['e_yeb'])
                    else:
                        P.op('dve', lambda e: e.tensor_copy(out=yeb[:, q, 512:1024], in_=ps_y[:]), r=[pyk], w=['e_yeb'])
            fin.append(P.op('sp', lambda e: e.dma_start(out=ye_all[ex].rearrange("(q p) d -> p q d", p=128), in_=yeb[:]), r=['e_yeb'], w=[('scr', 'ye', ex)], dma=True))
        P.barrier()
        esE.close()
        esH.close()
        esF = ExitStack()
        yeA = sbs(esF, "f_yeA", [128, 16, 4, D], BF16)
        for ex in range(16):
            P.op('sp', lambda e: e.dma_start(out=yeA[:, ex, :, :], in_=ye_all[ex].rearrange("(q p) d -> p q d", p=128)), r=[('scr', 'ye', ex)], w=['f_yeA'], dma=True)
        Sg = [sbs(esF, f"f_Sg{i}", [128, 512], BF16) for i in range(2)]
        SgT = sbs(esF, "f_SgT", [128, 16, 4, 128], BF16)
        x1l = [sbs(esF, f"f_x1l{i}", [128, D], F32) for i in range(2)]
        ft = sbs(esF, "f_ft", [128, D], F32)
        ot = [sbs(esF, f"f_ot{i}", [128, D], F32) for i in range(2)]
        junk2 = sbs(esF, "f_junk", [128, D], F32)
        for t in range(NT // 2):
            i = t % 2
            ts_ = slice(t * 128, (t + 1) * 128)
            P.op('sp', lambda e: e.dma_start(out=x1l[i][:], in_=x1s[ts_, :]), r=[('scr', 'x1s', t)], w=[f'f_x1l{i}'], dma=True)
            for ex in range(16):
                j = ex % 2
                P.op('dve', lambda e: e.tensor_scalar(out=Sg[j][:], in0=iot[:], scalar1=posm[:, t, ex:ex + 1], scalar2=affTM[:, t, ex:ex + 1], op0=ALU.is_equal, op1=ALU.mult),
                     r=['iot', 'posm', ('affTM', t)], w=[f'f_Sg{j}'])
                for q in range(4):
                    P.op('pe', lambda e: e.transpose(out=psT[j][:, q * 128:(q + 1) * 128], in_=Sg[j][:, q * 128:(q + 1) * 128], identity=identb[:]), r=[f'f_Sg{j}', 'identb'], w=[f'psT{j}'])
                if j == 0:
                    P.op('act', lambda e: e.copy(out=SgT[:, ex, :, :], in_=psT[j][:].rearrange("p (q n) -> p q n", q=4)), r=[f'psT{j}'], w=[('f_SgT', ex)])
                else:
                    P.op('pool', lambda e: e.tensor_copy(out=SgT[:, ex, :, :], in_=psT[j][:].rearrange("p (q n) -> p q n", q=4)), r=[f'psT{j}'], w=[('f_SgT', ex)]) if False else \
                        P.op('act', lambda e: e.copy(out=SgT[:, ex, :, :], in_=psT[j][:].rearrange("p (q n) -> p q n", q=4)), r=[f'psT{j}'], w=[('f_SgT', ex)])
            for hc in range(2):
                hs_ = slice(hc * 512, (hc + 1) * 512)
                pt, pk = psA[(t % 2) * 2 + hc], f'psA{(t % 2) * 2 + hc}'
                n = 0
                for ex in range(16):
                    for q in range(4):
                        P.op('pe', lambda e: e.matmul(pt[:], lhsT=SgT[:, ex, q, :], rhs=yeA[:, ex, q, hs_], start=(n == 0), stop=(n == 63)), r=[('f_SgT', ex), 'f_yeA'], w=[pk])
                        n += 1
                P.op('dve', lambda e: e.tensor_tensor(out=ft[:, hs_], in0=pt[:], in1=bcs[:, 3, hs_], op=ALU.mult), r=[pk, 'bcs'], w=['f_ft'])
            P.op('pool', lambda e: e.tensor_tensor(out=ft[:], in0=ft[:], in1=x1l[i][:], op=ALU.add), r=['f_ft', f'f_x1l{i}'], w=['f_ft'])
            P.op('dve', lambda e: e.memset(st2[:], 0.0), w=['st2'])
            P.op('act', lambda e: e.activation(out=junk2[:], in_=ft[:], func=AF.Square, accum_out=st2[:, 0:1]), r=['f_ft'], w=['f_junk', 'st2'])
            P.op('act', lambda e: e.activation(out=st2[:, 1:2], in_=st2[:, 0:1], func=AF.Sqrt, scale=1.0 / D, bias=epsb[:]), r=['st2', 'epsb'], w=['st2'])
            P.op('dve', lambda e: e.reciprocal(out=st2[:, 2:3], in_=st2[:, 1:2]), r=['st2'], w=['st2'])
            P.op('dve', lambda e: e.scalar_tensor_tensor(out=ot[i][:], in0=ft[:], scalar=st2[:, 2:3], in1=gfin[:], op0=ALU.mult, op1=ALU.mult), r=['f_ft', 'st2', 'gfin'], w=[f'f_ot{i}'])
            fin.append(P.op('sp', lambda e: e.dma_start(out=out[ts_, :], in_=ot[i][:]), r=[f'f_ot{i}'], dma=True))
        P.barrier()
        esF.close()
        esP.close()

    if 'A' in stages:
        phase_A()
    if 'MLA' in stages:
        phase_MLA()
    if 'DN' in stages:
        try:
            phase_DN()
        except _Stop:
            P.barrier()
    if 'MG' in stages:
        phase_MG_MOE()
    P.finish(fin)
    print("instructions:", P.n)
    return nc


_invf = (10000.0 ** (-np.arange(32, dtype=np.float32) / np.float32(32))).astype(np.float32)
INVF2 = np.concatenate([_invf, _invf])[:, None].astype(np.float32)
SGN2 = np.concatenate([-np.ones(32), np.ones(32)])[:, None].astype(np.float32)
SEL64 = np.zeros((128, 65), np.float32)
SEL64[:, 64] = 1.0
_xi = np.arange(128)[:, None]
_yi = np.arange(128)[None, :]
BIG = 1.0e4
DMASKS = np.stack([np.where(_xi > _yi, 0.0, BIG), np.where(_yi >= _xi, 0.0, -BIG),
                   np.where(_xi < _yi, 0.0, BIG), np.where(_yi <= _xi, 0.0, -BIG)], axis=1).astype(np.float32)
def _bd(s_):
    return ((_xi // s_) == (_yi // s_)).astype(np.float32)
DN_LMASK = np.ascontiguousarray(np.stack([np.tile(m_, (1, 4)) for m_ in
                                          (_bd(16), _bd(32) - _bd(16), _bd(64) - _bd(32), 1.0 - _bd(64), np.eye(128, dtype=np.float32))], axis=1))
IOTA512 = np.ascontiguousarray(np.broadcast_to(np.arange(512, dtype=np.float32)[None, :], (128, 512)))
TRI = (_xi < _yi).astype(np.float32)
IN_SPL = np.cumsum([3072, 1024, 16, 16, 512, 256, 64, 2048])


def prep_inputs(inputs, core):
    b, half = core // 2, core % 2
    f = lambda a: np.ascontiguousarray(a, dtype=np.float32)
    w_in = inputs['w_in'][0]
    qkv, z, bb, aa, cq, ckv, kr, g = np.split(w_in, IN_SPL[:-1], axis=1)
    xb = inputs['x'][b]
    posb = inputs['positions'][b]
    conv_w = inputs['conv_w'][0]
    a_log, dt_bias = inputs['a_log'][0], inputs['dt_bias'][0]
    if half == 1:
        xb = xb[::-1]
        posb = posb[::-1]
        conv_w = conv_w[::-1]
        bb = np.concatenate([bb[:, 8:16], bb[:, 0:8]], axis=1)
        aa = np.concatenate([aa[:, 8:16], aa[:, 0:8]], axis=1)
        a_log, dt_bias = a_log[::-1], dt_bias[::-1]
    swap = np.concatenate([np.arange(32, 64), np.arange(0, 32)])
    wuq_ = inputs['w_uq'][0].reshape(512, 8, 192)
    wukv_ = inputs['w_ukv'][0].reshape(256, 8, 256)
    m = {
        'x': f(xb),
        'cT': f(inputs['c'][b].reshape(8, 128).T),
        'w_mod': f(inputs['w_mod'][0]),
        'b_mod': f(inputs['b_mod'][0][None, :]),
        'g_mix': f(np.broadcast_to(inputs['g_mix'][0][None, :], (128, D))),
        'w_qkv': f(qkv), 'w_z': f(z), 'w_g': f(g),
        'w_ba': f(np.concatenate([bb, aa], axis=1)),
        'w_cq': f(cq), 'w_ckv': f(ckv),
        'w_kr2': f(np.concatenate([kr, kr[:, swap]], axis=1)),
        'q_gain': f(np.broadcast_to(inputs['q_gain'][0][None, :], (128, 512))),
        'kv_gain': f(np.broadcast_to(inputs['kv_gain'][0][None, :], (128, 256))),
        'identf': np.eye(128, dtype=np.float32),
        'posr': np.ascontiguousarray(np.broadcast_to(posb[None, :], (64, S)).astype(np.int32)),
        'invf2': INVF2, 'sgn2': SGN2, 'sel64': SEL64,
        'w_uqh': f(np.stack([np.concatenate([wuq_[:, h, 0:128], wuq_[:, h, 128:192], wuq_[:, h, 128:192][:, swap]], axis=1) for h in range(8)])),
        'w_ukvh': f(np.stack([wukv_[:, h, :] for h in range(8)])),
        'conv_wT': f(conv_w.T),
        'dn_sc': f(np.stack([a_log[0], a_log[1], dt_bias[0], dt_bias[1]], axis=1)),
        'dn_gain': f(np.broadcast_to(inputs['dn_o_gain'][0][None, :], (128, 128))),
        'dmasks': DMASKS, 'dn_lmask': DN_LMASK,
        'w_o_dn': f(inputs['w_o_dn'][0]), 'w_o_mla': f(inputs['w_o_mla'][0]), 'w_out': f(inputs['w_out'][0]),
        'g_ffn': f(np.broadcast_to(inputs['g_ffn'][0][None, :], (128, D))),
        'g_final': f(np.broadcast_to(inputs['g_final'][None, :], (128, D))),
        'w_router': f(inputs['w_router'][0]),
        'w_gate': f(inputs['w_gate'][0]), 'w_up': f(inputs['w_up'][0]), 'w_down': f(inputs['w_down'][0]),
        'iota512': IOTA512, 'tri_in': TRI,
    }
    return m


def kernel(**inputs):
    inputs = {k: np.asarray(v) for k, v in inputs.items()}
    nc = build()
    in_maps = [prep_inputs(inputs, c) for c in range(8)]
    res = run_bass_kernel_spmd(nc, in_maps, core_ids=list(range(8)))
    outp = np.zeros((4, S, D), np.float32)
    for c in range(8):
        b, half = c // 2, c % 2
        o_ = res.results[c]["out"]
        if half == 0:
            outp[b, 0:2048] = o_
        else:
            outp[b, 2048:4096] = o_[::-1]
    return outp
```

```python
import numpy as np
import ml_dtypes
import concourse.bass as bass
import concourse.mybir as mybir
from concourse.bass_utils import run_bass_kernel_spmd
from contextlib import ExitStack

F32 = mybir.dt.float32
BF16 = mybir.dt.bfloat16
I32 = mybir.dt.int32
AF = mybir.ActivationFunctionType
ALU = mybir.AluOpType
AX = mybir.AxisListType

import os
DBGN = int(os.environ.get('DBGN', '99'))
S = 4096
D = 1024
NT = S // 128
EPS = 1e-6


class Prog:
    def __init__(self, nc, ndma=16):
        self.nc = nc
        self.eng = {'pe': nc.tensor, 'act': nc.scalar, 'dve': nc.vector,
                    'pool': nc.gpsimd, 'sp': nc.sync}
        self.sem = {k: nc.alloc_semaphore(name=f"s_{k}") for k in self.eng}
        self.cnt = {k: 0 for k in self.eng}
        self.waited = {k: {} for k in self.eng}
        self.dsem = {q: [nc.alloc_semaphore(name=f"s_dma_{q}{i}") for i in range(ndma)] for q in ('sp', 'pool')}
        self.dcnt = {q: [0] * ndma for q in ('sp', 'pool')}
        self.nd = {'sp': 0, 'pool': 0}
        self.lastw = {}
        self.readers = {}
        self.n = 0

    def _wait(self, e, tok):
        if tok is None:
            return
        sem, val, key = tok
        w = self.waited[e]
        if w.get(key, 0) >= val:
            return
        w[key] = val
        self.eng[e].wait_ge(sem, val)

    def op(self, e, fn, r=(), w=(), dma=False):
        w = list(w) + [t for t in r if isinstance(t, str) and t.startswith('ps') and t not in w]
        toks = []
        for t in r:
            toks.append(self.lastw.get(t))
        for t in w:
            toks.append(self.lastw.get(t))
            toks.extend(self.readers.get(t, ()))
        for tok in toks:
            self._wait(e, tok)
        if dma:
            i = self.nd[e] % len(self.dsem[e])
            if self.dcnt[e][i] > 0:
                self._wait(e, (self.dsem[e][i], self.dcnt[e][i], f"dma_{e}{i}"))
        ins = fn(self.eng[e])
        self.n += 1
        if dma:
            i = self.nd[e] % len(self.dsem[e])
            self.nd[e] += 1
            self.dcnt[e][i] += 16
            ins.then_inc(self.dsem[e][i], 16)
            tok = (self.dsem[e][i], self.dcnt[e][i], f"dma_{e}{i}")
        else:
            self.cnt[e] += 1
            ins.then_inc(self.sem[e], 1)
            tok = (self.sem[e], self.cnt[e], e)
        for t in r:
            self.readers.setdefault(t, []).append(tok)
        for t in w:
            self.lastw[t] = tok
            self.readers[t] = []
        return tok

    def pe_group(self, fns, r=(), w=()):
        e = 'pe'
        w = list(w) + [t for t in r if isinstance(t, str) and t.startswith('ps') and t not in w]
        toks = []
        for t in r:
            toks.append(self.lastw.get(t))
        for t in w:
            toks.append(self.lastw.get(t))
            toks.extend(self.readers.get(t, ()))
        for tok in toks:
            self._wait(e, tok)
        for fn in fns[:-1]:
            fn(self.eng[e])
            self.n += 1
        ins = fns[-1](self.eng[e])
        self.n += 1
        self.cnt[e] += 1
        ins.then_inc(self.sem[e], 1)
        tok = (self.sem[e], self.cnt[e], e)
        for t in r:
            self.readers.setdefault(t, []).append(tok)
        for t in w:
            self.lastw[t] = tok
            self.readers[t] = []
        return tok

    def barrier(self):
        toks = [(self.sem[k], self.cnt[k], k) for k in self.eng if self.cnt[k] > 0]
        toks += [(self.dsem[q][i], self.dcnt[q][i], f"dma_{q}{i}") for q in self.dsem for i in range(len(self.dsem[q])) if self.dcnt[q][i] > 0]
        for e in self.eng:
            for tok in toks:
                self._wait(e, tok)

    def finish(self, toks):
        for tok in toks:
            self._wait('sp', tok)


class _Stop(Exception):
    pass


def build(debug=None, stages=('A', 'MLA', 'DN', 'MG'), ext_in=(), dn_heads=range(8), dn_stop=0):
    nc = bass.Bass("TRN2", target_bir_lowering=False)
    P = Prog(nc)

    def din(name, shape, dt=F32):
        return nc.dram_tensor(name, list(shape), dt, kind="ExternalInput").ap()

    def dscr(name, shape, dt):
        kind = "ExternalOutput" if (debug and name in debug) else ("ExternalInput" if name in ext_in else "Internal")
        return nc.dram_tensor(name, list(shape), dt, kind=kind).ap()

    x = din("x", [S, D])
    cT = din("cT", [128, 8])
    w_mod = din("w_mod", [D, 6 * D])
    b_mod = din("b_mod", [1, 6 * D])
    g_mix = din("g_mix", [128, D])
    w_qkv = din("w_qkv", [D, 3072])
    w_z = din("w_z", [D, 1024])
    w_g = din("w_g", [D, 2048])
    w_ba = din("w_ba", [D, 32])
    w_cq = din("w_cq", [D, 512])
    w_ckv = din("w_ckv", [D, 256])
    w_kr2 = din("w_kr2", [D, 128])
    q_gain = din("q_gain", [128, 512])
    kv_gain = din("kv_gain", [128, 256])
    identf = din("identf", [128, 128])
    posr = din("posr", [64, S], I32)
    invf2 = din("invf2", [64, 1])
    sgn2 = din("sgn2", [64, 1])
    sel64 = din("sel64", [128, 65])
    w_uqh = din("w_uqh", [8, 512, 256])
    w_ukvh = din("w_ukvh", [8, 256, 256])
    conv_wT = din("conv_wT", [3072, 5])
    dn_sc = din("dn_sc", [8, 4])
    dn_gain = din("dn_gain", [128, 128])
    dmasks = din("dmasks", [128, 4, 128])
    dn_lmask = din("dn_lmask", [128, 5, 512])
    w_o_dn = din("w_o_dn", [D, D])
    w_o_mla = din("w_o_mla", [D, D])
    w_out = din("w_out", [D, D])
    g_ffn = din("g_ffn", [128, D])
    g_final = din("g_final", [128, D])
    w_router = din("w_router", [D, 16])
    w_gate = din("w_gate", [16, D, D])
    w_up = din("w_up", [16, D, D])
    w_down = din("w_down", [16, D, D])
    iota512 = din("iota512", [128, 512])
    tri_in = din("tri_in", [128, 128])
    out = nc.dram_tensor("out", [S // 2, D], F32, kind="ExternalOutput").ap()

    qkvT = dscr("qkvT", [24, 128, S], BF16)
    zs = dscr("zs", [S, 1024], BF16)
    gs = dscr("gs", [S, 2048], BF16)
    baT = dscr("baT", [32, S], F32)
    cqnT = dscr("cqnT", [4, 128, S], BF16)
    ckvnT = dscr("ckvnT", [2, 128, S], BF16)
    krT = dscr("krT", [2, 64, S], BF16)
    modrow = dscr("modrow", [1, 6 * D], F32)
    oT_mla = dscr("oT_mla", [8, 128, S], BF16)
    oT_dn = dscr("oT_dn", [8, 128, S], BF16)
    x1s = dscr("x1s", [S, D], F32)
    ye_all = dscr("ye_all", [16, 512, D], BF16)

    sb = lambda n, s, d: nc.alloc_sbuf_tensor(n, list(s), d)
    ps_ = lambda n, s, d=F32: nc.alloc_psum_tensor(n, list(s), d)

    ident = sb("ident", [128, 128], F32)
    identb = sb("identb", [128, 128], BF16)
    ones_f = sb("ones_f", [128, 128], F32)
    P.op('sp', lambda e: e.dma_start(out=ident[:], in_=identf[:, :]), w=['ident'], dma=True)
    P.op('dve', lambda e: e.tensor_copy(out=identb[:], in_=ident[:]), r=['ident'], w=['identb'])
    P.op('dve', lambda e: e.memset(ones_f[:], 1.0), w=['ones_f'])
    epsb = sb("epsb", [128, 1], F32)
    P.op('dve', lambda e: e.memset(epsb[:], EPS), w=['epsb'])

    psA = [ps_(f"psA{i}", [128, 512]) for i in range(4)]
    psT = [ps_(f"psT{i}", [128, 512], BF16) for i in range(2)]
    psS = [ps_(f"psS{i}", [128, 512]) for i in range(2)]
    pa_i = [0]

    def next_psA():
        i = pa_i[0] % 4
        pa_i[0] += 1
        return psA[i], f"psA{i}"

    sbs = lambda es, n, s_, d: es.enter_context(nc.sbuf_tensor(n, list(s_), d))
    fin = []

    def phase_A():
        cT_sb = sb("cT_sb", [128, 8], F32)
        scT = sb("scT", [128, 8], F32)
        P.op('sp', lambda e: e.dma_start(out=cT_sb[:], in_=cT[:, :]), w=['cT'], dma=True)
        P.op('act', lambda e: e.activation(out=scT[:], in_=cT_sb[:], func=AF.Silu), r=['cT'], w=['scT'])
        esA = ExitStack()
        modbc = sbs(esA, "modbc", [128, 6, D], F32)
        gmx = sbs(esA, "gmx", [128, D], F32)
        A1 = sbs(esA, "A1", [128, D], F32)
        es0 = ExitStack()
        mod_sb = sbs(es0, "mod_sb", [1, 6 * D], F32)
        bm_sb = sbs(es0, "bm_sb", [1, 6 * D], F32)
        P.op('sp', lambda e: e.dma_start(out=bm_sb[:], in_=b_mod[:, :]), w=['bm'], dma=True)
        wm = [sbs(es0, f"wm{i}", [128, 8, 512], F32) for i in range(2)]
        w_mod_v = w_mod.rearrange("(k p) n -> p k n", p=128)
        for j in range(12):
            wt, wk = wm[j % 2], f"wm{j % 2}"
            P.op('sp', lambda e: e.dma_start(out=wt[:], in_=w_mod_v[:, :, j * 512:(j + 1) * 512]), w=[wk], dma=True)
            pt, pk = next_psA()
            for k in range(8):
                P.op('pe', lambda e: e.matmul(pt[0:1, :], lhsT=scT[:, k:k + 1], rhs=wt[:, k, :], start=(k == 0), stop=(k == 7)),
                     r=[wk, 'scT'], w=[pk])
            P.op('dve', lambda e: e.tensor_tensor(out=mod_sb[:, j * 512:(j + 1) * 512], in0=pt[0:1, :], in1=bm_sb[:, j * 512:(j + 1) * 512], op=ALU.add),
                 r=[pk, 'bm'], w=['mod'])
        for j in range(12):
            pt, pk = next_psA()
            P.op('pe', lambda e: e.matmul(pt[:], lhsT=ones_f[0:1, :], rhs=mod_sb[:, j * 512:(j + 1) * 512], start=True, stop=True),
                 r=['ones_f', 'mod'], w=[pk])
            P.op('act', lambda e: e.copy(out=modbc[:, j // 2, (j % 2) * 512:(j % 2 + 1) * 512], in_=pt[:]), r=[pk], w=['modbc'])
        P.op('sp', lambda e: e.dma_start(out=gmx[:], in_=g_mix[:, :]), w=['gmx'], dma=True)
        P.op('dve', lambda e: e.scalar_tensor_tensor(out=A1[:], in0=modbc[:, 1, :], scalar=1.0, in1=gmx[:], op0=ALU.add, op1=ALU.mult),
             r=['modbc', 'gmx'], w=['A1'])

        fin.append(P.op('sp', lambda e: e.dma_start(out=modrow[:, :], in_=mod_sb[:]), r=['mod'], dma=True))
        P.barrier()
        es0.close()
        hT = sbs(esA, "hT", [128, 8, S], BF16)
        xt = [sbs(esA, f"xt{i}", [128, D], F32) for i in range(2)]
        xn = [sbs(esA, f"xn{i}", [128, D], F32) for i in range(2)]
        hb = [sbs(esA, f"hb{i}", [128, D], BF16) for i in range(2)]
        st = [sbs(esA, f"st{i}", [128, 4], F32) for i in range(2)]
        junk = sbs(esA, "junk", [128, D], F32)
        for t in range(NT):
            i = t % 2
            P.op('sp', lambda e: e.dma_start(out=xt[i][:], in_=x[t * 128:(t + 1) * 128, :]), w=[f'xt{i}'], dma=True)
            P.op('dve', lambda e: e.memset(st[i][:], 0.0), w=[f'st{i}'])
            P.op('act', lambda e: e.activation(out=junk[:], in_=xt[i][:], func=AF.Square, accum_out=st[i][:, 0:1]),
                 r=[f'xt{i}'], w=['junk', f'st{i}'])
            P.op('act', lambda e: e.activation(out=st[i][:, 1:2], in_=st[i][:, 0:1], func=AF.Sqrt, scale=1.0 / D, bias=epsb[:]),
                 r=[f'st{i}', 'epsb'], w=[f'st{i}'])
            P.op('dve', lambda e: e.reciprocal(out=st[i][:, 2:3], in_=st[i][:, 1:2]), r=[f'st{i}'], w=[f'st{i}'])
            P.op('dve', lambda e: e.scalar_tensor_tensor(out=xn[i][:], in0=xt[i][:], scalar=st[i][:, 2:3], in1=A1[:], op0=ALU.mult, op1=ALU.mult),
                 r=[f'xt{i}', f'st{i}', 'A1'], w=[f'xn{i}'])
            P.op('pool', lambda e: e.tensor_tensor(out=hb[i][:], in0=xn[i][:], in1=modbc[:, 0, :], op=ALU.add),
                 r=[f'xn{i}', 'modbc'], w=[f'hb{i}'])
            for half in range(2):
                pt, pk = psT[half], f'psT{half}'
                for k4 in range(4):
                    k = half * 4 + k4
                    P.op('pe', lambda e: e.transpose(out=pt[:, k4 * 128:(k4 + 1) * 128], in_=hb[i][:, k * 128:(k + 1) * 128], identity=identb[:]),
                         r=[f'hb{i}', 'identb'], w=[pk])
                eng = 'act' if half == 0 else 'dve'
                if eng == 'act':
                    P.op('act', lambda e: e.copy(out=hT[:, half * 4:(half + 1) * 4, t * 128:(t + 1) * 128],
                                                 in_=pt[:].rearrange("p (k n) -> p k n", k=4)), r=[pk], w=[('hT', t)])
                else:
                    P.op('dve', lambda e: e.tensor_copy(out=hT[:, half * 4:(half + 1) * 4, t * 128:(t + 1) * 128],
                                                        in_=pt[:].rearrange("p (k n) -> p k n", k=4)), r=[pk], w=[('hT', t)])
        hT_all = [('hT', t) for t in range(NT)]

        wb = [sbs(esA, f"wb{i}", [128, 8, 512], BF16) for i in range(2)]
        wb_i = [0]

        def load_w(src, c0, ncols):
            i = wb_i[0] % 2
            wb_i[0] += 1
            v = src.rearrange("(k p) n -> p k n", p=128)
            P.op('pool', lambda e: e.dma_start(out=wb[i][:, :, 0:ncols], in_=v[:, :, c0:c0 + ncols]), w=[f'wb{i}'], dma=True)
            return wb[i], f'wb{i}'

        stg = [sbs(esA, f"stg{i}", [128, S], BF16) for i in range(2)]
        stg_i = [0]

        def chan_major(src, ncols_total, dst_fn, M, dt_out=BF16, stgs=stg):
            nblk = (ncols_total + 511) // 512
            for blk in range(nblk):
                nc_ = min(512, ncols_total - blk * 512)
                wt, wk = load_w(src, blk * 512, nc_)
                for c in range(nc_ // M):
                    si = stg_i[0] % 2
                    stg_i[0] += 1
                    sg, sk = stgs[si], f'{stgs[si].name}'
                    for g in range(8):
                        pt, pk = next_psA()
                        P.pe_group([(lambda e, k=k: e.matmul(pt[0:M, :], lhsT=wt[:, k, c * M:(c + 1) * M], rhs=hT[:, k, g * 512:(g + 1) * 512],
                                                            start=(k == 0), stop=(k == 7))) for k in range(8)],
                                   r=[wk] + hT_all[g * 4:(g + 1) * 4], w=[pk])
                        if g % 2 == 0:
                            P.op('act', lambda e: e.copy(out=sg[0:M, g * 512:(g + 1) * 512], in_=pt[0:M, :]), r=[pk], w=[sk])
                        else:
                            P.op('dve', lambda e: e.tensor_copy(out=sg[0:M, g * 512:(g + 1) * 512], in_=pt[0:M, :]), r=[pk], w=[sk])
                    fin.append(P.op('sp', lambda e: e.dma_start(out=dst_fn(blk * (512 // M) + c), in_=sg[0:M, :]), r=[sk], w=[('scr', dst_fn.__name__)], dma=True))

        def dst_qkv(c):
            return qkvT[c, :, :]
        chan_major(w_qkv, 3072, dst_qkv, 128)

        def dst_kr(c):
            return krT[c, :, :]
        chan_major(w_kr2, 128, dst_kr, 64)
        stgf0 = sbs(esA, "stgf0", [32, S], F32)
        stgf = [stgf0, stgf0]

        def dst_ba(c):
            return baT[:, :]
        chan_major(w_ba, 32, dst_ba, 32, F32, stgf)

        tst = [sbs(esA, f"tst{i}", [128, 512], BF16) for i in range(4)]
        tst_i = [0]

        def tok_major_act(src, ncols_total, dst, func):
            for blk in range(ncols_total // 512):
                wt, wk = load_w(src, blk * 512, 512)
                for t in range(NT):
                    pt, pk = next_psA()
                    for k in range(8):
                        P.op('pe', lambda e: e.matmul(pt[:], lhsT=hT[:, k, t * 128:(t + 1) * 128], rhs=wt[:, k, :], start=(k == 0), stop=(k == 7)),
                             r=[wk, ('hT', t)], w=[pk])
                    si = tst_i[0] % 4
                    tst_i[0] += 1
                    P.op('act', lambda e: e.activation(out=tst[si][:], in_=pt[:], func=func), r=[pk], w=[f'tst{si}'])
                    fin.append(P.op('sp', lambda e: e.dma_start(out=dst[t * 128:(t + 1) * 128, blk * 512:(blk + 1) * 512], in_=tst[si][:]),
                                    r=[f'tst{si}'], w=[('scr', dst.name, t, blk)], dma=True))
        tok_major_act(w_z, 1024, zs, AF.Silu)
        tok_major_act(w_g, 2048, gs, AF.Sigmoid)

        def latent(src, ncols, gain_in, dstT, nm):
            gsb = sbs(esA, f"gain_{nm}", [128, ncols], F32)
            P.op('sp', lambda e: e.dma_start(out=gsb[:], in_=gain_in[:, :]), w=[f'gain_{nm}'], dma=True)
            lst = [sbs(esA, f"lst_{nm}{i_}", [128, ncols // 128, 128], BF16) for i_ in range(2)]
            wt, wk = load_w(src, 0, ncols)
            for t in range(NT):
                i = t % 2
                pt, pk = next_psA()
                for k in range(8):
                    P.op('pe', lambda e: e.matmul(pt[:, 0:ncols], lhsT=hT[:, k, t * 128:(t + 1) * 128], rhs=wt[:, k, 0:ncols], start=(k == 0), stop=(k == 7)),
                         r=[wk, ('hT', t)], w=[pk])
                P.op('dve', lambda e: e.memset(st[i][:], 0.0), w=[f'st{i}'])
                P.op('act', lambda e: e.activation(out=junk[:, 0:ncols], in_=pt[:, 0:ncols], func=AF.Square, accum_out=st[i][:, 0:1]),
                     r=[pk], w=['junk', f'st{i}'])
                P.op('act', lambda e: e.activation(out=st[i][:, 1:2], in_=st[i][:, 0:1], func=AF.Sqrt, scale=1.0 / ncols, bias=epsb[:]),
                     r=[f'st{i}', 'epsb'], w=[f'st{i}'])
                P.op('dve', lambda e: e.reciprocal(out=st[i][:, 2:3], in_=st[i][:, 1:2]), r=[f'st{i}'], w=[f'st{i}'])
                P.op('dve', lambda e: e.scalar_tensor_tensor(out=hb[i][:, 0:ncols], in0=pt[:, 0:ncols], scalar=st[i][:, 2:3], in1=gsb[:], op0=ALU.mult, op1=ALU.mult),
                     r=[pk, f'st{i}', f'gain_{nm}'], w=[f'hb{i}'])
                tp, tk = psT[i], f'psT{i}'
                for c in range(ncols // 128):
                    P.op('pe', lambda e: e.transpose(out=tp[:, c * 128:(c + 1) * 128], in_=hb[i][:, c * 128:(c + 1) * 128], identity=identb[:]),
                         r=[f'hb{i}', 'identb'], w=[tk])
                P.op('act', lambda e: e.copy(out=lst[i][:], in_=tp[:, 0:ncols].rearrange("p (k n) -> p k n", k=ncols // 128)),
                     r=[tk], w=[f'lst_{nm}{i}'])
                fin.append(P.op('sp', lambda e: e.dma_start(out=dstT[:, :, t * 128:(t + 1) * 128].rearrange("c p n -> p c n"), in_=lst[i][:]),
                                r=[f'lst_{nm}{i}'], w=[('scr', nm, t)], dma=True))
        latent(w_cq, 512, q_gain, cqnT, 'cq')
        latent(w_ckv, 256, kv_gain, ckvnT, 'ckv')


        P.barrier()
        esA.close()

    def phase_MLA():
        esM = ExitStack()
        TWO_PI = float(2 * np.pi)
        SCL = float(192 ** -0.5)
        cos2 = sbs(esM, "cos2", [64, S], F32)
        sin2 = sbs(esM, "sin2", [64, S], F32)
        krA = sbs(esM, "krA", [65, S], BF16)
        QrA = sbs(esM, "QrA", [65, S], BF16)
        onesb = sbs(esM, "onesb", [128, 128], BF16)
        sel_b = sbs(esM, "sel_b", [128, 65], BF16)
        if True:
            es1 = ExitStack()
            posi = sbs(es1, "posi", [64, S], I32)
            ang = sbs(es1, "ang", [64, S], F32)
            ti = sbs(es1, "ti", [64, S], I32)
            tf = sbs(es1, "tf", [64, S], F32)
            tg = sbs(es1, "tg", [64, S], F32)
            ivf = sbs(es1, "ivf", [64, 2], F32)
            kr0 = sbs(es1, "kr0", [64, S], BF16)
            kr1 = sbs(es1, "kr1", [64, S], BF16)
            self_f = sbs(es1, "self_f", [128, 65], F32)
            P.op('sp', lambda e: e.dma_start(out=posi[:], in_=posr[:, :]), w=['posi'], dma=True)
            P.op('sp', lambda e: e.dma_start(out=ivf[:, 0:1], in_=invf2[:, :]), w=['ivf'], dma=True)
            P.op('sp', lambda e: e.dma_start(out=ivf[:, 1:2], in_=sgn2[:, :]), w=['ivf'], dma=True)
            P.op('sp', lambda e: e.dma_start(out=self_f[:], in_=sel64[:, :]), w=['self_f'], dma=True)
            P.op('dve', lambda e: e.tensor_copy(out=sel_b[:], in_=self_f[:]), r=['self_f'], w=['sel_b'])
            P.op('dve', lambda e: e.memset(onesb[:], 1.0), w=['onesb'])
            P.op('dve', lambda e: e.tensor_copy(out=ang[:], in_=posi[:]), r=['posi'], w=['ang'])
            P.op('dve', lambda e: e.tensor_scalar(out=ang[:], in0=ang[:], scalar1=ivf[:, 0:1], scalar2=float(1.0 / TWO_PI), op0=ALU.mult, op1=ALU.mult),
                 r=['ang', 'ivf'], w=['ang'])
            for which, dst in ((0, sin2), (1, cos2)):
                dk_ = 'sin2' if which == 0 else 'cos2'
                P.op('dve', lambda e: e.tensor_scalar(out=tg[:], in0=ang[:], scalar1=0.25 * which, scalar2=None, op0=ALU.add), r=['ang'], w=['tg'])
                P.op('dve', lambda e: e.tensor_copy(out=ti[:], in_=tg[:]), r=['tg'], w=['ti'])
                P.op('dve', lambda e: e.tensor_copy(out=tf[:], in_=ti[:]), r=['ti'], w=['tf'])
                P.op('dve', lambda e: e.tensor_tensor(out=tg[:], in0=tg[:], in1=tf[:], op=ALU.subtract), r=['tg', 'tf'], w=['tg'])
                P.op('dve', lambda e: e.tensor_scalar(out=tf[:], in0=tg[:], scalar1=0.5, scalar2=None, op0=ALU.is_gt), r=['tg'], w=['tf'])
                P.op('dve', lambda e: e.tensor_tensor(out=tg[:], in0=tg[:], in1=tf[:], op=ALU.subtract), r=['tg', 'tf'], w=['tg'])
                P.op('dve', lambda e: e.tensor_scalar(out=tf[:], in0=tg[:], scalar1=-0.5, scalar2=None, op0=ALU.is_lt), r=['tg'], w=['tf'])
                P.op('dve', lambda e: e.tensor_tensor(out=tg[:], in0=tg[:], in1=tf[:], op=ALU.add), r=['tg', 'tf'], w=['tg'])
                P.op('act', lambda e: e.activation(out=dst[:], in_=tg[:], func=AF.Sin, scale=TWO_PI), r=['tg'], w=[dk_])
            P.op('dve', lambda e: e.tensor_scalar(out=sin2[:], in0=sin2[:], scalar1=ivf[:, 1:2], scalar2=None, op0=ALU.mult), r=['sin2', 'ivf'], w=['sin2'])
            P.op('sp', lambda e: e.dma_start(out=kr0[:], in_=krT[0, :, :]), r=[('scr', 'dst_kr')], w=['kr0'], dma=True)
            P.op('sp', lambda e: e.dma_start(out=kr1[:], in_=krT[1, :, :]), r=[('scr', 'dst_kr')], w=['kr1'], dma=True)
            P.op('dve', lambda e: e.tensor_tensor(out=tg[:], in0=kr0[:], in1=cos2[:], op=ALU.mult), r=['kr0', 'cos2'], w=['tg'])
            P.op('dve', lambda e: e.tensor_tensor(out=tf[:], in0=kr1[:], in1=sin2[:], op=ALU.mult), r=['kr1', 'sin2'], w=['tf'])
            P.op('dve', lambda e: e.memset(krA[:], 1.0), w=['krA'])
            P.op('dve', lambda e: e.tensor_tensor(out=krA[0:64, :], in0=tg[:], in1=tf[:], op=ALU.add), r=['tg', 'tf'], w=['krA'])
            P.op('dve', lambda e: e.memset(QrA[:], 0.0), w=['QrA'])
            P.barrier()
            es1.close()
        cqn = sbs(esM, "cqn", [128, 4, S], BF16)
        ckvn = sbs(esM, "ckvn", [128, 2, S], BF16)
        QnT = sbs(esM, "QnT", [128, S], BF16)
        KnT = sbs(esM, "KnT", [128, S], BF16)
        Vt = sbs(esM, "Vt", [128, NT, 128], BF16)
        oTs = sbs(esM, "oTs", [128, S], BF16)
        kmx = sbs(esM, "kmx", [65, 16], F32)
        wuq = sbs(esM, "wuq", [128, 4, 256], BF16)
        wukv = sbs(esM, "wukv", [128, 2, 256], BF16)
        sq = sbs(esM, "sq", [128, 512], BF16)
        t1 = sbs(esM, "t1", [64, 512], F32)
        t2 = sbs(esM, "t2", [64, 512], F32)
        rowt = sbs(esM, "rowt", [65, 512], F32)
        pT = [sbs(esM, f"pT{i}", [128, 512], BF16) for i in range(3)]
        rden = sbs(esM, "rden", [128, 512], F32)
        for c in range(4):
            P.op('sp', lambda e: e.dma_start(out=cqn[:, c, :], in_=cqnT[c, :, :]), r=[('scr', 'cq', t_) for t_ in range(NT)], w=['cqn'], dma=True)
        for c in range(2):
            P.op('sp', lambda e: e.dma_start(out=ckvn[:, c, :], in_=ckvnT[c, :, :]), r=[('scr', 'ckv', t_) for t_ in range(NT)], w=['ckvn'], dma=True)
        for h in range(8):
            P.op('pool', lambda e: e.dma_start(out=wuq[:], in_=w_uqh[h].rearrange("(k p) n -> p k n", p=128)), w=['wuq'], dma=True)
            P.op('pool', lambda e: e.dma_start(out=wukv[:], in_=w_ukvh[h].rearrange("(k p) n -> p k n", p=128)), w=['wukv'], dma=True)
            for g in range(8):
                gs_ = slice(g * 512, (g + 1) * 512)
                pt, pk = psA[2], 'psA2'
                for k in range(4):
                    P.op('pe', lambda e: e.matmul(pt[:], lhsT=wuq[:, k, 0:128], rhs=cqn[:, k, gs_], start=(k == 0), stop=(k == 3)), r=['wuq', 'cqn'], w=[pk])
                P.op('act', lambda e: e.copy(out=QnT[:, gs_], in_=pt[:]), r=[pk], w=[('QnT', g)])
                pt, pk = psA[3], 'psA3'
                for k in range(4):
                    P.op('pe', lambda e: e.matmul(pt[0:64, :], lhsT=wuq[:, k, 128:192], rhs=cqn[:, k, gs_], start=(k == 0), stop=(k == 3)), r=['wuq', 'cqn'], w=[pk])
                P.op('dve', lambda e: e.tensor_tensor(out=t1[:], in0=pt[0:64, :], in1=cos2[:, gs_], op=ALU.mult), r=[pk, 'cos2'], w=['t1'])
                for k in range(4):
                    P.op('pe', lambda e: e.matmul(pt[0:64, :], lhsT=wuq[:, k, 192:256], rhs=cqn[:, k, gs_], start=(k == 0), stop=(k == 3)), r=['wuq', 'cqn'], w=[pk])
                P.op('dve', lambda e: e.tensor_tensor(out=t2[:], in0=pt[0:64, :], in1=sin2[:, gs_], op=ALU.mult), r=[pk, 'sin2'], w=['t2'])
                P.op('dve', lambda e: e.tensor_tensor(out=QrA[0:64, gs_], in0=t1[:], in1=t2[:], op=ALU.add), r=['t1', 't2'], w=[('QrA', g)])
                pt, pk = psA[2], 'psA2'
                for k in range(2):
                    P.op('pe', lambda e: e.matmul(pt[:], lhsT=wukv[:, k, 0:128], rhs=ckvn[:, k, gs_], start=(k == 0), stop=(k == 1)), r=['wukv', 'ckvn'], w=[pk])
                P.op('act', lambda e: e.copy(out=KnT[:, gs_], in_=pt[:]), r=[pk], w=[('KnT', g)])
                pt, pk = psA[3], 'psA3'
                for j in range(4):
                    t_ = g * 4 + j
                    for k in range(2):
                        P.op('pe', lambda e: e.matmul(pt[:, j * 128:(j + 1) * 128], lhsT=ckvn[:, k, t_ * 128:(t_ + 1) * 128], rhs=wukv[:, k, 128:256], start=(k == 0), stop=(k == 1)),
                             r=['wukv', 'ckvn'], w=[pk])
                P.op('dve', lambda e: e.tensor_copy(out=Vt[:, g * 4:(g + 1) * 4, :], in_=pt[:].rearrange("p (j n) -> p j n", j=4)), r=[pk], w=[('Vt', g)])
                pt, pk = psA[2], 'psA2'
                P.op('act', lambda e: e.activation(out=sq[:], in_=KnT[:, gs_], func=AF.Square), r=[('KnT', g)], w=['sq'])
                P.op('pe', lambda e: e.matmul(pt[0:65, :], lhsT=sel_b[:, :], rhs=sq[:], start=True, stop=False), r=['sq', 'sel_b'], w=[pk])
                P.op('act', lambda e: e.activation(out=sq[0:64, :], in_=krA[0:64, gs_], func=AF.Square), r=['krA'], w=['sq'])
                P.op('pe', lambda e: e.matmul(pt[0:65, :], lhsT=sel_b[0:64, :], rhs=sq[0:64, :], start=False, stop=True), r=['sq', 'sel_b'], w=[pk])
                P.op('dve', lambda e: e.tensor_reduce(out=kmx[64:65, g:g + 1], in_=pt[64:65, :], axis=AX.X, op=ALU.max), r=[pk], w=['kmx'])
            P.op('dve', lambda e: e.tensor_reduce(out=kmx[64:65, 8:9], in_=kmx[64:65, 0:8], axis=AX.X, op=ALU.max), r=['kmx'], w=['kmx'])
            for g in range(8):
                gs_ = slice(g * 512, (g + 1) * 512)
                pt, pk = psA[2], 'psA2'
                P.op('act', lambda e: e.activation(out=sq[:], in_=QnT[:, gs_], func=AF.Square), r=[('QnT', g)], w=['sq'])
                P.op('pe', lambda e: e.matmul(pt[0:65, :], lhsT=sel_b[:, :], rhs=sq[:], start=True, stop=False), r=['sq', 'sel_b'], w=[pk])
                P.op('act', lambda e: e.activation(out=sq[0:64, :], in_=QrA[0:64, gs_], func=AF.Square), r=[('QrA', g)], w=['sq'])
                P.op('pe', lambda e: e.matmul(pt[0:65, :], lhsT=sel_b[0:64, :], rhs=sq[0:64, :], start=False, stop=True), r=['sq', 'sel_b'], w=[pk])
                P.op('act', lambda e: e.activation(out=rowt[64:65, :], in_=pt[64:65, :], func=AF.Sqrt, scale=kmx[64:65, 8:9]), r=[pk, 'kmx'], w=['rowt'])
                P.op('dve', lambda e: e.tensor_scalar(out=QrA[64:65, gs_], in0=rowt[64:65, :], scalar1=-1.0, scalar2=None, op0=ALU.mult), r=['rowt'], w=[('QrA', g)])
            for g in range(8):
                gs_ = slice(g * 512, (g + 1) * 512)
                po, pd = psA[(g % 2) * 2], psA[(g % 2) * 2 + 1]
                kpo, kpd = f'psA{(g % 2) * 2}', f'psA{(g % 2) * 2 + 1}'

                def scores(kt):
                    ks_ = slice(kt * 128, (kt + 1) * 128)
                    sc_, sck = psS[kt % 2], f'psS{kt % 2}'
                    P.pe_group([lambda e: e.matmul(sc_[:], lhsT=KnT[:, ks_], rhs=QnT[:, gs_], start=True, stop=False),
                                lambda e: e.matmul(sc_[:], lhsT=krA[:, ks_], rhs=QrA[:, gs_], start=False, stop=True)],
                               r=[('KnT', kt // 4), ('QnT', g), 'krA', ('QrA', g)], w=[sck])
                scores(0)
                for kt in range(NT):
                    sc_, sck = psS[kt % 2], f'psS{kt % 2}'
                    pi = kt % 3
                    P.op('act', lambda e: e.activation(out=pT[pi][:], in_=sc_[:], func=AF.Exp, scale=SCL), r=[sck], w=[f'pT{pi}'])
                    if kt + 1 < NT:
                        scores(kt + 1)
                    P.pe_group([lambda e: e.matmul(po[:], lhsT=Vt[:, kt, :], rhs=pT[pi][:], start=(kt == 0), stop=(kt == NT - 1)),
                                lambda e: e.matmul(pd[:], lhsT=onesb[:], rhs=pT[pi][:], start=(kt == 0), stop=(kt == NT - 1))],
                               r=[('Vt', kt // 4), f'pT{pi}', 'onesb'], w=[kpo, kpd])
                P.op('dve', lambda e: e.reciprocal(out=rden[:], in_=pd[:]), r=[kpd], w=['rden'])
                P.op('dve', lambda e: e.tensor_tensor(out=oTs[:, gs_], in0=po[:], in1=rden[:], op=ALU.mult), r=[kpo, 'rden'], w=['oTs'])
            fin.append(P.op('sp', lambda e: e.dma_start(out=oT_mla[h, :, :], in_=oTs[:]), r=['oTs'], w=[('scr', 'oT_mla', h)], dma=True))
        P.barrier()
        esM.close()


    def phase_DN():
        def stop(k):
            if dn_stop == k:
                raise _Stop()
        esD = ExitStack()
        pM, pG, pX0, pX1, pZT, pU = psA[0], psA[1], psA[2], psA[3], psS[0], psS[1]
        kM, kG, kX0, kX1, kZT, kU = 'psA0', 'psA1', 'psA2', 'psA3', 'psS0', 'psS1'
        onesb = sbs(esD, "d_onesb", [128, 128], BF16)
        P.op('dve', lambda e: e.memset(onesb[:], 1.0), w=['d_onesb'])
        msk = sbs(esD, "d_msk", [128, 4, 128], F32)
        P.op('sp', lambda e: e.dma_start(out=msk[:], in_=dmasks[:, :, :]), w=['d_msk'], dma=True)
        gain = sbs(esD, "d_gain", [128, 128], F32)
        P.op('sp', lambda e: e.dma_start(out=gain[:], in_=dn_gain[:, :]), w=['d_gain'], dma=True)
        tokS = sbs(esD, "tokS", [128, NT, 48], F32)
        one1 = sbs(esD, "one1", [128, 1], F32)
        P.op('dve', lambda e: e.memset(one1[:], 1.0), w=['one1'])
        eps_l2 = sbs(esD, "eps_l2", [128, 1], F32)
        P.op('dve', lambda e: e.memset(eps_l2[:], EPS), w=['eps_l2'])
        es1 = ExitStack()
        sc = sbs(es1, "d_sc", [8, 4], F32)
        nA = sbs(es1, "d_nA", [8, 2], F32)
        P.op('sp', lambda e: e.dma_start(out=sc[:], in_=dn_sc[:, :]), w=['d_sc'], dma=True)
        P.op('act', lambda e: e.activation(out=nA[:], in_=sc[:, 0:2], func=AF.Exp), r=['d_sc'], w=['d_nA'])
        P.op('dve', lambda e: e.tensor_scalar(out=nA[:], in0=nA[:], scalar1=-1.0, scalar2=None, op0=ALU.mult), r=['d_nA'], w=['d_nA'])
        rows = {}
        ra = sbs(es1, "d_ra", [8, S], F32)
        rb = sbs(es1, "d_rb", [8, S], F32)
        rc = sbs(es1, "d_rc", [8, S], F32)
        for d in range(2):
            beta = sbs(es1, f"d_beta{d}", [8, S], F32)
            nbeta = sbs(es1, f"d_nbeta{d}", [8, S], F32)
            gc = sbs(es1, f"d_gc{d}", [8, S], F32)
            rows[d] = (beta, nbeta, gc)
            P.op('sp', lambda e: e.dma_start(out=ra[:], in_=baT[d * 8:(d + 1) * 8, :]), r=[('scr', 'dst_ba')], w=['d_ra'], dma=True)
            P.op('act', lambda e: e.activation(out=beta[:], in_=ra[:], func=AF.Sigmoid), r=['d_ra'], w=[f'd_beta{d}'])
            P.op('dve', lambda e: e.tensor_scalar(out=nbeta[:], in0=beta[:], scalar1=-1.0, scalar2=None, op0=ALU.mult), r=[f'd_beta{d}'], w=[f'd_nbeta{d}'])
            P.op('sp', lambda e: e.dma_start(out=ra[:], in_=baT[16 + d * 8:16 + (d + 1) * 8, :]), r=[('scr', 'dst_ba')], w=['d_ra'], dma=True)
            P.op('dve', lambda e: e.tensor_scalar(out=ra[:], in0=ra[:], scalar1=sc[:, 2 + d:3 + d], scalar2=None, op0=ALU.add), r=['d_ra', 'd_sc'], w=['d_ra'])
            P.op('act', lambda e: e.activation(out=rb[:], in_=ra[:], func=AF.Abs), r=['d_ra'], w=['d_rb'])
            P.op('act', lambda e: e.activation(out=rb[:], in_=rb[:], func=AF.Exp, scale=-1.0), r=['d_rb'], w=['d_rb'])
            P.op('act', lambda e: e.activation(out=rb[:], in_=rb[:], func=AF.Ln, bias=one1[0:8, :], scale=1.0), r=['d_rb', 'one1'], w=['d_rb'])
            P.op('dve', lambda e: e.scalar_tensor_tensor(out=rc[:], in0=ra[:], scalar=0.0, in1=rb[:], op0=ALU.max, op1=ALU.add), r=['d_ra', 'd_rb'], w=['d_rc'])
            P.op('dve', lambda e: e.tensor_scalar(out=rc[:], in0=rc[:], scalar1=nA[:, d:d + 1], scalar2=None, op0=ALU.mult), r=['d_rc', 'd_nA'], w=['d_rc'])
            cur, curk, nxt, nxtk = rc, 'd_rc', gc, f'd_gc{d}'
            for sft in (1, 2, 4, 8, 16, 32, 64):
                c3 = cur[:].rearrange("p (t n) -> p t n", n=128)
                n3 = nxt[:].rearrange("p (t n) -> p t n", n=128)
                P.op('act', lambda e: e.copy(out=nxt[:], in_=cur[:]), r=[curk], w=[nxtk])
                if d == 0:
                    P.op('dve', lambda e: e.tensor_tensor(out=n3[:, :, sft:], in0=c3[:, :, sft:], in1=c3[:, :, :128 - sft], op=ALU.add), r=[curk], w=[nxtk])
                else:
                    P.op('dve', lambda e: e.tensor_tensor(out=n3[:, :, :128 - sft], in0=c3[:, :, :128 - sft], in1=c3[:, :, sft:], op=ALU.add), r=[curk], w=[nxtk])
                cur, curk, nxt, nxtk = nxt, nxtk, cur, curk
            if cur is not gc:
                P.op('act', lambda e: e.copy(out=gc[:], in_=cur[:]), r=[curk], w=[f'd_gc{d}'])
        for t in range(NT):
            for d in range(2):
                for j in range(3):
                    src = rows[d][j]
                    col = d * 24 + j * 8
                    P.op('pe', lambda e: e.transpose(out=pG[:, col:col + 8], in_=src[:, t * 128:(t + 1) * 128], identity=ident[0:8, 0:8]),
                         r=[f'd_beta{d}', f'd_nbeta{d}', f'd_gc{d}', 'ident'], w=[kG])
            P.op('act', lambda e: e.copy(out=tokS[:, t, :], in_=pG[:, 0:48]), r=[kG], w=['tokS'])
        P.barrier()
        stop(1)
        es1.close()
        lm = sbs(esD, "d_lm", [128, 5, 4 * 128], F32)
        for j5 in range(5):
            P.op('sp', lambda e: e.dma_start(out=lm[:, j5, :], in_=dn_lmask[:, j5, :]), w=['d_lm'], dma=True)
        QKV = [sbs(esD, f"d_qkv{i}", [128, S], BF16) for i in range(3)]
        Ust = sbs(esD, "d_U", [128, NT, 2, 128], BF16)
        WTst = sbs(esD, "d_WT", [128, NT, 2, 128], BF16)
        ITst = sbs(esD, "d_IT", [128, NT, 2, 128], BF16)
        QDst = sbs(esD, "d_QD", [128, NT, 2, 128], BF16)
        KSst = sbs(esD, "d_KS", [128, NT, 2, 128], BF16)
        egl = sbs(esD, "d_egl", [128, NT, 2], F32)
        Oacc = sbs(esD, "d_Oacc", [128, NT, 128], F32)
        zsh = sbs(esD, "d_zsh", [128, NT, 128], BF16)
        oTd = sbs(esD, "d_oTd", [128, S], BF16)
        S32 = [sbs(esD, f"d_S32{d}", [128, 128], F32) for d in range(2)]
        Sbf = [sbs(esD, f"d_Sbf{d}", [128, 128], BF16) for d in range(2)]
        Vn = [sbs(esD, f"d_Vn{d}", [128, 128], BF16) for d in range(2)]
        ost = sbs(esD, "d_ost", [128, 8], F32)
        on = sbs(esD, "d_on", [128, 128], F32)
        onb = sbs(esD, "d_onb", [128, 128], BF16)
        G = 4
        QSC = float(128 ** -0.5)
        for h in dn_heads:
            esC = ExitStack()
            xpad = sbs(esC, f"d_xpad_{h}", [128, S + 4], F32)
            acc = sbs(esC, f"d_acc_{h}", [128, S], F32)
            cw = sbs(esC, f"d_cw_{h}", [128, 5], F32)
            rst = sbs(esC, f"d_rst_{h}", [128, 512], F32)
            sqb = sbs(esC, f"d_sqb_{h}", [128, 512], BF16)
            P.op('dve', lambda e: e.memset(xpad[:, 0:2], 0.0), w=['d_xpad'])
            P.op('dve', lambda e: e.memset(xpad[:, S + 2:S + 4], 0.0), w=['d_xpad'])
            for ci in range(3):
                ch = ci * 8 + h
                P.op('sp', lambda e: e.dma_start(out=cw[:], in_=conv_wT[ch * 128:(ch + 1) * 128, :]), w=['d_cw'], dma=True)
                P.op('pool', lambda e: e.dma_start(out=xpad[:, 2:S + 2], in_=qkvT[ch, :, :]), r=[('scr', 'dst_qkv')], w=['d_xpad'], dma=True)
                eng = 'dve'
                P.op(eng, lambda e: e.tensor_scalar(out=acc[:], in0=xpad[:, 0:S], scalar1=cw[:, 0:1], scalar2=None, op0=ALU.mult), r=['d_xpad', 'd_cw'], w=['d_acc'])
                for j in range(1, 5):
                    P.op(eng, lambda e: e.scalar_tensor_tensor(out=acc[:], in0=xpad[:, j:j + S], scalar=cw[:, j:j + 1], in1=acc[:], op0=ALU.mult, op1=ALU.add),
                         r=['d_xpad', 'd_cw', 'd_acc'], w=['d_acc'])
                P.op('act', lambda e: e.activation(out=acc[:], in_=acc[:], func=AF.Silu), r=['d_acc'], w=['d_acc'])
                if ci == 2:
                    P.op('dve', lambda e: e.tensor_copy(out=QKV[2][:], in_=acc[:]), r=['d_acc'], w=['d_qkv2'])
                else:
                    for g in range(8):
                        gs_ = slice(g * 512, (g + 1) * 512)
                        P.op('act', lambda e: e.activation(out=sqb[:], in_=acc[:, gs_], func=AF.Square), r=['d_acc'], w=['d_sqb'])
                        P.op('pe', lambda e: e.matmul(pM[:], lhsT=onesb[:], rhs=sqb[:], start=True, stop=True), r=['d_onesb', 'd_sqb'], w=[kM])
                        P.op('act', lambda e: e.activation(out=rst[:], in_=pM[:], func=AF.Sqrt, bias=eps_l2[:], scale=1.0), r=[kM, 'eps_l2'], w=['d_rst'])
                        P.op('dve', lambda e: e.reciprocal(out=rst[:], in_=rst[:]), r=['d_rst'], w=['d_rst'])
                        P.op('dve', lambda e: e.scalar_tensor_tensor(out=QKV[ci][:, gs_], in0=acc[:, gs_], scalar=(QSC if ci == 0 else 1.0), in1=rst[:], op0=ALU.mult, op1=ALU.mult),
                             r=['d_acc', 'd_rst'], w=[f'd_qkv{ci}'])
            Qt, Kt, Vch = QKV
            stop(2)
            P.barrier()
            esC.close()
            esW = ExitStack()
            Ktok = sbs(esW, f"d_Ktok_{h}", [128, 2, 128], BF16)
            Vtok = sbs(esW, f"d_Vtok_{h}", [128, 2, 128], BF16)
            Dg = sbs(esW, f"d_Dg_{h}", [128, G, 128], F32)
            tA = sbs(esW, f"d_tA_{h}", [128, G, 128], F32)
            tI = sbs(esW, f"d_tI_{h}", [128, G, 128], F32)
            EGB = sbs(esW, f"d_EGB_{h}", [128, G, 128], F32)
            A32 = sbs(esW, f"d_A32_{h}", [128, G, 128], F32)
            AT32 = sbs(esW, f"d_AT32_{h}", [128, G, 128], F32)
            ZY = sbs(esW, f"d_ZY_{h}", [128, G, 2, 128], F32)
            ZTYT = sbs(esW, f"d_ZTYT_{h}", [128, G, 2, 128], F32)
            Lb = sbs(esW, f"d_Lb_{h}", [128, G, 128], BF16)
            LTb = sbs(esW, f"d_LTb_{h}", [128, G, 128], BF16)
            Qb = sbs(esW, f"d_Qb_{h}", [128, G, 128], BF16)
            Rb = sbs(esW, f"d_Rb_{h}", [128, G, 128], BF16)
            TT = sbs(esW, f"d_TT_{h}", [128, G, 128], BF16)
            Tb = sbs(esW, f"d_Tb_{h}", [128, G, 128], BF16)
            ZYb = sbs(esW, f"d_ZYb_{h}", [128, G, 2, 128], BF16)
            ZTYTb = sbs(esW, f"d_ZTYTb_{h}", [128, G, 2, 128], BF16)
            Kbe = sbs(esW, f"d_Kbe_{h}", [128, G, 128], BF16)
            Vb = sbs(esW, f"d_Vb_{h}", [128, G, 128], BF16)
            egc = sbs(esW, f"d_egc_{h}", [128, G], F32)
            ebh = sbs(esW, f"d_ebh_{h}", [128, NT, 2], F32)
            for d_ in range(2):
                P.op('act', lambda e: e.activation(out=ebh[:, :, d_], in_=tokS[:, :, d_ * 24 + 16 + h], func=AF.Exp), r=['tokS'], w=['d_ebh'])
                P.op('dve', lambda e: e.tensor_tensor(out=ebh[:, :, d_], in0=ebh[:, :, d_], in1=tokS[:, :, d_ * 24 + h], op=ALU.mult), r=['d_ebh', 'tokS'], w=['d_ebh'])
            zs_v = zs[:, h * 128:(h + 1) * 128].rearrange("(t p) c -> p t c", p=128)
            for q4 in range(8):
                P.op('sp', lambda e: e.dma_start(out=zsh[:, q4 * 4:(q4 + 1) * 4, :], in_=zs_v[:, q4 * 4:(q4 + 1) * 4, :]),
                     r=[('scr', 'zs', t_, b_) for t_ in range(q4 * 4, q4 * 4 + 4) for b_ in range(2)], w=['d_zsh'], dma=True)
            for t0 in range(0, NT, 2):
                units = [(ti, d) for ti in range(2) for d in range(2)]
                for ti in range(2):
                    ts_ = slice((t0 + ti) * 128, (t0 + ti + 1) * 128)
                    P.op('pe', lambda e: e.transpose(out=psT[0][:, ti * 256:ti * 256 + 128], in_=Kt[:, ts_], identity=identb[:]), r=['d_qkv1', 'identb'], w=['psT0'])
                    P.op('pe', lambda e: e.transpose(out=psT[0][:, ti * 256 + 128:ti * 256 + 256], in_=Vch[:, ts_], identity=identb[:]), r=['d_qkv2', 'identb'], w=['psT0'])
                    P.op('pe', lambda e: e.matmul(pM[:, ti * 256:ti * 256 + 128], lhsT=Kt[:, ts_], rhs=Kt[:, ts_], start=True, stop=True), r=['d_qkv1'], w=[kM])
                    P.op('pe', lambda e: e.matmul(pM[:, ti * 256 + 128:ti * 256 + 256], lhsT=Kt[:, ts_], rhs=Qt[:, ts_], start=True, stop=True), r=['d_qkv1', 'd_qkv0'], w=[kM])
                pT4 = psT[0][:].rearrange("p (a b n) -> p a b n", a=2, b=2)
                P.op('act', lambda e: e.copy(out=Ktok[:], in_=pT4[:, :, 0, :]), r=['psT0'], w=['d_Ktok'])
                P.op('act', lambda e: e.copy(out=Vtok[:], in_=pT4[:, :, 1, :]), r=['psT0'], w=['d_Vtok'])
                stop(31)
                for u, (ti, d) in enumerate(units):
                    t = t0 + ti
                    gcol = tokS[:, t, d * 24 + 16 + h:d * 24 + 17 + h]
                    P.op('dve', lambda e: e.tensor_scalar(out=Dg[:, u, :], in0=ident[:], scalar1=gcol, scalar2=None, op0=ALU.mult), r=['ident', 'tokS'], w=[('d_Dg', u)])
                    P.op('pe', lambda e: e.matmul(pG[:, u * 128:(u + 1) * 128], lhsT=ones_f[:], rhs=Dg[:, u, :], start=True, stop=True), r=['ones_f', ('d_Dg', u)], w=[kG])
                stop(32)
                P.op('act', lambda e: e.copy(out=EGB[:], in_=pG[:].rearrange("p (u n) -> p u n", u=G)), r=[kG], w=['d_GBs'])
                for u, (ti, d) in enumerate(units):
                    t = t0 + ti
                    gcol = tokS[:, t, d * 24 + 16 + h:d * 24 + 17 + h]
                    P.op('dve', lambda e: e.scalar_tensor_tensor(out=tA[:, u, :], in0=EGB[:, u, :], scalar=gcol, in1=msk[:, 2 * d, :], op0=ALU.subtract, op1=ALU.max),
                         r=['d_GBs', 'tokS', 'd_msk'], w=[('d_tA', u)])
                    P.op('dve', lambda e: e.scalar_tensor_tensor(out=tI[:, u, :], in0=EGB[:, u, :], scalar=gcol, in1=msk[:, 2 * d + 1, :], op0=ALU.subtract, op1=ALU.min),
                         r=['d_GBs', 'tokS', 'd_msk'], w=[('d_tI', u)])
                kTA = [('d_tA', u) for u in range(G)]
                kTI = [('d_tI', u) for u in range(G)]
                P.op('act', lambda e: e.activation(out=tA[:], in_=tA[:], func=AF.Exp, scale=-1.0), r=kTA, w=kTA)
                P.op('act', lambda e: e.activation(out=tI[:], in_=tI[:], func=AF.Exp), r=kTI, w=kTI)
                P.op('act', lambda e: e.activation(out=EGB[:], in_=EGB[:], func=AF.Exp), r=['d_GBs'] + kTA + kTI, w=['d_GBs'])
                stop(33)
                for u, (ti, d) in enumerate(units):
                    t = t0 + ti
                    ts_ = slice(t * 128, (t + 1) * 128)
                    bcol = tokS[:, t, d * 24 + h:d * 24 + h + 1]
                    lastc = 127 if d == 0 else 0
                    P.op('dve', lambda e: e.scalar_tensor_tensor(out=A32[:, u, :], in0=pM[:, ti * 256:ti * 256 + 128], scalar=bcol, in1=tA[:, u, :], op0=ALU.mult, op1=ALU.mult),
                         r=[kM, 'tokS', ('d_tA', u)], w=[('d_A32', u)])
                    P.op('pe', lambda e: e.transpose(out=pX0[:, u * 128:(u + 1) * 128], in_=A32[:, u, :], identity=ident[:]), r=[('d_A32', u), 'ident'], w=[kX0])
                for u, (ti, d) in enumerate(units):
                    t = t0 + ti
                    ts_ = slice(t * 128, (t + 1) * 128)
                    bcol = tokS[:, t, d * 24 + h:d * 24 + h + 1]
                    lastc = 127 if d == 0 else 0
                    P.op('dve', lambda e: e.tensor_tensor(out=ITst[:, t, d, :], in0=pM[:, ti * 256 + 128:ti * 256 + 256], in1=tI[:, u, :], op=ALU.mult),
                         r=[kM, ('d_tI', u)], w=[('d_IT', t, d)])
                    P.op('dve', lambda e: e.tensor_tensor(out=QDst[:, t, d, :], in0=Qt[:, ts_], in1=EGB[:, u, :], op=ALU.mult), r=['d_qkv0', 'd_GBs'], w=[('d_QD', t, d)])
                    P.op('act', lambda e: e.copy(out=egl[:, t, d:d + 1], in_=EGB[:, u, lastc:lastc + 1]), r=['d_GBs'], w=[('d_egl', t, d)])
                    P.op('act', lambda e: e.activation(out=KSst[:, t, d, :], in_=Ktok[:, ti, :], func=AF.Copy, scale=tI[:, u, lastc:lastc + 1]),
                         r=['d_Ktok', ('d_tI', u)], w=[('d_KS', t, d)])
                    P.op('act', lambda e: e.activation(out=Kbe[:, u, :], in_=Ktok[:, ti, :], func=AF.Copy, scale=ebh[:, t, d:d + 1]),
                         r=['d_Ktok', 'd_ebh'], w=[('d_Kbe', u)])
                    P.op('act', lambda e: e.activation(out=Vb[:, u, :], in_=Vtok[:, ti, :], func=AF.Copy, scale=bcol), r=['d_Vtok', 'tokS'], w=[('d_Vb', u)])
                stop(34)
                kA = [('d_A32', u) for u in range(G)]
                v3 = lambda p_: p_[:].rearrange("p (u n) -> p u n", u=G)
                lmv = lambda j_: lm[:, j_, :].rearrange("p (u n) -> p u n", u=G)
                P.op('act', lambda e: e.copy(out=AT32[:], in_=v3(pX0)), r=[kX0], w=['d_AT32'])
                P.op('dve', lambda e: e.scalar_tensor_tensor(out=ZY[:, :, 0, :], in0=A32[:], scalar=-1.0, in1=lmv(0), op0=ALU.mult, op1=ALU.mult), r=kA + ['d_lm'], w=['d_ZY'])
                P.op('dve', lambda e: e.scalar_tensor_tensor(out=ZTYT[:, :, 0, :], in0=AT32[:], scalar=-1.0, in1=lmv(0), op0=ALU.mult, op1=ALU.mult), r=['d_AT32', 'd_lm'], w=['d_ZTYT'])
                P.op('pool', lambda e: e.tensor_tensor(out=ZY[:, :, 1, :], in0=ZY[:, :, 0, :], in1=lmv(4), op=ALU.add), r=['d_ZY', 'd_lm'], w=['d_ZY'])
                P.op('dve', lambda e: e.tensor_tensor(out=ZTYT[:, :, 1, :], in0=ZTYT[:, :, 0, :], in1=lmv(4), op=ALU.add), r=['d_ZTYT', 'd_lm'], w=['d_ZTYT'])
                stop(35)
                P.op('act', lambda e: e.copy(out=ZYb[:], in_=ZY[:]), r=['d_ZY'], w=['d_ZYb'])
                P.op('dve', lambda e: e.tensor_copy(out=ZTYTb[:], in_=ZTYT[:]), r=['d_ZTYT'], w=['d_ZTYTb'])
                for u in range(G):
                    us_ = slice(u * 128, (u + 1) * 128)
                    P.op('pe', lambda e: e.matmul(pX0[:, us_], lhsT=ZTYTb[:, u, 0, :], rhs=ZYb[:, u, 0, :], start=True, stop=True), r=['d_ZYb', 'd_ZTYTb'], w=[kX0])
                    P.op('pe', lambda e: e.matmul(pX1[:, us_], lhsT=ZYb[:, u, 0, :], rhs=ZTYTb[:, u, 0, :], start=True, stop=True), r=['d_ZYb', 'd_ZTYTb'], w=[kX1])
                P.op('act', lambda e: e.copy(out=ZYb[:, :, 0, :], in_=v3(pX0)), r=[kX0], w=['d_ZYb'])
                P.op('dve', lambda e: e.tensor_copy(out=ZTYTb[:, :, 0, :], in_=v3(pX1)), r=[kX1], w=['d_ZTYTb'])
                for lvl in (1, 2):
                    for u in range(G):
                        px, kx = (pX0, kX0) if u < 2 else (pX1, kX1)
                        pz, kz = (pZT, kZT) if u < 2 else (pU, kU)
                        cs_ = slice((u % 2) * 256, (u % 2) * 256 + 256)
                        P.op('pe', lambda e: e.matmul(px[:, cs_], lhsT=ZTYTb[:, u, 0, :], rhs=ZYb[:, u, :, :].rearrange("p c n -> p (c n)"), start=True, stop=True), r=['d_ZYb', 'd_ZTYTb'], w=[kx])
                        P.op('pe', lambda e: e.matmul(pz[:, cs_], lhsT=ZYb[:, u, 0, :], rhs=ZTYTb[:, u, :, :].rearrange("p c n -> p (c n)"), start=True, stop=True), r=['d_ZYb', 'd_ZTYTb'], w=[kz])
                    for hf, (px, kx, pz, kz) in enumerate(((pX0, kX0, pZT, kZT), (pX1, kX1, pU, kU))):
                        p4 = px[:].rearrange("p (u c n) -> p u c n", u=2, c=2)
                        z4 = pz[:].rearrange("p (u c n) -> p u c n", u=2, c=2)
                        hs2 = slice(hf * 2, hf * 2 + 2)
                        P.op('act', lambda e: e.copy(out=ZYb[:, hs2, 0, :], in_=p4[:, :, 0, :]), r=[kx], w=['d_ZYb'])
                        P.op('dve', lambda e: e.tensor_tensor(out=ZY[:, hs2, 1, :], in0=p4[:, :, 1, :], in1=ZY[:, hs2, 1, :], op=ALU.add), r=[kx, 'd_ZY'], w=['d_ZY'])
                        P.op('act', lambda e: e.copy(out=ZTYTb[:, hs2, 0, :], in_=z4[:, :, 0, :]), r=[kz], w=['d_ZTYTb'])
                        P.op('dve', lambda e: e.tensor_tensor(out=ZTYT[:, hs2, 1, :], in0=z4[:, :, 1, :], in1=ZTYT[:, hs2, 1, :], op=ALU.add), r=[kz, 'd_ZTYT'], w=['d_ZTYT'])
                    P.op('act', lambda e: e.copy(out=ZYb[:, :, 1, :], in_=ZY[:, :, 1, :]), r=['d_ZY'], w=['d_ZYb'])
                    P.op('dve', lambda e: e.tensor_copy(out=ZTYTb[:, :, 1, :], in_=ZTYT[:, :, 1, :]), r=['d_ZTYT'], w=['d_ZTYTb'])
                for u in range(G):
                    us_ = slice(u * 128, (u + 1) * 128)
                    P.op('pe', lambda e: e.matmul(pX0[:, us_], lhsT=ZTYTb[:, u, 0, :], rhs=ZYb[:, u, 1, :], start=True, stop=True), r=['d_ZYb', 'd_ZTYTb'], w=[kX0])
                    P.op('pe', lambda e: e.matmul(pX1[:, us_], lhsT=ZYb[:, u, 0, :], rhs=ZTYTb[:, u, 1, :], start=True, stop=True), r=['d_ZYb', 'd_ZTYTb'], w=[kX1])
                P.op('dve', lambda e: e.tensor_tensor(out=ZY[:, :, 1, :], in0=v3(pX0), in1=ZY[:, :, 1, :], op=ALU.add), r=[kX0, 'd_ZY'], w=['d_ZY'])
                P.op('dve', lambda e: e.tensor_tensor(out=ZTYT[:, :, 1, :], in0=v3(pX1), in1=ZTYT[:, :, 1, :], op=ALU.add), r=[kX1, 'd_ZTYT'], w=['d_ZTYT'])
                P.op('act', lambda e: e.copy(out=TT[:], in_=ZTYT[:, :, 1, :]), r=['d_ZTYT'], w=['d_TT'])
                P.op('dve', lambda e: e.tensor_copy(out=Tb[:], in_=ZY[:, :, 1, :]), r=['d_ZY'], w=['d_Tb'])
                for li in range(3):
                    last = (li == 2)
                    P.op('pool', lambda e: e.tensor_tensor(out=Lb[:], in0=A32[:], in1=lmv(1 + li), op=ALU.mult), r=kA + ['d_lm'], w=['d_Lb'])
                    if not last:
                        P.op('dve', lambda e: e.tensor_tensor(out=LTb[:], in0=AT32[:], in1=lmv(1 + li), op=ALU.mult), r=['d_AT32', 'd_lm'], w=['d_LTb'])
                    for u in range(G):
                        us_ = slice(u * 128, (u + 1) * 128)
                        P.op('pe', lambda e: e.matmul(pX1[:, us_], lhsT=Lb[:, u, :], rhs=TT[:, u, :], start=True, stop=True), r=['d_Lb', 'd_TT'], w=[kX1])
                        if not last:
                            P.op('pe', lambda e: e.matmul(pX0[:, us_], lhsT=LTb[:, u, :], rhs=Tb[:, u, :], start=True, stop=True), r=['d_LTb', 'd_Tb'], w=[kX0])
                    P.op('act', lambda e: e.copy(out=Rb[:], in_=v3(pX1)), r=[kX1], w=['d_Rb'])
                    if not last:
                        P.op('dve', lambda e: e.tensor_copy(out=Qb[:], in_=v3(pX0)), r=[kX0], w=['d_Qb'])
                    for u in range(G):
                        us_ = slice(u * 128, (u + 1) * 128)
                        P.op('pe', lambda e: e.matmul(pU[:, us_], lhsT=Tb[:, u, :], rhs=Rb[:, u, :], start=True, stop=True), r=['d_Tb', 'd_Rb'], w=[kU])
                        if not last:
                            P.op('pe', lambda e: e.matmul(pZT[:, us_], lhsT=TT[:, u, :], rhs=Qb[:, u, :], start=True, stop=True), r=['d_TT', 'd_Qb'], w=[kZT])
                    if not last:
                        P.op('dve', lambda e: e.tensor_tensor(out=ZTYT[:, :, 1, :], in0=ZTYT[:, :, 1, :], in1=v3(pU), op=ALU.subtract), r=[kU, 'd_ZTYT'], w=['d_ZTYT'])
                        P.op('dve', lambda e: e.tensor_tensor(out=ZY[:, :, 1, :], in0=ZY[:, :, 1, :], in1=v3(pZT), op=ALU.subtract), r=[kZT, 'd_ZY'], w=['d_ZY'])
                        P.op('act', lambda e: e.copy(out=TT[:], in_=ZTYT[:, :, 1, :]), r=['d_ZTYT'], w=['d_TT'])
                        P.op('act', lambda e: e.copy(out=Tb[:], in_=ZY[:, :, 1, :]), r=['d_ZY'], w=['d_Tb'])
                    else:
                        P.op('dve', lambda e: e.tensor_tensor(out=TT[:], in0=ZTYT[:, :, 1, :], in1=v3(pU), op=ALU.subtract), r=[kU, 'd_ZTYT'], w=['d_TT'])
                stop(36)
                for u, (ti, d) in enumerate(units):
                    P.op('pe', lambda e: e.matmul(pU[:, u * 128:(u + 1) * 128], lhsT=TT[:, u, :], rhs=Vb[:, u, :], start=True, stop=True), r=['d_TT', ('d_Vb', u)], w=[kU])
                    P.op('pe', lambda e: e.matmul(pG[:, u * 128:(u + 1) * 128], lhsT=Kbe[:, u, :], rhs=TT[:, u, :], start=True, stop=True), r=['d_TT', ('d_Kbe', u)], w=[kG])
                P.op('act', lambda e: e.copy(out=Ust[:, t0:t0 + 2, :, :].rearrange("p a b n -> p (a b) n"), in_=pU[:].rearrange("p (u n) -> p u n", u=G)), r=[kU], w=[('d_U', t0)])
                P.op('dve', lambda e: e.tensor_copy(out=WTst[:, t0:t0 + 2, :, :].rearrange("p a b n -> p (a b) n"), in_=pG[:].rearrange("p (u n) -> p u n", u=G)), r=[kG], w=[('d_WT', t0)])
                stop(3)
            P.barrier()
            stop(4)
            for d in range(2):
                P.op('dve', lambda e: e.memset(S32[d][:], 0.0), w=[f'd_S32{d}'])
                P.op('dve', lambda e: e.memset(Sbf[d][:], 0.0), w=[f'd_Sbf{d}'])
            for step in range(NT):
                tt = [step, NT - 1 - step]
                bank = [((pM, kM), (pX0, kX0), (pZT, kZT)), ((pG, kG), (pX1, kX1), (pU, kU))]
                for d in range(2):
                    t = tt[d]; t0 = (t // 2) * 2
                    (pW, kW) = bank[d][0]
                    P.op('pe', lambda e: e.matmul(pW[:, 0:128], lhsT=WTst[:, t, d, :], rhs=Sbf[d][:], start=True, stop=True), r=[('d_WT', t0), f'd_Sbf{d}'], w=[kW])
                for d in range(2):
                    t = tt[d]; t0 = (t // 2) * 2
                    (pW, kW), (pO, kO) = bank[d][0], bank[d][1]
                    P.op('dve', lambda e: e.tensor_tensor(out=Vn[d][:], in0=Ust[:, t, d, :], in1=pW[:, 0:128], op=ALU.subtract), r=[('d_U', t0), kW], w=[f'd_Vn{d}'])
                    P.op('pe', lambda e: e.matmul(pO[:, 0:128], lhsT=QDst[:, t, d, :], rhs=Sbf[d][:], start=True, stop=False), r=[('d_QD', t, d), f'd_Sbf{d}'], w=[kO])
                for d in range(2):
                    t = tt[d]
                    (pO, kO), (pD, kD) = bank[d][1], bank[d][2]
                    P.op('pe', lambda e: e.matmul(pO[:, 0:128], lhsT=ITst[:, t, d, :], rhs=Vn[d][:], start=False, stop=True), r=[('d_IT', t, d), f'd_Vn{d}'], w=[kO])
                    P.op('pe', lambda e: e.matmul(pD[:, 0:128], lhsT=KSst[:, t, d, :], rhs=Vn[d][:], start=True, stop=True), r=[('d_KS', t, d), f'd_Vn{d}'], w=[kD])
                for d in range(2):
                    t = tt[d]
                    (pO, kO), (pD, kD) = bank[d][1], bank[d][2]
                    P.op('dve', lambda e: e.scalar_tensor_tensor(out=Sbf[d][:], in0=S32[d][:], scalar=egl[:, t, d:d + 1], in1=pD[:, 0:128], op0=ALU.mult, op1=ALU.add),
                         r=[f'd_S32{d}', ('d_egl', t, d), kD], w=[f'd_Sbf{d}'])
                    P.op('dve', lambda e: e.scalar_tensor_tensor(out=S32[d][:], in0=S32[d][:], scalar=egl[:, t, d:d + 1], in1=pD[:, 0:128], op0=ALU.mult, op1=ALU.add),
                         r=[f'd_S32{d}', ('d_egl', t, d), kD], w=[f'd_S32{d}'])
                    if step < NT // 2:
                        P.op('act', lambda e: e.copy(out=Oacc[:, t, :], in_=pO[:, 0:128]), r=[kO], w=[('d_Oacc', t)])
                    else:
                        P.op('pool', lambda e: e.tensor_tensor(out=Oacc[:, t, :], in0=Oacc[:, t, :], in1=Oacc[:, t, :], op=ALU.add), r=[('d_Oacc', t)], w=[('d_Oacc', t)]) if False else \
                            P.op('dve', lambda e: e.tensor_tensor(out=Oacc[:, t, :], in0=pO[:, 0:128], in1=Oacc[:, t, :], op=ALU.add), r=[kO, ('d_Oacc', t)], w=[('d_Oacc', t)])
            stop(5)
            osq = [sbs(esW, f"d_osq{i_}_{h}", [128, 128], F32) for i_ in range(2)]
            ors = sbs(esW, f"d_ors_{h}", [128, 2, NT], F32)
            ont = [sbs(esW, f"d_ont{i_}_{h}", [128, 128], F32) for i_ in range(4)]
            onbt = [sbs(esW, f"d_onbt{i_}_{h}", [128, 128], BF16) for i_ in range(4)]
            kO_all = [('d_Oacc', t_) for t_ in range(NT)]
            P.op('dve', lambda e: e.memset(ors[:], 0.0), w=['d_ors'])
            for t in range(NT):
                P.op('act', lambda e: e.activation(out=osq[t % 2][:], in_=Oacc[:, t, :], func=AF.Square, accum_out=ors[:, 0, t:t + 1]), r=[('d_Oacc', t), 'd_ors'], w=[f'd_osq{t % 2}', ('d_ors_acc', t)])
            P.op('act', lambda e: e.activation(out=ors[:, 1, :], in_=ors[:, 0, :], func=AF.Sqrt, scale=1.0 / 128, bias=eps_l2[:]), r=['d_ors', 'eps_l2'] + [('d_ors_acc', t_) for t_ in range(NT)], w=['d_ors'])
            P.op('dve', lambda e: e.reciprocal(out=ors[:, 1, :], in_=ors[:, 1, :]), r=['d_ors'], w=['d_ors'])
            for t4 in range(0, NT, 4):
                for j in range(4):
                    t = t4 + j
                    P.op('dve', lambda e: e.scalar_tensor_tensor(out=ont[j][:], in0=Oacc[:, t, :], scalar=ors[:, 1, t:t + 1], in1=gain[:], op0=ALU.mult, op1=ALU.mult),
                         r=[('d_Oacc', t), 'd_ors', 'd_gain'], w=[f'd_ont{j}'])
                    P.op('pool', lambda e: e.tensor_tensor(out=onbt[j][:], in0=ont[j][:], in1=zsh[:, t, :], op=ALU.mult), r=[f'd_ont{j}', 'd_zsh'], w=[f'd_onbt{j}'])
                    P.op('pe', lambda e: e.transpose(out=psT[1][:, j * 128:(j + 1) * 128], in_=onbt[j][:], identity=identb[:]), r=[f'd_onbt{j}', 'identb'], w=['psT1'])
                P.op('act', lambda e: e.copy(out=oTd[:, t4 * 128:(t4 + 4) * 128], in_=psT[1][:]), r=['psT1'], w=['d_oTd'])
            fin.append(P.op('sp', lambda e: e.dma_start(out=oT_dn[h, :, :], in_=oTd[:]), r=['d_oTd'], w=[('scr', 'oT_dn', h)], dma=True))
            P.barrier()
            esW.close()
        P.barrier()
        esD.close()

    def phase_MG_MOE():
        esP = ExitStack()
        affTM = sbs(esP, "affTM", [128, NT, 16], F32)
        posm = sbs(esP, "posm", [128, NT, 16], F32)
        iot = sbs(esP, "iot", [128, 512], F32)
        bcs = sbs(esP, "bcs", [128, 4, D], F32)
        gfin = sbs(esP, "gfin", [128, D], F32)
        onesb = sbs(esP, "m_onesb", [128, 128], BF16)
        trib = sbs(esP, "trib", [128, 128], BF16)
        st2 = sbs(esP, "st2", [128, 8], F32)
        P.op('dve', lambda e: e.memset(onesb[:], 1.0), w=['m_onesb'])
        P.op('sp', lambda e: e.dma_start(out=iot[:], in_=iota512[:, :]), w=['iot'], dma=True)
        P.op('sp', lambda e: e.dma_start(out=gfin[:], in_=g_final[:, :]), w=['gfin'], dma=True)
        esH = ExitStack()
        h2b = sbs(esH, "h2b", [128, NT, D], BF16)
        esT = ExitStack()
        affT = sbs(esT, "affT", [16, S], F32)
        esG = ExitStack()
        es1 = ExitStack()
        mrow = sbs(es1, "g_mrow", [1, 4 * D], F32)
        trif = sbs(es1, "g_trif", [128, 128], F32)
        gff = sbs(es1, "g_gff", [128, D], F32)
        P.op('sp', lambda e: e.dma_start(out=mrow[:], in_=modrow[:, 2 * D:6 * D]), r=['mod'], w=['g_mrow'], dma=True)
        P.op('sp', lambda e: e.dma_start(out=trif[:], in_=tri_in[:, :]), w=['g_trif'], dma=True)
        P.op('dve', lambda e: e.tensor_copy(out=trib[:], in_=trif[:]), r=['g_trif'], w=['trib'])
        P.op('sp', lambda e: e.dma_start(out=gff[:], in_=g_ffn[:, :]), w=['g_gff'], dma=True)
        for j in range(8):
            src = j // 2
            dsti = {0: 0, 1: 2, 2: 1, 3: 3}[src]
            pt, pk = next_psA()
            P.op('pe', lambda e: e.matmul(pt[:], lhsT=ones_f[0:1, :], rhs=mrow[:, j * 512:(j + 1) * 512], start=True, stop=True), r=['ones_f', 'g_mrow'], w=[pk])
            P.op('act', lambda e: e.copy(out=bcs[:, dsti, (j % 2) * 512:(j % 2 + 1) * 512], in_=pt[:]), r=[pk], w=['bcs'])
        P.op('dve', lambda e: e.scalar_tensor_tensor(out=bcs[:, 1, :], in0=bcs[:, 1, :], scalar=1.0, in1=gff[:], op0=ALU.add, op1=ALU.mult), r=['bcs', 'g_gff'], w=['bcs'])
        P.barrier()
        es1.close()
        wod = sbs(esG, "g_wod", [128, 8, D], BF16)
        wom = sbs(esG, "g_wom", [128, 8, D], BF16)
        wou = sbs(esG, "g_wou", [128, 8, D], BF16)
        wr = sbs(esG, "g_wr", [128, 8, 16], F32)
        for wt_, src_, k_ in ((wod, w_o_dn, 'g_wod'), (wom, w_o_mla, 'g_wom'), (wou, w_out, 'g_wou')):
            v = src_.rearrange("(k p) n -> p k n", p=128)
            for kk in range(0, 8, 2):
                P.op('pool', lambda e: e.dma_start(out=wt_[:, kk:kk + 2, :], in_=v[:, kk:kk + 2, :]), w=[k_], dma=True)
        P.op('sp', lambda e: e.dma_start(out=wr[:], in_=w_router.rearrange("(k p) n -> p k n", p=128)), w=['g_wr'], dma=True)
        odn = [sbs(esG, f"g_odn{i}", [128, 8, 128], BF16) for i in range(2)]
        oml = [sbs(esG, f"g_oml{i}", [128, 8, 128], BF16) for i in range(2)]
        gst = [sbs(esG, f"g_gst{i}", [128, 2 * D], BF16) for i in range(2)]
        xt0 = sbs(esG, "g_xt0", [128, D], F32)
        xt = [xt0, xt0]
        m1 = sbs(esG, "g_m1", [128, D], F32)
        m2 = sbs(esG, "g_m2", [128, D], F32)
        mb = sbs(esG, "g_mb", [128, D], BF16)
        mT = sbs(esG, "g_mT", [128, 8, 128], BF16)
        x1t = [sbs(esG, f"g_x1t{i}", [128, D], F32) for i in range(2)]
        h2f = sbs(esG, "g_h2f", [128, D], F32)
        h2T = sbs(esG, "g_h2T", [128, 8, 128], F32)
        lg = sbs(esG, "g_lg", [128, 16], F32)
        for t in range(NT):
            i = t % 2
            ts_ = slice(t * 128, (t + 1) * 128)
            P.op('sp', lambda e: e.dma_start(out=odn[i][:], in_=oT_dn[:, :, ts_].rearrange("h p n -> p h n")), r=[('scr', 'oT_dn', h_) for h_ in range(8)], w=[f'g_odn{i}'], dma=True)
            P.op('sp', lambda e: e.dma_start(out=oml[i][:], in_=oT_mla[:, :, ts_].rearrange("h p n -> p h n")), r=[('scr', 'oT_mla', h_) for h_ in range(8)], w=[f'g_oml{i}'], dma=True)
            P.op('sp', lambda e: e.dma_start(out=gst[i][:], in_=gs[ts_, :]), r=[('scr', 'gs', t, b_) for b_ in range(4)], w=[f'g_gst{i}'], dma=True)
            P.op('sp', lambda e: e.dma_start(out=xt[i][:], in_=x[ts_, :]), w=['g_xt0'], dma=True)
            for br, (o_, ok_, w_, wk_) in enumerate(((odn[i], f'g_odn{i}', wod, 'g_wod'), (oml[i], f'g_oml{i}', wom, 'g_wom'))):
                for hc in range(2):
                    pt, pk = psA[br * 2 + hc], f'psA{br * 2 + hc}'
                    for k in range(8):
                        P.op('pe', lambda e: e.matmul(pt[:], lhsT=o_[:, k, :], rhs=w_[:, k, hc * 512:(hc + 1) * 512], start=(k == 0), stop=(k == 7)), r=[ok_, wk_], w=[pk])
            for hc in range(2):
                hs_ = slice(hc * 512, (hc + 1) * 512)
                P.op('dve', lambda e: e.tensor_tensor(out=m1[:, hs_], in0=psA[hc][:], in1=gst[i][:, hs_], op=ALU.mult), r=[f'psA{hc}', f'g_gst{i}'], w=['g_m1'])
                P.op('dve', lambda e: e.tensor_tensor(out=m2[:, hs_], in0=psA[2 + hc][:], in1=gst[i][:, D + hc * 512:D + (hc + 1) * 512], op=ALU.mult), r=[f'psA{2 + hc}', f'g_gst{i}'], w=['g_m2'])
            P.op('pool', lambda e: e.tensor_tensor(out=mb[:], in0=m1[:], in1=m2[:], op=ALU.add), r=['g_m1', 'g_m2'], w=['g_mb'])
            for half in range(2):
                for k4 in range(4):
                    k = half * 4 + k4
                    P.op('pe', lambda e: e.transpose(out=psT[half][:, k4 * 128:(k4 + 1) * 128], in_=mb[:, k * 128:(k + 1) * 128], identity=identb[:]), r=['g_mb', 'identb'], w=[f'psT{half}'])
                P.op('act', lambda e: e.copy(out=mT[:, half * 4:(half + 1) * 4, :], in_=psT[half][:].rearrange("p (k n) -> p k n", k=4)), r=[f'psT{half}'], w=['g_mT'])
            for hc in range(2):
                hs_ = slice(hc * 512, (hc + 1) * 512)
                for k in range(8):
                    P.op('pe', lambda e: e.matmul(psS[hc][:], lhsT=mT[:, k, :], rhs=wou[:, k, hs_], start=(k == 0), stop=(k == 7)), r=['g_mT', 'g_wou'], w=[f'psS{hc}'])
                P.op('dve', lambda e: e.tensor_tensor(out=x1t[i][:, hs_], in0=psS[hc][:], in1=bcs[:, 0, hs_], op=ALU.mult), r=[f'psS{hc}', 'bcs'], w=[f'g_x1t{i}'])
            P.op('pool', lambda e: e.tensor_tensor(out=x1t[i][:], in0=x1t[i][:], in1=xt[i][:], op=ALU.add), r=[f'g_x1t{i}', 'g_xt0'], w=[f'g_x1t{i}'])
            fin.append(P.op('sp', lambda e: e.dma_start(out=x1s[ts_, :], in_=x1t[i][:]), r=[f'g_x1t{i}'], w=[('scr', 'x1s', t)], dma=True))
            P.op('dve', lambda e: e.memset(st2[:], 0.0), w=['st2'])
            P.op('act', lambda e: e.activation(out=m1[:], in_=x1t[i][:], func=AF.Square, accum_out=st2[:, 0:1]), r=[f'g_x1t{i}'], w=['g_m1', 'st2'])
            P.op('act', lambda e: e.activation(out=st2[:, 1:2], in_=st2[:, 0:1], func=AF.Sqrt, scale=1.0 / D, bias=epsb[:]), r=['st2', 'epsb'], w=['st2'])
            P.op('dve', lambda e: e.reciprocal(out=st2[:, 2:3], in_=st2[:, 1:2]), r=['st2'], w=['st2'])
            P.op('dve', lambda e: e.scalar_tensor_tensor(out=h2f[:], in0=x1t[i][:], scalar=st2[:, 2:3], in1=bcs[:, 1, :], op0=ALU.mult, op1=ALU.mult), r=[f'g_x1t{i}', 'st2', 'bcs'], w=['g_h2f'])
            P.op('pool', lambda e: e.tensor_tensor(out=h2f[:], in0=h2f[:], in1=bcs[:, 2, :], op=ALU.add), r=['g_h2f', 'bcs'], w=['g_h2f'])
            P.op('act', lambda e: e.copy(out=h2b[:, t, :], in_=h2f[:]), r=['g_h2f'], w=[('h2b', t)])
            for half in range(2):
                for k4 in range(4):
                    k = half * 4 + k4
                    P.op('pe', lambda e: e.transpose(out=psA[half][:, k4 * 128:(k4 + 1) * 128], in_=h2f[:, k * 128:(k + 1) * 128], identity=ident[:]), r=['g_h2f', 'ident'], w=[f'psA{half}'])
                P.op('act', lambda e: e.copy(out=h2T[:, half * 4:(half + 1) * 4, :], in_=psA[half][:].rearrange("p (k n) -> p k n", k=4)), r=[f'psA{half}'], w=['g_h2T'])
            for k in range(8):
                P.op('pe', lambda e: e.matmul(psA[2][:, 0:16], lhsT=h2T[:, k, :], rhs=wr[:, k, :], start=(k == 0), stop=(k == 7)), r=['g_h2T', 'g_wr'], w=['psA2'])
            P.op('dve', lambda e: e.tensor_reduce(out=st2[:, 3:4], in_=psA[2][:, 0:16], axis=AX.X, op=ALU.max), r=['psA2'], w=['st2'])
            P.op('dve', lambda e: e.tensor_scalar(out=st2[:, 4:5], in0=st2[:, 3:4], scalar1=-1.0, scalar2=None, op0=ALU.mult), r=['st2'], w=['st2'])
            P.op('dve', lambda e: e.memset(st2[:, 5:6], 0.0), r=['st2'], w=['st2'])
            P.op('act', lambda e: e.activation(out=lg[:], in_=psA[2][:, 0:16], func=AF.Exp, bias=st2[:, 4:5], scale=1.0, accum_out=st2[:, 5:6]), r=['psA2', 'st2'], w=['g_lg', 'st2'])
            P.op('dve', lambda e: e.reciprocal(out=st2[:, 6:7], in_=st2[:, 5:6]), r=['st2'], w=['st2'])
            P.op('dve', lambda e: e.tensor_scalar(out=affTM[:, t, :], in0=lg[:], scalar1=st2[:, 6:7], scalar2=None, op0=ALU.mult), r=['g_lg', 'st2'], w=[('affTM', t)])
            P.op('pe', lambda e: e.transpose(out=psA[3][0:16, 0:128], in_=affTM[:, t, :], identity=ident[:]), r=[('affTM', t), 'ident'], w=['psA3'])
            P.op('act', lambda e: e.copy(out=affT[:, ts_], in_=psA[3][0:16, 0:128]), r=['psA3'], w=['affT'])
        P.barrier()
        esG.close()
        esE = ExitStack()
        es1 = ExitStack()
        cmpb = sbs(es1, "e_cmp", [16, S], F32)
        bis = sbs(es1, "e_bis", [16, 8], F32)
        maskT = sbs(es1, "e_maskT", [16, S], F32)
        P.op('dve', lambda e: e.memset(bis[:], 0.0), w=['e_bis'])
        P.op('dve', lambda e: e.memset(bis[:, 1:2], 1.0), r=['e_bis'], w=['e_bis'])
        for it in range(32):
            P.op('dve', lambda e: e.tensor_tensor(out=bis[:, 2:3], in0=bis[:, 0:1], in1=bis[:, 1:2], op=ALU.add), r=['e_bis'], w=['e_bis'])
            P.op('dve', lambda e: e.tensor_scalar(out=bis[:, 2:3], in0=bis[:, 2:3], scalar1=0.5, scalar2=None, op0=ALU.mult), r=['e_bis'], w=['e_bis'])
            P.op('dve', lambda e: e.tensor_scalar(out=cmpb[:], in0=affT[:], scalar1=bis[:, 2:3], scalar2=None, op0=ALU.is_ge), r=['affT', 'e_bis'], w=['e_cmp'])
            P.op('dve', lambda e: e.tensor_reduce(out=bis[:, 3:4], in_=cmpb[:], axis=AX.X, op=ALU.add), r=['e_cmp'], w=['e_bis'])
            P.op('dve', lambda e: e.tensor_scalar(out=bis[:, 4:5], in0=bis[:, 3:4], scalar1=511.5, scalar2=None, op0=ALU.is_ge), r=['e_bis'], w=['e_bis'])
            P.op('dve', lambda e: e.tensor_tensor(out=bis[:, 5:6], in0=bis[:, 2:3], in1=bis[:, 0:1], op=ALU.subtract), r=['e_bis'], w=['e_bis'])
            P.op('dve', lambda e: e.tensor_tensor(out=bis[:, 6:7], in0=bis[:, 1:2], in1=bis[:, 2:3], op=ALU.subtract), r=['e_bis'], w=['e_bis'])
            P.op('dve', lambda e: e.scalar_tensor_tensor(out=bis[:, 0:1], in0=bis[:, 5:6], scalar=bis[:, 4:5], in1=bis[:, 0:1], op0=ALU.mult, op1=ALU.add), r=['e_bis'], w=['e_bis'])
            P.op('dve', lambda e: e.scalar_tensor_tensor(out=bis[:, 1:2], in0=bis[:, 6:7], scalar=bis[:, 4:5], in1=bis[:, 2:3], op0=ALU.mult, op1=ALU.add), r=['e_bis'], w=['e_bis'])
        P.op('dve', lambda e: e.tensor_scalar(out=maskT[:], in0=affT[:], scalar1=bis[:, 0:1], scalar2=None, op0=ALU.is_ge), r=['affT', 'e_bis'], w=['e_maskT'])
        mk32 = sbs(es1, "e_mk32", [128, 16], F32)
        mkb = sbs(es1, "e_mkb", [128, 16], BF16)
        base = sbs(es1, "e_base", [128, 16], F32)
        ptmp = sbs(es1, "e_ptmp", [128, 16], F32)
        P.op('dve', lambda e: e.memset(base[:], 0.0), w=['e_base'])
        for t in range(NT):
            ts_ = slice(t * 128, (t + 1) * 128)
            P.op('pe', lambda e: e.transpose(out=psA[0][:, 0:16], in_=maskT[:, ts_], identity=ident[0:16, 0:16]), r=['e_maskT', 'ident'], w=['psA0'])
            P.op('act', lambda e: e.copy(out=mk32[:], in_=psA[0][:, 0:16]), r=['psA0'], w=['e_mk32'])
            P.op('dve', lambda e: e.tensor_copy(out=mkb[:], in_=mk32[:]), r=['e_mk32'], w=['e_mkb'])
            P.op('pe', lambda e: e.matmul(psA[1][:, 0:16], lhsT=trib[:], rhs=mkb[:], start=True, stop=True), r=['trib', 'e_mkb'], w=['psA1'])
            P.op('pe', lambda e: e.matmul(psA[2][:, 0:16], lhsT=onesb[:], rhs=mkb[:], start=True, stop=True), r=['m_onesb', 'e_mkb'], w=['psA2'])
            P.op('dve', lambda e: e.tensor_tensor(out=ptmp[:], in0=psA[1][:, 0:16], in1=base[:], op=ALU.add), r=['psA1', 'e_base'], w=['e_ptmp'])
            P.op('dve', lambda e: e.scalar_tensor_tensor(out=ptmp[:], in0=ptmp[:], scalar=1.0, in1=mk32[:], op0=ALU.add, op1=ALU.mult), r=['e_ptmp', 'e_mk32'], w=['e_ptmp'])
            P.op('dve', lambda e: e.tensor_scalar(out=posm[:, t, :], in0=ptmp[:], scalar1=-1.0, scalar2=None, op0=ALU.add), r=['e_ptmp'], w=['posm'])
            P.op('dve', lambda e: e.tensor_tensor(out=base[:], in0=psA[2][:, 0:16], in1=base[:], op=ALU.add), r=['psA2', 'e_base'], w=['e_base'])
        P.barrier()
        es1.close()
        esT.close()
        wg = sbs(esE, "e_wg", [128, 8, D], BF16)
        wu = sbs(esE, "e_wu", [128, 8, D], BF16)
        wd = sbs(esE, "e_wd", [128, 8, D], BF16)
        Sel = sbs(esE, "e_Sel", [128, NT, 512], BF16)
        xeT = sbs(esE, "e_xeT", [128, 8, 512], BF16)
        hid = sbs(esE, "e_hid", [128, 8, 512], BF16)
        sg0 = sbs(esE, "e_sg0", [128, 512], F32)
        sg = [sg0, sg0]
        yeb = sbs(esE, "e_yeb", [128, 4, D], BF16)
        stgw = [sbs(esE, f"e_stg{i}", [128, D], F32) for i in range(2)]
        stg_n = [0]
        for ex in range(16):
            for wt_, src_, k_ in ((wg, w_gate, 'e_wg'), (wu, w_up, 'e_wu'), (wd, w_down, 'e_wd')):
                v = src_[ex].rearrange("(k p) n -> p k n", p=128)
                for kk in range(8):
                    if kk % 2 == 0:
                        P.op('pool', lambda e: e.dma_start(out=wt_[:, kk, :], in_=v[:, kk, :]), w=[k_], dma=True)
                    else:
                        j = stg_n[0] % 2
                        stg_n[0] += 1
                        P.op('sp', lambda e: e.dma_start(out=stgw[j][:], in_=v[:, kk, :]), w=[f'e_stg{j}'], dma=True)
                        P.op('act', lambda e: e.copy(out=wt_[:, kk, :], in_=stgw[j][:]), r=[f'e_stg{j}'], w=[k_])
            for t in range(NT):
                P.op('dve', lambda e: e.tensor_scalar(out=Sel[:, t, :], in0=iot[:], scalar1=posm[:, t, ex:ex + 1], scalar2=None, op0=ALU.is_equal), r=['iot', 'posm'], w=[('e_Sel', t)])
            for k in range(8):
                pt, pk = psA[k % 4], f'psA{k % 4}'
                P.pe_group([(lambda e, t=t: e.matmul(pt[:], lhsT=h2b[:, t, k * 128:(k + 1) * 128], rhs=Sel[:, t, :], start=(t == 0), stop=(t == NT - 1))) for t in range(NT)],
                           r=[('h2b', t) for t in range(NT)] + [('e_Sel', t) for t in range(NT)], w=[pk])
                if k % 2 == 0:
                    P.op('act', lambda e: e.copy(out=xeT[:, k, :], in_=pt[:]), r=[pk], w=['e_xeT'])
                else:
                    P.op('dve', lambda e: e.tensor_copy(out=xeT[:, k, :], in_=pt[:]), r=[pk], w=['e_xeT'])
            for f in range(8):
                j = f % 2
                pg, pgk = psA[j * 2], f'psA{j * 2}'
                pu, puk = psA[j * 2 + 1], f'psA{j * 2 + 1}'
                P.pe_group([(lambda e, k=k: e.matmul(pg[:], lhsT=wg[:, k, f * 128:(f + 1) * 128], rhs=xeT[:, k, :], start=(k == 0), stop=(k == 7))) for k in range(8)], r=['e_wg', 'e_xeT'], w=[pgk])
                P.pe_group([(lambda e, k=k: e.matmul(pu[:], lhsT=wu[:, k, f * 128:(f + 1) * 128], rhs=xeT[:, k, :], start=(k == 0), stop=(k == 7))) for k in range(8)], r=['e_wu', 'e_xeT'], w=[puk])
                P.op('act', lambda e: e.activation(out=sg[j][:], in_=pg[:], func=AF.Silu), r=[pgk], w=['e_sg0'])
                P.op('dve', lambda e: e.tensor_tensor(out=hid[:, f, :], in0=pu[:], in1=sg[j][:], op=ALU.mult), r=[puk, 'e_sg0'], w=['e_hid'])
            for q in range(4):
                for hc in range(2):
                    ps_y, pyk = psS[hc], f'psS{hc}'
                    P.pe_group([(lambda e, f=f: e.matmul(ps_y[:], lhsT=hid[:, f, q * 128:(q + 1) * 128], rhs=wd[:, f, hc * 512:(hc + 1) * 512], start=(f == 0), stop=(f == 7))) for f in range(8)], r=['e_hid', 'e_wd'], w=[pyk])
                    if hc == 0:
                        P.op('act', lambda e: e.copy(out=yeb[:, q, 0:512], in_=ps_y[:]), r=[pyk], w=['e_yeb'])
                    else:
                        P.op('dve', lambda e: e.tensor_copy(out=yeb[:, q, 512:1024], in_=ps_y[:]), r=[pyk], w=['e_yeb'])
            fin.append(P.op('sp', lambda e: e.dma_start(out=ye_all[ex].rearrange("(q p) d -> p q d", p=128), in_=yeb[:]), r=['e_yeb'], w=[('scr', 'ye', ex)], dma=True))
        P.barrier()
        esE.close()
        esH.close()
        esF = ExitStack()
        yeA = sbs(esF, "f_yeA", [128, 16, 4, D], BF16)
        for ex in range(16):
            P.op('sp', lambda e: e.dma_start(out=yeA[:, ex, :, :], in_=ye_all[ex].rearrange("(q p) d -> p q d", p=128)), r=[('scr', 'ye', ex)], w=['f_yeA'], dma=True)
        Sg = [sbs(esF, f"f_Sg{i}", [128, 512], BF16) for i in range(2)]
        SgT = sbs(esF, "f_SgT", [128, 16, 4, 128], BF16)
        x1l = [sbs(esF, f"f_x1l{i}", [128, D], F32) for i in range(2)]
        ft = sbs(esF, "f_ft", [128, D], F32)
        ot = [sbs(esF, f"f_ot{i}", [128, D], F32) for i in range(2)]
        junk2 = sbs(esF, "f_junk", [128, D], F32)
        for t in range(NT // 2):
            i = t % 2
            ts_ = slice(t * 128, (t + 1) * 128)
            P.op('sp', lambda e: e.dma_start(out=x1l[i][:], in_=x1s[ts_, :]), r=[('scr', 'x1s', t)], w=[f'f_x1l{i}'], dma=True)
            for ex in range(16):
                j = ex % 2
                P.op('dve', lambda e: e.tensor_scalar(out=Sg[j][:], in0=iot[:], scalar1=posm[:, t, ex:ex + 1], scalar2=affTM[:, t, ex:ex + 1], op0=ALU.is_equal, op1=ALU.mult),
                     r=['iot', 'posm', ('affTM', t)], w=[f'f_Sg{j}'])
                for q in range(4):
                    P.op('pe', lambda e: e.transpose(out=psT[j][:, q * 128:(q + 1) * 128], in_=Sg[j][:, q * 128:(q + 1) * 128], identity=identb[:]), r=[f'f_Sg{j}', 'identb'], w=[f'psT{j}'])
                if j == 0:
                    P.op('act', lambda e: e.copy(out=SgT[:, ex, :, :], in_=psT[j][:].rearrange("p (q n) -> p q n", q=4)), r=[f'psT{j}'], w=[('f_SgT', ex)])
                else:
                    P.op('pool', lambda e: e.tensor_copy(out=SgT[:, ex, :, :], in_=psT[j][:].rearrange("p (q n) -> p q n", q=4)), r=[f'psT{j}'], w=[('f_SgT', ex)]) if False else \
                        P.op('act', lambda e: e.copy(out=SgT[:, ex, :, :], in_=psT[j][:].rearrange("p (q n) -> p q n", q=4)), r=[f'psT{j}'], w=[('f_SgT', ex)])
            for hc in range(2):
                hs_ = slice(hc * 512, (hc + 1) * 512)
                pt, pk = psA[(t % 2) * 2 + hc], f'psA{(t % 2) * 2 + hc}'
                P.pe_group([(lambda e, ex=ex, q=q: e.matmul(pt[:], lhsT=SgT[:, ex, q, :], rhs=yeA[:, ex, q, hs_], start=(ex == 0 and q == 0), stop=(ex == 15 and q == 3)))
                            for ex in range(16) for q in range(4)], r=[('f_SgT', ex) for ex in range(16)] + ['f_yeA'], w=[pk])
                P.op('dve', lambda e: e.tensor_tensor(out=ft[:, hs_], in0=pt[:], in1=bcs[:, 3, hs_], op=ALU.mult), r=[pk, 'bcs'], w=['f_ft'])
            P.op('pool', lambda e: e.tensor_tensor(out=ft[:], in0=ft[:], in1=x1l[i][:], op=ALU.add), r=['f_ft', f'f_x1l{i}'], w=['f_ft'])
            P.op('dve', lambda e: e.memset(st2[:], 0.0), w=['st2'])
            P.op('act', lambda e: e.activation(out=junk2[:], in_=ft[:], func=AF.Square, accum_out=st2[:, 0:1]), r=['f_ft'], w=['f_junk', 'st2'])
            P.op('act', lambda e: e.activation(out=st2[:, 1:2], in_=st2[:, 0:1], func=AF.Sqrt, scale=1.0 / D, bias=epsb[:]), r=['st2', 'epsb'], w=['st2'])
            P.op('dve', lambda e: e.reciprocal(out=st2[:, 2:3], in_=st2[:, 1:2]), r=['st2'], w=['st2'])
            P.op('dve', lambda e: e.scalar_tensor_tensor(out=ot[i][:], in0=ft[:], scalar=st2[:, 2:3], in1=gfin[:], op0=ALU.mult, op1=ALU.mult), r=['f_ft', 'st2', 'gfin'], w=[f'f_ot{i}'])
            fin.append(P.op('sp', lambda e: e.dma_start(out=out[ts_, :], in_=ot[i][:]), r=[f'f_ot{i}'], dma=True))
        P.barrier()
        esF.close()
        esP.close()

    if 'A' in stages:
        phase_A()
    if 'MLA' in stages:
        phase_MLA()
    if 'DN' in stages:
        try:
            phase_DN()
        except _Stop:
            P.barrier()
    if 'MG' in stages:
        phase_MG_MOE()
    P.finish(fin)
    print("instructions:", P.n)
    return nc


_invf = (10000.0 ** (-np.arange(32, dtype=np.float32) / np.float32(32))).astype(np.float32)
INVF2 = np.concatenate([_invf, _invf])[:, None].astype(np.float32)
SGN2 = np.concatenate([-np.ones(32), np.ones(32)])[:, None].astype(np.float32)
SEL64 = np.zeros((128, 65), np.float32)
SEL64[:, 64] = 1.0
_xi = np.arange(128)[:, None]
_yi = np.arange(128)[None, :]
BIG = 1.0e4
DMASKS = np.stack([np.where(_xi > _yi, 0.0, BIG), np.where(_yi >= _xi, 0.0, -BIG),
                   np.where(_xi < _yi, 0.0, BIG), np.where(_yi <= _xi, 0.0, -BIG)], axis=1).astype(np.float32)
def _bd(s_):
    return ((_xi // s_) == (_yi // s_)).astype(np.float32)
DN_LMASK = np.ascontiguousarray(np.stack([np.tile(m_, (1, 4)) for m_ in
                                          (_bd(16), _bd(32) - _bd(16), _bd(64) - _bd(32), 1.0 - _bd(64), np.eye(128, dtype=np.float32))], axis=1))
IOTA512 = np.ascontiguousarray(np.broadcast_to(np.arange(512, dtype=np.float32)[None, :], (128, 512)))
TRI = (_xi < _yi).astype(np.float32)
IN_SPL = np.cumsum([3072, 1024, 16, 16, 512, 256, 64, 2048])


def prep_inputs(inputs, core):
    b, half = core // 2, core % 2
    f = lambda a: np.ascontiguousarray(a, dtype=np.float32)
    w_in = inputs['w_in'][0]
    qkv, z, bb, aa, cq, ckv, kr, g = np.split(w_in, IN_SPL[:-1], axis=1)
    xb = inputs['x'][b]
    posb = inputs['positions'][b]
    conv_w = inputs['conv_w'][0]
    a_log, dt_bias = inputs['a_log'][0], inputs['dt_bias'][0]
    if half == 1:
        xb = xb[::-1]
        posb = posb[::-1]
        conv_w = conv_w[::-1]
        bb = np.concatenate([bb[:, 8:16], bb[:, 0:8]], axis=1)
        aa = np.concatenate([aa[:, 8:16], aa[:, 0:8]], axis=1)
        a_log, dt_bias = a_log[::-1], dt_bias[::-1]
    swap = np.concatenate([np.arange(32, 64), np.arange(0, 32)])
    wuq_ = inputs['w_uq'][0].reshape(512, 8, 192)
    wukv_ = inputs['w_ukv'][0].reshape(256, 8, 256)
    m = {
        'x': f(xb),
        'cT': f(inputs['c'][b].reshape(8, 128).T),
        'w_mod': f(inputs['w_mod'][0]),
        'b_mod': f(inputs['b_mod'][0][None, :]),
        'g_mix': f(np.broadcast_to(inputs['g_mix'][0][None, :], (128, D))),
        'w_qkv': f(qkv), 'w_z': f(z), 'w_g': f(g),
        'w_ba': f(np.concatenate([bb, aa], axis=1)),
        'w_cq': f(cq), 'w_ckv': f(ckv),
        'w_kr2': f(np.concatenate([kr, kr[:, swap]], axis=1)),
        'q_gain': f(np.broadcast_to(inputs['q_gain'][0][None, :], (128, 512))),
        'kv_gain': f(np.broadcast_to(inputs['kv_gain'][0][None, :], (128, 256))),
        'identf': np.eye(128, dtype=np.float32),
        'posr': np.ascontiguousarray(np.broadcast_to(posb[None, :], (64, S)).astype(np.int32)),
        'invf2': INVF2, 'sgn2': SGN2, 'sel64': SEL64,
        'w_uqh': f(np.stack([np.concatenate([wuq_[:, h, 0:128], wuq_[:, h, 128:192], wuq_[:, h, 128:192][:, swap]], axis=1) for h in range(8)])),
        'w_ukvh': f(np.stack([wukv_[:, h, :] for h in range(8)])),
        'conv_wT': f(conv_w.T),
        'dn_sc': f(np.stack([a_log[0], a_log[1], dt_bias[0], dt_bias[1]], axis=1)),
        'dn_gain': f(np.broadcast_to(inputs['dn_o_gain'][0][None, :], (128, 128))),
        'dmasks': DMASKS, 'dn_lmask': DN_LMASK,
        'w_o_dn': f(inputs['w_o_dn'][0]), 'w_o_mla': f(inputs['w_o_mla'][0]), 'w_out': f(inputs['w_out'][0]),
        'g_ffn': f(np.broadcast_to(inputs['g_ffn'][0][None, :], (128, D))),
        'g_final': f(np.broadcast_to(inputs['g_final'][None, :], (128, D))),
        'w_router': f(inputs['w_router'][0]),
        'w_gate': f(inputs['w_gate'][0]), 'w_up': f(inputs['w_up'][0]), 'w_down': f(inputs['w_down'][0]),
        'iota512': IOTA512, 'tri_in': TRI,
    }
    return m


def kernel(**inputs):
    inputs = {k: np.asarray(v) for k, v in inputs.items()}
    nc = build()
    in_maps = [prep_inputs(inputs, c) for c in range(8)]
    res = run_bass_kernel_spmd(nc, in_maps, core_ids=list(range(8)))
    outp = np.zeros((4, S, D), np.float32)
    for c in range(8):
        b, half = c // 2, c % 2
        o_ = res.results[c]["out"]
        if half == 0:
            outp[b, 0:2048] = o_
        else:
            outp[b, 2048:4096] = o_[::-1]
    return outp
```

```python
import numpy as np
import ml_dtypes
import concourse.bass as bass
import concourse.mybir as mybir
from concourse.bass_utils import run_bass_kernel_spmd
from contextlib import ExitStack

F32 = mybir.dt.float32
BF16 = mybir.dt.bfloat16
I32 = mybir.dt.int32
AF = mybir.ActivationFunctionType
ALU = mybir.AluOpType
AX = mybir.AxisListType

import os
DBGN = int(os.environ.get('DBGN', '99'))
S = 4096
D = 1024
NT = S // 128
EPS = 1e-6


class Prog:
    def __init__(self, nc, ndma=16):
        self.nc = nc
        self.eng = {'pe': nc.tensor, 'act': nc.scalar, 'dve': nc.vector,
                    'pool': nc.gpsimd, 'sp': nc.sync}
        self.sem = {k: nc.alloc_semaphore(name=f"s_{k}") for k in self.eng}
        self.cnt = {k: 0 for k in self.eng}
        self.waited = {k: {} for k in self.eng}
        self.dsem = {q: [nc.alloc_semaphore(name=f"s_dma_{q}{i}") for i in range(ndma)] for q in ('sp', 'pool')}
        self.dcnt = {q: [0] * ndma for q in ('sp', 'pool')}
        self.nd = {'sp': 0, 'pool': 0}
        self.lastw = {}
        self.readers = {}
        self.n = 0

    def _wait(self, e, tok):
        if tok is None:
            return
        sem, val, key = tok
        w = self.waited[e]
        if w.get(key, 0) >= val:
            return
        w[key] = val
        self.eng[e].wait_ge(sem, val)

    def op(self, e, fn, r=(), w=(), dma=False):
        w = list(w) + [t for t in r if isinstance(t, str) and t.startswith('ps') and t not in w]
        toks = []
        for t in r:
            toks.append(self.lastw.get(t))
        for t in w:
            toks.append(self.lastw.get(t))
            toks.extend(self.readers.get(t, ()))
        for tok in toks:
            self._wait(e, tok)
        if dma:
            i = self.nd[e] % len(self.dsem[e])
            if self.dcnt[e][i] > 0:
                self._wait(e, (self.dsem[e][i], self.dcnt[e][i], f"dma_{e}{i}"))
        ins = fn(self.eng[e])
        self.n += 1
        if dma:
            i = self.nd[e] % len(self.dsem[e])
            self.nd[e] += 1
            self.dcnt[e][i] += 16
            ins.then_inc(self.dsem[e][i], 16)
            tok = (self.dsem[e][i], self.dcnt[e][i], f"dma_{e}{i}")
        else:
            self.cnt[e] += 1
            ins.then_inc(self.sem[e], 1)
            tok = (self.sem[e], self.cnt[e], e)
        for t in r:
            self.readers.setdefault(t, []).append(tok)
        for t in w:
            self.lastw[t] = tok
            self.readers[t] = []
        return tok

    def pe_group(self, fns, r=(), w=()):
        e = 'pe'
        w = list(w) + [t for t in r if isinstance(t, str) and t.startswith('ps') and t not in w]
        toks = []
        for t in r:
            toks.append(self.lastw.get(t))
        for t in w:
            toks.append(self.lastw.get(t))
            toks.extend(self.readers.get(t, ()))
        for tok in toks:
            self._wait(e, tok)
        for fn in fns[:-1]:
            fn(self.eng[e])
            self.n += 1
        ins = fns[-1](self.eng[e])
        self.n += 1
        self.cnt[e] += 1
        ins.then_inc(self.sem[e], 1)
        tok = (self.sem[e], self.cnt[e], e)
        for t in r:
            self.readers.setdefault(t, []).append(tok)
        for t in w:
            self.lastw[t] = tok
            self.readers[t] = []
        return tok

    def barrier(self):
        toks = [(self.sem[k], self.cnt[k], k) for k in self.eng if self.cnt[k] > 0]
        toks += [(self.dsem[q][i], self.dcnt[q][i], f"dma_{q}{i}") for q in self.dsem for i in range(len(self.dsem[q])) if self.dcnt[q][i] > 0]
        for e in self.eng:
            for tok in toks:
                self._wait(e, tok)

    def finish(self, toks):
        for tok in toks:
            self._wait('sp', tok)


class _Stop(Exception):
    pass


def build(debug=None, stages=('A', 'MLA', 'DN', 'MG'), ext_in=(), dn_heads=range(8), dn_stop=0):
    nc = bass.Bass("TRN2", target_bir_lowering=False)
    P = Prog(nc)

    def din(name, shape, dt=F32):
        return nc.dram_tensor(name, list(shape), dt, kind="ExternalInput").ap()

    def dscr(name, shape, dt):
        kind = "ExternalOutput" if (debug and name in debug) else ("ExternalInput" if name in ext_in else "Internal")
        return nc.dram_tensor(name, list(shape), dt, kind=kind).ap()

    x = din("x", [S, D])
    cT = din("cT", [128, 8])
    w_mod = din("w_mod", [D, 6 * D])
    b_mod = din("b_mod", [1, 6 * D])
    g_mix = din("g_mix", [128, D])
    w_qkv = din("w_qkv", [D, 3072])
    w_z = din("w_z", [D, 1024])
    w_g = din("w_g", [D, 2048])
    w_ba = din("w_ba", [D, 32])
    w_cq = din("w_cq", [D, 512])
    w_ckv = din("w_ckv", [D, 256])
    w_kr2 = din("w_kr2", [D, 128])
    q_gain = din("q_gain", [128, 512])
    kv_gain = din("kv_gain", [128, 256])
    identf = din("identf", [128, 128])
    posr = din("posr", [64, S], I32)
    invf2 = din("invf2", [64, 1])
    sgn2 = din("sgn2", [64, 1])
    sel64 = din("sel64", [128, 65])
    w_uqh = din("w_uqh", [8, 512, 256])
    w_ukvh = din("w_ukvh", [8, 256, 256])
    conv_wT = din("conv_wT", [3072, 5])
    dn_sc = din("dn_sc", [8, 4])
    dn_gain = din("dn_gain", [128, 128])
    dmasks = din("dmasks", [128, 4, 128])
    dn_lmask = din("dn_lmask", [128, 5, 512])
    w_o_dn = din("w_o_dn", [D, D])
    w_o_mla = din("w_o_mla", [D, D])
    w_out = din("w_out", [D, D])
    g_ffn = din("g_ffn", [128, D])
    g_final = din("g_final", [128, D])
    w_router = din("w_router", [D, 16])
    w_gate = din("w_gate", [16, D, D])
    w_up = din("w_up", [16, D, D])
    w_down = din("w_down", [16, D, D])
    iota512 = din("iota512", [128, 512])
    tri_in = din("tri_in", [128, 128])
    out = nc.dram_tensor("out", [S // 2, D], F32, kind="ExternalOutput").ap()

    qkvT = dscr("qkvT", [24, 128, S], BF16)
    zs = dscr("zs", [S, 1024], BF16)
    gs = dscr("gs", [S, 2048], BF16)
    baT = dscr("baT", [32, S], F32)
    cqnT = dscr("cqnT", [4, 128, S], BF16)
    ckvnT = dscr("ckvnT", [2, 128, S], BF16)
    krT = dscr("krT", [2, 64, S], BF16)
    modrow = dscr("modrow", [1, 6 * D], F32)
    oT_mla = dscr("oT_mla", [8, 128, S], BF16)
    oT_dn = dscr("oT_dn", [8, 128, S], BF16)
    x1s = dscr("x1s", [S, D], F32)
    ye_all = dscr("ye_all", [16, 512, D], BF16)

    sb = lambda n, s, d: nc.alloc_sbuf_tensor(n, list(s), d)
    ps_ = lambda n, s, d=F32: nc.alloc_psum_tensor(n, list(s), d)

    ident = sb("ident", [128, 128], F32)
    identb = sb("identb", [128, 128], BF16)
    ones_f = sb("ones_f", [128, 128], F32)
    P.op('sp', lambda e: e.dma_start(out=ident[:], in_=identf[:, :]), w=['ident'], dma=True)
    P.op('dve', lambda e: e.tensor_copy(out=identb[:], in_=ident[:]), r=['ident'], w=['identb'])
    P.op('dve', lambda e: e.memset(ones_f[:], 1.0), w=['ones_f'])
    epsb = sb("epsb", [128, 1], F32)
    P.op('dve', lambda e: e.memset(epsb[:], EPS), w=['epsb'])

    psA = [ps_(f"psA{i}", [128, 512]) for i in range(4)]
    psT = [ps_(f"psT{i}", [128, 512], BF16) for i in range(2)]
    psS = [ps_(f"psS{i}", [128, 512]) for i in range(2)]
    pa_i = [0]

    def next_psA():
        i = pa_i[0] % 4
        pa_i[0] += 1
        return psA[i], f"psA{i}"

    sbs = lambda es, n, s_, d: es.enter_context(nc.sbuf_tensor(n, list(s_), d))
    fin = []

    def phase_A():
        cT_sb = sb("cT_sb", [128, 8], F32)
        scT = sb("scT", [128, 8], F32)
        P.op('sp', lambda e: e.dma_start(out=cT_sb[:], in_=cT[:, :]), w=['cT'], dma=True)
        P.op('act', lambda e: e.activation(out=scT[:], in_=cT_sb[:], func=AF.Silu), r=['cT'], w=['scT'])
        esA = ExitStack()
        modbc = sbs(esA, "modbc", [128, 6, D], F32)
        gmx = sbs(esA, "gmx", [128, D], F32)
        A1 = sbs(esA, "A1", [128, D], F32)
        es0 = ExitStack()
        mod_sb = sbs(es0, "mod_sb", [1, 6 * D], F32)
        bm_sb = sbs(es0, "bm_sb", [1, 6 * D], F32)
        P.op('sp', lambda e: e.dma_start(out=bm_sb[:], in_=b_mod[:, :]), w=['bm'], dma=True)
        wm = [sbs(es0, f"wm{i}", [128, 8, 512], F32) for i in range(2)]
        w_mod_v = w_mod.rearrange("(k p) n -> p k n", p=128)
        for j in range(12):
            wt, wk = wm[j % 2], f"wm{j % 2}"
            P.op('sp', lambda e: e.dma_start(out=wt[:], in_=w_mod_v[:, :, j * 512:(j + 1) * 512]), w=[wk], dma=True)
            pt, pk = next_psA()
            for k in range(8):
                P.op('pe', lambda e: e.matmul(pt[0:1, :], lhsT=scT[:, k:k + 1], rhs=wt[:, k, :], start=(k == 0), stop=(k == 7)),
                     r=[wk, 'scT'], w=[pk])
            P.op('dve', lambda e: e.tensor_tensor(out=mod_sb[:, j * 512:(j + 1) * 512], in0=pt[0:1, :], in1=bm_sb[:, j * 512:(j + 1) * 512], op=ALU.add),
                 r=[pk, 'bm'], w=['mod'])
        for j in range(12):
            pt, pk = next_psA()
            P.op('pe', lambda e: e.matmul(pt[:], lhsT=ones_f[0:1, :], rhs=mod_sb[:, j * 512:(j + 1) * 512], start=True, stop=True),
                 r=['ones_f', 'mod'], w=[pk])
            P.op('act', lambda e: e.copy(out=modbc[:, j // 2, (j % 2) * 512:(j % 2 + 1) * 512], in_=pt[:]), r=[pk], w=['modbc'])
        P.op('sp', lambda e: e.dma_start(out=gmx[:], in_=g_mix[:, :]), w=['gmx'], dma=True)
        P.op('dve', lambda e: e.scalar_tensor_tensor(out=A1[:], in0=modbc[:, 1, :], scalar=1.0, in1=gmx[:], op0=ALU.add, op1=ALU.mult),
             r=['modbc', 'gmx'], w=['A1'])

        fin.append(P.op('sp', lambda e: e.dma_start(out=modrow[:, :], in_=mod_sb[:]), r=['mod'], dma=True))
        P.barrier()
        es0.close()
        hT = sbs(esA, "hT", [128, 8, S], BF16)
        xt = [sbs(esA, f"xt{i}", [128, D], F32) for i in range(2)]
        xn = [sbs(esA, f"xn{i}", [128, D], F32) for i in range(2)]
        hb = [sbs(esA, f"hb{i}", [128, D], BF16) for i in range(2)]
        st = [sbs(esA, f"st{i}", [128, 4], F32) for i in range(2)]
        junk = sbs(esA, "junk", [128, D], F32)
        for t in range(NT):
            i = t % 2
            P.op('sp', lambda e: e.dma_start(out=xt[i][:], in_=x[t * 128:(t + 1) * 128, :]), w=[f'xt{i}'], dma=True)
            P.op('dve', lambda e: e.memset(st[i][:], 0.0), w=[f'st{i}'])
            P.op('act', lambda e: e.activation(out=junk[:], in_=xt[i][:], func=AF.Square, accum_out=st[i][:, 0:1]),
                 r=[f'xt{i}'], w=['junk', f'st{i}'])
            P.op('act', lambda e: e.activation(out=st[i][:, 1:2], in_=st[i][:, 0:1], func=AF.Sqrt, scale=1.0 / D, bias=epsb[:]),
                 r=[f'st{i}', 'epsb'], w=[f'st{i}'])
            P.op('dve', lambda e: e.reciprocal(out=st[i][:, 2:3], in_=st[i][:, 1:2]), r=[f'st{i}'], w=[f'st{i}'])
            P.op('dve', lambda e: e.scalar_tensor_tensor(out=xn[i][:], in0=xt[i][:], scalar=st[i][:, 2:3], in1=A1[:], op0=ALU.mult, op1=ALU.mult),
                 r=[f'xt{i}', f'st{i}', 'A1'], w=[f'xn{i}'])
            P.op('pool', lambda e: e.tensor_tensor(out=hb[i][:], in0=xn[i][:], in1=modbc[:, 0, :], op=ALU.add),
                 r=[f'xn{i}', 'modbc'], w=[f'hb{i}'])
            for half in range(2):
                pt, pk = psT[half], f'psT{half}'
                for k4 in range(4):
                    k = half * 4 + k4
                    P.op('pe', lambda e: e.transpose(out=pt[:, k4 * 128:(k4 + 1) * 128], in_=hb[i][:, k * 128:(k + 1) * 128], identity=identb[:]),
                         r=[f'hb{i}', 'identb'], w=[pk])
                eng = 'act' if half == 0 else 'dve'
                if eng == 'act':
                    P.op('act', lambda e: e.copy(out=hT[:, half * 4:(half + 1) * 4, t * 128:(t + 1) * 128],
                                                 in_=pt[:].rearrange("p (k n) -> p k n", k=4)), r=[pk], w=[('hT', t)])
                else:
                    P.op('dve', lambda e: e.tensor_copy(out=hT[:, half * 4:(half + 1) * 4, t * 128:(t + 1) * 128],
                                                        in_=pt[:].rearrange("p (k n) -> p k n", k=4)), r=[pk], w=[('hT', t)])
        hT_all = [('hT', t) for t in range(NT)]

        wb = [sbs(esA, f"wb{i}", [128, 8, 512], BF16) for i in range(2)]
        wb_i = [0]

        def load_w(src, c0, ncols):
            i = wb_i[0] % 2
            wb_i[0] += 1
            v = src.rearrange("(k p) n -> p k n", p=128)
            P.op('pool', lambda e: e.dma_start(out=wb[i][:, :, 0:ncols], in_=v[:, :, c0:c0 + ncols]), w=[f'wb{i}'], dma=True)
            return wb[i], f'wb{i}'

        stg = [sbs(esA, f"stg{i}", [128, S], BF16) for i in range(2)]
        stg_i = [0]

        def chan_major(src, ncols_total, dst_fn, M, dt_out=BF16, stgs=stg):
            nblk = (ncols_total + 511) // 512
            for blk in range(nblk):
                nc_ = min(512, ncols_total - blk * 512)
                wt, wk = load_w(src, blk * 512, nc_)
                for c in range(nc_ // M):
                    si = stg_i[0] % 2
                    stg_i[0] += 1
                    sg, sk = stgs[si], f'{stgs[si].name}'
                    for g in range(8):
                        pt, pk = next_psA()
                        P.pe_group([(lambda e, k=k: e.matmul(pt[0:M, :], lhsT=wt[:, k, c * M:(c + 1) * M], rhs=hT[:, k, g * 512:(g + 1) * 512],
                                                            start=(k == 0), stop=(k == 7))) for k in range(8)],
                                   r=[wk] + hT_all[g * 4:(g + 1) * 4], w=[pk])
                        if g % 2 == 0:
                            P.op('act', lambda e: e.copy(out=sg[0:M, g * 512:(g + 1) * 512], in_=pt[0:M, :]), r=[pk], w=[sk])
                        else:
                            P.op('dve', lambda e: e.tensor_copy(out=sg[0:M, g * 512:(g + 1) * 512], in_=pt[0:M, :]), r=[pk], w=[sk])
                    fin.append(P.op('sp', lambda e: e.dma_start(out=dst_fn(blk * (512 // M) + c), in_=sg[0:M, :]), r=[sk], w=[('scr', dst_fn.__name__)], dma=True))

        def dst_qkv(c):
            return qkvT[c, :, :]
        chan_major(w_qkv, 3072, dst_qkv, 128)

        def dst_kr(c):
            return krT[c, :, :]
        chan_major(w_kr2, 128, dst_kr, 64)
        stgf0 = sbs(esA, "stgf0", [32, S], F32)
        stgf = [stgf0, stgf0]

        def dst_ba(c):
            return baT[:, :]
        chan_major(w_ba, 32, dst_ba, 32, F32, stgf)

        tst = [sbs(esA, f"tst{i}", [128, 512], BF16) for i in range(4)]
        tst_i = [0]

        def tok_major_act(src, ncols_total, dst, func):
            for blk in range(ncols_total // 512):
                wt, wk = load_w(src, blk * 512, 512)
                for t in range(NT):
                    pt, pk = next_psA()
                    for k in range(8):
                        P.op('pe', lambda e: e.matmul(pt[:], lhsT=hT[:, k, t * 128:(t + 1) * 128], rhs=wt[:, k, :], start=(k == 0), stop=(k == 7)),
                             r=[wk, ('hT', t)], w=[pk])
                    si = tst_i[0] % 4
                    tst_i[0] += 1
                    P.op('act', lambda e: e.activation(out=tst[si][:], in_=pt[:], func=func), r=[pk], w=[f'tst{si}'])
                    fin.append(P.op('sp', lambda e: e.dma_start(out=dst[t * 128:(t + 1) * 128, blk * 512:(blk + 1) * 512], in_=tst[si][:]),
                                    r=[f'tst{si}'], w=[('scr', dst.name, t, blk)], dma=True))
        tok_major_act(w_z, 1024, zs, AF.Silu)
        tok_major_act(w_g, 2048, gs, AF.Sigmoid)

        def latent(src, ncols, gain_in, dstT, nm):
            gsb = sbs(esA, f"gain_{nm}", [128, ncols], F32)
            P.op('sp', lambda e: e.dma_start(out=gsb[:], in_=gain_in[:, :]), w=[f'gain_{nm}'], dma=True)
            lst = [sbs(esA, f"lst_{nm}{i_}", [128, ncols // 128, 128], BF16) for i_ in range(2)]
            wt, wk = load_w(src, 0, ncols)
            for t in range(NT):
                i = t % 2
                pt, pk = next_psA()
                for k in range(8):
                    P.op('pe', lambda e: e.matmul(pt[:, 0:ncols], lhsT=hT[:, k, t * 128:(t + 1) * 128], rhs=wt[:, k, 0:ncols], start=(k == 0), stop=(k == 7)),
                         r=[wk, ('hT', t)], w=[pk])
                P.op('dve', lambda e: e.memset(st[i][:], 0.0), w=[f'st{i}'])
                P.op('act', lambda e: e.activation(out=junk[:, 0:ncols], in_=pt[:, 0:ncols], func=AF.Square, accum_out=st[i][:, 0:1]),
                     r=[pk], w=['junk', f'st{i}'])
                P.op('act', lambda e: e.activation(out=st[i][:, 1:2], in_=st[i][:, 0:1], func=AF.Sqrt, scale=1.0 / ncols, bias=epsb[:]),
                     r=[f'st{i}', 'epsb'], w=[f'st{i}'])
                P.op('dve', lambda e: e.reciprocal(out=st[i][:, 2:3], in_=st[i][:, 1:2]), r=[f'st{i}'], w=[f'st{i}'])
                P.op('dve', lambda e: e.scalar_tensor_tensor(out=hb[i][:, 0:ncols], in0=pt[:, 0:ncols], scalar=st[i][:, 2:3], in1=gsb[:], op0=ALU.mult, op1=ALU.mult),
                     r=[pk, f'st{i}', f'gain_{nm}'], w=[f'hb{i}'])
                tp, tk = psT[i], f'psT{i}'
                for c in range(ncols // 128):
                    P.op('pe', lambda e: e.transpose(out=tp[:, c * 128:(c + 1) * 128], in_=hb[i][:, c * 128:(c + 1) * 128], identity=identb[:]),
                         r=[f'hb{i}', 'identb'], w=[tk])
                P.op('act', lambda e: e.copy(out=lst[i][:], in_=tp[:, 0:ncols].rearrange("p (k n) -> p k n", k=ncols // 128)),
                     r=[tk], w=[f'lst_{nm}{i}'])
                fin.append(P.op('sp', lambda e: e.dma_start(out=dstT[:, :, t * 128:(t + 1) * 128].rearrange("c p n -> p c n"), in_=lst[i][:]),
                                r=[f'lst_{nm}{i}'], w=[('scr', nm, t)], dma=True))
        latent(w_cq, 512, q_gain, cqnT, 'cq')
        latent(w_ckv, 256, kv_gain, ckvnT, 'ckv')


        P.barrier()
        esA.close()

    def phase_MLA():
        esM = ExitStack()
        TWO_PI = float(2 * np.pi)
        SCL = float(192 ** -0.5)
        cos2 = sbs(esM, "cos2", [64, S], F32)
        sin2 = sbs(esM, "sin2", [64, S], F32)
        krA = sbs(esM, "krA", [65, S], BF16)
        QrA = sbs(esM, "QrA", [65, S], BF16)
        onesb = sbs(esM, "onesb", [128, 128], BF16)
        sel_b = sbs(esM, "sel_b", [128, 65], BF16)
        if True:
            es1 = ExitStack()
            posi = sbs(es1, "posi", [64, S], I32)
            ang = sbs(es1, "ang", [64, S], F32)
            ti = sbs(es1, "ti", [64, S], I32)
            tf = sbs(es1, "tf", [64, S], F32)
            tg = sbs(es1, "tg", [64, S], F32)
            ivf = sbs(es1, "ivf", [64, 2], F32)
            kr0 = sbs(es1, "kr0", [64, S], BF16)
            kr1 = sbs(es1, "kr1", [64, S], BF16)
            self_f = sbs(es1, "self_f", [128, 65], F32)
            P.op('sp', lambda e: e.dma_start(out=posi[:], in_=posr[:, :]), w=['posi'], dma=True)
            P.op('sp', lambda e: e.dma_start(out=ivf[:, 0:1], in_=invf2[:, :]), w=['ivf'], dma=True)
            P.op('sp', lambda e: e.dma_start(out=ivf[:, 1:2], in_=sgn2[:, :]), w=['ivf'], dma=True)
            P.op('sp', lambda e: e.dma_start(out=self_f[:], in_=sel64[:, :]), w=['self_f'], dma=True)
            P.op('dve', lambda e: e.tensor_copy(out=sel_b[:], in_=self_f[:]), r=['self_f'], w=['sel_b'])
            P.op('dve', lambda e: e.memset(onesb[:], 1.0), w=['onesb'])
            P.op('dve', lambda e: e.tensor_copy(out=ang[:], in_=posi[:]), r=['posi'], w=['ang'])
            P.op('dve', lambda e: e.tensor_scalar(out=ang[:], in0=ang[:], scalar1=ivf[:, 0:1], scalar2=float(1.0 / TWO_PI), op0=ALU.mult, op1=ALU.mult),
                 r=['ang', 'ivf'], w=['ang'])
            for which, dst in ((0, sin2), (1, cos2)):
                dk_ = 'sin2' if which == 0 else 'cos2'
                P.op('dve', lambda e: e.tensor_scalar(out=tg[:], in0=ang[:], scalar1=0.25 * which, scalar2=None, op0=ALU.add), r=['ang'], w=['tg'])
                P.op('dve', lambda e: e.tensor_copy(out=ti[:], in_=tg[:]), r=['tg'], w=['ti'])
                P.op('dve', lambda e: e.tensor_copy(out=tf[:], in_=ti[:]), r=['ti'], w=['tf'])
                P.op('dve', lambda e: e.tensor_tensor(out=tg[:], in0=tg[:], in1=tf[:], op=ALU.subtract), r=['tg', 'tf'], w=['tg'])
                P.op('dve', lambda e: e.tensor_scalar(out=tf[:], in0=tg[:], scalar1=0.5, scalar2=None, op0=ALU.is_gt), r=['tg'], w=['tf'])
                P.op('dve', lambda e: e.tensor_tensor(out=tg[:], in0=tg[:], in1=tf[:], op=ALU.subtract), r=['tg', 'tf'], w=['tg'])
                P.op('dve', lambda e: e.tensor_scalar(out=tf[:], in0=tg[:], scalar1=-0.5, scalar2=None, op0=ALU.is_lt), r=['tg'], w=['tf'])
                P.op('dve', lambda e: e.tensor_tensor(out=tg[:], in0=tg[:], in1=tf[:], op=ALU.add), r=['tg', 'tf'], w=['tg'])
                P.op('act', lambda e: e.activation(out=dst[:], in_=tg[:], func=AF.Sin, scale=TWO_PI), r=['tg'], w=[dk_])
            P.op('dve', lambda e: e.tensor_scalar(out=sin2[:], in0=sin2[:], scalar1=ivf[:, 1:2], scalar2=None, op0=ALU.mult), r=['sin2', 'ivf'], w=['sin2'])
            P.op('sp', lambda e: e.dma_start(out=kr0[:], in_=krT[0, :, :]), r=[('scr', 'dst_kr')], w=['kr0'], dma=True)
            P.op('sp', lambda e: e.dma_start(out=kr1[:], in_=krT[1, :, :]), r=[('scr', 'dst_kr')], w=['kr1'], dma=True)
            P.op('dve', lambda e: e.tensor_tensor(out=tg[:], in0=kr0[:], in1=cos2[:], op=ALU.mult), r=['kr0', 'cos2'], w=['tg'])
            P.op('dve', lambda e: e.tensor_tensor(out=tf[:], in0=kr1[:], in1=sin2[:], op=ALU.mult), r=['kr1', 'sin2'], w=['tf'])
            P.op('dve', lambda e: e.memset(krA[:], 1.0), w=['krA'])
            P.op('dve', lambda e: e.tensor_tensor(out=krA[0:64, :], in0=tg[:], in1=tf[:], op=ALU.add), r=['tg', 'tf'], w=['krA'])
            P.op('dve', lambda e: e.memset(QrA[:], 0.0), w=['QrA'])
            P.barrier()
            es1.close()
        cqn = sbs(esM, "cqn", [128, 4, S], BF16)
        ckvn = sbs(esM, "ckvn", [128, 2, S], BF16)
        QnT = sbs(esM, "QnT", [128, S], BF16)
        KnT = sbs(esM, "KnT", [128, S], BF16)
        Vt = sbs(esM, "Vt", [128, NT, 128], BF16)
        oTs = sbs(esM, "oTs", [128, S], BF16)
        kmx = sbs(esM, "kmx", [65, 16], F32)
        wuq = sbs(esM, "wuq", [128, 4, 256], BF16)
        wukv = sbs(esM, "wukv", [128, 2, 256], BF16)
        sq = sbs(esM, "sq", [128, 512], BF16)
        t1 = sbs(esM, "t1", [64, 512], F32)
        t2 = sbs(esM, "t2", [64, 512], F32)
        rowt = sbs(esM, "rowt", [65, 512], F32)
        pT = [sbs(esM, f"pT{i}", [128, 512], BF16) for i in range(3)]
        rden = sbs(esM, "rden", [128, 512], F32)
        for c in range(4):
            P.op('sp', lambda e: e.dma_start(out=cqn[:, c, :], in_=cqnT[c, :, :]), r=[('scr', 'cq', t_) for t_ in range(NT)], w=['cqn'], dma=True)
        for c in range(2):
            P.op('sp', lambda e: e.dma_start(out=ckvn[:, c, :], in_=ckvnT[c, :, :]), r=[('scr', 'ckv', t_) for t_ in range(NT)], w=['ckvn'], dma=True)
        for h in range(8):
            P.op('pool', lambda e: e.dma_start(out=wuq[:], in_=w_uqh[h].rearrange("(k p) n -> p k n", p=128)), w=['wuq'], dma=True)
            P.op('pool', lambda e: e.dma_start(out=wukv[:], in_=w_ukvh[h].rearrange("(k p) n -> p k n", p=128)), w=['wukv'], dma=True)
            for g in range(8):
                gs_ = slice(g * 512, (g + 1) * 512)
                pt, pk = psA[2], 'psA2'
                for k in range(4):
                    P.op('pe', lambda e: e.matmul(pt[:], lhsT=wuq[:, k, 0:128], rhs=cqn[:, k, gs_], start=(k == 0), stop=(k == 3)), r=['wuq', 'cqn'], w=[pk])
                P.op('act', lambda e: e.copy(out=QnT[:, gs_], in_=pt[:]), r=[pk], w=[('QnT', g)])
                pt, pk = psA[3], 'psA3'
                for k in range(4):
                    P.op('pe', lambda e: e.matmul(pt[0:64, :], lhsT=wuq[:, k, 128:192], rhs=cqn[:, k, gs_], start=(k == 0), stop=(k == 3)), r=['wuq', 'cqn'], w=[pk])
                P.op('dve', lambda e: e.tensor_tensor(out=t1[:], in0=pt[0:64, :], in1=cos2[:, gs_], op=ALU.mult), r=[pk, 'cos2'], w=['t1'])
                for k in range(4):
                    P.op('pe', lambda e: e.matmul(pt[0:64, :], lhsT=wuq[:, k, 192:256], rhs=cqn[:, k, gs_], start=(k == 0), stop=(k == 3)), r=['wuq', 'cqn'], w=[pk])
                P.op('dve', lambda e: e.tensor_tensor(out=t2[:], in0=pt[0:64, :], in1=sin2[:, gs_], op=ALU.mult), r=[pk, 'sin2'], w=['t2'])
                P.op('dve', lambda e: e.tensor_tensor(out=QrA[0:64, gs_], in0=t1[:], in1=t2[:], op=ALU.add), r=['t1', 't2'], w=[('QrA', g)])
                pt, pk = psA[2], 'psA2'
                for k in range(2):
                    P.op('pe', lambda e: e.matmul(pt[:], lhsT=wukv[:, k, 0:128], rhs=ckvn[:, k, gs_], start=(k == 0), stop=(k == 1)), r=['wukv', 'ckvn'], w=[pk])
                P.op('act', lambda e: e.copy(out=KnT[:, gs_], in_=pt[:]), r=[pk], w=[('KnT', g)])
                pt, pk = psA[3], 'psA3'
                for j in range(4):
                    t_ = g * 4 + j
                    for k in range(2):
                        P.op('pe', lambda e: e.matmul(pt[:, j * 128:(j + 1) * 128], lhsT=ckvn[:, k, t_ * 128:(t_ + 1) * 128], rhs=wukv[:, k, 128:256], start=(k == 0), stop=(k == 1)),
                             r=['wukv', 'ckvn'], w=[pk])
                P.op('dve', lambda e: e.tensor_copy(out=Vt[:, g * 4:(g + 1) * 4, :], in_=pt[:].rearrange("p (j n) -> p j n", j=4)), r=[pk], w=[('Vt', g)])
                pt, pk = psA[2], 'psA2'
                P.op('act', lambda e: e.activation(out=sq[:], in_=KnT[:, gs_], func=AF.Square), r=[('KnT', g)], w=['sq'])
                P.op('pe', lambda e: e.matmul(pt[0:65, :], lhsT=sel_b[:, :], rhs=sq[:], start=True, stop=False), r=['sq', 'sel_b'], w=[pk])
                P.op('act', lambda e: e.activation(out=sq[0:64, :], in_=krA[0:64, gs_], func=AF.Square), r=['krA'], w=['sq'])
                P.op('pe', lambda e: e.matmul(pt[0:65, :], lhsT=sel_b[0:64, :], rhs=sq[0:64, :], start=False, stop=True), r=['sq', 'sel_b'], w=[pk])
                P.op('dve', lambda e: e.tensor_reduce(out=kmx[64:65, g:g + 1], in_=pt[64:65, :], axis=AX.X, op=ALU.max), r=[pk], w=['kmx'])
            P.op('dve', lambda e: e.tensor_reduce(out=kmx[64:65, 8:9], in_=kmx[64:65, 0:8], axis=AX.X, op=ALU.max), r=['kmx'], w=['kmx'])
            for g in range(8):
                gs_ = slice(g * 512, (g + 1) * 512)
                pt, pk = psA[2], 'psA2'
                P.op('act', lambda e: e.activation(out=sq[:], in_=QnT[:, gs_], func=AF.Square), r=[('QnT', g)], w=['sq'])
                P.op('pe', lambda e: e.matmul(pt[0:65, :], lhsT=sel_b[:, :], rhs=sq[:], start=True, stop=False), r=['sq', 'sel_b'], w=[pk])
                P.op('act', lambda e: e.activation(out=sq[0:64, :], in_=QrA[0:64, gs_], func=AF.Square), r=[('QrA', g)], w=['sq'])
                P.op('pe', lambda e: e.matmul(pt[0:65, :], lhsT=sel_b[0:64, :], rhs=sq[0:64, :], start=False, stop=True), r=['sq', 'sel_b'], w=[pk])
                P.op('act', lambda e: e.activation(out=rowt[64:65, :], in_=pt[64:65, :], func=AF.Sqrt, scale=kmx[64:65, 8:9]), r=[pk, 'kmx'], w=['rowt'])
                P.op('dve', lambda e: e.tensor_scalar(out=QrA[64:65, gs_], in0=rowt[64:65, :], scalar1=-1.0, scalar2=None, op0=ALU.mult), r=['rowt'], w=[('QrA', g)])
            for g in range(8):
                gs_ = slice(g * 512, (g + 1) * 512)
                po, pd = psA[(g % 2) * 2], psA[(g % 2) * 2 + 1]
                kpo, kpd = f'psA{(g % 2) * 2}', f'psA{(g % 2) * 2 + 1}'

                def scores(kt):
                    ks_ = slice(kt * 128, (kt + 1) * 128)
                    sc_, sck = psS[kt % 2], f'psS{kt % 2}'
                    P.pe_group([lambda e: e.matmul(sc_[:], lhsT=KnT[:, ks_], rhs=QnT[:, gs_], start=True, stop=False),
                                lambda e: e.matmul(sc_[:], lhsT=krA[:, ks_], rhs=QrA[:, gs_], start=False, stop=True)],
                               r=[('KnT', kt // 4), ('QnT', g), 'krA', ('QrA', g)], w=[sck])
                scores(0)
                for kt in range(NT):
                    sc_, sck = psS[kt % 2], f'psS{kt % 2}'
                    pi = kt % 3
                    P.op('act', lambda e: e.activation(out=pT[pi][:], in_=sc_[:], func=AF.Exp, scale=SCL), r=[sck], w=[f'pT{pi}'])
                    if kt + 1 < NT:
                        scores(kt + 1)
                    P.pe_group([lambda e: e.matmul(po[:], lhsT=Vt[:, kt, :], rhs=pT[pi][:], start=(kt == 0), stop=(kt == NT - 1)),
                                lambda e: e.matmul(pd[:], lhsT=onesb[:], rhs=pT[pi][:], start=(kt == 0), stop=(kt == NT - 1))],
                               r=[('Vt', kt // 4), f'pT{pi}', 'onesb'], w=[kpo, kpd])
                P.op('dve', lambda e: e.reciprocal(out=rden[:], in_=pd[:]), r=[kpd], w=['rden'])
                P.op('dve', lambda e: e.tensor_tensor(out=oTs[:, gs_], in0=po[:], in1=rden[:], op=ALU.mult), r=[kpo, 'rden'], w=['oTs'])
            fin.append(P.op('sp', lambda e: e.dma_start(out=oT_mla[h, :, :], in_=oTs[:]), r=['oTs'], w=[('scr', 'oT_mla', h)], dma=True))
        P.barrier()
        esM.close()


    def phase_DN():
        def stop(k):
            if dn_stop == k:
                raise _Stop()
        esD = ExitStack()
        pM, pG, pX0, pX1, pZT, pU = psA[0], psA[1], psA[2], psA[3], psS[0], psS[1]
        kM, kG, kX0, kX1, kZT, kU = 'psA0', 'psA1', 'psA2', 'psA3', 'psS0', 'psS1'
        onesb = sbs(esD, "d_onesb", [128, 128], BF16)
        P.op('dve', lambda e: e.memset(onesb[:], 1.0), w=['d_onesb'])
        msk = sbs(esD, "d_msk", [128, 4, 128], F32)
        P.op('sp', lambda e: e.dma_start(out=msk[:], in_=dmasks[:, :, :]), w=['d_msk'], dma=True)
        gain = sbs(esD, "d_gain", [128, 128], F32)
        P.op('sp', lambda e: e.dma_start(out=gain[:], in_=dn_gain[:, :]), w=['d_gain'], dma=True)
        tokS = sbs(esD, "tokS", [128, NT, 48], F32)
        one1 = sbs(esD, "one1", [128, 1], F32)
        P.op('dve', lambda e: e.memset(one1[:], 1.0), w=['one1'])
        eps_l2 = sbs(esD, "eps_l2", [128, 1], F32)
        P.op('dve', lambda e: e.memset(eps_l2[:], EPS), w=['eps_l2'])
        es1 = ExitStack()
        sc = sbs(es1, "d_sc", [8, 4], F32)
        nA = sbs(es1, "d_nA", [8, 2], F32)
        P.op('sp', lambda e: e.dma_start(out=sc[:], in_=dn_sc[:, :]), w=['d_sc'], dma=True)
        P.op('act', lambda e: e.activation(out=nA[:], in_=sc[:, 0:2], func=AF.Exp), r=['d_sc'], w=['d_nA'])
        P.op('dve', lambda e: e.tensor_scalar(out=nA[:], in0=nA[:], scalar1=-1.0, scalar2=None, op0=ALU.mult), r=['d_nA'], w=['d_nA'])
        rows = {}
        ra = sbs(es1, "d_ra", [8, S], F32)
        rb = sbs(es1, "d_rb", [8, S], F32)
        rc = sbs(es1, "d_rc", [8, S], F32)
        for d in range(2):
            beta = sbs(es1, f"d_beta{d}", [8, S], F32)
            nbeta = sbs(es1, f"d_nbeta{d}", [8, S], F32)
            gc = sbs(es1, f"d_gc{d}", [8, S], F32)
            rows[d] = (beta, nbeta, gc)
            P.op('sp', lambda e: e.dma_start(out=ra[:], in_=baT[d * 8:(d + 1) * 8, :]), r=[('scr', 'dst_ba')], w=['d_ra'], dma=True)
            P.op('act', lambda e: e.activation(out=beta[:], in_=ra[:], func=AF.Sigmoid), r=['d_ra'], w=[f'd_beta{d}'])
            P.op('dve', lambda e: e.tensor_scalar(out=nbeta[:], in0=beta[:], scalar1=-1.0, scalar2=None, op0=ALU.mult), r=[f'd_beta{d}'], w=[f'd_nbeta{d}'])
            P.op('sp', lambda e: e.dma_start(out=ra[:], in_=baT[16 + d * 8:16 + (d + 1) * 8, :]), r=[('scr', 'dst_ba')], w=['d_ra'], dma=True)
            P.op('dve', lambda e: e.tensor_scalar(out=ra[:], in0=ra[:], scalar1=sc[:, 2 + d:3 + d], scalar2=None, op0=ALU.add), r=['d_ra', 'd_sc'], w=['d_ra'])
            P.op('act', lambda e: e.activation(out=rb[:], in_=ra[:], func=AF.Abs), r=['d_ra'], w=['d_rb'])
            P.op('act', lambda e: e.activation(out=rb[:], in_=rb[:], func=AF.Exp, scale=-1.0), r=['d_rb'], w=['d_rb'])
            P.op('act', lambda e: e.activation(out=rb[:], in_=rb[:], func=AF.Ln, bias=one1[0:8, :], scale=1.0), r=['d_rb', 'one1'], w=['d_rb'])
            P.op('dve', lambda e: e.scalar_tensor_tensor(out=rc[:], in0=ra[:], scalar=0.0, in1=rb[:], op0=ALU.max, op1=ALU.add), r=['d_ra', 'd_rb'], w=['d_rc'])
            P.op('dve', lambda e: e.tensor_scalar(out=rc[:], in0=rc[:], scalar1=nA[:, d:d + 1], scalar2=None, op0=ALU.mult), r=['d_rc', 'd_nA'], w=['d_rc'])
            cur, curk, nxt, nxtk = rc, 'd_rc', gc, f'd_gc{d}'
            for sft in (1, 2, 4, 8, 16, 32, 64):
                c3 = cur[:].rearrange("p (t n) -> p t n", n=128)
                n3 = nxt[:].rearrange("p (t n) -> p t n", n=128)
                P.op('act', lambda e: e.copy(out=nxt[:], in_=cur[:]), r=[curk], w=[nxtk])
                if d == 0:
                    P.op('dve', lambda e: e.tensor_tensor(out=n3[:, :, sft:], in0=c3[:, :, sft:], in1=c3[:, :, :128 - sft], op=ALU.add), r=[curk], w=[nxtk])
                else:
                    P.op('dve', lambda e: e.tensor_tensor(out=n3[:, :, :128 - sft], in0=c3[:, :, :128 - sft], in1=c3[:, :, sft:], op=ALU.add), r=[curk], w=[nxtk])
                cur, curk, nxt, nxtk = nxt, nxtk, cur, curk
            if cur is not gc:
                P.op('act', lambda e: e.copy(out=gc[:], in_=cur[:]), r=[curk], w=[f'd_gc{d}'])
        for t in range(NT):
            for d in range(2):
                for j in range(3):
                    src = rows[d][j]
                    col = d * 24 + j * 8
                    P.op('pe', lambda e: e.transpose(out=pG[:, col:col + 8], in_=src[:, t * 128:(t + 1) * 128], identity=ident[0:8, 0:8]),
                         r=[f'd_beta{d}', f'd_nbeta{d}', f'd_gc{d}', 'ident'], w=[kG])
            P.op('act', lambda e: e.copy(out=tokS[:, t, :], in_=pG[:, 0:48]), r=[kG], w=['tokS'])
        P.barrier()
        stop(1)
        es1.close()
        lm = sbs(esD, "d_lm", [128, 5, 4 * 128], F32)
        for j5 in range(5):
            P.op('sp', lambda e: e.dma_start(out=lm[:, j5, :], in_=dn_lmask[:, j5, :]), w=['d_lm'], dma=True)
        QKV = [sbs(esD, f"d_qkv{i}", [128, S], BF16) for i in range(3)]
        Ust = sbs(esD, "d_U", [128, NT, 2, 128], BF16)
        WTst = sbs(esD, "d_WT", [128, NT, 2, 128], BF16)
        ITst = sbs(esD, "d_IT", [128, NT, 2, 128], BF16)
        QDst = sbs(esD, "d_QD", [128, NT, 2, 128], BF16)
        KSst = sbs(esD, "d_KS", [128, NT, 2, 128], BF16)
        egl = sbs(esD, "d_egl", [128, NT, 2], F32)
        Oacc = sbs(esD, "d_Oacc", [128, NT, 128], F32)
        zsh = sbs(esD, "d_zsh", [128, NT, 128], BF16)
        oTd = sbs(esD, "d_oTd", [128, S], BF16)
        S32 = [sbs(esD, f"d_S32{d}", [128, 128], F32) for d in range(2)]
        Sbf = [sbs(esD, f"d_Sbf{d}", [128, 128], BF16) for d in range(2)]
        Vn = [sbs(esD, f"d_Vn{d}", [128, 128], BF16) for d in range(2)]
        ost = sbs(esD, "d_ost", [128, 8], F32)
        on = sbs(esD, "d_on", [128, 128], F32)
        onb = sbs(esD, "d_onb", [128, 128], BF16)
        G = 4
        QSC = float(128 ** -0.5)
        for h in dn_heads:
            esC = ExitStack()
            xpad = sbs(esC, f"d_xpad_{h}", [128, S + 4], F32)
            acc = sbs(esC, f"d_acc_{h}", [128, S], F32)
            cw = sbs(esC, f"d_cw_{h}", [128, 5], F32)
            rst = sbs(esC, f"d_rst_{h}", [128, 512], F32)
            sqb = sbs(esC, f"d_sqb_{h}", [128, 512], BF16)
            P.op('dve', lambda e: e.memset(xpad[:, 0:2], 0.0), w=['d_xpad'])
            P.op('dve', lambda e: e.memset(xpad[:, S + 2:S + 4], 0.0), w=['d_xpad'])
            for ci in range(3):
                ch = ci * 8 + h
                P.op('sp', lambda e: e.dma_start(out=cw[:], in_=conv_wT[ch * 128:(ch + 1) * 128, :]), w=['d_cw'], dma=True)
                P.op('pool', lambda e: e.dma_start(out=xpad[:, 2:S + 2], in_=qkvT[ch, :, :]), r=[('scr', 'dst_qkv')], w=['d_xpad'], dma=True)
                eng = 'dve'
                P.op(eng, lambda e: e.tensor_scalar(out=acc[:], in0=xpad[:, 0:S], scalar1=cw[:, 0:1], scalar2=None, op0=ALU.mult), r=['d_xpad', 'd_cw'], w=['d_acc'])
                for j in range(1, 5):
                    P.op(eng, lambda e: e.scalar_tensor_tensor(out=acc[:], in0=xpad[:, j:j + S], scalar=cw[:, j:j + 1], in1=acc[:], op0=ALU.mult, op1=ALU.add),
                         r=['d_xpad', 'd_cw', 'd_acc'], w=['d_acc'])
                P.op('act', lambda e: e.activation(out=acc[:], in_=acc[:], func=AF.Silu), r=['d_acc'], w=['d_acc'])
                if ci == 2:
                    P.op('dve', lambda e: e.tensor_copy(out=QKV[2][:], in_=acc[:]), r=['d_acc'], w=['d_qkv2'])
                else:
                    for g in range(8):
                        gs_ = slice(g * 512, (g + 1) * 512)
                        P.op('act', lambda e: e.activation(out=sqb[:], in_=acc[:, gs_], func=AF.Square), r=['d_acc'], w=['d_sqb'])
                        P.op('pe', lambda e: e.matmul(pM[:], lhsT=onesb[:], rhs=sqb[:], start=True, stop=True), r=['d_onesb', 'd_sqb'], w=[kM])
                        P.op('act', lambda e: e.activation(out=rst[:], in_=pM[:], func=AF.Sqrt, bias=eps_l2[:], scale=1.0), r=[kM, 'eps_l2'], w=['d_rst'])
                        P.op('dve', lambda e: e.reciprocal(out=rst[:], in_=rst[:]), r=['d_rst'], w=['d_rst'])
                        P.op('dve', lambda e: e.scalar_tensor_tensor(out=QKV[ci][:, gs_], in0=acc[:, gs_], scalar=(QSC if ci == 0 else 1.0), in1=rst[:], op0=ALU.mult, op1=ALU.mult),
                             r=['d_acc', 'd_rst'], w=[f'd_qkv{ci}'])
            Qt, Kt, Vch = QKV
            stop(2)
            P.barrier()
            esC.close()
            esW = ExitStack()
            Ktok = sbs(esW, f"d_Ktok_{h}", [128, 2, 128], BF16)
            Vtok = sbs(esW, f"d_Vtok_{h}", [128, 2, 128], BF16)
            Dg = sbs(esW, f"d_Dg_{h}", [128, G, 128], F32)
            tA = sbs(esW, f"d_tA_{h}", [128, G, 128], F32)
            tI = sbs(esW, f"d_tI_{h}", [128, G, 128], F32)
            EGB = sbs(esW, f"d_EGB_{h}", [128, G, 128], F32)
            A32 = sbs(esW, f"d_A32_{h}", [128, G, 128], F32)
            AT32 = sbs(esW, f"d_AT32_{h}", [128, G, 128], F32)
            ZY = sbs(esW, f"d_ZY_{h}", [128, G, 2, 128], F32)
            ZTYT = sbs(esW, f"d_ZTYT_{h}", [128, G, 2, 128], F32)
            Lb = sbs(esW, f"d_Lb_{h}", [128, G, 128], BF16)
            LTb = sbs(esW, f"d_LTb_{h}", [128, G, 128], BF16)
            Qb = sbs(esW, f"d_Qb_{h}", [128, G, 128], BF16)
            Rb = sbs(esW, f"d_Rb_{h}", [128, G, 128], BF16)
            TT = sbs(esW, f"d_TT_{h}", [128, G, 128], BF16)
            Tb = sbs(esW, f"d_Tb_{h}", [128, G, 128], BF16)
            ZYb = sbs(esW, f"d_ZYb_{h}", [128, G, 2, 128], BF16)
            ZTYTb = sbs(esW, f"d_ZTYTb_{h}", [128, G, 2, 128], BF16)
            Kbe = sbs(esW, f"d_Kbe_{h}", [128, G, 128], BF16)
            Vb = sbs(esW, f"d_Vb_{h}", [128, G, 128], BF16)
            egc = sbs(esW, f"d_egc_{h}", [128, G], F32)
            ebh = sbs(esW, f"d_ebh_{h}", [128, NT, 2], F32)
            for d_ in range(2):
                P.op('act', lambda e: e.activation(out=ebh[:, :, d_], in_=tokS[:, :, d_ * 24 + 16 + h], func=AF.Exp), r=['tokS'], w=['d_ebh'])
                P.op('dve', lambda e: e.tensor_tensor(out=ebh[:, :, d_], in0=ebh[:, :, d_], in1=tokS[:, :, d_ * 24 + h], op=ALU.mult), r=['d_ebh', 'tokS'], w=['d_ebh'])
            zs_v = zs[:, h * 128:(h + 1) * 128].rearrange("(t p) c -> p t c", p=128)
            for q4 in range(8):
                P.op('sp', lambda e: e.dma_start(out=zsh[:, q4 * 4:(q4 + 1) * 4, :], in_=zs_v[:, q4 * 4:(q4 + 1) * 4, :]),
                     r=[('scr', 'zs', t_, b_) for t_ in range(q4 * 4, q4 * 4 + 4) for b_ in range(2)], w=['d_zsh'], dma=True)
            for t0 in range(0, NT, 2):
                units = [(ti, d) for ti in range(2) for d in range(2)]
                fl = []
                for ti in range(2):
                    ts_ = slice((t0 + ti) * 128, (t0 + ti + 1) * 128)
                    fl.append(lambda e, ti=ti, ts_=ts_: e.transpose(out=psT[0][:, ti * 256:ti * 256 + 128], in_=Kt[:, ts_], identity=identb[:]))
                    fl.append(lambda e, ti=ti, ts_=ts_: e.transpose(out=psT[0][:, ti * 256 + 128:ti * 256 + 256], in_=Vch[:, ts_], identity=identb[:]))
                    fl.append(lambda e, ti=ti, ts_=ts_: e.matmul(pM[:, ti * 256:ti * 256 + 128], lhsT=Kt[:, ts_], rhs=Kt[:, ts_], start=True, stop=True))
                    fl.append(lambda e, ti=ti, ts_=ts_: e.matmul(pM[:, ti * 256 + 128:ti * 256 + 256], lhsT=Kt[:, ts_], rhs=Qt[:, ts_], start=True, stop=True))
                P.pe_group(fl, r=['d_qkv0', 'd_qkv1', 'd_qkv2', 'identb'], w=['psT0', kM])
                pT4 = psT[0][:].rearrange("p (a b n) -> p a b n", a=2, b=2)
                P.op('act', lambda e: e.copy(out=Ktok[:], in_=pT4[:, :, 0, :]), r=['psT0'], w=['d_Ktok'])
                P.op('act', lambda e: e.copy(out=Vtok[:], in_=pT4[:, :, 1, :]), r=['psT0'], w=['d_Vtok'])
                stop(31)
                for u, (ti, d) in enumerate(units):
                    t = t0 + ti
                    gcol = tokS[:, t, d * 24 + 16 + h:d * 24 + 17 + h]
                    P.op('dve', lambda e: e.tensor_scalar(out=Dg[:, u, :], in0=ident[:], scalar1=gcol, scalar2=None, op0=ALU.mult), r=['ident', 'tokS'], w=[('d_Dg', u)])
                P.pe_group([(lambda e, u=u: e.matmul(pG[:, u * 128:(u + 1) * 128], lhsT=ones_f[:], rhs=Dg[:, u, :], start=True, stop=True)) for u in range(G)],
                           r=['ones_f'] + [('d_Dg', u) for u in range(G)], w=[kG])
                stop(32)
                P.op('act', lambda e: e.copy(out=EGB[:], in_=pG[:].rearrange("p (u n) -> p u n", u=G)), r=[kG], w=['d_GBs'])
                for u, (ti, d) in enumerate(units):
                    t = t0 + ti
                    gcol = tokS[:, t, d * 24 + 16 + h:d * 24 + 17 + h]
                    P.op('dve', lambda e: e.scalar_tensor_tensor(out=tA[:, u, :], in0=EGB[:, u, :], scalar=gcol, in1=msk[:, 2 * d, :], op0=ALU.subtract, op1=ALU.max),
                         r=['d_GBs', 'tokS', 'd_msk'], w=[('d_tA', u)])
                    P.op('dve', lambda e: e.scalar_tensor_tensor(out=tI[:, u, :], in0=EGB[:, u, :], scalar=gcol, in1=msk[:, 2 * d + 1, :], op0=ALU.subtract, op1=ALU.min),
                         r=['d_GBs', 'tokS', 'd_msk'], w=[('d_tI', u)])
                kTA = [('d_tA', u) for u in range(G)]
                kTI = [('d_tI', u) for u in range(G)]
                P.op('act', lambda e: e.activation(out=tA[:], in_=tA[:], func=AF.Exp, scale=-1.0), r=kTA, w=kTA)
                P.op('act', lambda e: e.activation(out=tI[:], in_=tI[:], func=AF.Exp), r=kTI, w=kTI)
                P.op('act', lambda e: e.activation(out=EGB[:], in_=EGB[:], func=AF.Exp), r=['d_GBs'] + kTA + kTI, w=['d_GBs'])
                stop(33)
                for u, (ti, d) in enumerate(units):
                    t = t0 + ti
                    ts_ = slice(t * 128, (t + 1) * 128)
                    bcol = tokS[:, t, d * 24 + h:d * 24 + h + 1]
                    lastc = 127 if d == 0 else 0
                    P.op('dve', lambda e: e.scalar_tensor_tensor(out=A32[:, u, :], in0=pM[:, ti * 256:ti * 256 + 128], scalar=bcol, in1=tA[:, u, :], op0=ALU.mult, op1=ALU.mult),
                         r=[kM, 'tokS', ('d_tA', u)], w=[('d_A32', u)])
                P.pe_group([(lambda e, u=u: e.transpose(out=pX0[:, u * 128:(u + 1) * 128], in_=A32[:, u, :], identity=ident[:])) for u in range(G)],
                           r=[('d_A32', u) for u in range(G)] + ['ident'], w=[kX0])
                for u, (ti, d) in enumerate(units):
                    t = t0 + ti
                    ts_ = slice(t * 128, (t + 1) * 128)
                    bcol = tokS[:, t, d * 24 + h:d * 24 + h + 1]
                    lastc = 127 if d == 0 else 0
                    P.op('dve', lambda e: e.tensor_tensor(out=ITst[:, t, d, :], in0=pM[:, ti * 256 + 128:ti * 256 + 256], in1=tI[:, u, :], op=ALU.mult),
                         r=[kM, ('d_tI', u)], w=[('d_IT', t, d)])
                    P.op('dve', lambda e: e.tensor_tensor(out=QDst[:, t, d, :], in0=Qt[:, ts_], in1=EGB[:, u, :], op=ALU.mult), r=['d_qkv0', 'd_GBs'], w=[('d_QD', t, d)])
                    P.op('act', lambda e: e.copy(out=egl[:, t, d:d + 1], in_=EGB[:, u, lastc:lastc + 1]), r=['d_GBs'], w=[('d_egl', t, d)])
                    P.op('act', lambda e: e.activation(out=KSst[:, t, d, :], in_=Ktok[:, ti, :], func=AF.Copy, scale=tI[:, u, lastc:lastc + 1]),
                         r=['d_Ktok', ('d_tI', u)], w=[('d_KS', t, d)])
                    P.op('act', lambda e: e.activation(out=Kbe[:, u, :], in_=Ktok[:, ti, :], func=AF.Copy, scale=ebh[:, t, d:d + 1]),
                         r=['d_Ktok', 'd_ebh'], w=[('d_Kbe', u)])
                    P.op('act', lambda e: e.activation(out=Vb[:, u, :], in_=Vtok[:, ti, :], func=AF.Copy, scale=bcol), r=['d_Vtok', 'tokS'], w=[('d_Vb', u)])
                stop(34)
                kA = [('d_A32', u) for u in range(G)]
                v3 = lambda p_: p_[:].rearrange("p (u n) -> p u n", u=G)
                lmv = lambda j_: lm[:, j_, :].rearrange("p (u n) -> p u n", u=G)
                P.op('act', lambda e: e.copy(out=AT32[:], in_=v3(pX0)), r=[kX0], w=['d_AT32'])
                P.op('dve', lambda e: e.scalar_tensor_tensor(out=ZY[:, :, 0, :], in0=A32[:], scalar=-1.0, in1=lmv(0), op0=ALU.mult, op1=ALU.mult), r=kA + ['d_lm'], w=['d_ZY'])
                P.op('dve', lambda e: e.scalar_tensor_tensor(out=ZTYT[:, :, 0, :], in0=AT32[:], scalar=-1.0, in1=lmv(0), op0=ALU.mult, op1=ALU.mult), r=['d_AT32', 'd_lm'], w=['d_ZTYT'])
                P.op('pool', lambda e: e.tensor_tensor(out=ZY[:, :, 1, :], in0=ZY[:, :, 0, :], in1=lmv(4), op=ALU.add), r=['d_ZY', 'd_lm'], w=['d_ZY'])
                P.op('dve', lambda e: e.tensor_tensor(out=ZTYT[:, :, 1, :], in0=ZTYT[:, :, 0, :], in1=lmv(4), op=ALU.add), r=['d_ZTYT', 'd_lm'], w=['d_ZTYT'])
                stop(35)
                P.op('act', lambda e: e.copy(out=ZYb[:], in_=ZY[:]), r=['d_ZY'], w=['d_ZYb'])
                P.op('dve', lambda e: e.tensor_copy(out=ZTYTb[:], in_=ZTYT[:]), r=['d_ZTYT'], w=['d_ZTYTb'])
                P.pe_group([f_ for u in range(G) for f_ in (
                    (lambda e, u=u: e.matmul(pX0[:, u * 128:(u + 1) * 128], lhsT=ZTYTb[:, u, 0, :], rhs=ZYb[:, u, 0, :], start=True, stop=True)),
                    (lambda e, u=u: e.matmul(pX1[:, u * 128:(u + 1) * 128], lhsT=ZYb[:, u, 0, :], rhs=ZTYTb[:, u, 0, :], start=True, stop=True)))],
                    r=['d_ZYb', 'd_ZTYTb'], w=[kX0, kX1])
                P.op('act', lambda e: e.copy(out=ZYb[:, :, 0, :], in_=v3(pX0)), r=[kX0], w=['d_ZYb'])
                P.op('dve', lambda e: e.tensor_copy(out=ZTYTb[:, :, 0, :], in_=v3(pX1)), r=[kX1], w=['d_ZTYTb'])
                for lvl in (1, 2):
                    P.pe_group([f_ for u in range(G) for f_ in (
                        (lambda e, u=u: e.matmul((pX0 if u < 2 else pX1)[:, (u % 2) * 256:(u % 2) * 256 + 256], lhsT=ZTYTb[:, u, 0, :], rhs=ZYb[:, u, :, :].rearrange("p c n -> p (c n)"), start=True, stop=True)),
                        (lambda e, u=u: e.matmul((pZT if u < 2 else pU)[:, (u % 2) * 256:(u % 2) * 256 + 256], lhsT=ZYb[:, u, 0, :], rhs=ZTYTb[:, u, :, :].rearrange("p c n -> p (c n)"), start=True, stop=True)))],
                        r=['d_ZYb', 'd_ZTYTb'], w=[kX0, kX1, kZT, kU])
                    for hf, (px, kx, pz, kz) in enumerate(((pX0, kX0, pZT, kZT), (pX1, kX1, pU, kU))):
                        p4 = px[:].rearrange("p (u c n) -> p u c n", u=2, c=2)
                        z4 = pz[:].rearrange("p (u c n) -> p u c n", u=2, c=2)
                        hs2 = slice(hf * 2, hf * 2 + 2)
                        P.op('act', lambda e: e.copy(out=ZYb[:, hs2, 0, :], in_=p4[:, :, 0, :]), r=[kx], w=['d_ZYb'])
                        P.op('dve', lambda e: e.tensor_tensor(out=ZY[:, hs2, 1, :], in0=p4[:, :, 1, :], in1=ZY[:, hs2, 1, :], op=ALU.add), r=[kx, 'd_ZY'], w=['d_ZY'])
                        P.op('act', lambda e: e.copy(out=ZTYTb[:, hs2, 0, :], in_=z4[:, :, 0, :]), r=[kz], w=['d_ZTYTb'])
                        P.op('dve', lambda e: e.tensor_tensor(out=ZTYT[:, hs2, 1, :], in0=z4[:, :, 1, :], in1=ZTYT[:, hs2, 1, :], op=ALU.add), r=[kz, 'd_ZTYT'], w=['d_ZTYT'])
                    P.op('act', lambda e: e.copy(out=ZYb[:, :, 1, :], in_=ZY[:, :, 1, :]), r=['d_ZY'], w=['d_ZYb'])
                    P.op('dve', lambda e: e.tensor_copy(out=ZTYTb[:, :, 1, :], in_=ZTYT[:, :, 1, :]), r=['d_ZTYT'], w=['d_ZTYTb'])
                P.pe_group([f_ for u in range(G) for f_ in (
                    (lambda e, u=u: e.matmul(pX0[:, u * 128:(u + 1) * 128], lhsT=ZTYTb[:, u, 0, :], rhs=ZYb[:, u, 1, :], start=True, stop=True)),
                    (lambda e, u=u: e.matmul(pX1[:, u * 128:(u + 1) * 128], lhsT=ZYb[:, u, 0, :], rhs=ZTYTb[:, u, 1, :], start=True, stop=True)))],
                    r=['d_ZYb', 'd_ZTYTb'], w=[kX0, kX1])
                P.op('dve', lambda e: e.tensor_tensor(out=ZY[:, :, 1, :], in0=v3(pX0), in1=ZY[:, :, 1, :], op=ALU.add), r=[kX0, 'd_ZY'], w=['d_ZY'])
                P.op('dve', lambda e: e.tensor_tensor(out=ZTYT[:, :, 1, :], in0=v3(pX1), in1=ZTYT[:, :, 1, :], op=ALU.add), r=[kX1, 'd_ZTYT'], w=['d_ZTYT'])
                P.op('act', lambda e: e.copy(out=TT[:], in_=ZTYT[:, :, 1, :]), r=['d_ZTYT'], w=['d_TT'])
                P.op('dve', lambda e: e.tensor_copy(out=Tb[:], in_=ZY[:, :, 1, :]), r=['d_ZY'], w=['d_Tb'])
                for li in range(3):
                    last = (li == 2)
                    P.op('pool', lambda e: e.tensor_tensor(out=Lb[:], in0=A32[:], in1=lmv(1 + li), op=ALU.mult), r=kA + ['d_lm'], w=['d_Lb'])
                    if not last:
                        P.op('dve', lambda e: e.tensor_tensor(out=LTb[:], in0=AT32[:], in1=lmv(1 + li), op=ALU.mult), r=['d_AT32', 'd_lm'], w=['d_LTb'])
                    fl = [(lambda e, u=u: e.matmul(pX1[:, u * 128:(u + 1) * 128], lhsT=Lb[:, u, :], rhs=TT[:, u, :], start=True, stop=True)) for u in range(G)]
                    if not last:
                        fl += [(lambda e, u=u: e.matmul(pX0[:, u * 128:(u + 1) * 128], lhsT=LTb[:, u, :], rhs=Tb[:, u, :], start=True, stop=True)) for u in range(G)]
                    P.pe_group(fl, r=['d_Lb', 'd_TT'] + ([] if last else ['d_LTb', 'd_Tb']), w=[kX1] + ([] if last else [kX0]))
                    P.op('act', lambda e: e.copy(out=Rb[:], in_=v3(pX1)), r=[kX1], w=['d_Rb'])
                    if not last:
                        P.op('dve', lambda e: e.tensor_copy(out=Qb[:], in_=v3(pX0)), r=[kX0], w=['d_Qb'])
                    fl = [(lambda e, u=u: e.matmul(pU[:, u * 128:(u + 1) * 128], lhsT=Tb[:, u, :], rhs=Rb[:, u, :], start=True, stop=True)) for u in range(G)]
                    if not last:
                        fl += [(lambda e, u=u: e.matmul(pZT[:, u * 128:(u + 1) * 128], lhsT=TT[:, u, :], rhs=Qb[:, u, :], start=True, stop=True)) for u in range(G)]
                    P.pe_group(fl, r=['d_Tb', 'd_Rb'] + ([] if last else ['d_TT', 'd_Qb']), w=[kU] + ([] if last else [kZT]))
                    if not last:
                        P.op('dve', lambda e: e.tensor_tensor(out=ZTYT[:, :, 1, :], in0=ZTYT[:, :, 1, :], in1=v3(pU), op=ALU.subtract), r=[kU, 'd_ZTYT'], w=['d_ZTYT'])
                        P.op('dve', lambda e: e.tensor_tensor(out=ZY[:, :, 1, :], in0=ZY[:, :, 1, :], in1=v3(pZT), op=ALU.subtract), r=[kZT, 'd_ZY'], w=['d_ZY'])
                        P.op('act', lambda e: e.copy(out=TT[:], in_=ZTYT[:, :, 1, :]), r=['d_ZTYT'], w=['d_TT'])
                        P.op('act', lambda e: e.copy(out=Tb[:], in_=ZY[:, :, 1, :]), r=['d_ZY'], w=['d_Tb'])
                    else:
                        P.op('dve', lambda e: e.tensor_tensor(out=TT[:], in0=ZTYT[:, :, 1, :], in1=v3(pU), op=ALU.subtract), r=[kU, 'd_ZTYT'], w=['d_TT'])
                stop(36)
                P.pe_group([f_ for u in range(G) for f_ in (
                    (lambda e, u=u: e.matmul(pU[:, u * 128:(u + 1) * 128], lhsT=TT[:, u, :], rhs=Vb[:, u, :], start=True, stop=True)),
                    (lambda e, u=u: e.matmul(pG[:, u * 128:(u + 1) * 128], lhsT=Kbe[:, u, :], rhs=TT[:, u, :], start=True, stop=True)))],
                    r=['d_TT'] + [('d_Vb', u) for u in range(G)] + [('d_Kbe', u) for u in range(G)], w=[kU, kG])
                P.op('act', lambda e: e.copy(out=Ust[:, t0:t0 + 2, :, :].rearrange("p a b n -> p (a b) n"), in_=pU[:].rearrange("p (u n) -> p u n", u=G)), r=[kU], w=[('d_U', t0)])
                P.op('dve', lambda e: e.tensor_copy(out=WTst[:, t0:t0 + 2, :, :].rearrange("p a b n -> p (a b) n"), in_=pG[:].rearrange("p (u n) -> p u n", u=G)), r=[kG], w=[('d_WT', t0)])
                stop(3)
            P.barrier()
            stop(4)
            for d in range(2):
                P.op('dve', lambda e: e.memset(S32[d][:], 0.0), w=[f'd_S32{d}'])
                P.op('dve', lambda e: e.memset(Sbf[d][:], 0.0), w=[f'd_Sbf{d}'])
            for step in range(NT):
                tt = [step, NT - 1 - step]
                bank = [((pM, kM), (pX0, kX0), (pZT, kZT)), ((pG, kG), (pX1, kX1), (pU, kU))]
                for d in range(2):
                    t = tt[d]; t0 = (t // 2) * 2
                    (pW, kW) = bank[d][0]
                    P.op('pe', lambda e: e.matmul(pW[:, 0:128], lhsT=WTst[:, t, d, :], rhs=Sbf[d][:], start=True, stop=True), r=[('d_WT', t0), f'd_Sbf{d}'], w=[kW])
                for d in range(2):
                    t = tt[d]; t0 = (t // 2) * 2
                    (pW, kW), (pO, kO) = bank[d][0], bank[d][1]
                    P.op('dve', lambda e: e.tensor_tensor(out=Vn[d][:], in0=Ust[:, t, d, :], in1=pW[:, 0:128], op=ALU.subtract), r=[('d_U', t0), kW], w=[f'd_Vn{d}'])
                    P.op('pe', lambda e: e.matmul(pO[:, 0:128], lhsT=QDst[:, t, d, :], rhs=Sbf[d][:], start=True, stop=False), r=[('d_QD', t, d), f'd_Sbf{d}'], w=[kO])
                for d in range(2):
                    t = tt[d]
                    (pO, kO), (pD, kD) = bank[d][1], bank[d][2]
                    P.op('pe', lambda e: e.matmul(pO[:, 0:128], lhsT=ITst[:, t, d, :], rhs=Vn[d][:], start=False, stop=True), r=[('d_IT', t, d), f'd_Vn{d}'], w=[kO])
                    P.op('pe', lambda e: e.matmul(pD[:, 0:128], lhsT=KSst[:, t, d, :], rhs=Vn[d][:], start=True, stop=True), r=[('d_KS', t, d), f'd_Vn{d}'], w=[kD])
                for d in range(2):
                    t = tt[d]
                    (pO, kO), (pD, kD) = bank[d][1], bank[d][2]
                    P.op('dve', lambda e: e.scalar_tensor_tensor(out=Sbf[d][:], in0=S32[d][:], scalar=egl[:, t, d:d + 1], in1=pD[:, 0:128], op0=ALU.mult, op1=ALU.add),
                         r=[f'd_S32{d}', ('d_egl', t, d), kD], w=[f'd_Sbf{d}'])
                    P.op('dve', lambda e: e.scalar_tensor_tensor(out=S32[d][:], in0=S32[d][:], scalar=egl[:, t, d:d + 1], in1=pD[:, 0:128], op0=ALU.mult, op1=ALU.add),
                         r=[f'd_S32{d}', ('d_egl', t, d), kD], w=[f'd_S32{d}'])
                    if step < NT // 2:
                        P.op('act', lambda e: e.copy(out=Oacc[:, t, :], in_=pO[:, 0:128]), r=[kO], w=[('d_Oacc', t)])
                    else:
                        P.op('pool', lambda e: e.tensor_tensor(out=Oacc[:, t, :], in0=Oacc[:, t, :], in1=Oacc[:, t, :], op=ALU.add), r=[('d_Oacc', t)], w=[('d_Oacc', t)]) if False else \
                            P.op('dve', lambda e: e.tensor_tensor(out=Oacc[:, t, :], in0=pO[:, 0:128], in1=Oacc[:, t, :], op=ALU.add), r=[kO, ('d_Oacc', t)], w=[('d_Oacc', t)])
            stop(5)
            osq = [sbs(esW, f"d_osq{i_}_{h}", [128, 128], F32) for i_ in range(2)]
            ors = sbs(esW, f"d_ors_{h}", [128, 2, NT], F32)
            ont = [sbs(esW, f"d_ont{i_}_{h}", [128, 128], F32) for i_ in range(4)]
            onbt = [sbs(esW, f"d_onbt{i_}_{h}", [128, 128], BF16) for i_ in range(4)]
            kO_all = [('d_Oacc', t_) for t_ in range(NT)]
            P.op('dve', lambda e: e.memset(ors[:], 0.0), w=['d_ors'])
            for t in range(NT):
                P.op('act', lambda e: e.activation(out=osq[t % 2][:], in_=Oacc[:, t, :], func=AF.Square, accum_out=ors[:, 0, t:t + 1]), r=[('d_Oacc', t), 'd_ors'], w=[f'd_osq{t % 2}', ('d_ors_acc', t)])
            P.op('act', lambda e: e.activation(out=ors[:, 1, :], in_=ors[:, 0, :], func=AF.Sqrt, scale=1.0 / 128, bias=eps_l2[:]), r=['d_ors', 'eps_l2'] + [('d_ors_acc', t_) for t_ in range(NT)], w=['d_ors'])
            P.op('dve', lambda e: e.reciprocal(out=ors[:, 1, :], in_=ors[:, 1, :]), r=['d_ors'], w=['d_ors'])
            for t4 in range(0, NT, 4):
                for j in range(4):
                    t = t4 + j
                    P.op('dve', lambda e: e.scalar_tensor_tensor(out=ont[j][:], in0=Oacc[:, t, :], scalar=ors[:, 1, t:t + 1], in1=gain[:], op0=ALU.mult, op1=ALU.mult),
                         r=[('d_Oacc', t), 'd_ors', 'd_gain'], w=[f'd_ont{j}'])
                    P.op('pool', lambda e: e.tensor_tensor(out=onbt[j][:], in0=ont[j][:], in1=zsh[:, t, :], op=ALU.mult), r=[f'd_ont{j}', 'd_zsh'], w=[f'd_onbt{j}'])
                    P.op('pe', lambda e: e.transpose(out=psT[1][:, j * 128:(j + 1) * 128], in_=onbt[j][:], identity=identb[:]), r=[f'd_onbt{j}', 'identb'], w=['psT1'])
                P.op('act', lambda e: e.copy(out=oTd[:, t4 * 128:(t4 + 4) * 128], in_=psT[1][:]), r=['psT1'], w=['d_oTd'])
            fin.append(P.op('sp', lambda e: e.dma_start(out=oT_dn[h, :, :], in_=oTd[:]), r=['d_oTd'], w=[('scr', 'oT_dn', h)], dma=True))
            P.barrier()
            esW.close()
        P.barrier()
        esD.close()

    def phase_MG_MOE():
        esP = ExitStack()
        affTM = sbs(esP, "affTM", [128, NT, 16], F32)
        posm = sbs(esP, "posm", [128, NT, 16], F32)
        iot = sbs(esP, "iot", [128, 512], F32)
        bcs = sbs(esP, "bcs", [128, 4, D], F32)
        gfin = sbs(esP, "gfin", [128, D], F32)
        onesb = sbs(esP, "m_onesb", [128, 128], BF16)
        trib = sbs(esP, "trib", [128, 128], BF16)
        st2 = sbs(esP, "st2", [128, 8], F32)
        P.op('dve', lambda e: e.memset(onesb[:], 1.0), w=['m_onesb'])
        P.op('sp', lambda e: e.dma_start(out=iot[:], in_=iota512[:, :]), w=['iot'], dma=True)
        P.op('sp', lambda e: e.dma_start(out=gfin[:], in_=g_final[:, :]), w=['gfin'], dma=True)
        esH = ExitStack()
        h2b = sbs(esH, "h2b", [128, NT, D], BF16)
        esT = ExitStack()
        affT = sbs(esT, "affT", [16, S], F32)
        esG = ExitStack()
        es1 = ExitStack()
        mrow = sbs(es1, "g_mrow", [1, 4 * D], F32)
        trif = sbs(es1, "g_trif", [128, 128], F32)
        gff = sbs(es1, "g_gff", [128, D], F32)
        P.op('sp', lambda e: e.dma_start(out=mrow[:], in_=modrow[:, 2 * D:6 * D]), r=['mod'], w=['g_mrow'], dma=True)
        P.op('sp', lambda e: e.dma_start(out=trif[:], in_=tri_in[:, :]), w=['g_trif'], dma=True)
        P.op('dve', lambda e: e.tensor_copy(out=trib[:], in_=trif[:]), r=['g_trif'], w=['trib'])
        P.op('sp', lambda e: e.dma_start(out=gff[:], in_=g_ffn[:, :]), w=['g_gff'], dma=True)
        for j in range(8):
            src = j // 2
            dsti = {0: 0, 1: 2, 2: 1, 3: 3}[src]
            pt, pk = next_psA()
            P.op('pe', lambda e: e.matmul(pt[:], lhsT=ones_f[0:1, :], rhs=mrow[:, j * 512:(j + 1) * 512], start=True, stop=True), r=['ones_f', 'g_mrow'], w=[pk])
            P.op('act', lambda e: e.copy(out=bcs[:, dsti, (j % 2) * 512:(j % 2 + 1) * 512], in_=pt[:]), r=[pk], w=['bcs'])
        P.op('dve', lambda e: e.scalar_tensor_tensor(out=bcs[:, 1, :], in0=bcs[:, 1, :], scalar=1.0, in1=gff[:], op0=ALU.add, op1=ALU.mult), r=['bcs', 'g_gff'], w=['bcs'])
        P.barrier()
        es1.close()
        wod = sbs(esG, "g_wod", [128, 8, D], BF16)
        wom = sbs(esG, "g_wom", [128, 8, D], BF16)
        wou = sbs(esG, "g_wou", [128, 8, D], BF16)
        wr = sbs(esG, "g_wr", [128, 8, 16], F32)
        for wt_, src_, k_ in ((wod, w_o_dn, 'g_wod'), (wom, w_o_mla, 'g_wom'), (wou, w_out, 'g_wou')):
            v = src_.rearrange("(k p) n -> p k n", p=128)
            for kk in range(0, 8, 2):
                P.op('pool', lambda e: e.dma_start(out=wt_[:, kk:kk + 2, :], in_=v[:, kk:kk + 2, :]), w=[k_], dma=True)
        P.op('sp', lambda e: e.dma_start(out=wr[:], in_=w_router.rearrange("(k p) n -> p k n", p=128)), w=['g_wr'], dma=True)
        odn = [sbs(esG, f"g_odn{i}", [128, 8, 128], BF16) for i in range(2)]
        oml = [sbs(esG, f"g_oml{i}", [128, 8, 128], BF16) for i in range(2)]
        gst = [sbs(esG, f"g_gst{i}", [128, 2 * D], BF16) for i in range(2)]
        xt0 = sbs(esG, "g_xt0", [128, D], F32)
        xt = [xt0, xt0]
        m1 = sbs(esG, "g_m1", [128, D], F32)
        m2 = sbs(esG, "g_m2", [128, D], F32)
        mb = sbs(esG, "g_mb", [128, D], BF16)
        mT = sbs(esG, "g_mT", [128, 8, 128], BF16)
        x1t = [sbs(esG, f"g_x1t{i}", [128, D], F32) for i in range(2)]
        h2f = sbs(esG, "g_h2f", [128, D], F32)
        h2T = sbs(esG, "g_h2T", [128, 8, 128], F32)
        lg = sbs(esG, "g_lg", [128, 16], F32)
        for t in range(NT):
            i = t % 2
            ts_ = slice(t * 128, (t + 1) * 128)
            P.op('sp', lambda e: e.dma_start(out=odn[i][:], in_=oT_dn[:, :, ts_].rearrange("h p n -> p h n")), r=[('scr', 'oT_dn', h_) for h_ in range(8)], w=[f'g_odn{i}'], dma=True)
            P.op('sp', lambda e: e.dma_start(out=oml[i][:], in_=oT_mla[:, :, ts_].rearrange("h p n -> p h n")), r=[('scr', 'oT_mla', h_) for h_ in range(8)], w=[f'g_oml{i}'], dma=True)
            P.op('sp', lambda e: e.dma_start(out=gst[i][:], in_=gs[ts_, :]), r=[('scr', 'gs', t, b_) for b_ in range(4)], w=[f'g_gst{i}'], dma=True)
            P.op('sp', lambda e: e.dma_start(out=xt[i][:], in_=x[ts_, :]), w=['g_xt0'], dma=True)
            for br, (o_, ok_, w_, wk_) in enumerate(((odn[i], f'g_odn{i}', wod, 'g_wod'), (oml[i], f'g_oml{i}', wom, 'g_wom'))):
                for hc in range(2):
                    pt, pk = psA[br * 2 + hc], f'psA{br * 2 + hc}'
                    P.pe_group([(lambda e, k=k: e.matmul(pt[:], lhsT=o_[:, k, :], rhs=w_[:, k, hc * 512:(hc + 1) * 512], start=(k == 0), stop=(k == 7))) for k in range(8)], r=[ok_, wk_], w=[pk])
            for hc in range(2):
                hs_ = slice(hc * 512, (hc + 1) * 512)
                P.op('dve', lambda e: e.tensor_tensor(out=m1[:, hs_], in0=psA[hc][:], in1=gst[i][:, hs_], op=ALU.mult), r=[f'psA{hc}', f'g_gst{i}'], w=['g_m1'])
                P.op('dve', lambda e: e.tensor_tensor(out=m2[:, hs_], in0=psA[2 + hc][:], in1=gst[i][:, D + hc * 512:D + (hc + 1) * 512], op=ALU.mult), r=[f'psA{2 + hc}', f'g_gst{i}'], w=['g_m2'])
            P.op('pool', lambda e: e.tensor_tensor(out=mb[:], in0=m1[:], in1=m2[:], op=ALU.add), r=['g_m1', 'g_m2'], w=['g_mb'])
            for half in range(2):
                P.pe_group([(lambda e, k4=k4: e.transpose(out=psT[half][:, k4 * 128:(k4 + 1) * 128], in_=mb[:, (half * 4 + k4) * 128:(half * 4 + k4 + 1) * 128], identity=identb[:])) for k4 in range(4)], r=['g_mb', 'identb'], w=[f'psT{half}'])
                P.op('act', lambda e: e.copy(out=mT[:, half * 4:(half + 1) * 4, :], in_=psT[half][:].rearrange("p (k n) -> p k n", k=4)), r=[f'psT{half}'], w=['g_mT'])
            for hc in range(2):
                hs_ = slice(hc * 512, (hc + 1) * 512)
                P.pe_group([(lambda e, k=k: e.matmul(psS[hc][:], lhsT=mT[:, k, :], rhs=wou[:, k, hs_], start=(k == 0), stop=(k == 7))) for k in range(8)], r=['g_mT', 'g_wou'], w=[f'psS{hc}'])
                P.op('dve', lambda e: e.tensor_tensor(out=x1t[i][:, hs_], in0=psS[hc][:], in1=bcs[:, 0, hs_], op=ALU.mult), r=[f'psS{hc}', 'bcs'], w=[f'g_x1t{i}'])
            P.op('pool', lambda e: e.tensor_tensor(out=x1t[i][:], in0=x1t[i][:], in1=xt[i][:], op=ALU.add), r=[f'g_x1t{i}', 'g_xt0'], w=[f'g_x1t{i}'])
            fin.append(P.op('sp', lambda e: e.dma_start(out=x1s[ts_, :], in_=x1t[i][:]), r=[f'g_x1t{i}'], w=[('scr', 'x1s', t)], dma=True))
            P.op('dve', lambda e: e.memset(st2[:], 0.0), w=['st2'])
            P.op('act', lambda e: e.activation(out=m1[:], in_=x1t[i][:], func=AF.Square, accum_out=st2[:, 0:1]), r=[f'g_x1t{i}'], w=['g_m1', 'st2'])
            P.op('act', lambda e: e.activation(out=st2[:, 1:2], in_=st2[:, 0:1], func=AF.Sqrt, scale=1.0 / D, bias=epsb[:]), r=['st2', 'epsb'], w=['st2'])
            P.op('dve', lambda e: e.reciprocal(out=st2[:, 2:3], in_=st2[:, 1:2]), r=['st2'], w=['st2'])
            P.op('dve', lambda e: e.scalar_tensor_tensor(out=h2f[:], in0=x1t[i][:], scalar=st2[:, 2:3], in1=bcs[:, 1, :], op0=ALU.mult, op1=ALU.mult), r=[f'g_x1t{i}', 'st2', 'bcs'], w=['g_h2f'])
            P.op('pool', lambda e: e.tensor_tensor(out=h2f[:], in0=h2f[:], in1=bcs[:, 2, :], op=ALU.add), r=['g_h2f', 'bcs'], w=['g_h2f'])
            P.op('act', lambda e: e.copy(out=h2b[:, t, :], in_=h2f[:]), r=['g_h2f'], w=[('h2b', t)])
            for half in range(2):
                P.pe_group([(lambda e, k4=k4: e.transpose(out=psA[half][:, k4 * 128:(k4 + 1) * 128], in_=h2f[:, (half * 4 + k4) * 128:(half * 4 + k4 + 1) * 128], identity=ident[:])) for k4 in range(4)], r=['g_h2f', 'ident'], w=[f'psA{half}'])
                P.op('act', lambda e: e.copy(out=h2T[:, half * 4:(half + 1) * 4, :], in_=psA[half][:].rearrange("p (k n) -> p k n", k=4)), r=[f'psA{half}'], w=['g_h2T'])
            P.pe_group([(lambda e, k=k: e.matmul(psA[2][:, 0:16], lhsT=h2T[:, k, :], rhs=wr[:, k, :], start=(k == 0), stop=(k == 7))) for k in range(8)], r=['g_h2T', 'g_wr'], w=['psA2'])
            P.op('dve', lambda e: e.tensor_reduce(out=st2[:, 3:4], in_=psA[2][:, 0:16], axis=AX.X, op=ALU.max), r=['psA2'], w=['st2'])
            P.op('dve', lambda e: e.tensor_scalar(out=st2[:, 4:5], in0=st2[:, 3:4], scalar1=-1.0, scalar2=None, op0=ALU.mult), r=['st2'], w=['st2'])
            P.op('dve', lambda e: e.memset(st2[:, 5:6], 0.0), r=['st2'], w=['st2'])
            P.op('act', lambda e: e.activation(out=lg[:], in_=psA[2][:, 0:16], func=AF.Exp, bias=st2[:, 4:5], scale=1.0, accum_out=st2[:, 5:6]), r=['psA2', 'st2'], w=['g_lg', 'st2'])
            P.op('dve', lambda e: e.reciprocal(out=st2[:, 6:7], in_=st2[:, 5:6]), r=['st2'], w=['st2'])
            P.op('dve', lambda e: e.tensor_scalar(out=affTM[:, t, :], in0=lg[:], scalar1=st2[:, 6:7], scalar2=None, op0=ALU.mult), r=['g_lg', 'st2'], w=[('affTM', t)])
            P.op('pe', lambda e: e.transpose(out=psA[3][0:16, 0:128], in_=affTM[:, t, :], identity=ident[:]), r=[('affTM', t), 'ident'], w=['psA3'])
            P.op('act', lambda e: e.copy(out=affT[:, ts_], in_=psA[3][0:16, 0:128]), r=['psA3'], w=['affT'])
        P.barrier()
        esG.close()
        esE = ExitStack()
        es1 = ExitStack()
        cmpb = sbs(es1, "e_cmp", [16, S], F32)
        bis = sbs(es1, "e_bis", [16, 8], F32)
        maskT = sbs(es1, "e_maskT", [16, S], F32)
        P.op('dve', lambda e: e.memset(bis[:], 0.0), w=['e_bis'])
        P.op('dve', lambda e: e.memset(bis[:, 1:2], 1.0), r=['e_bis'], w=['e_bis'])
        for it in range(32):
            P.op('dve', lambda e: e.tensor_tensor(out=bis[:, 2:3], in0=bis[:, 0:1], in1=bis[:, 1:2], op=ALU.add), r=['e_bis'], w=['e_bis'])
            P.op('dve', lambda e: e.tensor_scalar(out=bis[:, 2:3], in0=bis[:, 2:3], scalar1=0.5, scalar2=None, op0=ALU.mult), r=['e_bis'], w=['e_bis'])
            P.op('dve', lambda e: e.tensor_scalar(out=cmpb[:], in0=affT[:], scalar1=bis[:, 2:3], scalar2=None, op0=ALU.is_ge), r=['affT', 'e_bis'], w=['e_cmp'])
            P.op('dve', lambda e: e.tensor_reduce(out=bis[:, 3:4], in_=cmpb[:], axis=AX.X, op=ALU.add), r=['e_cmp'], w=['e_bis'])
            P.op('dve', lambda e: e.tensor_scalar(out=bis[:, 4:5], in0=bis[:, 3:4], scalar1=511.5, scalar2=None, op0=ALU.is_ge), r=['e_bis'], w=['e_bis'])
            P.op('dve', lambda e: e.tensor_tensor(out=bis[:, 5:6], in0=bis[:, 2:3], in1=bis[:, 0:1], op=ALU.subtract), r=['e_bis'], w=['e_bis'])
            P.op('dve', lambda e: e.tensor_tensor(out=bis[:, 6:7], in0=bis[:, 1:2], in1=bis[:, 2:3], op=ALU.subtract), r=['e_bis'], w=['e_bis'])
            P.op('dve', lambda e: e.scalar_tensor_tensor(out=bis[:, 0:1], in0=bis[:, 5:6], scalar=bis[:, 4:5], in1=bis[:, 0:1], op0=ALU.mult, op1=ALU.add), r=['e_bis'], w=['e_bis'])
            P.op('dve', lambda e: e.scalar_tensor_tensor(out=bis[:, 1:2], in0=bis[:, 6:7], scalar=bis[:, 4:5], in1=bis[:, 2:3], op0=ALU.mult, op1=ALU.add), r=['e_bis'], w=['e_bis'])
        P.op('dve', lambda e: e.tensor_scalar(out=maskT[:], in0=affT[:], scalar1=bis[:, 0:1], scalar2=None, op0=ALU.is_ge), r=['affT', 'e_bis'], w=['e_maskT'])
        mk32 = sbs(es1, "e_mk32", [128, 16], F32)
        mkb = sbs(es1, "e_mkb", [128, 16], BF16)
        base = sbs(es1, "e_base", [128, 16], F32)
        ptmp = sbs(es1, "e_ptmp", [128, 16], F32)
        P.op('dve', lambda e: e.memset(base[:], 0.0), w=['e_base'])
        for t in range(NT):
            ts_ = slice(t * 128, (t + 1) * 128)
            P.op('pe', lambda e: e.transpose(out=psA[0][:, 0:16], in_=maskT[:, ts_], identity=ident[0:16, 0:16]), r=['e_maskT', 'ident'], w=['psA0'])
            P.op('act', lambda e: e.copy(out=mk32[:], in_=psA[0][:, 0:16]), r=['psA0'], w=['e_mk32'])
            P.op('dve', lambda e: e.tensor_copy(out=mkb[:], in_=mk32[:]), r=['e_mk32'], w=['e_mkb'])
            P.op('pe', lambda e: e.matmul(psA[1][:, 0:16], lhsT=trib[:], rhs=mkb[:], start=True, stop=True), r=['trib', 'e_mkb'], w=['psA1'])
            P.op('pe', lambda e: e.matmul(psA[2][:, 0:16], lhsT=onesb[:], rhs=mkb[:], start=True, stop=True), r=['m_onesb', 'e_mkb'], w=['psA2'])
            P.op('dve', lambda e: e.tensor_tensor(out=ptmp[:], in0=psA[1][:, 0:16], in1=base[:], op=ALU.add), r=['psA1', 'e_base'], w=['e_ptmp'])
            P.op('dve', lambda e: e.scalar_tensor_tensor(out=ptmp[:], in0=ptmp[:], scalar=1.0, in1=mk32[:], op0=ALU.add, op1=ALU.mult), r=['e_ptmp', 'e_mk32'], w=['e_ptmp'])
            P.op('dve', lambda e: e.tensor_scalar(out=posm[:, t, :], in0=ptmp[:], scalar1=-1.0, scalar2=None, op0=ALU.add), r=['e_ptmp'], w=['posm'])
            P.op('dve', lambda e: e.tensor_tensor(out=base[:], in0=psA[2][:, 0:16], in1=base[:], op=ALU.add), r=['psA2', 'e_base'], w=['e_base'])
        P.barrier()
        es1.close()
        esT.close()
        wg = sbs(esE, "e_wg", [128, 8, D], BF16)
        wu = sbs(esE, "e_wu", [128, 8, D], BF16)
        wd = sbs(esE, "e_wd", [128, 8, D], BF16)
        Sel = sbs(esE, "e_Sel", [128, NT, 512], BF16)
        xeT = sbs(esE, "e_xeT", [128, 8, 512], BF16)
        hid = sbs(esE, "e_hid", [128, 8, 512], BF16)
        sg0 = sbs(esE, "e_sg0", [128, 512], F32)
        sg = [sg0, sg0]
        yeb = sbs(esE, "e_yeb", [128, 4, D], BF16)
        stgw = [sbs(esE, f"e_stg{i}", [128, D], F32) for i in range(2)]
        stg_n = [0]
        for ex in range(16):
            for wt_, src_, k_ in ((wg, w_gate, 'e_wg'), (wu, w_up, 'e_wu'), (wd, w_down, 'e_wd')):
                v = src_[ex].rearrange("(k p) n -> p k n", p=128)
                for kk in range(8):
                    if kk % 2 == 0:
                        P.op('pool', lambda e: e.dma_start(out=wt_[:, kk, :], in_=v[:, kk, :]), w=[k_], dma=True)
                    else:
                        j = stg_n[0] % 2
                        stg_n[0] += 1
                        P.op('sp', lambda e: e.dma_start(out=stgw[j][:], in_=v[:, kk, :]), w=[f'e_stg{j}'], dma=True)
                        P.op('act', lambda e: e.copy(out=wt_[:, kk, :], in_=stgw[j][:]), r=[f'e_stg{j}'], w=[k_])
            for t in range(NT):
                P.op('dve', lambda e: e.tensor_scalar(out=Sel[:, t, :], in0=iot[:], scalar1=posm[:, t, ex:ex + 1], scalar2=None, op0=ALU.is_equal), r=['iot', 'posm'], w=[('e_Sel', t)])
            for k in range(8):
                pt, pk = psA[k % 4], f'psA{k % 4}'
                P.pe_group([(lambda e, t=t: e.matmul(pt[:], lhsT=h2b[:, t, k * 128:(k + 1) * 128], rhs=Sel[:, t, :], start=(t == 0), stop=(t == NT - 1))) for t in range(NT)],
                           r=[('h2b', t) for t in range(NT)] + [('e_Sel', t) for t in range(NT)], w=[pk])
                if k % 2 == 0:
                    P.op('act', lambda e: e.copy(out=xeT[:, k, :], in_=pt[:]), r=[pk], w=['e_xeT'])
                else:
                    P.op('dve', lambda e: e.tensor_copy(out=xeT[:, k, :], in_=pt[:]), r=[pk], w=['e_xeT'])
            for f in range(8):
                j = f % 2
                pg, pgk = psA[j * 2], f'psA{j * 2}'
                pu, puk = psA[j * 2 + 1], f'psA{j * 2 + 1}'
                P.pe_group([(lambda e, k=k: e.matmul(pg[:], lhsT=wg[:, k, f * 128:(f + 1) * 128], rhs=xeT[:, k, :], start=(k == 0), stop=(k == 7))) for k in range(8)], r=['e_wg', 'e_xeT'], w=[pgk])
                P.pe_group([(lambda e, k=k: e.matmul(pu[:], lhsT=wu[:, k, f * 128:(f + 1) * 128], rhs=xeT[:, k, :], start=(k == 0), stop=(k == 7))) for k in range(8)], r=['e_wu', 'e_xeT'], w=[puk])
                P.op('act', lambda e: e.activation(out=sg[j][:], in_=pg[:], func=AF.Silu), r=[pgk], w=['e_sg0'])
                P.op('dve', lambda e: e.tensor_tensor(out=hid[:, f, :], in0=pu[:], in1=sg[j][:], op=ALU.mult), r=[puk, 'e_sg0'], w=['e_hid'])
            for q in range(4):
                for hc in range(2):
                    ps_y, pyk = psS[hc], f'psS{hc}'
                    P.pe_group([(lambda e, f=f: e.matmul(ps_y[:], lhsT=hid[:, f, q * 128:(q + 1) * 128], rhs=wd[:, f, hc * 512:(hc + 1) * 512], start=(f == 0), stop=(f == 7))) for f in range(8)], r=['e_hid', 'e_wd'], w=[pyk])
                    if hc == 0:
                        P.op('act', lambda e: e.copy(out=yeb[:, q, 0:512], in_=ps_y[:]), r=[pyk], w=['e_yeb'])
                    else:
                        P.op('dve', lambda e: e.tensor_copy(out=yeb[:, q, 512:1024], in_=ps_y[:]), r=[pyk], w=['e_yeb'])
            fin.append(P.op('sp', lambda e: e.dma_start(out=ye_all[ex].rearrange("(q p) d -> p q d", p=128), in_=yeb[:]), r=['e_yeb'], w=[('scr', 'ye', ex)], dma=True))
        P.barrier()
        esE.close()
        esH.close()
        esF = ExitStack()
        yeA = sbs(esF, "f_yeA", [128, 16, 4, D], BF16)
        for ex in range(16):
            P.op('sp', lambda e: e.dma_start(out=yeA[:, ex, :, :], in_=ye_all[ex].rearrange("(q p) d -> p q d", p=128)), r=[('scr', 'ye', ex)], w=['f_yeA'], dma=True)
        Sg = [sbs(esF, f"f_Sg{i}", [128, 512], BF16) for i in range(2)]
        SgT = sbs(esF, "f_SgT", [128, 16, 4, 128], BF16)
        x1l = [sbs(esF, f"f_x1l{i}", [128, D], F32) for i in range(2)]
        ft = sbs(esF, "f_ft", [128, D], F32)
        ot = [sbs(esF, f"f_ot{i}", [128, D], F32) for i in range(2)]
        junk2 = sbs(esF, "f_junk", [128, D], F32)
        for t in range(NT // 2):
            i = t % 2
            ts_ = slice(t * 128, (t + 1) * 128)
            P.op('sp', lambda e: e.dma_start(out=x1l[i][:], in_=x1s[ts_, :]), r=[('scr', 'x1s', t)], w=[f'f_x1l{i}'], dma=True)
            for ex in range(16):
                j = ex % 2
                P.op('dve', lambda e: e.tensor_scalar(out=Sg[j][:], in0=iot[:], scalar1=posm[:, t, ex:ex + 1], scalar2=affTM[:, t, ex:ex + 1], op0=ALU.is_equal, op1=ALU.mult),
                     r=['iot', 'posm', ('affTM', t)], w=[f'f_Sg{j}'])
                P.pe_group([(lambda e, q=q: e.transpose(out=psT[j][:, q * 128:(q + 1) * 128], in_=Sg[j][:, q * 128:(q + 1) * 128], identity=identb[:])) for q in range(4)], r=[f'f_Sg{j}', 'identb'], w=[f'psT{j}'])
                if j == 0:
                    P.op('act', lambda e: e.copy(out=SgT[:, ex, :, :], in_=psT[j][:].rearrange("p (q n) -> p q n", q=4)), r=[f'psT{j}'], w=[('f_SgT', ex)])
                else:
                    P.op('pool', lambda e: e.tensor_copy(out=SgT[:, ex, :, :], in_=psT[j][:].rearrange("p (q n) -> p q n", q=4)), r=[f'psT{j}'], w=[('f_SgT', ex)]) if False else \
                        P.op('act', lambda e: e.copy(out=SgT[:, ex, :, :], in_=psT[j][:].rearrange("p (q n) -> p q n", q=4)), r=[f'psT{j}'], w=[('f_SgT', ex)])
            for hc in range(2):
                hs_ = slice(hc * 512, (hc + 1) * 512)
                pt, pk = psA[(t % 2) * 2 + hc], f'psA{(t % 2) * 2 + hc}'
                P.pe_group([(lambda e, ex=ex, q=q: e.matmul(pt[:], lhsT=SgT[:, ex, q, :], rhs=yeA[:, ex, q, hs_], start=(ex == 0 and q == 0), stop=(ex == 15 and q == 3)))
                            for ex in range(16) for q in range(4)], r=[('f_SgT', ex) for ex in range(16)] + ['f_yeA'], w=[pk])
                P.op('dve', lambda e: e.tensor_tensor(out=ft[:, hs_], in0=pt[:], in1=bcs[:, 3, hs_], op=ALU.mult), r=[pk, 'bcs'], w=['f_ft'])
            P.op('pool', lambda e: e.tensor_tensor(out=ft[:], in0=ft[:], in1=x1l[i][:], op=ALU.add), r=['f_ft', f'f_x1l{i}'], w=['f_ft'])
            P.op('dve', lambda e: e.memset(st2[:], 0.0), w=['st2'])
            P.op('act', lambda e: e.activation(out=junk2[:], in_=ft[:], func=AF.Square, accum_out=st2[:, 0:1]), r=['f_ft'], w=['f_junk', 'st2'])
            P.op('act', lambda e: e.activation(out=st2[:, 1:2], in_=st2[:, 0:1], func=AF.Sqrt, scale=1.0 / D, bias=epsb[:]), r=['st2', 'epsb'], w=['st2'])
            P.op('dve', lambda e: e.reciprocal(out=st2[:, 2:3], in_=st2[:, 1:2]), r=['st2'], w=['st2'])
            P.op('dve', lambda e: e.scalar_tensor_tensor(out=ot[i][:], in0=ft[:], scalar=st2[:, 2:3], in1=gfin[:], op0=ALU.mult, op1=ALU.mult), r=['f_ft', 'st2', 'gfin'], w=[f'f_ot{i}'])
            fin.append(P.op('sp', lambda e: e.dma_start(out=out[ts_, :], in_=ot[i][:]), r=[f'f_ot{i}'], dma=True))
        P.barrier()
        esF.close()
        esP.close()

    if 'A' in stages:
        phase_A()
    if 'MLA' in stages:
        phase_MLA()
    if 'DN' in stages:
        try:
            phase_DN()
        except _Stop:
            P.barrier()
    if 'MG' in stages:
        phase_MG_MOE()
    P.finish(fin)
    print("instructions:", P.n)
    return nc


_invf = (10000.0 ** (-np.arange(32, dtype=np.float32) / np.float32(32))).astype(np.float32)
INVF2 = np.concatenate([_invf, _invf])[:, None].astype(np.float32)
SGN2 = np.concatenate([-np.ones(32), np.ones(32)])[:, None].astype(np.float32)
SEL64 = np.zeros((128, 65), np.float32)
SEL64[:, 64] = 1.0
_xi = np.arange(128)[:, None]
_yi = np.arange(128)[None, :]
BIG = 1.0e4
DMASKS = np.stack([np.where(_xi > _yi, 0.0, BIG), np.where(_yi >= _xi, 0.0, -BIG),
                   np.where(_xi < _yi, 0.0, BIG), np.where(_yi <= _xi, 0.0, -BIG)], axis=1).astype(np.float32)
def _bd(s_):
    return ((_xi // s_) == (_yi // s_)).astype(np.float32)
DN_LMASK = np.ascontiguousarray(np.stack([np.tile(m_, (1, 4)) for m_ in
                                          (_bd(16), _bd(32) - _bd(16), _bd(64) - _bd(32), 1.0 - _bd(64), np.eye(128, dtype=np.float32))], axis=1))
IOTA512 = np.ascontiguousarray(np.broadcast_to(np.arange(512, dtype=np.float32)[None, :], (128, 512)))
TRI = (_xi < _yi).astype(np.float32)
IN_SPL = np.cumsum([3072, 1024, 16, 16, 512, 256, 64, 2048])


def prep_inputs(inputs, core):
    b, half = core // 2, core % 2
    f = lambda a: np.ascontiguousarray(a, dtype=np.float32)
    w_in = inputs['w_in'][0]
    qkv, z, bb, aa, cq, ckv, kr, g = np.split(w_in, IN_SPL[:-1], axis=1)
    xb = inputs['x'][b]
    posb = inputs['positions'][b]
    conv_w = inputs['conv_w'][0]
    a_log, dt_bias = inputs['a_log'][0], inputs['dt_bias'][0]
    if half == 1:
        xb = xb[::-1]
        posb = posb[::-1]
        conv_w = conv_w[::-1]
        bb = np.concatenate([bb[:, 8:16], bb[:, 0:8]], axis=1)
        aa = np.concatenate([aa[:, 8:16], aa[:, 0:8]], axis=1)
        a_log, dt_bias = a_log[::-1], dt_bias[::-1]
    swap = np.concatenate([np.arange(32, 64), np.arange(0, 32)])
    wuq_ = inputs['w_uq'][0].reshape(512, 8, 192)
    wukv_ = inputs['w_ukv'][0].reshape(256, 8, 256)
    m = {
        'x': f(xb),
        'cT': f(inputs['c'][b].reshape(8, 128).T),
        'w_mod': f(inputs['w_mod'][0]),
        'b_mod': f(inputs['b_mod'][0][None, :]),
        'g_mix': f(np.broadcast_to(inputs['g_mix'][0][None, :], (128, D))),
        'w_qkv': f(qkv), 'w_z': f(z), 'w_g': f(g),
        'w_ba': f(np.concatenate([bb, aa], axis=1)),
        'w_cq': f(cq), 'w_ckv': f(ckv),
        'w_kr2': f(np.concatenate([kr, kr[:, swap]], axis=1)),
        'q_gain': f(np.broadcast_to(inputs['q_gain'][0][None, :], (128, 512))),
        'kv_gain': f(np.broadcast_to(inputs['kv_gain'][0][None, :], (128, 256))),
        'identf': np.eye(128, dtype=np.float32),
        'posr': np.ascontiguousarray(np.broadcast_to(posb[None, :], (64, S)).astype(np.int32)),
        'invf2': INVF2, 'sgn2': SGN2, 'sel64': SEL64,
        'w_uqh': f(np.stack([np.concatenate([wuq_[:, h, 0:128], wuq_[:, h, 128:192], wuq_[:, h, 128:192][:, swap]], axis=1) for h in range(8)])),
        'w_ukvh': f(np.stack([wukv_[:, h, :] for h in range(8)])),
        'conv_wT': f(conv_w.T),
        'dn_sc': f(np.stack([a_log[0], a_log[1], dt_bias[0], dt_bias[1]], axis=1)),
        'dn_gain': f(np.broadcast_to(inputs['dn_o_gain'][0][None, :], (128, 128))),
        'dmasks': DMASKS, 'dn_lmask': DN_LMASK,
        'w_o_dn': f(inputs['w_o_dn'][0]), 'w_o_mla': f(inputs['w_o_mla'][0]), 'w_out': f(inputs['w_out'][0]),
        'g_ffn': f(np.broadcast_to(inputs['g_ffn'][0][None, :], (128, D))),
        'g_final': f(np.broadcast_to(inputs['g_final'][None, :], (128, D))),
        'w_router': f(inputs['w_router'][0]),
        'w_gate': f(inputs['w_gate'][0]), 'w_up': f(inputs['w_up'][0]), 'w_down': f(inputs['w_down'][0]),
        'iota512': IOTA512, 'tri_in': TRI,
    }
    return m


def kernel(**inputs):
    inputs = {k: np.asarray(v) for k, v in inputs.items()}
    nc = build()
    in_maps = [prep_inputs(inputs, c) for c in range(8)]
    res = run_bass_kernel_spmd(nc, in_maps, core_ids=list(range(8)))
    outp = np.zeros((4, S, D), np.float32)
    for c in range(8):
        b, half = c // 2, c % 2
        o_ = res.results[c]["out"]
        if half == 0:
            outp[b, 0:2048] = o_
        else:
            outp[b, 2048:4096] = o_[::-1]
    return outp
```

```python
import numpy as np
import ml_dtypes
import concourse.bass as bass
import concourse.mybir as mybir
from concourse.bass_utils import run_bass_kernel_spmd
from contextlib import ExitStack

F32 = mybir.dt.float32
BF16 = mybir.dt.bfloat16
I32 = mybir.dt.int32
AF = mybir.ActivationFunctionType
ALU = mybir.AluOpType
AX = mybir.AxisListType

import os
DBGN = int(os.environ.get('DBGN', '99'))
S = 4096
D = 1024
NT = S // 128
EPS = 1e-6


class Prog:
    def __init__(self, nc, ndma=16):
        self.nc = nc
        self.eng = {'pe': nc.tensor, 'act': nc.scalar, 'dve': nc.vector,
                    'pool': nc.gpsimd, 'sp': nc.sync}
        self.sem = {k: nc.alloc_semaphore(name=f"s_{k}") for k in self.eng}
        self.cnt = {k: 0 for k in self.eng}
        self.waited = {k: {} for k in self.eng}
        self.dsem = {q: [nc.alloc_semaphore(name=f"s_dma_{q}{i}") for i in range(ndma)] for q in ('sp', 'pool')}
        self.dcnt = {q: [0] * ndma for q in ('sp', 'pool')}
        self.nd = {'sp': 0, 'pool': 0}
        self.lastw = {}
        self.readers = {}
        self.n = 0

    def _wait(self, e, tok):
        if tok is None:
            return
        sem, val, key = tok
        w = self.waited[e]
        if w.get(key, 0) >= val:
            return
        w[key] = val
        self.eng[e].wait_ge(sem, val)

    def op(self, e, fn, r=(), w=(), dma=False):
        w = list(w) + [t for t in r if isinstance(t, str) and t.startswith('ps') and t not in w]
        toks = []
        for t in r:
            toks.append(self.lastw.get(t))
        for t in w:
            toks.append(self.lastw.get(t))
            toks.extend(self.readers.get(t, ()))
        for tok in toks:
            self._wait(e, tok)
        if dma:
            i = self.nd[e] % len(self.dsem[e])
            if self.dcnt[e][i] > 0:
                self._wait(e, (self.dsem[e][i], self.dcnt[e][i], f"dma_{e}{i}"))
        ins = fn(self.eng[e])
        self.n += 1
        if dma:
            i = self.nd[e] % len(self.dsem[e])
            self.nd[e] += 1
            self.dcnt[e][i] += 16
            ins.then_inc(self.dsem[e][i], 16)
            tok = (self.dsem[e][i], self.dcnt[e][i], f"dma_{e}{i}")
        else:
            self.cnt[e] += 1
            ins.then_inc(self.sem[e], 1)
            tok = (self.sem[e], self.cnt[e], e)
        for t in r:
            self.readers.setdefault(t, []).append(tok)
        for t in w:
            self.lastw[t] = tok
            self.readers[t] = []
        return tok

    def pe_group(self, fns, r=(), w=()):
        e = 'pe'
        w = list(w) + [t for t in r if isinstance(t, str) and t.startswith('ps') and t not in w]
        toks = []
        for t in r:
            toks.append(self.lastw.get(t))
        for t in w:
            toks.append(self.lastw.get(t))
            toks.extend(self.readers.get(t, ()))
        for tok in toks:
            self._wait(e, tok)
        for fn in fns[:-1]:
            fn(self.eng[e])
            self.n += 1
        ins = fns[-1](self.eng[e])
        self.n += 1
        self.cnt[e] += 1
        ins.then_inc(self.sem[e], 1)
        tok = (self.sem[e], self.cnt[e], e)
        for t in r:
            self.readers.setdefault(t, []).append(tok)
        for t in w:
            self.lastw[t] = tok
            self.readers[t] = []
        return tok

    def barrier(self):
        toks = [(self.sem[k], self.cnt[k], k) for k in self.eng if self.cnt[k] > 0]
        toks += [(self.dsem[q][i], self.dcnt[q][i], f"dma_{q}{i}") for q in self.dsem for i in range(len(self.dsem[q])) if self.dcnt[q][i] > 0]
        for e in self.eng:
            for tok in toks:
                self._wait(e, tok)

    def finish(self, toks):
        for tok in toks:
            self._wait('sp', tok)


class _Stop(Exception):
    pass


def build(debug=None, stages=('A', 'MLA', 'DN', 'MG'), ext_in=(), dn_heads=range(8), dn_stop=0):
    nc = bass.Bass("TRN2", target_bir_lowering=False)
    P = Prog(nc)

    def din(name, shape, dt=F32):
        return nc.dram_tensor(name, list(shape), dt, kind="ExternalInput").ap()

    def dscr(name, shape, dt):
        kind = "ExternalOutput" if (debug and name in debug) else ("ExternalInput" if name in ext_in else "Internal")
        return nc.dram_tensor(name, list(shape), dt, kind=kind).ap()

    x = din("x", [S, D])
    cT = din("cT", [128, 8])
    w_mod = din("w_mod", [D, 6 * D])
    b_mod = din("b_mod", [1, 6 * D])
    g_mix = din("g_mix", [128, D])
    w_qkv = din("w_qkv", [D, 3072])
    w_z = din("w_z", [D, 1024])
    w_g = din("w_g", [D, 2048])
    w_ba = din("w_ba", [D, 32])
    w_cq = din("w_cq", [D, 512])
    w_ckv = din("w_ckv", [D, 256])
    w_kr2 = din("w_kr2", [D, 128])
    q_gain = din("q_gain", [128, 512])
    kv_gain = din("kv_gain", [128, 256])
    identf = din("identf", [128, 128])
    posr = din("posr", [64, S], I32)
    invf2 = din("invf2", [64, 1])
    sgn2 = din("sgn2", [64, 1])
    sel64 = din("sel64", [128, 65])
    w_uqh = din("w_uqh", [8, 512, 256])
    w_ukvh = din("w_ukvh", [8, 256, 256])
    conv_wT = din("conv_wT", [3072, 5])
    dn_sc = din("dn_sc", [8, 4])
    dn_gain = din("dn_gain", [128, 128])
    dmasks = din("dmasks", [128, 4, 128])
    dn_lmask = din("dn_lmask", [128, 5, 512])
    w_o_dn = din("w_o_dn", [D, D])
    w_o_mla = din("w_o_mla", [D, D])
    w_out = din("w_out", [D, D])
    g_ffn = din("g_ffn", [128, D])
    g_final = din("g_final", [128, D])
    w_router = din("w_router", [D, 16])
    w_gate = din("w_gate", [16, D, D])
    w_up = din("w_up", [16, D, D])
    w_down = din("w_down", [16, D, D])
    iota512 = din("iota512", [128, 512])
    tri_in = din("tri_in", [128, 128])
    out = nc.dram_tensor("out", [S // 2, D], F32, kind="ExternalOutput").ap()

    qkvT = dscr("qkvT", [24, 128, S], BF16)
    zs = dscr("zs", [S, 1024], BF16)
    gs = dscr("gs", [S, 2048], BF16)
    baT = dscr("baT", [32, S], F32)
    cqnT = dscr("cqnT", [4, 128, S], BF16)
    ckvnT = dscr("ckvnT", [2, 128, S], BF16)
    krT = dscr("krT", [2, 64, S], BF16)
    modrow = dscr("modrow", [1, 6 * D], F32)
    oT_mla = dscr("oT_mla", [8, 128, S], BF16)
    oT_dn = dscr("oT_dn", [8, 128, S], BF16)
    x1s = dscr("x1s", [S, D], F32)
    ye_all = dscr("ye_all", [16, 512, D], BF16)

    sb = lambda n, s, d: nc.alloc_sbuf_tensor(n, list(s), d)
    ps_ = lambda n, s, d=F32: nc.alloc_psum_tensor(n, list(s), d)

    ident = sb("ident", [128, 128], F32)
    identb = sb("identb", [128, 128], BF16)
    ones_f = sb("ones_f", [128, 128], F32)
    P.op('sp', lambda e: e.dma_start(out=ident[:], in_=identf[:, :]), w=['ident'], dma=True)
    P.op('dve', lambda e: e.tensor_copy(out=identb[:], in_=ident[:]), r=['ident'], w=['identb'])
    P.op('dve', lambda e: e.memset(ones_f[:], 1.0), w=['ones_f'])
    epsb = sb("epsb", [128, 1], F32)
    P.op('dve', lambda e: e.memset(epsb[:], EPS), w=['epsb'])

    psA = [ps_(f"psA{i}", [128, 512]) for i in range(4)]
    psT = [ps_(f"psT{i}", [128, 512], BF16) for i in range(2)]
    psS = [ps_(f"psS{i}", [128, 512]) for i in range(2)]
    pa_i = [0]

    def next_psA():
        i = pa_i[0] % 4
        pa_i[0] += 1
        return psA[i], f"psA{i}"

    sbs = lambda es, n, s_, d: es.enter_context(nc.sbuf_tensor(n, list(s_), d))
    fin = []

    def phase_A():
        cT_sb = sb("cT_sb", [128, 8], F32)
        scT = sb("scT", [128, 8], F32)
        P.op('sp', lambda e: e.dma_start(out=cT_sb[:], in_=cT[:, :]), w=['cT'], dma=True)
        P.op('act', lambda e: e.activation(out=scT[:], in_=cT_sb[:], func=AF.Silu), r=['cT'], w=['scT'])
        esA = ExitStack()
        modbc = sbs(esA, "modbc", [128, 6, D], F32)
        gmx = sbs(esA, "gmx", [128, D], F32)
        A1 = sbs(esA, "A1", [128, D], F32)
        es0 = ExitStack()
        mod_sb = sbs(es0, "mod_sb", [1, 6 * D], F32)
        bm_sb = sbs(es0, "bm_sb", [1, 6 * D], F32)
        P.op('sp', lambda e: e.dma_start(out=bm_sb[:], in_=b_mod[:, :]), w=['bm'], dma=True)
        wm = [sbs(es0, f"wm{i}", [128, 8, 512], F32) for i in range(2)]
        w_mod_v = w_mod.rearrange("(k p) n -> p k n", p=128)
        for j in range(12):
            wt, wk = wm[j % 2], f"wm{j % 2}"
            P.op('sp', lambda e: e.dma_start(out=wt[:], in_=w_mod_v[:, :, j * 512:(j + 1) * 512]), w=[wk], dma=True)
            pt, pk = next_psA()
            for k in range(8):
                P.op('pe', lambda e: e.matmul(pt[0:1, :], lhsT=scT[:, k:k + 1], rhs=wt[:, k, :], start=(k == 0), stop=(k == 7)),
                     r=[wk, 'scT'], w=[pk])
            P.op('dve', lambda e: e.tensor_tensor(out=mod_sb[:, j * 512:(j + 1) * 512], in0=pt[0:1, :], in1=bm_sb[:, j * 512:(j + 1) * 512], op=ALU.add),
                 r=[pk, 'bm'], w=['mod'])
        for j in range(12):
            pt, pk = next_psA()
            P.op('pe', lambda e: e.matmul(pt[:], lhsT=ones_f[0:1, :], rhs=mod_sb[:, j * 512:(j + 1) * 512], start=True, stop=True),
                 r=['ones_f', 'mod'], w=[pk])
            P.op('act', lambda e: e.copy(out=modbc[:, j // 2, (j % 2) * 512:(j % 2 + 1) * 512], in_=pt[:]), r=[pk], w=['modbc'])
        P.op('sp', lambda e: e.dma_start(out=gmx[:], in_=g_mix[:, :]), w=['gmx'], dma=True)
        P.op('dve', lambda e: e.scalar_tensor_tensor(out=A1[:], in0=modbc[:, 1, :], scalar=1.0, in1=gmx[:], op0=ALU.add, op1=ALU.mult),
             r=['modbc', 'gmx'], w=['A1'])

        fin.append(P.op('sp', lambda e: e.dma_start(out=modrow[:, :], in_=mod_sb[:]), r=['mod'], dma=True))
        P.barrier()
        es0.close()
        hT = sbs(esA, "hT", [128, 8, S], BF16)
        xt = [sbs(esA, f"xt{i}", [128, D], F32) for i in range(2)]
        xn = [sbs(esA, f"xn{i}", [128, D], F32) for i in range(2)]
        hb = [sbs(esA, f"hb{i}", [128, D], BF16) for i in range(2)]
        st = [sbs(esA, f"st{i}", [128, 4], F32) for i in range(2)]
        junk = sbs(esA, "junk", [128, D], F32)
        for t in range(NT):
            i = t % 2
            P.op('sp', lambda e: e.dma_start(out=xt[i][:], in_=x[t * 128:(t + 1) * 128, :]), w=[f'xt{i}'], dma=True)
            P.op('dve', lambda e: e.memset(st[i][:], 0.0), w=[f'st{i}'])
            P.op('act', lambda e: e.activation(out=junk[:], in_=xt[i][:], func=AF.Square, accum_out=st[i][:, 0:1]),
                 r=[f'xt{i}'], w=['junk', f'st{i}'])
            P.op('act', lambda e: e.activation(out=st[i][:, 1:2], in_=st[i][:, 0:1], func=AF.Sqrt, scale=1.0 / D, bias=epsb[:]),
                 r=[f'st{i}', 'epsb'], w=[f'st{i}'])
            P.op('dve', lambda e: e.reciprocal(out=st[i][:, 2:3], in_=st[i][:, 1:2]), r=[f'st{i}'], w=[f'st{i}'])
            P.op('dve', lambda e: e.scalar_tensor_tensor(out=xn[i][:], in0=xt[i][:], scalar=st[i][:, 2:3], in1=A1[:], op0=ALU.mult, op1=ALU.mult),
                 r=[f'xt{i}', f'st{i}', 'A1'], w=[f'xn{i}'])
            P.op('pool', lambda e: e.tensor_tensor(out=hb[i][:], in0=xn[i][:], in1=modbc[:, 0, :], op=ALU.add),
                 r=[f'xn{i}', 'modbc'], w=[f'hb{i}'])
            for half in range(2):
                pt, pk = psT[half], f'psT{half}'
                for k4 in range(4):
                    k = half * 4 + k4
                    P.op('pe', lambda e: e.transpose(out=pt[:, k4 * 128:(k4 + 1) * 128], in_=hb[i][:, k * 128:(k + 1) * 128], identity=identb[:]),
                         r=[f'hb{i}', 'identb'], w=[pk])
                eng = 'act' if half == 0 else 'dve'
                if eng == 'act':
                    P.op('act', lambda e: e.copy(out=hT[:, half * 4:(half + 1) * 4, t * 128:(t + 1) * 128],
                                                 in_=pt[:].rearrange("p (k n) -> p k n", k=4)), r=[pk], w=[('hT', t)])
                else:
                    P.op('dve', lambda e: e.tensor_copy(out=hT[:, half * 4:(half + 1) * 4, t * 128:(t + 1) * 128],
                                                        in_=pt[:].rearrange("p (k n) -> p k n", k=4)), r=[pk], w=[('hT', t)])
        hT_all = [('hT', t) for t in range(NT)]

        wb = [sbs(esA, f"wb{i}", [128, 8, 512], BF16) for i in range(2)]
        wb_i = [0]

        def load_w(src, c0, ncols):
            i = wb_i[0] % 2
            wb_i[0] += 1
            v = src.rearrange("(k p) n -> p k n", p=128)
            P.op('pool', lambda e: e.dma_start(out=wb[i][:, :, 0:ncols], in_=v[:, :, c0:c0 + ncols]), w=[f'wb{i}'], dma=True)
            return wb[i], f'wb{i}'

        stg = [sbs(esA, f"stg{i}", [128, S], BF16) for i in range(2)]
        stg_i = [0]

        def chan_major(src, ncols_total, dst_fn, M, dt_out=BF16, stgs=stg):
            nblk = (ncols_total + 511) // 512
            for blk in range(nblk):
                nc_ = min(512, ncols_total - blk * 512)
                wt, wk = load_w(src, blk * 512, nc_)
                for c in range(nc_ // M):
                    si = stg_i[0] % 2
                    stg_i[0] += 1
                    sg, sk = stgs[si], f'{stgs[si].name}'
                    for g in range(8):
                        pt, pk = next_psA()
                        P.pe_group([(lambda e, k=k: e.matmul(pt[0:M, :], lhsT=wt[:, k, c * M:(c + 1) * M], rhs=hT[:, k, g * 512:(g + 1) * 512],
                                                            start=(k == 0), stop=(k == 7))) for k in range(8)],
                                   r=[wk] + hT_all[g * 4:(g + 1) * 4], w=[pk])
                        if g % 2 == 0:
                            P.op('act', lambda e: e.copy(out=sg[0:M, g * 512:(g + 1) * 512], in_=pt[0:M, :]), r=[pk], w=[sk])
                        else:
                            P.op('dve', lambda e: e.tensor_copy(out=sg[0:M, g * 512:(g + 1) * 512], in_=pt[0:M, :]), r=[pk], w=[sk])
                    fin.append(P.op('sp', lambda e: e.dma_start(out=dst_fn(blk * (512 // M) + c), in_=sg[0:M, :]), r=[sk], w=[('scr', dst_fn.__name__)], dma=True))

        def dst_qkv(c):
            return qkvT[c, :, :]
        chan_major(w_qkv, 3072, dst_qkv, 128)

        def dst_kr(c):
            return krT[c, :, :]
        chan_major(w_kr2, 128, dst_kr, 64)
        stgf0 = sbs(esA, "stgf0", [32, S], F32)
        stgf = [stgf0, stgf0]

        def dst_ba(c):
            return baT[:, :]
        chan_major(w_ba, 32, dst_ba, 32, F32, stgf)

        tst = [sbs(esA, f"tst{i}", [128, 512], BF16) for i in range(4)]
        tst_i = [0]

        def tok_major_act(src, ncols_total, dst, func):
            for blk in range(ncols_total // 512):
                wt, wk = load_w(src, blk * 512, 512)
                for t in range(NT):
                    pt, pk = next_psA()
                    P.pe_group([(lambda e, k=k: e.matmul(pt[:], lhsT=hT[:, k, t * 128:(t + 1) * 128], rhs=wt[:, k, :], start=(k == 0), stop=(k == 7))) for k in range(8)],
                               r=[wk, ('hT', t)], w=[pk])
                    si = tst_i[0] % 4
                    tst_i[0] += 1
                    P.op('act', lambda e: e.activation(out=tst[si][:], in_=pt[:], func=func), r=[pk], w=[f'tst{si}'])
                    fin.append(P.op('sp', lambda e: e.dma_start(out=dst[t * 128:(t + 1) * 128, blk * 512:(blk + 1) * 512], in_=tst[si][:]),
                                    r=[f'tst{si}'], w=[('scr', dst.name, t, blk)], dma=True))
        tok_major_act(w_z, 1024, zs, AF.Silu)
        tok_major_act(w_g, 2048, gs, AF.Sigmoid)

        def latent(src, ncols, gain_in, dstT, nm):
            gsb = sbs(esA, f"gain_{nm}", [128, ncols], F32)
            P.op('sp', lambda e: e.dma_start(out=gsb[:], in_=gain_in[:, :]), w=[f'gain_{nm}'], dma=True)
            lst = [sbs(esA, f"lst_{nm}{i_}", [128, ncols // 128, 128], BF16) for i_ in range(2)]
            wt, wk = load_w(src, 0, ncols)
            for t in range(NT):
                i = t % 2
                pt, pk = next_psA()
                P.pe_group([(lambda e, k=k: e.matmul(pt[:, 0:ncols], lhsT=hT[:, k, t * 128:(t + 1) * 128], rhs=wt[:, k, 0:ncols], start=(k == 0), stop=(k == 7))) for k in range(8)],
                           r=[wk, ('hT', t)], w=[pk])
                P.op('dve', lambda e: e.memset(st[i][:], 0.0), w=[f'st{i}'])
                P.op('act', lambda e: e.activation(out=junk[:, 0:ncols], in_=pt[:, 0:ncols], func=AF.Square, accum_out=st[i][:, 0:1]),
                     r=[pk], w=['junk', f'st{i}'])
                P.op('act', lambda e: e.activation(out=st[i][:, 1:2], in_=st[i][:, 0:1], func=AF.Sqrt, scale=1.0 / ncols, bias=epsb[:]),
                     r=[f'st{i}', 'epsb'], w=[f'st{i}'])
                P.op('dve', lambda e: e.reciprocal(out=st[i][:, 2:3], in_=st[i][:, 1:2]), r=[f'st{i}'], w=[f'st{i}'])
                P.op('dve', lambda e: e.scalar_tensor_tensor(out=hb[i][:, 0:ncols], in0=pt[:, 0:ncols], scalar=st[i][:, 2:3], in1=gsb[:], op0=ALU.mult, op1=ALU.mult),
                     r=[pk, f'st{i}', f'gain_{nm}'], w=[f'hb{i}'])
                tp, tk = psT[i], f'psT{i}'
                for c in range(ncols // 128):
                    P.op('pe', lambda e: e.transpose(out=tp[:, c * 128:(c + 1) * 128], in_=hb[i][:, c * 128:(c + 1) * 128], identity=identb[:]),
                         r=[f'hb{i}', 'identb'], w=[tk])
                P.op('act', lambda e: e.copy(out=lst[i][:], in_=tp[:, 0:ncols].rearrange("p (k n) -> p k n", k=ncols // 128)),
                     r=[tk], w=[f'lst_{nm}{i}'])
                fin.append(P.op('sp', lambda e: e.dma_start(out=dstT[:, :, t * 128:(t + 1) * 128].rearrange("c p n -> p c n"), in_=lst[i][:]),
                                r=[f'lst_{nm}{i}'], w=[('scr', nm, t)], dma=True))
        latent(w_cq, 512, q_gain, cqnT, 'cq')
        latent(w_ckv, 256, kv_gain, ckvnT, 'ckv')


        P.barrier()
        esA.close()

    def phase_MLA():
        esM = ExitStack()
        TWO_PI = float(2 * np.pi)
        SCL = float(192 ** -0.5)
        cos2 = sbs(esM, "cos2", [64, S], F32)
        sin2 = sbs(esM, "sin2", [64, S], F32)
        krA = sbs(esM, "krA", [65, S], BF16)
        QrA = sbs(esM, "QrA", [65, S], BF16)
        onesb = sbs(esM, "onesb", [128, 128], BF16)
        sel_b = sbs(esM, "sel_b", [128, 65], BF16)
        if True:
            es1 = ExitStack()
            posi = sbs(es1, "posi", [64, S], I32)
            ang = sbs(es1, "ang", [64, S], F32)
            ti = sbs(es1, "ti", [64, S], I32)
            tf = sbs(es1, "tf", [64, S], F32)
            tg = sbs(es1, "tg", [64, S], F32)
            ivf = sbs(es1, "ivf", [64, 2], F32)
            kr0 = sbs(es1, "kr0", [64, S], BF16)
            kr1 = sbs(es1, "kr1", [64, S], BF16)
            self_f = sbs(es1, "self_f", [128, 65], F32)
            P.op('sp', lambda e: e.dma_start(out=posi[:], in_=posr[:, :]), w=['posi'], dma=True)
            P.op('sp', lambda e: e.dma_start(out=ivf[:, 0:1], in_=invf2[:, :]), w=['ivf'], dma=True)
            P.op('sp', lambda e: e.dma_start(out=ivf[:, 1:2], in_=sgn2[:, :]), w=['ivf'], dma=True)
            P.op('sp', lambda e: e.dma_start(out=self_f[:], in_=sel64[:, :]), w=['self_f'], dma=True)
            P.op('dve', lambda e: e.tensor_copy(out=sel_b[:], in_=self_f[:]), r=['self_f'], w=['sel_b'])
            P.op('dve', lambda e: e.memset(onesb[:], 1.0), w=['onesb'])
            P.op('dve', lambda e: e.tensor_copy(out=ang[:], in_=posi[:]), r=['posi'], w=['ang'])
            P.op('dve', lambda e: e.tensor_scalar(out=ang[:], in0=ang[:], scalar1=ivf[:, 0:1], scalar2=float(1.0 / TWO_PI), op0=ALU.mult, op1=ALU.mult),
                 r=['ang', 'ivf'], w=['ang'])
            for which, dst in ((0, sin2), (1, cos2)):
                dk_ = 'sin2' if which == 0 else 'cos2'
                P.op('dve', lambda e: e.tensor_scalar(out=tg[:], in0=ang[:], scalar1=0.25 * which, scalar2=None, op0=ALU.add), r=['ang'], w=['tg'])
                P.op('dve', lambda e: e.tensor_copy(out=ti[:], in_=tg[:]), r=['tg'], w=['ti'])
                P.op('dve', lambda e: e.tensor_copy(out=tf[:], in_=ti[:]), r=['ti'], w=['tf'])
                P.op('dve', lambda e: e.tensor_tensor(out=tg[:], in0=tg[:], in1=tf[:], op=ALU.subtract), r=['tg', 'tf'], w=['tg'])
                P.op('dve', lambda e: e.tensor_scalar(out=tf[:], in0=tg[:], scalar1=0.5, scalar2=None, op0=ALU.is_gt), r=['tg'], w=['tf'])
                P.op('dve', lambda e: e.tensor_tensor(out=tg[:], in0=tg[:], in1=tf[:], op=ALU.subtract), r=['tg', 'tf'], w=['tg'])
                P.op('dve', lambda e: e.tensor_scalar(out=tf[:], in0=tg[:], scalar1=-0.5, scalar2=None, op0=ALU.is_lt), r=['tg'], w=['tf'])
                P.op('dve', lambda e: e.tensor_tensor(out=tg[:], in0=tg[:], in1=tf[:], op=ALU.add), r=['tg', 'tf'], w=['tg'])
                P.op('act', lambda e: e.activation(out=dst[:], in_=tg[:], func=AF.Sin, scale=TWO_PI), r=['tg'], w=[dk_])
            P.op('dve', lambda e: e.tensor_scalar(out=sin2[:], in0=sin2[:], scalar1=ivf[:, 1:2], scalar2=None, op0=ALU.mult), r=['sin2', 'ivf'], w=['sin2'])
            P.op('sp', lambda e: e.dma_start(out=kr0[:], in_=krT[0, :, :]), r=[('scr', 'dst_kr')], w=['kr0'], dma=True)
            P.op('sp', lambda e: e.dma_start(out=kr1[:], in_=krT[1, :, :]), r=[('scr', 'dst_kr')], w=['kr1'], dma=True)
            P.op('dve', lambda e: e.tensor_tensor(out=tg[:], in0=kr0[:], in1=cos2[:], op=ALU.mult), r=['kr0', 'cos2'], w=['tg'])
            P.op('dve', lambda e: e.tensor_tensor(out=tf[:], in0=kr1[:], in1=sin2[:], op=ALU.mult), r=['kr1', 'sin2'], w=['tf'])
            P.op('dve', lambda e: e.memset(krA[:], 1.0), w=['krA'])
            P.op('dve', lambda e: e.tensor_tensor(out=krA[0:64, :], in0=tg[:], in1=tf[:], op=ALU.add), r=['tg', 'tf'], w=['krA'])
            P.op('dve', lambda e: e.memset(QrA[:], 0.0), w=['QrA'])
            P.barrier()
            es1.close()
        cqn = sbs(esM, "cqn", [128, 4, S], BF16)
        ckvn = sbs(esM, "ckvn", [128, 2, S], BF16)
        QnT = sbs(esM, "QnT", [128, S], BF16)
        KnT = sbs(esM, "KnT", [128, S], BF16)
        Vt = sbs(esM, "Vt", [128, NT, 128], BF16)
        oTs = sbs(esM, "oTs", [128, S], BF16)
        kmx = sbs(esM, "kmx", [65, 16], F32)
        wuq = sbs(esM, "wuq", [128, 4, 256], BF16)
        wukv = sbs(esM, "wukv", [128, 2, 256], BF16)
        sq = sbs(esM, "sq", [128, 512], BF16)
        t1 = sbs(esM, "t1", [64, 512], F32)
        t2 = sbs(esM, "t2", [64, 512], F32)
        rowt = sbs(esM, "rowt", [65, 512], F32)
        pT = [sbs(esM, f"pT{i}", [128, 512], BF16) for i in range(3)]
        rden = sbs(esM, "rden", [128, 512], F32)
        for c in range(4):
            P.op('sp', lambda e: e.dma_start(out=cqn[:, c, :], in_=cqnT[c, :, :]), r=[('scr', 'cq', t_) for t_ in range(NT)], w=['cqn'], dma=True)
        for c in range(2):
            P.op('sp', lambda e: e.dma_start(out=ckvn[:, c, :], in_=ckvnT[c, :, :]), r=[('scr', 'ckv', t_) for t_ in range(NT)], w=['ckvn'], dma=True)
        for h in range(8):
            P.op('pool', lambda e: e.dma_start(out=wuq[:], in_=w_uqh[h].rearrange("(k p) n -> p k n", p=128)), w=['wuq'], dma=True)
            P.op('pool', lambda e: e.dma_start(out=wukv[:], in_=w_ukvh[h].rearrange("(k p) n -> p k n", p=128)), w=['wukv'], dma=True)
            for g in range(8):
                gs_ = slice(g * 512, (g + 1) * 512)
                pt, pk = psA[2], 'psA2'
                P.pe_group([(lambda e, k=k: e.matmul(pt[:], lhsT=wuq[:, k, 0:128], rhs=cqn[:, k, gs_], start=(k == 0), stop=(k == 3))) for k in range(4)], r=['wuq', 'cqn'], w=[pk])
                P.op('act', lambda e: e.copy(out=QnT[:, gs_], in_=pt[:]), r=[pk], w=[('QnT', g)])
                pt, pk = psA[3], 'psA3'
                P.pe_group([(lambda e, k=k: e.matmul(pt[0:64, :], lhsT=wuq[:, k, 128:192], rhs=cqn[:, k, gs_], start=(k == 0), stop=(k == 3))) for k in range(4)], r=['wuq', 'cqn'], w=[pk])
                P.op('dve', lambda e: e.tensor_tensor(out=t1[:], in0=pt[0:64, :], in1=cos2[:, gs_], op=ALU.mult), r=[pk, 'cos2'], w=['t1'])
                P.pe_group([(lambda e, k=k: e.matmul(pt[0:64, :], lhsT=wuq[:, k, 192:256], rhs=cqn[:, k, gs_], start=(k == 0), stop=(k == 3))) for k in range(4)], r=['wuq', 'cqn'], w=[pk])
                P.op('dve', lambda e: e.tensor_tensor(out=t2[:], in0=pt[0:64, :], in1=sin2[:, gs_], op=ALU.mult), r=[pk, 'sin2'], w=['t2'])
                P.op('dve', lambda e: e.tensor_tensor(out=QrA[0:64, gs_], in0=t1[:], in1=t2[:], op=ALU.add), r=['t1', 't2'], w=[('QrA', g)])
                pt, pk = psA[2], 'psA2'
                P.pe_group([(lambda e, k=k: e.matmul(pt[:], lhsT=wukv[:, k, 0:128], rhs=ckvn[:, k, gs_], start=(k == 0), stop=(k == 1))) for k in range(2)], r=['wukv', 'ckvn'], w=[pk])
                P.op('act', lambda e: e.copy(out=KnT[:, gs_], in_=pt[:]), r=[pk], w=[('KnT', g)])
                pt, pk = psA[3], 'psA3'
                for j in range(4):
                    t_ = g * 4 + j
                    for k in range(2):
                        P.op('pe', lambda e: e.matmul(pt[:, j * 128:(j + 1) * 128], lhsT=ckvn[:, k, t_ * 128:(t_ + 1) * 128], rhs=wukv[:, k, 128:256], start=(k == 0), stop=(k == 1)),
                             r=['wukv', 'ckvn'], w=[pk])
                P.op('dve', lambda e: e.tensor_copy(out=Vt[:, g * 4:(g + 1) * 4, :], in_=pt[:].rearrange("p (j n) -> p j n", j=4)), r=[pk], w=[('Vt', g)])
                pt, pk = psA[2], 'psA2'
                P.op('act', lambda e: e.activation(out=sq[:], in_=KnT[:, gs_], func=AF.Square), r=[('KnT', g)], w=['sq'])
                P.op('pe', lambda e: e.matmul(pt[0:65, :], lhsT=sel_b[:, :], rhs=sq[:], start=True, stop=False), r=['sq', 'sel_b'], w=[pk])
                P.op('act', lambda e: e.activation(out=sq[0:64, :], in_=krA[0:64, gs_], func=AF.Square), r=['krA'], w=['sq'])
                P.op('pe', lambda e: e.matmul(pt[0:65, :], lhsT=sel_b[0:64, :], rhs=sq[0:64, :], start=False, stop=True), r=['sq', 'sel_b'], w=[pk])
                P.op('dve', lambda e: e.tensor_reduce(out=kmx[64:65, g:g + 1], in_=pt[64:65, :], axis=AX.X, op=ALU.max), r=[pk], w=['kmx'])
            P.op('dve', lambda e: e.tensor_reduce(out=kmx[64:65, 8:9], in_=kmx[64:65, 0:8], axis=AX.X, op=ALU.max), r=['kmx'], w=['kmx'])
            for g in range(8):
                gs_ = slice(g * 512, (g + 1) * 512)
                pt, pk = psA[2], 'psA2'
                P.op('act', lambda e: e.activation(out=sq[:], in_=QnT[:, gs_], func=AF.Square), r=[('QnT', g)], w=['sq'])
                P.op('pe', lambda e: e.matmul(pt[0:65, :], lhsT=sel_b[:, :], rhs=sq[:], start=True, stop=False), r=['sq', 'sel_b'], w=[pk])
                P.op('act', lambda e: e.activation(out=sq[0:64, :], in_=QrA[0:64, gs_], func=AF.Square), r=[('QrA', g)], w=['sq'])
                P.op('pe', lambda e: e.matmul(pt[0:65, :], lhsT=sel_b[0:64, :], rhs=sq[0:64, :], start=False, stop=True), r=['sq', 'sel_b'], w=[pk])
                P.op('act', lambda e: e.activation(out=rowt[64:65, :], in_=pt[64:65, :], func=AF.Sqrt, scale=kmx[64:65, 8:9]), r=[pk, 'kmx'], w=['rowt'])
                P.op('dve', lambda e: e.tensor_scalar(out=QrA[64:65, gs_], in0=rowt[64:65, :], scalar1=-1.0, scalar2=None, op0=ALU.mult), r=['rowt'], w=[('QrA', g)])
            for g in range(8):
                gs_ = slice(g * 512, (g + 1) * 512)
                po, pd = psA[(g % 2) * 2], psA[(g % 2) * 2 + 1]
                kpo, kpd = f'psA{(g % 2) * 2}', f'psA{(g % 2) * 2 + 1}'

                def scores(kt):
                    ks_ = slice(kt * 128, (kt + 1) * 128)
                    sc_, sck = psS[kt % 2], f'psS{kt % 2}'
                    P.pe_group([lambda e: e.matmul(sc_[:], lhsT=KnT[:, ks_], rhs=QnT[:, gs_], start=True, stop=False),
                                lambda e: e.matmul(sc_[:], lhsT=krA[:, ks_], rhs=QrA[:, gs_], start=False, stop=True)],
                               r=[('KnT', kt // 4), ('QnT', g), 'krA', ('QrA', g)], w=[sck])
                scores(0)
                for kt in range(NT):
                    sc_, sck = psS[kt % 2], f'psS{kt % 2}'
                    pi = kt % 3
                    P.op('act', lambda e: e.activation(out=pT[pi][:], in_=sc_[:], func=AF.Exp, scale=SCL), r=[sck], w=[f'pT{pi}'])
                    if kt + 1 < NT:
                        scores(kt + 1)
                    P.pe_group([lambda e: e.matmul(po[:], lhsT=Vt[:, kt, :], rhs=pT[pi][:], start=(kt == 0), stop=(kt == NT - 1)),
                                lambda e: e.matmul(pd[:], lhsT=onesb[:], rhs=pT[pi][:], start=(kt == 0), stop=(kt == NT - 1))],
                               r=[('Vt', kt // 4), f'pT{pi}', 'onesb'], w=[kpo, kpd])
                P.op('dve', lambda e: e.reciprocal(out=rden[:], in_=pd[:]), r=[kpd], w=['rden'])
                P.op('dve', lambda e: e.tensor_tensor(out=oTs[:, gs_], in0=po[:], in1=rden[:], op=ALU.mult), r=[kpo, 'rden'], w=['oTs'])
            fin.append(P.op('sp', lambda e: e.dma_start(out=oT_mla[h, :, :], in_=oTs[:]), r=['oTs'], w=[('scr', 'oT_mla', h)], dma=True))
        P.barrier()
        esM.close()


    def phase_DN():
        def stop(k):
            if dn_stop == k:
                raise _Stop()
        esD = ExitStack()
        pM, pG, pX0, pX1, pZT, pU = psA[0], psA[1], psA[2], psA[3], psS[0], psS[1]
        kM, kG, kX0, kX1, kZT, kU = 'psA0', 'psA1', 'psA2', 'psA3', 'psS0', 'psS1'
        onesb = sbs(esD, "d_onesb", [128, 128], BF16)
        P.op('dve', lambda e: e.memset(onesb[:], 1.0), w=['d_onesb'])
        msk = sbs(esD, "d_msk", [128, 4, 128], F32)
        P.op('sp', lambda e: e.dma_start(out=msk[:], in_=dmasks[:, :, :]), w=['d_msk'], dma=True)
        gain = sbs(esD, "d_gain", [128, 128], F32)
        P.op('sp', lambda e: e.dma_start(out=gain[:], in_=dn_gain[:, :]), w=['d_gain'], dma=True)
        tokS = sbs(esD, "tokS", [128, NT, 48], F32)
        one1 = sbs(esD, "one1", [128, 1], F32)
        P.op('dve', lambda e: e.memset(one1[:], 1.0), w=['one1'])
        eps_l2 = sbs(esD, "eps_l2", [128, 1], F32)
        P.op('dve', lambda e: e.memset(eps_l2[:], EPS), w=['eps_l2'])
        es1 = ExitStack()
        sc = sbs(es1, "d_sc", [8, 4], F32)
        nA = sbs(es1, "d_nA", [8, 2], F32)
        P.op('sp', lambda e: e.dma_start(out=sc[:], in_=dn_sc[:, :]), w=['d_sc'], dma=True)
        P.op('act', lambda e: e.activation(out=nA[:], in_=sc[:, 0:2], func=AF.Exp), r=['d_sc'], w=['d_nA'])
        P.op('dve', lambda e: e.tensor_scalar(out=nA[:], in0=nA[:], scalar1=-1.0, scalar2=None, op0=ALU.mult), r=['d_nA'], w=['d_nA'])
        rows = {}
        ra = sbs(es1, "d_ra", [8, S], F32)
        rb = sbs(es1, "d_rb", [8, S], F32)
        rc = sbs(es1, "d_rc", [8, S], F32)
        for d in range(2):
            beta = sbs(es1, f"d_beta{d}", [8, S], F32)
            nbeta = sbs(es1, f"d_nbeta{d}", [8, S], F32)
            gc = sbs(es1, f"d_gc{d}", [8, S], F32)
            rows[d] = (beta, nbeta, gc)
            P.op('sp', lambda e: e.dma_start(out=ra[:], in_=baT[d * 8:(d + 1) * 8, :]), r=[('scr', 'dst_ba')], w=['d_ra'], dma=True)
            P.op('act', lambda e: e.activation(out=beta[:], in_=ra[:], func=AF.Sigmoid), r=['d_ra'], w=[f'd_beta{d}'])
            P.op('dve', lambda e: e.tensor_scalar(out=nbeta[:], in0=beta[:], scalar1=-1.0, scalar2=None, op0=ALU.mult), r=[f'd_beta{d}'], w=[f'd_nbeta{d}'])
            P.op('sp', lambda e: e.dma_start(out=ra[:], in_=baT[16 + d * 8:16 + (d + 1) * 8, :]), r=[('scr', 'dst_ba')], w=['d_ra'], dma=True)
            P.op('dve', lambda e: e.tensor_scalar(out=ra[:], in0=ra[:], scalar1=sc[:, 2 + d:3 + d], scalar2=None, op0=ALU.add), r=['d_ra', 'd_sc'], w=['d_ra'])
            P.op('act', lambda e: e.activation(out=rb[:], in_=ra[:], func=AF.Abs), r=['d_ra'], w=['d_rb'])
            P.op('act', lambda e: e.activation(out=rb[:], in_=rb[:], func=AF.Exp, scale=-1.0), r=['d_rb'], w=['d_rb'])
            P.op('act', lambda e: e.activation(out=rb[:], in_=rb[:], func=AF.Ln, bias=one1[0:8, :], scale=1.0), r=['d_rb', 'one1'], w=['d_rb'])
            P.op('dve', lambda e: e.scalar_tensor_tensor(out=rc[:], in0=ra[:], scalar=0.0, in1=rb[:], op0=ALU.max, op1=ALU.add), r=['d_ra', 'd_rb'], w=['d_rc'])
            P.op('dve', lambda e: e.tensor_scalar(out=rc[:], in0=rc[:], scalar1=nA[:, d:d + 1], scalar2=None, op0=ALU.mult), r=['d_rc', 'd_nA'], w=['d_rc'])
            cur, curk, nxt, nxtk = rc, 'd_rc', gc, f'd_gc{d}'
            for sft in (1, 2, 4, 8, 16, 32, 64):
                c3 = cur[:].rearrange("p (t n) -> p t n", n=128)
                n3 = nxt[:].rearrange("p (t n) -> p t n", n=128)
                P.op('act', lambda e: e.copy(out=nxt[:], in_=cur[:]), r=[curk], w=[nxtk])
                if d == 0:
                    P.op('dve', lambda e: e.tensor_tensor(out=n3[:, :, sft:], in0=c3[:, :, sft:], in1=c3[:, :, :128 - sft], op=ALU.add), r=[curk], w=[nxtk])
                else:
                    P.op('dve', lambda e: e.tensor_tensor(out=n3[:, :, :128 - sft], in0=c3[:, :, :128 - sft], in1=c3[:, :, sft:], op=ALU.add), r=[curk], w=[nxtk])
                cur, curk, nxt, nxtk = nxt, nxtk, cur, curk
            if cur is not gc:
                P.op('act', lambda e: e.copy(out=gc[:], in_=cur[:]), r=[curk], w=[f'd_gc{d}'])
        for t in range(NT):
            for d in range(2):
                for j in range(3):
                    src = rows[d][j]
                    col = d * 24 + j * 8
                    P.op('pe', lambda e: e.transpose(out=pG[:, col:col + 8], in_=src[:, t * 128:(t + 1) * 128], identity=ident[0:8, 0:8]),
                         r=[f'd_beta{d}', f'd_nbeta{d}', f'd_gc{d}', 'ident'], w=[kG])
            P.op('act', lambda e: e.copy(out=tokS[:, t, :], in_=pG[:, 0:48]), r=[kG], w=['tokS'])
        P.barrier()
        stop(1)
        es1.close()
        lm = sbs(esD, "d_lm", [128, 5, 4 * 128], F32)
        for j5 in range(5):
            P.op('sp', lambda e: e.dma_start(out=lm[:, j5, :], in_=dn_lmask[:, j5, :]), w=['d_lm'], dma=True)
        QKV = [sbs(esD, f"d_qkv{i}", [128, S], BF16) for i in range(3)]
        Ust = sbs(esD, "d_U", [128, NT, 2, 128], BF16)
        WTst = sbs(esD, "d_WT", [128, NT, 2, 128], BF16)
        ITst = sbs(esD, "d_IT", [128, NT, 2, 128], BF16)
        QDst = sbs(esD, "d_QD", [128, NT, 2, 128], BF16)
        KSst = sbs(esD, "d_KS", [128, NT, 2, 128], BF16)
        egl = sbs(esD, "d_egl", [128, NT, 2], F32)
        Oacc = sbs(esD, "d_Oacc", [128, NT, 128], F32)
        zsh = sbs(esD, "d_zsh", [128, NT, 128], BF16)
        oTd = sbs(esD, "d_oTd", [128, S], BF16)
        S32 = [sbs(esD, f"d_S32{d}", [128, 128], F32) for d in range(2)]
        Sbf = [sbs(esD, f"d_Sbf{d}", [128, 128], BF16) for d in range(2)]
        Vn = [sbs(esD, f"d_Vn{d}", [128, 128], BF16) for d in range(2)]
        ost = sbs(esD, "d_ost", [128, 8], F32)
        on = sbs(esD, "d_on", [128, 128], F32)
        onb = sbs(esD, "d_onb", [128, 128], BF16)
        G = 4
        QSC = float(128 ** -0.5)
        for h in dn_heads:
            esC = ExitStack()
            xpad = sbs(esC, f"d_xpad_{h}", [128, S + 4], F32)
            acc = sbs(esC, f"d_acc_{h}", [128, S], F32)
            cw = sbs(esC, f"d_cw_{h}", [128, 5], F32)
            rst = sbs(esC, f"d_rst_{h}", [128, 512], F32)
            sqb = sbs(esC, f"d_sqb_{h}", [128, 512], BF16)
            P.op('dve', lambda e: e.memset(xpad[:, 0:2], 0.0), w=['d_xpad'])
            P.op('dve', lambda e: e.memset(xpad[:, S + 2:S + 4], 0.0), w=['d_xpad'])
            for ci in range(3):
                ch = ci * 8 + h
                P.op('sp', lambda e: e.dma_start(out=cw[:], in_=conv_wT[ch * 128:(ch + 1) * 128, :]), w=['d_cw'], dma=True)
                P.op('pool', lambda e: e.dma_start(out=xpad[:, 2:S + 2], in_=qkvT[ch, :, :]), r=[('scr', 'dst_qkv')], w=['d_xpad'], dma=True)
                eng = 'dve'
                P.op(eng, lambda e: e.tensor_scalar(out=acc[:], in0=xpad[:, 0:S], scalar1=cw[:, 0:1], scalar2=None, op0=ALU.mult), r=['d_xpad', 'd_cw'], w=['d_acc'])
                for j in range(1, 5):
                    P.op(eng, lambda e: e.scalar_tensor_tensor(out=acc[:], in0=xpad[:, j:j + S], scalar=cw[:, j:j + 1], in1=acc[:], op0=ALU.mult, op1=ALU.add),
                         r=['d_xpad', 'd_cw', 'd_acc'], w=['d_acc'])
                P.op('act', lambda e: e.activation(out=acc[:], in_=acc[:], func=AF.Silu), r=['d_acc'], w=['d_acc'])
                if ci == 2:
                    P.op('dve', lambda e: e.tensor_copy(out=QKV[2][:], in_=acc[:]), r=['d_acc'], w=['d_qkv2'])
                else:
                    for g in range(8):
                        gs_ = slice(g * 512, (g + 1) * 512)
                        P.op('act', lambda e: e.activation(out=sqb[:], in_=acc[:, gs_], func=AF.Square), r=['d_acc'], w=['d_sqb'])
                        P.op('pe', lambda e: e.matmul(pM[:], lhsT=onesb[:], rhs=sqb[:], start=True, stop=True), r=['d_onesb', 'd_sqb'], w=[kM])
                        P.op('act', lambda e: e.activation(out=rst[:], in_=pM[:], func=AF.Sqrt, bias=eps_l2[:], scale=1.0), r=[kM, 'eps_l2'], w=['d_rst'])
                        P.op('dve', lambda e: e.reciprocal(out=rst[:], in_=rst[:]), r=['d_rst'], w=['d_rst'])
                        P.op('dve', lambda e: e.scalar_tensor_tensor(out=QKV[ci][:, gs_], in0=acc[:, gs_], scalar=(QSC if ci == 0 else 1.0), in1=rst[:], op0=ALU.mult, op1=ALU.mult),
                             r=['d_acc', 'd_rst'], w=[f'd_qkv{ci}'])
            Qt, Kt, Vch = QKV
            stop(2)
            P.barrier()
            esC.close()
            esW = ExitStack()
            Ktok = sbs(esW, f"d_Ktok_{h}", [128, 2, 128], BF16)
            Vtok = sbs(esW, f"d_Vtok_{h}", [128, 2, 128], BF16)
            Dg = sbs(esW, f"d_Dg_{h}", [128, G, 128], F32)
            tA = sbs(esW, f"d_tA_{h}", [128, G, 128], F32)
            tI = sbs(esW, f"d_tI_{h}", [128, G, 128], F32)
            EGB = sbs(esW, f"d_EGB_{h}", [128, G, 128], F32)
            A32 = sbs(esW, f"d_A32_{h}", [128, G, 128], F32)
            AT32 = sbs(esW, f"d_AT32_{h}", [128, G, 128], F32)
            ZY = sbs(esW, f"d_ZY_{h}", [128, G, 2, 128], F32)
            ZTYT = sbs(esW, f"d_ZTYT_{h}", [128, G, 2, 128], F32)
            Lb = sbs(esW, f"d_Lb_{h}", [128, G, 128], BF16)
            LTb = sbs(esW, f"d_LTb_{h}", [128, G, 128], BF16)
            Qb = sbs(esW, f"d_Qb_{h}", [128, G, 128], BF16)
            Rb = sbs(esW, f"d_Rb_{h}", [128, G, 128], BF16)
            TT = sbs(esW, f"d_TT_{h}", [128, G, 128], BF16)
            Tb = sbs(esW, f"d_Tb_{h}", [128, G, 128], BF16)
            ZYb = sbs(esW, f"d_ZYb_{h}", [128, G, 2, 128], BF16)
            ZTYTb = sbs(esW, f"d_ZTYTb_{h}", [128, G, 2, 128], BF16)
            Kbe = sbs(esW, f"d_Kbe_{h}", [128, G, 128], BF16)
            Vb = sbs(esW, f"d_Vb_{h}", [128, G, 128], BF16)
            egc = sbs(esW, f"d_egc_{h}", [128, G], F32)
            ebh = sbs(esW, f"d_ebh_{h}", [128, NT, 2], F32)
            for d_ in range(2):
                P.op('act', lambda e: e.activation(out=ebh[:, :, d_], in_=tokS[:, :, d_ * 24 + 16 + h], func=AF.Exp), r=['tokS'], w=['d_ebh'])
                P.op('dve', lambda e: e.tensor_tensor(out=ebh[:, :, d_], in0=ebh[:, :, d_], in1=tokS[:, :, d_ * 24 + h], op=ALU.mult), r=['d_ebh', 'tokS'], w=['d_ebh'])
            zs_v = zs[:, h * 128:(h + 1) * 128].rearrange("(t p) c -> p t c", p=128)
            for q4 in range(8):
                P.op('sp', lambda e: e.dma_start(out=zsh[:, q4 * 4:(q4 + 1) * 4, :], in_=zs_v[:, q4 * 4:(q4 + 1) * 4, :]),
                     r=[('scr', 'zs', t_, b_) for t_ in range(q4 * 4, q4 * 4 + 4) for b_ in range(2)], w=['d_zsh'], dma=True)
            for t0 in range(0, NT, 2):
                units = [(ti, d) for ti in range(2) for d in range(2)]
                fl = []
                for ti in range(2):
                    ts_ = slice((t0 + ti) * 128, (t0 + ti + 1) * 128)
                    fl.append(lambda e, ti=ti, ts_=ts_: e.transpose(out=psT[0][:, ti * 256:ti * 256 + 128], in_=Kt[:, ts_], identity=identb[:]))
                    fl.append(lambda e, ti=ti, ts_=ts_: e.transpose(out=psT[0][:, ti * 256 + 128:ti * 256 + 256], in_=Vch[:, ts_], identity=identb[:]))
                    fl.append(lambda e, ti=ti, ts_=ts_: e.matmul(pM[:, ti * 256:ti * 256 + 128], lhsT=Kt[:, ts_], rhs=Kt[:, ts_], start=True, stop=True))
                    fl.append(lambda e, ti=ti, ts_=ts_: e.matmul(pM[:, ti * 256 + 128:ti * 256 + 256], lhsT=Kt[:, ts_], rhs=Qt[:, ts_], start=True, stop=True))
                P.pe_group(fl, r=['d_qkv0', 'd_qkv1', 'd_qkv2', 'identb'], w=['psT0', kM])
                pT4 = psT[0][:].rearrange("p (a b n) -> p a b n", a=2, b=2)
                P.op('act', lambda e: e.copy(out=Ktok[:], in_=pT4[:, :, 0, :]), r=['psT0'], w=['d_Ktok'])
                P.op('act', lambda e: e.copy(out=Vtok[:], in_=pT4[:, :, 1, :]), r=['psT0'], w=['d_Vtok'])
                stop(31)
                for u, (ti, d) in enumerate(units):
                    t = t0 + ti
                    gcol = tokS[:, t, d * 24 + 16 + h:d * 24 + 17 + h]
                    P.op('dve', lambda e: e.tensor_scalar(out=Dg[:, u, :], in0=ident[:], scalar1=gcol, scalar2=None, op0=ALU.mult), r=['ident', 'tokS'], w=[('d_Dg', u)])
                P.pe_group([(lambda e, u=u: e.matmul(pG[:, u * 128:(u + 1) * 128], lhsT=ones_f[:], rhs=Dg[:, u, :], start=True, stop=True)) for u in range(G)],
                           r=['ones_f'] + [('d_Dg', u) for u in range(G)], w=[kG])
                stop(32)
                P.op('act', lambda e: e.copy(out=EGB[:], in_=pG[:].rearrange("p (u n) -> p u n", u=G)), r=[kG], w=['d_GBs'])
                for u, (ti, d) in enumerate(units):
                    t = t0 + ti
                    gcol = tokS[:, t, d * 24 + 16 + h:d * 24 + 17 + h]
                    P.op('dve', lambda e: e.scalar_tensor_tensor(out=tA[:, u, :], in0=EGB[:, u, :], scalar=gcol, in1=msk[:, 2 * d, :], op0=ALU.subtract, op1=ALU.max),
                         r=['d_GBs', 'tokS', 'd_msk'], w=[('d_tA', u)])
                    P.op('dve', lambda e: e.scalar_tensor_tensor(out=tI[:, u, :], in0=EGB[:, u, :], scalar=gcol, in1=msk[:, 2 * d + 1, :], op0=ALU.subtract, op1=ALU.min),
                         r=['d_GBs', 'tokS', 'd_msk'], w=[('d_tI', u)])
                kTA = [('d_tA', u) for u in range(G)]
                kTI = [('d_tI', u) for u in range(G)]
                P.op('act', lambda e: e.activation(out=tA[:], in_=tA[:], func=AF.Exp, scale=-1.0), r=kTA, w=kTA)
                P.op('act', lambda e: e.activation(out=tI[:], in_=tI[:], func=AF.Exp), r=kTI, w=kTI)
                P.op('act', lambda e: e.activation(out=EGB[:], in_=EGB[:], func=AF.Exp), r=['d_GBs'] + kTA + kTI, w=['d_GBs'])
                stop(33)
                for u, (ti, d) in enumerate(units):
                    t = t0 + ti
                    ts_ = slice(t * 128, (t + 1) * 128)
                    bcol = tokS[:, t, d * 24 + h:d * 24 + h + 1]
                    lastc = 127 if d == 0 else 0
                    P.op('dve', lambda e: e.scalar_tensor_tensor(out=A32[:, u, :], in0=pM[:, ti * 256:ti * 256 + 128], scalar=bcol, in1=tA[:, u, :], op0=ALU.mult, op1=ALU.mult),
                         r=[kM, 'tokS', ('d_tA', u)], w=[('d_A32', u)])
                P.pe_group([(lambda e, u=u: e.transpose(out=pX0[:, u * 128:(u + 1) * 128], in_=A32[:, u, :], identity=ident[:])) for u in range(G)],
                           r=[('d_A32', u) for u in range(G)] + ['ident'], w=[kX0])
                for u, (ti, d) in enumerate(units):
                    t = t0 + ti
                    ts_ = slice(t * 128, (t + 1) * 128)
                    bcol = tokS[:, t, d * 24 + h:d * 24 + h + 1]
                    lastc = 127 if d == 0 else 0
                    P.op('dve', lambda e: e.tensor_tensor(out=ITst[:, t, d, :], in0=pM[:, ti * 256 + 128:ti * 256 + 256], in1=tI[:, u, :], op=ALU.mult),
                         r=[kM, ('d_tI', u)], w=[('d_IT', t, d)])
                    P.op('dve', lambda e: e.tensor_tensor(out=QDst[:, t, d, :], in0=Qt[:, ts_], in1=EGB[:, u, :], op=ALU.mult), r=['d_qkv0', 'd_GBs'], w=[('d_QD', t, d)])
                    P.op('act', lambda e: e.copy(out=egl[:, t, d:d + 1], in_=EGB[:, u, lastc:lastc + 1]), r=['d_GBs'], w=[('d_egl', t, d)])
                    P.op('act', lambda e: e.activation(out=KSst[:, t, d, :], in_=Ktok[:, ti, :], func=AF.Copy, scale=tI[:, u, lastc:lastc + 1]),
                         r=['d_Ktok', ('d_tI', u)], w=[('d_KS', t, d)])
                    P.op('act', lambda e: e.activation(out=Kbe[:, u, :], in_=Ktok[:, ti, :], func=AF.Copy, scale=ebh[:, t, d:d + 1]),
                         r=['d_Ktok', 'd_ebh'], w=[('d_Kbe', u)])
                    P.op('act', lambda e: e.activation(out=Vb[:, u, :], in_=Vtok[:, ti, :], func=AF.Copy, scale=bcol), r=['d_Vtok', 'tokS'], w=[('d_Vb', u)])
                stop(34)
                kA = [('d_A32', u) for u in range(G)]
                v3 = lambda p_: p_[:].rearrange("p (u n) -> p u n", u=G)
                lmv = lambda j_: lm[:, j_, :].rearrange("p (u n) -> p u n", u=G)
                P.op('act', lambda e: e.copy(out=AT32[:], in_=v3(pX0)), r=[kX0], w=['d_AT32'])
                P.op('dve', lambda e: e.scalar_tensor_tensor(out=ZY[:, :, 0, :], in0=A32[:], scalar=-1.0, in1=lmv(0), op0=ALU.mult, op1=ALU.mult), r=kA + ['d_lm'], w=['d_ZY'])
                P.op('dve', lambda e: e.scalar_tensor_tensor(out=ZTYT[:, :, 0, :], in0=AT32[:], scalar=-1.0, in1=lmv(0), op0=ALU.mult, op1=ALU.mult), r=['d_AT32', 'd_lm'], w=['d_ZTYT'])
                P.op('pool', lambda e: e.tensor_tensor(out=ZY[:, :, 1, :], in0=ZY[:, :, 0, :], in1=lmv(4), op=ALU.add), r=['d_ZY', 'd_lm'], w=['d_ZY'])
                P.op('dve', lambda e: e.tensor_tensor(out=ZTYT[:, :, 1, :], in0=ZTYT[:, :, 0, :], in1=lmv(4), op=ALU.add), r=['d_ZTYT', 'd_lm'], w=['d_ZTYT'])
                stop(35)
                P.op('act', lambda e: e.copy(out=ZYb[:], in_=ZY[:]), r=['d_ZY'], w=['d_ZYb'])
                P.op('dve', lambda e: e.tensor_copy(out=ZTYTb[:], in_=ZTYT[:]), r=['d_ZTYT'], w=['d_ZTYTb'])
                P.pe_group([f_ for u in range(G) for f_ in (
                    (lambda e, u=u: e.matmul(pX0[:, u * 128:(u + 1) * 128], lhsT=ZTYTb[:, u, 0, :], rhs=ZYb[:, u, 0, :], start=True, stop=True)),
                    (lambda e, u=u: e.matmul(pX1[:, u * 128:(u + 1) * 128], lhsT=ZYb[:, u, 0, :], rhs=ZTYTb[:, u, 0, :], start=True, stop=True)))],
                    r=['d_ZYb', 'd_ZTYTb'], w=[kX0, kX1])
                P.op('act', lambda e: e.copy(out=ZYb[:, :, 0, :], in_=v3(pX0)), r=[kX0], w=['d_ZYb'])
                P.op('dve', lambda e: e.tensor_copy(out=ZTYTb[:, :, 0, :], in_=v3(pX1)), r=[kX1], w=['d_ZTYTb'])
                for lvl in (1, 2):
                    P.pe_group([f_ for u in range(G) for f_ in (
                        (lambda e, u=u: e.matmul((pX0 if u < 2 else pX1)[:, (u % 2) * 256:(u % 2) * 256 + 256], lhsT=ZTYTb[:, u, 0, :], rhs=ZYb[:, u, :, :].rearrange("p c n -> p (c n)"), start=True, stop=True)),
                        (lambda e, u=u: e.matmul((pZT if u < 2 else pU)[:, (u % 2) * 256:(u % 2) * 256 + 256], lhsT=ZYb[:, u, 0, :], rhs=ZTYTb[:, u, :, :].rearrange("p c n -> p (c n)"), start=True, stop=True)))],
                        r=['d_ZYb', 'd_ZTYTb'], w=[kX0, kX1, kZT, kU])
                    for hf, (px, kx, pz, kz) in enumerate(((pX0, kX0, pZT, kZT), (pX1, kX1, pU, kU))):
                        p4 = px[:].rearrange("p (u c n) -> p u c n", u=2, c=2)
                        z4 = pz[:].rearrange("p (u c n) -> p u c n", u=2, c=2)
                        hs2 = slice(hf * 2, hf * 2 + 2)
                        P.op('act', lambda e: e.copy(out=ZYb[:, hs2, 0, :], in_=p4[:, :, 0, :]), r=[kx], w=['d_ZYb'])
                        P.op('dve', lambda e: e.tensor_tensor(out=ZY[:, hs2, 1, :], in0=p4[:, :, 1, :], in1=ZY[:, hs2, 1, :], op=ALU.add), r=[kx, 'd_ZY'], w=['d_ZY'])
                        P.op('act', lambda e: e.copy(out=ZTYTb[:, hs2, 0, :], in_=z4[:, :, 0, :]), r=[kz], w=['d_ZTYTb'])
                        P.op('dve', lambda e: e.tensor_tensor(out=ZTYT[:, hs2, 1, :], in0=z4[:, :, 1, :], in1=ZTYT[:, hs2, 1, :], op=ALU.add), r=[kz, 'd_ZTYT'], w=['d_ZTYT'])
                    P.op('act', lambda e: e.copy(out=ZYb[:, :, 1, :], in_=ZY[:, :, 1, :]), r=['d_ZY'], w=['d_ZYb'])
                    P.op('dve', lambda e: e.tensor_copy(out=ZTYTb[:, :, 1, :], in_=ZTYT[:, :, 1, :]), r=['d_ZTYT'], w=['d_ZTYTb'])
                P.pe_group([f_ for u in range(G) for f_ in (
                    (lambda e, u=u: e.matmul(pX0[:, u * 128:(u + 1) * 128], lhsT=ZTYTb[:, u, 0, :], rhs=ZYb[:, u, 1, :], start=True, stop=True)),
                    (lambda e, u=u: e.matmul(pX1[:, u * 128:(u + 1) * 128], lhsT=ZYb[:, u, 0, :], rhs=ZTYTb[:, u, 1, :], start=True, stop=True)))],
                    r=['d_ZYb', 'd_ZTYTb'], w=[kX0, kX1])
                P.op('dve', lambda e: e.tensor_tensor(out=ZY[:, :, 1, :], in0=v3(pX0), in1=ZY[:, :, 1, :], op=ALU.add), r=[kX0, 'd_ZY'], w=['d_ZY'])
                P.op('dve', lambda e: e.tensor_tensor(out=ZTYT[:, :, 1, :], in0=v3(pX1), in1=ZTYT[:, :, 1, :], op=ALU.add), r=[kX1, 'd_ZTYT'], w=['d_ZTYT'])
                P.op('act', lambda e: e.copy(out=TT[:], in_=ZTYT[:, :, 1, :]), r=['d_ZTYT'], w=['d_TT'])
                P.op('dve', lambda e: e.tensor_copy(out=Tb[:], in_=ZY[:, :, 1, :]), r=['d_ZY'], w=['d_Tb'])
                for li in range(3):
                    last = (li == 2)
                    P.op('pool', lambda e: e.tensor_tensor(out=Lb[:], in0=A32[:], in1=lmv(1 + li), op=ALU.mult), r=kA + ['d_lm'], w=['d_Lb'])
                    if not last:
                        P.op('dve', lambda e: e.tensor_tensor(out=LTb[:], in0=AT32[:], in1=lmv(1 + li), op=ALU.mult), r=['d_AT32', 'd_lm'], w=['d_LTb'])
                    fl = [(lambda e, u=u: e.matmul(pX1[:, u * 128:(u + 1) * 128], lhsT=Lb[:, u, :], rhs=TT[:, u, :], start=True, stop=True)) for u in range(G)]
                    if not last:
                        fl += [(lambda e, u=u: e.matmul(pX0[:, u * 128:(u + 1) * 128], lhsT=LTb[:, u, :], rhs=Tb[:, u, :], start=True, stop=True)) for u in range(G)]
                    P.pe_group(fl, r=['d_Lb', 'd_TT'] + ([] if last else ['d_LTb', 'd_Tb']), w=[kX1] + ([] if last else [kX0]))
                    P.op('act', lambda e: e.copy(out=Rb[:], in_=v3(pX1)), r=[kX1], w=['d_Rb'])
                    if not last:
                        P.op('dve', lambda e: e.tensor_copy(out=Qb[:], in_=v3(pX0)), r=[kX0], w=['d_Qb'])
                    fl = [(lambda e, u=u: e.matmul(pU[:, u * 128:(u + 1) * 128], lhsT=Tb[:, u, :], rhs=Rb[:, u, :], start=True, stop=True)) for u in range(G)]
                    if not last:
                        fl += [(lambda e, u=u: e.matmul(pZT[:, u * 128:(u + 1) * 128], lhsT=TT[:, u, :], rhs=Qb[:, u, :], start=True, stop=True)) for u in range(G)]
                    P.pe_group(fl, r=['d_Tb', 'd_Rb'] + ([] if last else ['d_TT', 'd_Qb']), w=[kU] + ([] if last else [kZT]))
                    if not last:
                        P.op('dve', lambda e: e.tensor_tensor(out=ZTYT[:, :, 1, :], in0=ZTYT[:, :, 1, :], in1=v3(pU), op=ALU.subtract), r=[kU, 'd_ZTYT'], w=['d_ZTYT'])
                        P.op('dve', lambda e: e.tensor_tensor(out=ZY[:, :, 1, :], in0=ZY[:, :, 1, :], in1=v3(pZT), op=ALU.subtract), r=[kZT, 'd_ZY'], w=['d_ZY'])
                        P.op('act', lambda e: e.copy(out=TT[:], in_=ZTYT[:, :, 1, :]), r=['d_ZTYT'], w=['d_TT'])
                        P.op('act', lambda e: e.copy(out=Tb[:], in_=ZY[:, :, 1, :]), r=['d_ZY'], w=['d_Tb'])
                    else:
                        P.op('dve', lambda e: e.tensor_tensor(out=TT[:], in0=ZTYT[:, :, 1, :], in1=v3(pU), op=ALU.subtract), r=[kU, 'd_ZTYT'], w=['d_TT'])
                stop(36)
                P.pe_group([f_ for u in range(G) for f_ in (
                    (lambda e, u=u: e.matmul(pU[:, u * 128:(u + 1) * 128], lhsT=TT[:, u, :], rhs=Vb[:, u, :], start=True, stop=True)),
                    (lambda e, u=u: e.matmul(pG[:, u * 128:(u + 1) * 128], lhsT=Kbe[:, u, :], rhs=TT[:, u, :], start=True, stop=True)))],
                    r=['d_TT'] + [('d_Vb', u) for u in range(G)] + [('d_Kbe', u) for u in range(G)], w=[kU, kG])
                P.op('act', lambda e: e.copy(out=Ust[:, t0:t0 + 2, :, :].rearrange("p a b n -> p (a b) n"), in_=pU[:].rearrange("p (u n) -> p u n", u=G)), r=[kU], w=[('d_U', t0)])
                P.op('dve', lambda e: e.tensor_copy(out=WTst[:, t0:t0 + 2, :, :].rearrange("p a b n -> p (a b) n"), in_=pG[:].rearrange("p (u n) -> p u n", u=G)), r=[kG], w=[('d_WT', t0)])
                stop(3)
            P.barrier()
            stop(4)
            for d in range(2):
                P.op('dve', lambda e: e.memset(S32[d][:], 0.0), w=[f'd_S32{d}'])
                P.op('dve', lambda e: e.memset(Sbf[d][:], 0.0), w=[f'd_Sbf{d}'])
            for step in range(NT):
                tt = [step, NT - 1 - step]
                bank = [((pM, kM), (pX0, kX0), (pZT, kZT)), ((pG, kG), (pX1, kX1), (pU, kU))]
                for d in range(2):
                    t = tt[d]; t0 = (t // 2) * 2
                    (pW, kW) = bank[d][0]
                    P.op('pe', lambda e: e.matmul(pW[:, 0:128], lhsT=WTst[:, t, d, :], rhs=Sbf[d][:], start=True, stop=True), r=[('d_WT', t0), f'd_Sbf{d}'], w=[kW])
                for d in range(2):
                    t = tt[d]; t0 = (t // 2) * 2
                    (pW, kW), (pO, kO) = bank[d][0], bank[d][1]
                    P.op('dve', lambda e: e.tensor_tensor(out=Vn[d][:], in0=Ust[:, t, d, :], in1=pW[:, 0:128], op=ALU.subtract), r=[('d_U', t0), kW], w=[f'd_Vn{d}'])
                    P.op('pe', lambda e: e.matmul(pO[:, 0:128], lhsT=QDst[:, t, d, :], rhs=Sbf[d][:], start=True, stop=False), r=[('d_QD', t, d), f'd_Sbf{d}'], w=[kO])
                for d in range(2):
                    t = tt[d]
                    (pO, kO), (pD, kD) = bank[d][1], bank[d][2]
                    P.op('pe', lambda e: e.matmul(pO[:, 0:128], lhsT=ITst[:, t, d, :], rhs=Vn[d][:], start=False, stop=True), r=[('d_IT', t, d), f'd_Vn{d}'], w=[kO])
                    P.op('pe', lambda e: e.matmul(pD[:, 0:128], lhsT=KSst[:, t, d, :], rhs=Vn[d][:], start=True, stop=True), r=[('d_KS', t, d), f'd_Vn{d}'], w=[kD])
                for d in range(2):
                    t = tt[d]
                    (pO, kO), (pD, kD) = bank[d][1], bank[d][2]
                    P.op('dve', lambda e: e.scalar_tensor_tensor(out=Sbf[d][:], in0=S32[d][:], scalar=egl[:, t, d:d + 1], in1=pD[:, 0:128], op0=ALU.mult, op1=ALU.add),
                         r=[f'd_S32{d}', ('d_egl', t, d), kD], w=[f'd_Sbf{d}'])
                    P.op('dve', lambda e: e.scalar_tensor_tensor(out=S32[d][:], in0=S32[d][:], scalar=egl[:, t, d:d + 1], in1=pD[:, 0:128], op0=ALU.mult, op1=ALU.add),
                         r=[f'd_S32{d}', ('d_egl', t, d), kD], w=[f'd_S32{d}'])
                    if step < NT // 2:
                        P.op('act', lambda e: e.copy(out=Oacc[:, t, :], in_=pO[:, 0:128]), r=[kO], w=[('d_Oacc', t)])
                    else:
                        P.op('pool', lambda e: e.tensor_tensor(out=Oacc[:, t, :], in0=Oacc[:, t, :], in1=Oacc[:, t, :], op=ALU.add), r=[('d_Oacc', t)], w=[('d_Oacc', t)]) if False else \
                            P.op('dve', lambda e: e.tensor_tensor(out=Oacc[:, t, :], in0=pO[:, 0:128], in1=Oacc[:, t, :], op=ALU.add), r=[kO, ('d_Oacc', t)], w=[('d_Oacc', t)])
            stop(5)
            osq = [sbs(esW, f"d_osq{i_}_{h}", [128, 128], F32) for i_ in range(2)]
            ors = sbs(esW, f"d_ors_{h}", [128, 2, NT], F32)
            ont = [sbs(esW, f"d_ont{i_}_{h}", [128, 128], F32) for i_ in range(4)]
            onbt = [sbs(esW, f"d_onbt{i_}_{h}", [128, 128], BF16) for i_ in range(4)]
            kO_all = [('d_Oacc', t_) for t_ in range(NT)]
            P.op('dve', lambda e: e.memset(ors[:], 0.0), w=['d_ors'])
            for t in range(NT):
                P.op('act', lambda e: e.activation(out=osq[t % 2][:], in_=Oacc[:, t, :], func=AF.Square, accum_out=ors[:, 0, t:t + 1]), r=[('d_Oacc', t), 'd_ors'], w=[f'd_osq{t % 2}', ('d_ors_acc', t)])
            P.op('act', lambda e: e.activation(out=ors[:, 1, :], in_=ors[:, 0, :], func=AF.Sqrt, scale=1.0 / 128, bias=eps_l2[:]), r=['d_ors', 'eps_l2'] + [('d_ors_acc', t_) for t_ in range(NT)], w=['d_ors'])
            P.op('dve', lambda e: e.reciprocal(out=ors[:, 1, :], in_=ors[:, 1, :]), r=['d_ors'], w=['d_ors'])
            for t4 in range(0, NT, 4):
                for j in range(4):
                    t = t4 + j
                    P.op('dve', lambda e: e.scalar_tensor_tensor(out=ont[j][:], in0=Oacc[:, t, :], scalar=ors[:, 1, t:t + 1], in1=gain[:], op0=ALU.mult, op1=ALU.mult),
                         r=[('d_Oacc', t), 'd_ors', 'd_gain'], w=[f'd_ont{j}'])
                    P.op('pool', lambda e: e.tensor_tensor(out=onbt[j][:], in0=ont[j][:], in1=zsh[:, t, :], op=ALU.mult), r=[f'd_ont{j}', 'd_zsh'], w=[f'd_onbt{j}'])
                    P.op('pe', lambda e: e.transpose(out=psT[1][:, j * 128:(j + 1) * 128], in_=onbt[j][:], identity=identb[:]), r=[f'd_onbt{j}', 'identb'], w=['psT1'])
                P.op('act', lambda e: e.copy(out=oTd[:, t4 * 128:(t4 + 4) * 128], in_=psT[1][:]), r=['psT1'], w=['d_oTd'])
            fin.append(P.op('sp', lambda e: e.dma_start(out=oT_dn[h, :, :], in_=oTd[:]), r=['d_oTd'], w=[('scr', 'oT_dn', h)], dma=True))
            P.barrier()
            esW.close()
        P.barrier()
        esD.close()

    def phase_MG_MOE():
        esP = ExitStack()
        affTM = sbs(esP, "affTM", [128, NT, 16], F32)
        posm = sbs(esP, "posm", [128, NT, 16], F32)
        iot = sbs(esP, "iot", [128, 512], F32)
        bcs = sbs(esP, "bcs", [128, 4, D], F32)
        gfin = sbs(esP, "gfin", [128, D], F32)
        onesb = sbs(esP, "m_onesb", [128, 128], BF16)
        trib = sbs(esP, "trib", [128, 128], BF16)
        st2 = sbs(esP, "st2", [128, 8], F32)
        P.op('dve', lambda e: e.memset(onesb[:], 1.0), w=['m_onesb'])
        P.op('sp', lambda e: e.dma_start(out=iot[:], in_=iota512[:, :]), w=['iot'], dma=True)
        P.op('sp', lambda e: e.dma_start(out=gfin[:], in_=g_final[:, :]), w=['gfin'], dma=True)
        esH = ExitStack()
        h2b = sbs(esH, "h2b", [128, NT, D], BF16)
        esT = ExitStack()
        affT = sbs(esT, "affT", [16, S], F32)
        esG = ExitStack()
        es1 = ExitStack()
        mrow = sbs(es1, "g_mrow", [1, 4 * D], F32)
        trif = sbs(es1, "g_trif", [128, 128], F32)
        gff = sbs(es1, "g_gff", [128, D], F32)
        P.op('sp', lambda e: e.dma_start(out=mrow[:], in_=modrow[:, 2 * D:6 * D]), r=['mod'], w=['g_mrow'], dma=True)
        P.op('sp', lambda e: e.dma_start(out=trif[:], in_=tri_in[:, :]), w=['g_trif'], dma=True)
        P.op('dve', lambda e: e.tensor_copy(out=trib[:], in_=trif[:]), r=['g_trif'], w=['trib'])
        P.op('sp', lambda e: e.dma_start(out=gff[:], in_=g_ffn[:, :]), w=['g_gff'], dma=True)
        for j in range(8):
            src = j // 2
            dsti = {0: 0, 1: 2, 2: 1, 3: 3}[src]
            pt, pk = next_psA()
            P.op('pe', lambda e: e.matmul(pt[:], lhsT=ones_f[0:1, :], rhs=mrow[:, j * 512:(j + 1) * 512], start=True, stop=True), r=['ones_f', 'g_mrow'], w=[pk])
            P.op('act', lambda e: e.copy(out=bcs[:, dsti, (j % 2) * 512:(j % 2 + 1) * 512], in_=pt[:]), r=[pk], w=['bcs'])
        P.op('dve', lambda e: e.scalar_tensor_tensor(out=bcs[:, 1, :], in0=bcs[:, 1, :], scalar=1.0, in1=gff[:], op0=ALU.add, op1=ALU.mult), r=['bcs', 'g_gff'], w=['bcs'])
        P.barrier()
        es1.close()
        wod = sbs(esG, "g_wod", [128, 8, D], BF16)
        wom = sbs(esG, "g_wom", [128, 8, D], BF16)
        wou = sbs(esG, "g_wou", [128, 8, D], BF16)
        wr = sbs(esG, "g_wr", [128, 8, 16], F32)
        for wt_, src_, k_ in ((wod, w_o_dn, 'g_wod'), (wom, w_o_mla, 'g_wom'), (wou, w_out, 'g_wou')):
            v = src_.rearrange("(k p) n -> p k n", p=128)
            for kk in range(0, 8, 2):
                P.op('pool', lambda e: e.dma_start(out=wt_[:, kk:kk + 2, :], in_=v[:, kk:kk + 2, :]), w=[k_], dma=True)
        P.op('sp', lambda e: e.dma_start(out=wr[:], in_=w_router.rearrange("(k p) n -> p k n", p=128)), w=['g_wr'], dma=True)
        odn = [sbs(esG, f"g_odn{i}", [128, 8, 128], BF16) for i in range(2)]
        oml = [sbs(esG, f"g_oml{i}", [128, 8, 128], BF16) for i in range(2)]
        gst = [sbs(esG, f"g_gst{i}", [128, 2 * D], BF16) for i in range(2)]
        xt0 = sbs(esG, "g_xt0", [128, D], F32)
        xt = [xt0, xt0]
        m1 = sbs(esG, "g_m1", [128, D], F32)
        m2 = sbs(esG, "g_m2", [128, D], F32)
        mb = sbs(esG, "g_mb", [128, D], BF16)
        mT = sbs(esG, "g_mT", [128, 8, 128], BF16)
        x1t = [sbs(esG, f"g_x1t{i}", [128, D], F32) for i in range(2)]
        h2f = sbs(esG, "g_h2f", [128, D], F32)
        h2T = sbs(esG, "g_h2T", [128, 8, 128], F32)
        lg = sbs(esG, "g_lg", [128, 16], F32)
        for t in range(NT):
            i = t % 2
            ts_ = slice(t * 128, (t + 1) * 128)
            P.op('sp', lambda e: e.dma_start(out=odn[i][:], in_=oT_dn[:, :, ts_].rearrange("h p n -> p h n")), r=[('scr', 'oT_dn', h_) for h_ in range(8)], w=[f'g_odn{i}'], dma=True)
            P.op('sp', lambda e: e.dma_start(out=oml[i][:], in_=oT_mla[:, :, ts_].rearrange("h p n -> p h n")), r=[('scr', 'oT_mla', h_) for h_ in range(8)], w=[f'g_oml{i}'], dma=True)
            P.op('sp', lambda e: e.dma_start(out=gst[i][:], in_=gs[ts_, :]), r=[('scr', 'gs', t, b_) for b_ in range(4)], w=[f'g_gst{i}'], dma=True)
            P.op('sp', lambda e: e.dma_start(out=xt[i][:], in_=x[ts_, :]), w=['g_xt0'], dma=True)
            for br, (o_, ok_, w_, wk_) in enumerate(((odn[i], f'g_odn{i}', wod, 'g_wod'), (oml[i], f'g_oml{i}', wom, 'g_wom'))):
                for hc in range(2):
                    pt, pk = psA[br * 2 + hc], f'psA{br * 2 + hc}'
                    P.pe_group([(lambda e, k=k: e.matmul(pt[:], lhsT=o_[:, k, :], rhs=w_[:, k, hc * 512:(hc + 1) * 512], start=(k == 0), stop=(k == 7))) for k in range(8)], r=[ok_, wk_], w=[pk])
            for hc in range(2):
                hs_ = slice(hc * 512, (hc + 1) * 512)
                P.op('dve', lambda e: e.tensor_tensor(out=m1[:, hs_], in0=psA[hc][:], in1=gst[i][:, hs_], op=ALU.mult), r=[f'psA{hc}', f'g_gst{i}'], w=['g_m1'])
                P.op('dve', lambda e: e.tensor_tensor(out=m2[:, hs_], in0=psA[2 + hc][:], in1=gst[i][:, D + hc * 512:D + (hc + 1) * 512], op=ALU.mult), r=[f'psA{2 + hc}', f'g_gst{i}'], w=['g_m2'])
            P.op('pool', lambda e: e.tensor_tensor(out=mb[:], in0=m1[:], in1=m2[:], op=ALU.add), r=['g_m1', 'g_m2'], w=['g_mb'])
            for half in range(2):
                P.pe_group([(lambda e, k4=k4: e.transpose(out=psT[half][:, k4 * 128:(k4 + 1) * 128], in_=mb[:, (half * 4 + k4) * 128:(half * 4 + k4 + 1) * 128], identity=identb[:])) for k4 in range(4)], r=['g_mb', 'identb'], w=[f'psT{half}'])
                P.op('act', lambda e: e.copy(out=mT[:, half * 4:(half + 1) * 4, :], in_=psT[half][:].rearrange("p (k n) -> p k n", k=4)), r=[f'psT{half}'], w=['g_mT'])
            for hc in range(2):
                hs_ = slice(hc * 512, (hc + 1) * 512)
                P.pe_group([(lambda e, k=k: e.matmul(psS[hc][:], lhsT=mT[:, k, :], rhs=wou[:, k, hs_], start=(k == 0), stop=(k == 7))) for k in range(8)], r=['g_mT', 'g_wou'], w=[f'psS{hc}'])
                P.op('dve', lambda e: e.tensor_tensor(out=x1t[i][:, hs_], in0=psS[hc][:], in1=bcs[:, 0, hs_], op=ALU.mult), r=[f'psS{hc}', 'bcs'], w=[f'g_x1t{i}'])
            P.op('pool', lambda e: e.tensor_tensor(out=x1t[i][:], in0=x1t[i][:], in1=xt[i][:], op=ALU.add), r=[f'g_x1t{i}', 'g_xt0'], w=[f'g_x1t{i}'])
            fin.append(P.op('sp', lambda e: e.dma_start(out=x1s[ts_, :], in_=x1t[i][:]), r=[f'g_x1t{i}'], w=[('scr', 'x1s', t)], dma=True))
            P.op('dve', lambda e: e.memset(st2[:], 0.0), w=['st2'])
            P.op('act', lambda e: e.activation(out=m1[:], in_=x1t[i][:], func=AF.Square, accum_out=st2[:, 0:1]), r=[f'g_x1t{i}'], w=['g_m1', 'st2'])
            P.op('act', lambda e: e.activation(out=st2[:, 1:2], in_=st2[:, 0:1], func=AF.Sqrt, scale=1.0 / D, bias=epsb[:]), r=['st2', 'epsb'], w=['st2'])
            P.op('dve', lambda e: e.reciprocal(out=st2[:, 2:3], in_=st2[:, 1:2]), r=['st2'], w=['st2'])
            P.op('dve', lambda e: e.scalar_tensor_tensor(out=h2f[:], in0=x1t[i][:], scalar=st2[:, 2:3], in1=bcs[:, 1, :], op0=ALU.mult, op1=ALU.mult), r=[f'g_x1t{i}', 'st2', 'bcs'], w=['g_h2f'])
            P.op('pool', lambda e: e.tensor_tensor(out=h2f[:], in0=h2f[:], in1=bcs[:, 2, :], op=ALU.add), r=['g_h2f', 'bcs'], w=['g_h2f'])
            P.op('act', lambda e: e.copy(out=h2b[:, t, :], in_=h2f[:]), r=['g_h2f'], w=[('h2b', t)])
            for half in range(2):
                P.pe_group([(lambda e, k4=k4: e.transpose(out=psA[half][:, k4 * 128:(k4 + 1) * 128], in_=h2f[:, (half * 4 + k4) * 128:(half * 4 + k4 + 1) * 128], identity=ident[:])) for k4 in range(4)], r=['g_h2f', 'ident'], w=[f'psA{half}'])
                P.op('act', lambda e: e.copy(out=h2T[:, half * 4:(half + 1) * 4, :], in_=psA[half][:].rearrange("p (k n) -> p k n", k=4)), r=[f'psA{half}'], w=['g_h2T'])
            P.pe_group([(lambda e, k=k: e.matmul(psA[2][:, 0:16], lhsT=h2T[:, k, :], rhs=wr[:, k, :], start=(k == 0), stop=(k == 7))) for k in range(8)], r=['g_h2T', 'g_wr'], w=['psA2'])
            P.op('dve', lambda e: e.tensor_reduce(out=st2[:, 3:4], in_=psA[2][:, 0:16], axis=AX.X, op=ALU.max), r=['psA2'], w=['st2'])
            P.op('dve', lambda e: e.tensor_scalar(out=st2[:, 4:5], in0=st2[:, 3:4], scalar1=-1.0, scalar2=None, op0=ALU.mult), r=['st2'], w=['st2'])
            P.op('dve', lambda e: e.memset(st2[:, 5:6], 0.0), r=['st2'], w=['st2'])
            P.op('act', lambda e: e.activation(out=lg[:], in_=psA[2][:, 0:16], func=AF.Exp, bias=st2[:, 4:5], scale=1.0, accum_out=st2[:, 5:6]), r=['psA2', 'st2'], w=['g_lg', 'st2'])
            P.op('dve', lambda e: e.reciprocal(out=st2[:, 6:7], in_=st2[:, 5:6]), r=['st2'], w=['st2'])
            P.op('dve', lambda e: e.tensor_scalar(out=affTM[:, t, :], in0=lg[:], scalar1=st2[:, 6:7], scalar2=None, op0=ALU.mult), r=['g_lg', 'st2'], w=[('affTM', t)])
            P.op('pe', lambda e: e.transpose(out=psA[3][0:16, 0:128], in_=affTM[:, t, :], identity=ident[:]), r=[('affTM', t), 'ident'], w=['psA3'])
            P.op('act', lambda e: e.copy(out=affT[:, ts_], in_=psA[3][0:16, 0:128]), r=['psA3'], w=['affT'])
        P.barrier()
        esG.close()
        esE = ExitStack()
        es1 = ExitStack()
        cmpb = sbs(es1, "e_cmp", [16, S], F32)
        bis = sbs(es1, "e_bis", [16, 8], F32)
        maskT = sbs(es1, "e_maskT", [16, S], F32)
        P.op('dve', lambda e: e.memset(bis[:], 0.0), w=['e_bis'])
        P.op('dve', lambda e: e.memset(bis[:, 1:2], 1.0), r=['e_bis'], w=['e_bis'])
        for it in range(32):
            P.op('dve', lambda e: e.tensor_tensor(out=bis[:, 2:3], in0=bis[:, 0:1], in1=bis[:, 1:2], op=ALU.add), r=['e_bis'], w=['e_bis'])
            P.op('dve', lambda e: e.tensor_scalar(out=bis[:, 2:3], in0=bis[:, 2:3], scalar1=0.5, scalar2=None, op0=ALU.mult), r=['e_bis'], w=['e_bis'])
            P.op('dve', lambda e: e.tensor_scalar(out=cmpb[:], in0=affT[:], scalar1=bis[:, 2:3], scalar2=None, op0=ALU.is_ge), r=['affT', 'e_bis'], w=['e_cmp'])
            P.op('dve', lambda e: e.tensor_reduce(out=bis[:, 3:4], in_=cmpb[:], axis=AX.X, op=ALU.add), r=['e_cmp'], w=['e_bis'])
            P.op('dve', lambda e: e.tensor_scalar(out=bis[:, 4:5], in0=bis[:, 3:4], scalar1=511.5, scalar2=None, op0=ALU.is_ge), r=['e_bis'], w=['e_bis'])
            P.op('dve', lambda e: e.tensor_tensor(out=bis[:, 5:6], in0=bis[:, 2:3], in1=bis[:, 0:1], op=ALU.subtract), r=['e_bis'], w=['e_bis'])
            P.op('dve', lambda e: e.tensor_tensor(out=bis[:, 6:7], in0=bis[:, 1:2], in1=bis[:, 2:3], op=ALU.subtract), r=['e_bis'], w=['e_bis'])
            P.op('dve', lambda e: e.scalar_tensor_tensor(out=bis[:, 0:1], in0=bis[:, 5:6], scalar=bis[:, 4:5], in1=bis[:, 0:1], op0=ALU.mult, op1=ALU.add), r=['e_bis'], w=['e_bis'])
            P.op('dve', lambda e: e.scalar_tensor_tensor(out=bis[:, 1:2], in0=bis[:, 6:7], scalar=bis[:, 4:5], in1=bis[:, 2:3], op0=ALU.mult, op1=ALU.add), r=['e_bis'], w=['e_bis'])
        P.op('dve', lambda e: e.tensor_scalar(out=maskT[:], in0=affT[:], scalar1=bis[:, 0:1], scalar2=None, op0=ALU.is_ge), r=['affT', 'e_bis'], w=['e_maskT'])
        mk32 = sbs(es1, "e_mk32", [128, 16], F32)
        mkb = sbs(es1, "e_mkb", [128, 16], BF16)
        base = sbs(es1, "e_base", [128, 16], F32)
        ptmp = sbs(es1, "e_ptmp", [128, 16], F32)
        P.op('dve', lambda e: e.memset(base[:], 0.0), w=['e_base'])
        for t in range(NT):
            ts_ = slice(t * 128, (t + 1) * 128)
            P.op('pe', lambda e: e.transpose(out=psA[0][:, 0:16], in_=maskT[:, ts_], identity=ident[0:16, 0:16]), r=['e_maskT', 'ident'], w=['psA0'])
            P.op('act', lambda e: e.copy(out=mk32[:], in_=psA[0][:, 0:16]), r=['psA0'], w=['e_mk32'])
            P.op('dve', lambda e: e.tensor_copy(out=mkb[:], in_=mk32[:]), r=['e_mk32'], w=['e_mkb'])
            P.op('pe', lambda e: e.matmul(psA[1][:, 0:16], lhsT=trib[:], rhs=mkb[:], start=True, stop=True), r=['trib', 'e_mkb'], w=['psA1'])
            P.op('pe', lambda e: e.matmul(psA[2][:, 0:16], lhsT=onesb[:], rhs=mkb[:], start=True, stop=True), r=['m_onesb', 'e_mkb'], w=['psA2'])
            P.op('dve', lambda e: e.tensor_tensor(out=ptmp[:], in0=psA[1][:, 0:16], in1=base[:], op=ALU.add), r=['psA1', 'e_base'], w=['e_ptmp'])
            P.op('dve', lambda e: e.scalar_tensor_tensor(out=ptmp[:], in0=ptmp[:], scalar=1.0, in1=mk32[:], op0=ALU.add, op1=ALU.mult), r=['e_ptmp', 'e_mk32'], w=['e_ptmp'])
            P.op('dve', lambda e: e.tensor_scalar(out=posm[:, t, :], in0=ptmp[:], scalar1=-1.0, scalar2=None, op0=ALU.add), r=['e_ptmp'], w=['posm'])
            P.op('dve', lambda e: e.tensor_tensor(out=base[:], in0=psA[2][:, 0:16], in1=base[:], op=ALU.add), r=['psA2', 'e_base'], w=['e_base'])
        P.barrier()
        es1.close()
        esT.close()
        wg = sbs(esE, "e_wg", [128, 8, D], BF16)
        wu = sbs(esE, "e_wu", [128, 8, D], BF16)
        wd = sbs(esE, "e_wd", [128, 8, D], BF16)
        Sel = sbs(esE, "e_Sel", [128, NT, 512], BF16)
        xeT = sbs(esE, "e_xeT", [128, 8, 512], BF16)
        hid = sbs(esE, "e_hid", [128, 8, 512], BF16)
        sg0 = sbs(esE, "e_sg0", [128, 512], F32)
        sg = [sg0, sg0]
        yeb = sbs(esE, "e_yeb", [128, 4, D], BF16)
        stgw = [sbs(esE, f"e_stg{i}", [128, D], F32) for i in range(2)]
        stg_n = [0]
        for ex in range(16):
            for wt_, src_, k_ in ((wg, w_gate, 'e_wg'), (wu, w_up, 'e_wu'), (wd, w_down, 'e_wd')):
                v = src_[ex].rearrange("(k p) n -> p k n", p=128)
                for kk in range(8):
                    if kk % 2 == 0:
                        P.op('pool', lambda e: e.dma_start(out=wt_[:, kk, :], in_=v[:, kk, :]), w=[k_], dma=True)
                    else:
                        j = stg_n[0] % 2
                        stg_n[0] += 1
                        P.op('sp', lambda e: e.dma_start(out=stgw[j][:], in_=v[:, kk, :]), w=[f'e_stg{j}'], dma=True)
                        P.op('act', lambda e: e.copy(out=wt_[:, kk, :], in_=stgw[j][:]), r=[f'e_stg{j}'], w=[k_])
            for t in range(NT):
                P.op('dve', lambda e: e.tensor_scalar(out=Sel[:, t, :], in0=iot[:], scalar1=posm[:, t, ex:ex + 1], scalar2=None, op0=ALU.is_equal), r=['iot', 'posm'], w=[('e_Sel', t)])
            for k in range(8):
                pt, pk = psA[k % 4], f'psA{k % 4}'
                P.pe_group([(lambda e, t=t: e.matmul(pt[:], lhsT=h2b[:, t, k * 128:(k + 1) * 128], rhs=Sel[:, t, :], start=(t == 0), stop=(t == NT - 1))) for t in range(NT)],
                           r=[('h2b', t) for t in range(NT)] + [('e_Sel', t) for t in range(NT)], w=[pk])
                if k % 2 == 0:
                    P.op('act', lambda e: e.copy(out=xeT[:, k, :], in_=pt[:]), r=[pk], w=['e_xeT'])
                else:
                    P.op('dve', lambda e: e.tensor_copy(out=xeT[:, k, :], in_=pt[:]), r=[pk], w=['e_xeT'])
            for f in range(8):
                j = f % 2
                pg, pgk = psA[j * 2], f'psA{j * 2}'
                pu, puk = psA[j * 2 + 1], f'psA{j * 2 + 1}'
                P.pe_group([(lambda e, k=k: e.matmul(pg[:], lhsT=wg[:, k, f * 128:(f + 1) * 128], rhs=xeT[:, k, :], start=(k == 0), stop=(k == 7))) for k in range(8)], r=['e_wg', 'e_xeT'], w=[pgk])
                P.pe_group([(lambda e, k=k: e.matmul(pu[:], lhsT=wu[:, k, f * 128:(f + 1) * 128], rhs=xeT[:, k, :], start=(k == 0), stop=(k == 7))) for k in range(8)], r=['e_wu', 'e_xeT'], w=[puk])
                P.op('act', lambda e: e.activation(out=sg[j][:], in_=pg[:], func=AF.Silu), r=[pgk], w=['e_sg0'])
                P.op('dve', lambda e: e.tensor_tensor(out=hid[:, f, :], in0=pu[:], in1=sg[j][:], op=ALU.mult), r=[puk, 'e_sg0'], w=['e_hid'])
            for q in range(4):
                for hc in range(2):
                    ps_y, pyk = psS[hc], f'psS{hc}'
                    P.pe_group([(lambda e, f=f: e.matmul(ps_y[:], lhsT=hid[:, f, q * 128:(q + 1) * 128], rhs=wd[:, f, hc * 512:(hc + 1) * 512], start=(f == 0), stop=(f == 7))) for f in range(8)], r=['e_hid', 'e_wd'], w=[pyk])
                    if hc == 0:
                        P.op('act', lambda e: e.copy(out=yeb[:, q, 0:512], in_=ps_y[:]), r=[pyk], w=['e_yeb'])
                    else:
                        P.op('dve', lambda e: e.tensor_copy(out=yeb[:, q, 512:1024], in_=ps_y[:]), r=[pyk], w=['e_yeb'])
            fin.append(P.op('sp', lambda e: e.dma_start(out=ye_all[ex].rearrange("(q p) d -> p q d", p=128), in_=yeb[:]), r=['e_yeb'], w=[('scr', 'ye', ex)], dma=True))
        P.barrier()
        esE.close()
        esH.close()
        esF = ExitStack()
        yeA = sbs(esF, "f_yeA", [128, 16, 4, D], BF16)
        for ex in range(16):
            P.op('sp', lambda e: e.dma_start(out=yeA[:, ex, :, :], in_=ye_all[ex].rearrange("(q p) d -> p q d", p=128)), r=[('scr', 'ye', ex)], w=['f_yeA'], dma=True)
        Sg = [sbs(esF, f"f_Sg{i}", [128, 512], BF16) for i in range(2)]
        SgT = sbs(esF, "f_SgT", [128, 16, 4, 128], BF16)
        x1l = [sbs(esF, f"f_x1l{i}", [128, D], F32) for i in range(2)]
        ft = sbs(esF, "f_ft", [128, D], F32)
        ot = [sbs(esF, f"f_ot{i}", [128, D], F32) for i in range(2)]
        junk2 = sbs(esF, "f_junk", [128, D], F32)
        for t in range(NT // 2):
            i = t % 2
            ts_ = slice(t * 128, (t + 1) * 128)
            P.op('sp', lambda e: e.dma_start(out=x1l[i][:], in_=x1s[ts_, :]), r=[('scr', 'x1s', t)], w=[f'f_x1l{i}'], dma=True)
            for ex in range(16):
                j = ex % 2
                P.op('dve', lambda e: e.tensor_scalar(out=Sg[j][:], in0=iot[:], scalar1=posm[:, t, ex:ex + 1], scalar2=affTM[:, t, ex:ex + 1], op0=ALU.is_equal, op1=ALU.mult),
                     r=['iot', 'posm', ('affTM', t)], w=[f'f_Sg{j}'])
                P.pe_group([(lambda e, q=q: e.transpose(out=psT[j][:, q * 128:(q + 1) * 128], in_=Sg[j][:, q * 128:(q + 1) * 128], identity=identb[:])) for q in range(4)], r=[f'f_Sg{j}', 'identb'], w=[f'psT{j}'])
                if j == 0:
                    P.op('act', lambda e: e.copy(out=SgT[:, ex, :, :], in_=psT[j][:].rearrange("p (q n) -> p q n", q=4)), r=[f'psT{j}'], w=[('f_SgT', ex)])
                else:
                    P.op('pool', lambda e: e.tensor_copy(out=SgT[:, ex, :, :], in_=psT[j][:].rearrange("p (q n) -> p q n", q=4)), r=[f'psT{j}'], w=[('f_SgT', ex)]) if False else \
                        P.op('act', lambda e: e.copy(out=SgT[:, ex, :, :], in_=psT[j][:].rearrange("p (q n) -> p q n", q=4)), r=[f'psT{j}'], w=[('f_SgT', ex)])
            for hc in range(2):
                hs_ = slice(hc * 512, (hc + 1) * 512)
                pt, pk = psA[(t % 2) * 2 + hc], f'psA{(t % 2) * 2 + hc}'
                P.pe_group([(lambda e, ex=ex, q=q: e.matmul(pt[:], lhsT=SgT[:, ex, q, :], rhs=yeA[:, ex, q, hs_], start=(ex == 0 and q == 0), stop=(ex == 15 and q == 3)))
                            for ex in range(16) for q in range(4)], r=[('f_SgT', ex) for ex in range(16)] + ['f_yeA'], w=[pk])
                P.op('dve', lambda e: e.tensor_tensor(out=ft[:, hs_], in0=pt[:], in1=bcs[:, 3, hs_], op=ALU.mult), r=[pk, 'bcs'], w=['f_ft'])
            P.op('pool', lambda e: e.tensor_tensor(out=ft[:], in0=ft[:], in1=x1l[i][:], op=ALU.add), r=['f_ft', f'f_x1l{i}'], w=['f_ft'])
            P.op('dve', lambda e: e.memset(st2[:], 0.0), w=['st2'])
            P.op('act', lambda e: e.activation(out=junk2[:], in_=ft[:], func=AF.Square, accum_out=st2[:, 0:1]), r=['f_ft'], w=['f_junk', 'st2'])
            P.op('act', lambda e: e.activation(out=st2[:, 1:2], in_=st2[:, 0:1], func=AF.Sqrt, scale=1.0 / D, bias=epsb[:]), r=['st2', 'epsb'], w=['st2'])
            P.op('dve', lambda e: e.reciprocal(out=st2[:, 2:3], in_=st2[:, 1:2]), r=['st2'], w=['st2'])
            P.op('dve', lambda e: e.scalar_tensor_tensor(out=ot[i][:], in0=ft[:], scalar=st2[:, 2:3], in1=gfin[:], op0=ALU.mult, op1=ALU.mult), r=['f_ft', 'st2', 'gfin'], w=[f'f_ot{i}'])
            fin.append(P.op('sp', lambda e: e.dma_start(out=out[ts_, :], in_=ot[i][:]), r=[f'f_ot{i}'], dma=True))
        P.barrier()
        esF.close()
        esP.close()

    if 'A' in stages:
        phase_A()
    if 'MLA' in stages:
        phase_MLA()
    if 'DN' in stages:
        try:
            phase_DN()
        except _Stop:
            P.barrier()
    if 'MG' in stages:
        phase_MG_MOE()
    P.finish(fin)
    print("instructions:", P.n)
    return nc


_invf = (10000.0 ** (-np.arange(32, dtype=np.float32) / np.float32(32))).astype(np.float32)
INVF2 = np.concatenate([_invf, _invf])[:, None].astype(np.float32)
SGN2 = np.concatenate([-np.ones(32), np.ones(32)])[:, None].astype(np.float32)
SEL64 = np.zeros((128, 65), np.float32)
SEL64[:, 64] = 1.0
_xi = np.arange(128)[:, None]
_yi = np.arange(128)[None, :]
BIG = 1.0e4
DMASKS = np.stack([np.where(_xi > _yi, 0.0, BIG), np.where(_yi >= _xi, 0.0, -BIG),
                   np.where(_xi < _yi, 0.0, BIG), np.where(_yi <= _xi, 0.0, -BIG)], axis=1).astype(np.float32)
def _bd(s_):
    return ((_xi // s_) == (_yi // s_)).astype(np.float32)
DN_LMASK = np.ascontiguousarray(np.stack([np.tile(m_, (1, 4)) for m_ in
                                          (_bd(16), _bd(32) - _bd(16), _bd(64) - _bd(32), 1.0 - _bd(64), np.eye(128, dtype=np.float32))], axis=1))
IOTA512 = np.ascontiguousarray(np.broadcast_to(np.arange(512, dtype=np.float32)[None, :], (128, 512)))
TRI = (_xi < _yi).astype(np.float32)
IN_SPL = np.cumsum([3072, 1024, 16, 16, 512, 256, 64, 2048])


def prep_inputs(inputs, core):
    b, half = core // 2, core % 2
    f = lambda a: np.ascontiguousarray(a, dtype=np.float32)
    w_in = inputs['w_in'][0]
    qkv, z, bb, aa, cq, ckv, kr, g = np.split(w_in, IN_SPL[:-1], axis=1)
    xb = inputs['x'][b]
    posb = inputs['positions'][b]
    conv_w = inputs['conv_w'][0]
    a_log, dt_bias = inputs['a_log'][0], inputs['dt_bias'][0]
    if half == 1:
        xb = xb[::-1]
        posb = posb[::-1]
        conv_w = conv_w[::-1]
        bb = np.concatenate([bb[:, 8:16], bb[:, 0:8]], axis=1)
        aa = np.concatenate([aa[:, 8:16], aa[:, 0:8]], axis=1)
        a_log, dt_bias = a_log[::-1], dt_bias[::-1]
    swap = np.concatenate([np.arange(32, 64), np.arange(0, 32)])
    wuq_ = inputs['w_uq'][0].reshape(512, 8, 192)
    wukv_ = inputs['w_ukv'][0].reshape(256, 8, 256)
    m = {
        'x': f(xb),
        'cT': f(inputs['c'][b].reshape(8, 128).T),
        'w_mod': f(inputs['w_mod'][0]),
        'b_mod': f(inputs['b_mod'][0][None, :]),
        'g_mix': f(np.broadcast_to(inputs['g_mix'][0][None, :], (128, D))),
        'w_qkv': f(qkv), 'w_z': f(z), 'w_g': f(g),
        'w_ba': f(np.concatenate([bb, aa], axis=1)),
        'w_cq': f(cq), 'w_ckv': f(ckv),
        'w_kr2': f(np.concatenate([kr, kr[:, swap]], axis=1)),
        'q_gain': f(np.broadcast_to(inputs['q_gain'][0][None, :], (128, 512))),
        'kv_gain': f(np.broadcast_to(inputs['kv_gain'][0][None, :], (128, 256))),
        'identf': np.eye(128, dtype=np.float32),
        'posr': np.ascontiguousarray(np.broadcast_to(posb[None, :], (64, S)).astype(np.int32)),
        'invf2': INVF2, 'sgn2': SGN2, 'sel64': SEL64,
        'w_uqh': f(np.stack([np.concatenate([wuq_[:, h, 0:128], wuq_[:, h, 128:192], wuq_[:, h, 128:192][:, swap]], axis=1) for h in range(8)])),
        'w_ukvh': f(np.stack([wukv_[:, h, :] for h in range(8)])),
        'conv_wT': f(conv_w.T),
        'dn_sc': f(np.stack([a_log[0], a_log[1], dt_bias[0], dt_bias[1]], axis=1)),
        'dn_gain': f(np.broadcast_to(inputs['dn_o_gain'][0][None, :], (128, 128))),
        'dmasks': DMASKS, 'dn_lmask': DN_LMASK,
        'w_o_dn': f(inputs['w_o_dn'][0]), 'w_o_mla': f(inputs['w_o_mla'][0]), 'w_out': f(inputs['w_out'][0]),
        'g_ffn': f(np.broadcast_to(inputs['g_ffn'][0][None, :], (128, D))),
        'g_final': f(np.broadcast_to(inputs['g_final'][None, :], (128, D))),
        'w_router': f(inputs['w_router'][0]),
        'w_gate': f(inputs['w_gate'][0]), 'w_up': f(inputs['w_up'][0]), 'w_down': f(inputs['w_down'][0]),
        'iota512': IOTA512, 'tri_in': TRI,
    }
    return m


def kernel(**inputs):
    inputs = {k: np.asarray(v) for k, v in inputs.items()}
    nc = build()
    in_maps = [prep_inputs(inputs, c) for c in range(8)]
    res = run_bass_kernel_spmd(nc, in_maps, core_ids=list(range(8)))
    outp = np.zeros((4, S, D), np.float32)
    for c in range(8):
        b, half = c // 2, c % 2
        o_ = res.results[c]["out"]
        if half == 0:
            outp[b, 0:2048] = o_
        else:
            outp[b, 2048:4096] = o_[::-1]
    return outp
```

```python
import numpy as np
import ml_dtypes
import concourse.bass as bass
import concourse.mybir as mybir
from concourse.bass_utils import run_bass_kernel_spmd
from contextlib import ExitStack

F32 = mybir.dt.float32
BF16 = mybir.dt.bfloat16
I32 = mybir.dt.int32
AF = mybir.ActivationFunctionType
ALU = mybir.AluOpType
AX = mybir.AxisListType

import os
DBGN = int(os.environ.get('DBGN', '99'))
S = 4096
D = 1024
NT = S // 128
EPS = 1e-6


class Prog:
    def __init__(self, nc, ndma=16):
        self.nc = nc
        self.eng = {'pe': nc.tensor, 'act': nc.scalar, 'dve': nc.vector,
                    'pool': nc.gpsimd, 'sp': nc.sync}
        self.sem = {k: nc.alloc_semaphore(name=f"s_{k}") for k in self.eng}
        self.cnt = {k: 0 for k in self.eng}
        self.waited = {k: {} for k in self.eng}
        self.dsem = {q: [nc.alloc_semaphore(name=f"s_dma_{q}{i}") for i in range(ndma)] for q in ('sp', 'pool')}
        self.dcnt = {q: [0] * ndma for q in ('sp', 'pool')}
        self.nd = {'sp': 0, 'pool': 0}
        self.lastw = {}
        self.readers = {}
        self.n = 0

    def _wait(self, e, tok):
        if tok is None:
            return
        sem, val, key = tok
        w = self.waited[e]
        if w.get(key, 0) >= val:
            return
        w[key] = val
        self.eng[e].wait_ge(sem, val)

    def op(self, e, fn, r=(), w=(), dma=False):
        w = list(w) + [t for t in r if isinstance(t, str) and t.startswith('ps') and t not in w]
        toks = []
        for t in r:
            toks.append(self.lastw.get(t))
        for t in w:
            toks.append(self.lastw.get(t))
            toks.extend(self.readers.get(t, ()))
        for tok in toks:
            self._wait(e, tok)
        if dma:
            i = self.nd[e] % len(self.dsem[e])
            if self.dcnt[e][i] > 0:
                self._wait(e, (self.dsem[e][i], self.dcnt[e][i], f"dma_{e}{i}"))
        ins = fn(self.eng[e])
        self.n += 1
        if dma:
            i = self.nd[e] % len(self.dsem[e])
            self.nd[e] += 1
            self.dcnt[e][i] += 16
            ins.then_inc(self.dsem[e][i], 16)
            tok = (self.dsem[e][i], self.dcnt[e][i], f"dma_{e}{i}")
        else:
            self.cnt[e] += 1
            ins.then_inc(self.sem[e], 1)
            tok = (self.sem[e], self.cnt[e], e)
        for t in r:
            self.readers.setdefault(t, []).append(tok)
        for t in w:
            self.lastw[t] = tok
            self.readers[t] = []
        return tok

    def pe_group(self, fns, r=(), w=()):
        e = 'pe'
        w = list(w) + [t for t in r if isinstance(t, str) and t.startswith('ps') and t not in w]
        toks = []
        for t in r:
            toks.append(self.lastw.get(t))
        for t in w:
            toks.append(self.lastw.get(t))
            toks.extend(self.readers.get(t, ()))
        for tok in toks:
            self._wait(e, tok)
        for fn in fns[:-1]:
            fn(self.eng[e])
            self.n += 1
        ins = fns[-1](self.eng[e])
        self.n += 1
        self.cnt[e] += 1
        ins.then_inc(self.sem[e], 1)
        tok = (self.sem[e], self.cnt[e], e)
        for t in r:
            self.readers.setdefault(t, []).append(tok)
        for t in w:
            self.lastw[t] = tok
            self.readers[t] = []
        return tok

    def barrier(self):
        toks = [(self.sem[k], self.cnt[k], k) for k in self.eng if self.cnt[k] > 0]
        toks += [(self.dsem[q][i], self.dcnt[q][i], f"dma_{q}{i}") for q in self.dsem for i in range(len(self.dsem[q])) if self.dcnt[q][i] > 0]
        for e in self.eng:
            for tok in toks:
                self._wait(e, tok)

    def finish(self, toks):
        for tok in toks:
            self._wait('sp', tok)


class _Stop(Exception):
    pass


def build(debug=None, stages=('A', 'MLA', 'DN', 'MG'), ext_in=(), dn_heads=range(8), dn_stop=0):
    nc = bass.Bass("TRN2", target_bir_lowering=False)
    P = Prog(nc)

    def din(name, shape, dt=F32):
        return nc.dram_tensor(name, list(shape), dt, kind="ExternalInput").ap()

    def dscr(name, shape, dt):
        kind = "ExternalOutput" if (debug and name in debug) else ("ExternalInput" if name in ext_in else "Internal")
        return nc.dram_tensor(name, list(shape), dt, kind=kind).ap()

    x = din("x", [S, D])
    cT = din("cT", [128, 8])
    w_mod = din("w_mod", [D, 6 * D])
    b_mod = din("b_mod", [1, 6 * D])
    g_mix = din("g_mix", [128, D])
    w_qkv = din("w_qkv", [D, 3072])
    w_z = din("w_z", [D, 1024])
    w_g = din("w_g", [D, 2048])
    w_ba = din("w_ba", [D, 32])
    w_cq = din("w_cq", [D, 512])
    w_ckv = din("w_ckv", [D, 256])
    w_kr2 = din("w_kr2", [D, 128])
    q_gain = din("q_gain", [128, 512])
    kv_gain = din("kv_gain", [128, 256])
    identf = din("identf", [128, 128])
    posr = din("posr", [64, S], I32)
    invf2 = din("invf2", [64, 1])
    sgn2 = din("sgn2", [64, 1])
    sel64 = din("sel64", [128, 65])
    w_uqh = din("w_uqh", [8, 512, 256])
    w_ukvh = din("w_ukvh", [8, 256, 256])
    conv_wT = din("conv_wT", [3072, 5])
    dn_sc = din("dn_sc", [8, 4])
    dn_gain = din("dn_gain", [128, 128])
    dmasks = din("dmasks", [128, 4, 128])
    dn_lmask = din("dn_lmask", [128, 5, 512])
    w_o_dn = din("w_o_dn", [D, D])
    w_o_mla = din("w_o_mla", [D, D])
    w_out = din("w_out", [D, D])
    g_ffn = din("g_ffn", [128, D])
    g_final = din("g_final", [128, D])
    w_router = din("w_router", [D, 16])
    w_gate = din("w_gate", [16, D, D])
    w_up = din("w_up", [16, D, D])
    w_down = din("w_down", [16, D, D])
    iota512 = din("iota512", [128, 512])
    tri_in = din("tri_in", [128, 128])
    out = nc.dram_tensor("out", [S // 2, D], F32, kind="ExternalOutput").ap()

    qkvT = dscr("qkvT", [24, 128, S], BF16)
    zs = dscr("zs", [S, 1024], BF16)
    gs = dscr("gs", [S, 2048], BF16)
    baT = dscr("baT", [32, S], F32)
    cqnT = dscr("cqnT", [4, 128, S], BF16)
    ckvnT = dscr("ckvnT", [2, 128, S], BF16)
    krT = dscr("krT", [2, 64, S], BF16)
    modrow = dscr("modrow", [1, 6 * D], F32)
    oT_mla = dscr("oT_mla", [8, 128, S], BF16)
    oT_dn = dscr("oT_dn", [8, 128, S], BF16)
    x1s = dscr("x1s", [S, D], F32)
    ye_all = dscr("ye_all", [16, 512, D], BF16)

    sb = lambda n, s, d: nc.alloc_sbuf_tensor(n, list(s), d)
    ps_ = lambda n, s, d=F32: nc.alloc_psum_tensor(n, list(s), d)

    ident = sb("ident", [128, 128], F32)
    identb = sb("identb", [128, 128], BF16)
    ones_f = sb("ones_f", [128, 128], F32)
    P.op('sp', lambda e: e.dma_start(out=ident[:], in_=identf[:, :]), w=['ident'], dma=True)
    P.op('dve', lambda e: e.tensor_copy(out=identb[:], in_=ident[:]), r=['ident'], w=['identb'])
    P.op('dve', lambda e: e.memset(ones_f[:], 1.0), w=['ones_f'])
    epsb = sb("epsb", [128, 1], F32)
    P.op('dve', lambda e: e.memset(epsb[:], EPS), w=['epsb'])

    psA = [ps_(f"psA{i}", [128, 512]) for i in range(4)]
    psT = [ps_(f"psT{i}", [128, 512], BF16) for i in range(2)]
    psS = [ps_(f"psS{i}", [128, 512]) for i in range(2)]
    pa_i = [0]

    def next_psA():
        i = pa_i[0] % 4
        pa_i[0] += 1
        return psA[i], f"psA{i}"

    sbs = lambda es, n, s_, d: es.enter_context(nc.sbuf_tensor(n, list(s_), d))
    fin = []

    def phase_A():
        cT_sb = sb("cT_sb", [128, 8], F32)
        scT = sb("scT", [128, 8], F32)
        P.op('sp', lambda e: e.dma_start(out=cT_sb[:], in_=cT[:, :]), w=['cT'], dma=True)
        P.op('act', lambda e: e.activation(out=scT[:], in_=cT_sb[:], func=AF.Silu), r=['cT'], w=['scT'])
        esA = ExitStack()
        modbc = sbs(esA, "modbc", [128, 6, D], F32)
        gmx = sbs(esA, "gmx", [128, D], F32)
        A1 = sbs(esA, "A1", [128, D], F32)
        es0 = ExitStack()
        mod_sb = sbs(es0, "mod_sb", [1, 6 * D], F32)
        bm_sb = sbs(es0, "bm_sb", [1, 6 * D], F32)
        P.op('sp', lambda e: e.dma_start(out=bm_sb[:], in_=b_mod[:, :]), w=['bm'], dma=True)
        wm = [sbs(es0, f"wm{i}", [128, 8, 512], F32) for i in range(2)]
        w_mod_v = w_mod.rearrange("(k p) n -> p k n", p=128)
        for j in range(12):
            wt, wk = wm[j % 2], f"wm{j % 2}"
            P.op('sp', lambda e: e.dma_start(out=wt[:], in_=w_mod_v[:, :, j * 512:(j + 1) * 512]), w=[wk], dma=True)
            pt, pk = next_psA()
            for k in range(8):
                P.op('pe', lambda e: e.matmul(pt[0:1, :], lhsT=scT[:, k:k + 1], rhs=wt[:, k, :], start=(k == 0), stop=(k == 7)),
                     r=[wk, 'scT'], w=[pk])
            P.op('dve', lambda e: e.tensor_tensor(out=mod_sb[:, j * 512:(j + 1) * 512], in0=pt[0:1, :], in1=bm_sb[:, j * 512:(j + 1) * 512], op=ALU.add),
                 r=[pk, 'bm'], w=['mod'])
        for j in range(12):
            pt, pk = next_psA()
            P.op('pe', lambda e: e.matmul(pt[:], lhsT=ones_f[0:1, :], rhs=mod_sb[:, j * 512:(j + 1) * 512], start=True, stop=True),
                 r=['ones_f', 'mod'], w=[pk])
            P.op('act', lambda e: e.copy(out=modbc[:, j // 2, (j % 2) * 512:(j % 2 + 1) * 512], in_=pt[:]), r=[pk], w=['modbc'])
        P.op('sp', lambda e: e.dma_start(out=gmx[:], in_=g_mix[:, :]), w=['gmx'], dma=True)
        P.op('dve', lambda e: e.scalar_tensor_tensor(out=A1[:], in0=modbc[:, 1, :], scalar=1.0, in1=gmx[:], op0=ALU.add, op1=ALU.mult),
             r=['modbc', 'gmx'], w=['A1'])

        fin.append(P.op('sp', lambda e: e.dma_start(out=modrow[:, :], in_=mod_sb[:]), r=['mod'], dma=True))
        P.barrier()
        es0.close()
        hT = sbs(esA, "hT", [128, 8, S], BF16)
        xt = [sbs(esA, f"xt{i}", [128, D], F32) for i in range(2)]
        xn = [sbs(esA, f"xn{i}", [128, D], F32) for i in range(2)]
        hb = [sbs(esA, f"hb{i}", [128, D], BF16) for i in range(2)]
        st = [sbs(esA, f"st{i}", [128, 4], F32) for i in range(2)]
        junk = sbs(esA, "junk", [128, D], F32)
        for t in range(NT):
            i = t % 2
            P.op('sp', lambda e: e.dma_start(out=xt[i][:], in_=x[t * 128:(t + 1) * 128, :]), w=[f'xt{i}'], dma=True)
            P.op('dve', lambda e: e.memset(st[i][:], 0.0), w=[f'st{i}'])
            P.op('act', lambda e: e.activation(out=junk[:], in_=xt[i][:], func=AF.Square, accum_out=st[i][:, 0:1]),
                 r=[f'xt{i}'], w=['junk', f'st{i}'])
            P.op('act', lambda e: e.activation(out=st[i][:, 1:2], in_=st[i][:, 0:1], func=AF.Sqrt, scale=1.0 / D, bias=epsb[:]),
                 r=[f'st{i}', 'epsb'], w=[f'st{i}'])
            P.op('dve', lambda e: e.reciprocal(out=st[i][:, 2:3], in_=st[i][:, 1:2]), r=[f'st{i}'], w=[f'st{i}'])
            P.op('dve', lambda e: e.scalar_tensor_tensor(out=xn[i][:], in0=xt[i][:], scalar=st[i][:, 2:3], in1=A1[:], op0=ALU.mult, op1=ALU.mult),
                 r=[f'xt{i}', f'st{i}', 'A1'], w=[f'xn{i}'])
            P.op('pool', lambda e: e.tensor_tensor(out=hb[i][:], in0=xn[i][:], in1=modbc[:, 0, :], op=ALU.add),
                 r=[f'xn{i}', 'modbc'], w=[f'hb{i}'])
            for half in range(2):
                pt, pk = psT[half], f'psT{half}'
                P.pe_group([(lambda e, k4=k4: e.transpose(out=pt[:, k4 * 128:(k4 + 1) * 128], in_=hb[i][:, (half * 4 + k4) * 128:(half * 4 + k4 + 1) * 128], identity=identb[:])) for k4 in range(4)],
                           r=[f'hb{i}', 'identb'], w=[pk])
                eng = 'act' if half == 0 else 'dve'
                if eng == 'act':
                    P.op('act', lambda e: e.copy(out=hT[:, half * 4:(half + 1) * 4, t * 128:(t + 1) * 128],
                                                 in_=pt[:].rearrange("p (k n) -> p k n", k=4)), r=[pk], w=[('hT', t)])
                else:
                    P.op('dve', lambda e: e.tensor_copy(out=hT[:, half * 4:(half + 1) * 4, t * 128:(t + 1) * 128],
                                                        in_=pt[:].rearrange("p (k n) -> p k n", k=4)), r=[pk], w=[('hT', t)])
        hT_all = [('hT', t) for t in range(NT)]

        wb = [sbs(esA, f"wb{i}", [128, 8, 512], BF16) for i in range(2)]
        wb_i = [0]

        def load_w(src, c0, ncols):
            i = wb_i[0] % 2
            wb_i[0] += 1
            v = src.rearrange("(k p) n -> p k n", p=128)
            P.op('pool', lambda e: e.dma_start(out=wb[i][:, :, 0:ncols], in_=v[:, :, c0:c0 + ncols]), w=[f'wb{i}'], dma=True)
            return wb[i], f'wb{i}'

        stg = [sbs(esA, f"stg{i}", [128, S], BF16) for i in range(2)]
        stg_i = [0]

        def chan_major(src, ncols_total, dst_fn, M, dt_out=BF16, stgs=stg):
            nblk = (ncols_total + 511) // 512
            for blk in range(nblk):
                nc_ = min(512, ncols_total - blk * 512)
                wt, wk = load_w(src, blk * 512, nc_)
                for c in range(nc_ // M):
                    si = stg_i[0] % 2
                    stg_i[0] += 1
                    sg, sk = stgs[si], f'{stgs[si].name}'
                    for g in range(8):
                        pt, pk = next_psA()
                        P.pe_group([(lambda e, k=k: e.matmul(pt[0:M, :], lhsT=wt[:, k, c * M:(c + 1) * M], rhs=hT[:, k, g * 512:(g + 1) * 512],
                                                            start=(k == 0), stop=(k == 7))) for k in range(8)],
                                   r=[wk] + hT_all[g * 4:(g + 1) * 4], w=[pk])
                        if g % 2 == 0:
                            P.op('act', lambda e: e.copy(out=sg[0:M, g * 512:(g + 1) * 512], in_=pt[0:M, :]), r=[pk], w=[sk])
                        else:
                            P.op('dve', lambda e: e.tensor_copy(out=sg[0:M, g * 512:(g + 1) * 512], in_=pt[0:M, :]), r=[pk], w=[sk])
                    fin.append(P.op('sp', lambda e: e.dma_start(out=dst_fn(blk * (512 // M) + c), in_=sg[0:M, :]), r=[sk], w=[('scr', dst_fn.__name__)], dma=True))

        def dst_qkv(c):
            return qkvT[c, :, :]
        chan_major(w_qkv, 3072, dst_qkv, 128)

        def dst_kr(c):
            return krT[c, :, :]
        chan_major(w_kr2, 128, dst_kr, 64)
        stgf0 = sbs(esA, "stgf0", [32, S], F32)
        stgf = [stgf0, stgf0]

        def dst_ba(c):
            return baT[:, :]
        chan_major(w_ba, 32, dst_ba, 32, F32, stgf)

        tst = [sbs(esA, f"tst{i}", [128, 512], BF16) for i in range(4)]
        tst_i = [0]

        def tok_major_act(src, ncols_total, dst, func):
            for blk in range(ncols_total // 512):
                wt, wk = load_w(src, blk * 512, 512)
                for t in range(NT):
                    pt, pk = next_psA()
                    P.pe_group([(lambda e, k=k: e.matmul(pt[:], lhsT=hT[:, k, t * 128:(t + 1) * 128], rhs=wt[:, k, :], start=(k == 0), stop=(k == 7))) for k in range(8)],
                               r=[wk, ('hT', t)], w=[pk])
                    si = tst_i[0] % 4
                    tst_i[0] += 1
                    P.op('act', lambda e: e.activation(out=tst[si][:], in_=pt[:], func=func), r=[pk], w=[f'tst{si}'])
                    fin.append(P.op('sp', lambda e: e.dma_start(out=dst[t * 128:(t + 1) * 128, blk * 512:(blk + 1) * 512], in_=tst[si][:]),
                                    r=[f'tst{si}'], w=[('scr', dst.name, t, blk)], dma=True))
        tok_major_act(w_z, 1024, zs, AF.Silu)
        tok_major_act(w_g, 2048, gs, AF.Sigmoid)

        def latent(src, ncols, gain_in, dstT, nm):
            gsb = sbs(esA, f"gain_{nm}", [128, ncols], F32)
            P.op('sp', lambda e: e.dma_start(out=gsb[:], in_=gain_in[:, :]), w=[f'gain_{nm}'], dma=True)
            lst = [sbs(esA, f"lst_{nm}{i_}", [128, ncols // 128, 128], BF16) for i_ in range(2)]
            wt, wk = load_w(src, 0, ncols)
            for t in range(NT):
                i = t % 2
                pt, pk = next_psA()
                P.pe_group([(lambda e, k=k: e.matmul(pt[:, 0:ncols], lhsT=hT[:, k, t * 128:(t + 1) * 128], rhs=wt[:, k, 0:ncols], start=(k == 0), stop=(k == 7))) for k in range(8)],
                           r=[wk, ('hT', t)], w=[pk])
                P.op('dve', lambda e: e.memset(st[i][:], 0.0), w=[f'st{i}'])
                P.op('act', lambda e: e.activation(out=junk[:, 0:ncols], in_=pt[:, 0:ncols], func=AF.Square, accum_out=st[i][:, 0:1]),
                     r=[pk], w=['junk', f'st{i}'])
                P.op('act', lambda e: e.activation(out=st[i][:, 1:2], in_=st[i][:, 0:1], func=AF.Sqrt, scale=1.0 / ncols, bias=epsb[:]),
                     r=[f'st{i}', 'epsb'], w=[f'st{i}'])
                P.op('dve', lambda e: e.reciprocal(out=st[i][:, 2:3], in_=st[i][:, 1:2]), r=[f'st{i}'], w=[f'st{i}'])
                P.op('dve', lambda e: e.scalar_tensor_tensor(out=hb[i][:, 0:ncols], in0=pt[:, 0:ncols], scalar=st[i][:, 2:3], in1=gsb[:], op0=ALU.mult, op1=ALU.mult),
                     r=[pk, f'st{i}', f'gain_{nm}'], w=[f'hb{i}'])
                tp, tk = psT[i], f'psT{i}'
                for c in range(ncols // 128):
                    P.op('pe', lambda e: e.transpose(out=tp[:, c * 128:(c + 1) * 128], in_=hb[i][:, c * 128:(c + 1) * 128], identity=identb[:]),
                         r=[f'hb{i}', 'identb'], w=[tk])
                P.op('act', lambda e: e.copy(out=lst[i][:], in_=tp[:, 0:ncols].rearrange("p (k n) -> p k n", k=ncols // 128)),
                     r=[tk], w=[f'lst_{nm}{i}'])
                fin.append(P.op('sp', lambda e: e.dma_start(out=dstT[:, :, t * 128:(t + 1) * 128].rearrange("c p n -> p c n"), in_=lst[i][:]),
                                r=[f'lst_{nm}{i}'], w=[('scr', nm, t)], dma=True))
        latent(w_cq, 512, q_gain, cqnT, 'cq')
        latent(w_ckv, 256, kv_gain, ckvnT, 'ckv')


        P.barrier()
        esA.close()

    def phase_MLA():
        esM = ExitStack()
        TWO_PI = float(2 * np.pi)
        SCL = float(192 ** -0.5)
        cos2 = sbs(esM, "cos2", [64, S], F32)
        sin2 = sbs(esM, "sin2", [64, S], F32)
        krA = sbs(esM, "krA", [65, S], BF16)
        QrA = sbs(esM, "QrA", [65, S], BF16)
        onesb = sbs(esM, "onesb", [128, 128], BF16)
        sel_b = sbs(esM, "sel_b", [128, 65], BF16)
        if True:
            es1 = ExitStack()
            posi = sbs(es1, "posi", [64, S], I32)
            ang = sbs(es1, "ang", [64, S], F32)
            ti = sbs(es1, "ti", [64, S], I32)
            tf = sbs(es1, "tf", [64, S], F32)
            tg = sbs(es1, "tg", [64, S], F32)
            ivf = sbs(es1, "ivf", [64, 2], F32)
            kr0 = sbs(es1, "kr0", [64, S], BF16)
            kr1 = sbs(es1, "kr1", [64, S], BF16)
            self_f = sbs(es1, "self_f", [128, 65], F32)
            P.op('sp', lambda e: e.dma_start(out=posi[:], in_=posr[:, :]), w=['posi'], dma=True)
            P.op('sp', lambda e: e.dma_start(out=ivf[:, 0:1], in_=invf2[:, :]), w=['ivf'], dma=True)
            P.op('sp', lambda e: e.dma_start(out=ivf[:, 1:2], in_=sgn2[:, :]), w=['ivf'], dma=True)
            P.op('sp', lambda e: e.dma_start(out=self_f[:], in_=sel64[:, :]), w=['self_f'], dma=True)
            P.op('dve', lambda e: e.tensor_copy(out=sel_b[:], in_=self_f[:]), r=['self_f'], w=['sel_b'])
            P.op('dve', lambda e: e.memset(onesb[:], 1.0), w=['onesb'])
            P.op('dve', lambda e: e.tensor_copy(out=ang[:], in_=posi[:]), r=['posi'], w=['ang'])
            P.op('dve', lambda e: e.tensor_scalar(out=ang[:], in0=ang[:], scalar1=ivf[:, 0:1], scalar2=float(1.0 / TWO_PI), op0=ALU.mult, op1=ALU.mult),
                 r=['ang', 'ivf'], w=['ang'])
            for which, dst in ((0, sin2), (1, cos2)):
                dk_ = 'sin2' if which == 0 else 'cos2'
                P.op('dve', lambda e: e.tensor_scalar(out=tg[:], in0=ang[:], scalar1=0.25 * which, scalar2=None, op0=ALU.add), r=['ang'], w=['tg'])
                P.op('dve', lambda e: e.tensor_copy(out=ti[:], in_=tg[:]), r=['tg'], w=['ti'])
                P.op('dve', lambda e: e.tensor_copy(out=tf[:], in_=ti[:]), r=['ti'], w=['tf'])
                P.op('dve', lambda e: e.tensor_tensor(out=tg[:], in0=tg[:], in1=tf[:], op=ALU.subtract), r=['tg', 'tf'], w=['tg'])
                P.op('dve', lambda e: e.tensor_scalar(out=tf[:], in0=tg[:], scalar1=0.5, scalar2=None, op0=ALU.is_gt), r=['tg'], w=['tf'])
                P.op('dve', lambda e: e.tensor_tensor(out=tg[:], in0=tg[:], in1=tf[:], op=ALU.subtract), r=['tg', 'tf'], w=['tg'])
                P.op('dve', lambda e: e.tensor_scalar(out=tf[:], in0=tg[:], scalar1=-0.5, scalar2=None, op0=ALU.is_lt), r=['tg'], w=['tf'])
                P.op('dve', lambda e: e.tensor_tensor(out=tg[:], in0=tg[:], in1=tf[:], op=ALU.add), r=['tg', 'tf'], w=['tg'])
                P.op('act', lambda e: e.activation(out=dst[:], in_=tg[:], func=AF.Sin, scale=TWO_PI), r=['tg'], w=[dk_])
            P.op('dve', lambda e: e.tensor_scalar(out=sin2[:], in0=sin2[:], scalar1=ivf[:, 1:2], scalar2=None, op0=ALU.mult), r=['sin2', 'ivf'], w=['sin2'])
            P.op('sp', lambda e: e.dma_start(out=kr0[:], in_=krT[0, :, :]), r=[('scr', 'dst_kr')], w=['kr0'], dma=True)
            P.op('sp', lambda e: e.dma_start(out=kr1[:], in_=krT[1, :, :]), r=[('scr', 'dst_kr')], w=['kr1'], dma=True)
            P.op('dve', lambda e: e.tensor_tensor(out=tg[:], in0=kr0[:], in1=cos2[:], op=ALU.mult), r=['kr0', 'cos2'], w=['tg'])
            P.op('dve', lambda e: e.tensor_tensor(out=tf[:], in0=kr1[:], in1=sin2[:], op=ALU.mult), r=['kr1', 'sin2'], w=['tf'])
            P.op('dve', lambda e: e.memset(krA[:], 1.0), w=['krA'])
            P.op('dve', lambda e: e.tensor_tensor(out=krA[0:64, :], in0=tg[:], in1=tf[:], op=ALU.add), r=['tg', 'tf'], w=['krA'])
            P.op('dve', lambda e: e.memset(QrA[:], 0.0), w=['QrA'])
            P.barrier()
            es1.close()
        cqn = sbs(esM, "cqn", [128, 4, S], BF16)
        ckvn = sbs(esM, "ckvn", [128, 2, S], BF16)
        QnT = sbs(esM, "QnT", [128, S], BF16)
        KnT = sbs(esM, "KnT", [128, S], BF16)
        Vt = sbs(esM, "Vt", [128, NT, 128], BF16)
        oTs = sbs(esM, "oTs", [128, S], BF16)
        kmx = sbs(esM, "kmx", [65, 16], F32)
        wuq = sbs(esM, "wuq", [128, 4, 256], BF16)
        wukv = sbs(esM, "wukv", [128, 2, 256], BF16)
        sq = sbs(esM, "sq", [128, 512], BF16)
        t1 = sbs(esM, "t1", [64, 512], F32)
        t2 = sbs(esM, "t2", [64, 512], F32)
        rowt = sbs(esM, "rowt", [65, 512], F32)
        pT = [sbs(esM, f"pT{i}", [128, 512], BF16) for i in range(3)]
        rden = sbs(esM, "rden", [128, 512], F32)
        for c in range(4):
            P.op('sp', lambda e: e.dma_start(out=cqn[:, c, :], in_=cqnT[c, :, :]), r=[('scr', 'cq', t_) for t_ in range(NT)], w=['cqn'], dma=True)
        for c in range(2):
            P.op('sp', lambda e: e.dma_start(out=ckvn[:, c, :], in_=ckvnT[c, :, :]), r=[('scr', 'ckv', t_) for t_ in range(NT)], w=['ckvn'], dma=True)
        for h in range(8):
            P.op('pool', lambda e: e.dma_start(out=wuq[:], in_=w_uqh[h].rearrange("(k p) n -> p k n", p=128)), w=['wuq'], dma=True)
            P.op('pool', lambda e: e.dma_start(out=wukv[:], in_=w_ukvh[h].rearrange("(k p) n -> p k n", p=128)), w=['wukv'], dma=True)
            for g in range(8):
                gs_ = slice(g * 512, (g + 1) * 512)
                pt, pk = psA[2], 'psA2'
                P.pe_group([(lambda e, k=k: e.matmul(pt[:], lhsT=wuq[:, k, 0:128], rhs=cqn[:, k, gs_], start=(k == 0), stop=(k == 3))) for k in range(4)], r=['wuq', 'cqn'], w=[pk])
                P.op('act', lambda e: e.copy(out=QnT[:, gs_], in_=pt[:]), r=[pk], w=[('QnT', g)])
                pt, pk = psA[3], 'psA3'
                P.pe_group([(lambda e, k=k: e.matmul(pt[0:64, :], lhsT=wuq[:, k, 128:192], rhs=cqn[:, k, gs_], start=(k == 0), stop=(k == 3))) for k in range(4)], r=['wuq', 'cqn'], w=[pk])
                P.op('dve', lambda e: e.tensor_tensor(out=t1[:], in0=pt[0:64, :], in1=cos2[:, gs_], op=ALU.mult), r=[pk, 'cos2'], w=['t1'])
                P.pe_group([(lambda e, k=k: e.matmul(pt[0:64, :], lhsT=wuq[:, k, 192:256], rhs=cqn[:, k, gs_], start=(k == 0), stop=(k == 3))) for k in range(4)], r=['wuq', 'cqn'], w=[pk])
                P.op('dve', lambda e: e.tensor_tensor(out=t2[:], in0=pt[0:64, :], in1=sin2[:, gs_], op=ALU.mult), r=[pk, 'sin2'], w=['t2'])
                P.op('dve', lambda e: e.tensor_tensor(out=QrA[0:64, gs_], in0=t1[:], in1=t2[:], op=ALU.add), r=['t1', 't2'], w=[('QrA', g)])
                pt, pk = psA[2], 'psA2'
                P.pe_group([(lambda e, k=k: e.matmul(pt[:], lhsT=wukv[:, k, 0:128], rhs=ckvn[:, k, gs_], start=(k == 0), stop=(k == 1))) for k in range(2)], r=['wukv', 'ckvn'], w=[pk])
                P.op('act', lambda e: e.copy(out=KnT[:, gs_], in_=pt[:]), r=[pk], w=[('KnT', g)])
                pt, pk = psA[3], 'psA3'
                for j in range(4):
                    t_ = g * 4 + j
                    for k in range(2):
                        P.op('pe', lambda e: e.matmul(pt[:, j * 128:(j + 1) * 128], lhsT=ckvn[:, k, t_ * 128:(t_ + 1) * 128], rhs=wukv[:, k, 128:256], start=(k == 0), stop=(k == 1)),
                             r=['wukv', 'ckvn'], w=[pk])
                P.op('dve', lambda e: e.tensor_copy(out=Vt[:, g * 4:(g + 1) * 4, :], in_=pt[:].rearrange("p (j n) -> p j n", j=4)), r=[pk], w=[('Vt', g)])
                pt, pk = psA[2], 'psA2'
                P.op('act', lambda e: e.activation(out=sq[:], in_=KnT[:, gs_], func=AF.Square), r=[('KnT', g)], w=['sq'])
                P.op('pe', lambda e: e.matmul(pt[0:65, :], lhsT=sel_b[:, :], rhs=sq[:], start=True, stop=False), r=['sq', 'sel_b'], w=[pk])
                P.op('act', lambda e: e.activation(out=sq[0:64, :], in_=krA[0:64, gs_], func=AF.Square), r=['krA'], w=['sq'])
                P.op('pe', lambda e: e.matmul(pt[0:65, :], lhsT=sel_b[0:64, :], rhs=sq[0:64, :], start=False, stop=True), r=['sq', 'sel_b'], w=[pk])
                P.op('dve', lambda e: e.tensor_reduce(out=kmx[64:65, g:g + 1], in_=pt[64:65, :], axis=AX.X, op=ALU.max), r=[pk], w=['kmx'])
            P.op('dve', lambda e: e.tensor_reduce(out=kmx[64:65, 8:9], in_=kmx[64:65, 0:8], axis=AX.X, op=ALU.max), r=['kmx'], w=['kmx'])
            for g in range(8):
                gs_ = slice(g * 512, (g + 1) * 512)
                pt, pk = psA[2], 'psA2'
                P.op('act', lambda e: e.activation(out=sq[:], in_=QnT[:, gs_], func=AF.Square), r=[('QnT', g)], w=['sq'])
                P.op('pe', lambda e: e.matmul(pt[0:65, :], lhsT=sel_b[:, :], rhs=sq[:], start=True, stop=False), r=['sq', 'sel_b'], w=[pk])
                P.op('act', lambda e: e.activation(out=sq[0:64, :], in_=QrA[0:64, gs_], func=AF.Square), r=[('QrA', g)], w=['sq'])
                P.op('pe', lambda e: e.matmul(pt[0:65, :], lhsT=sel_b[0:64, :], rhs=sq[0:64, :], start=False, stop=True), r=['sq', 'sel_b'], w=[pk])
                P.op('act', lambda e: e.activation(out=rowt[64:65, :], in_=pt[64:65, :], func=AF.Sqrt, scale=kmx[64:65, 8:9]), r=[pk, 'kmx'], w=['rowt'])
                P.op('dve', lambda e: e.tensor_scalar(out=QrA[64:65, gs_], in0=rowt[64:65, :], scalar1=-1.0, scalar2=None, op0=ALU.mult), r=['rowt'], w=[('QrA', g)])
            for g in range(8):
                gs_ = slice(g * 512, (g + 1) * 512)
                po, pd = psA[(g % 2) * 2], psA[(g % 2) * 2 + 1]
                kpo, kpd = f'psA{(g % 2) * 2}', f'psA{(g % 2) * 2 + 1}'

                def scores(kt):
                    ks_ = slice(kt * 128, (kt + 1) * 128)
                    sc_, sck = psS[kt % 2], f'psS{kt % 2}'
                    P.pe_group([lambda e: e.matmul(sc_[:], lhsT=KnT[:, ks_], rhs=QnT[:, gs_], start=True, stop=False),
                                lambda e: e.matmul(sc_[:], lhsT=krA[:, ks_], rhs=QrA[:, gs_], start=False, stop=True)],
                               r=[('KnT', kt // 4), ('QnT', g), 'krA', ('QrA', g)], w=[sck])
                scores(0)
                for kt in range(NT):
                    sc_, sck = psS[kt % 2], f'psS{kt % 2}'
                    pi = kt % 3
                    P.op('act', lambda e: e.activation(out=pT[pi][:], in_=sc_[:], func=AF.Exp, scale=SCL), r=[sck], w=[f'pT{pi}'])
                    if kt + 1 < NT:
                        scores(kt + 1)
                    P.pe_group([lambda e: e.matmul(po[:], lhsT=Vt[:, kt, :], rhs=pT[pi][:], start=(kt == 0), stop=(kt == NT - 1)),
                                lambda e: e.matmul(pd[:], lhsT=onesb[:], rhs=pT[pi][:], start=(kt == 0), stop=(kt == NT - 1))],
                               r=[('Vt', kt // 4), f'pT{pi}', 'onesb'], w=[kpo, kpd])
                P.op('dve', lambda e: e.reciprocal(out=rden[:], in_=pd[:]), r=[kpd], w=['rden'])
                P.op('dve', lambda e: e.tensor_tensor(out=oTs[:, gs_], in0=po[:], in1=rden[:], op=ALU.mult), r=[kpo, 'rden'], w=['oTs'])
            fin.append(P.op('sp', lambda e: e.dma_start(out=oT_mla[h, :, :], in_=oTs[:]), r=['oTs'], w=[('scr', 'oT_mla', h)], dma=True))
        P.barrier()
        esM.close()


    def phase_DN():
        def stop(k):
            if dn_stop == k:
                raise _Stop()
        esD = ExitStack()
        pM, pG, pX0, pX1, pZT, pU = psA[0], psA[1], psA[2], psA[3], psS[0], psS[1]
        kM, kG, kX0, kX1, kZT, kU = 'psA0', 'psA1', 'psA2', 'psA3', 'psS0', 'psS1'
        onesb = sbs(esD, "d_onesb", [128, 128], BF16)
        P.op('dve', lambda e: e.memset(onesb[:], 1.0), w=['d_onesb'])
        msk = sbs(esD, "d_msk", [128, 4, 128], F32)
        P.op('sp', lambda e: e.dma_start(out=msk[:], in_=dmasks[:, :, :]), w=['d_msk'], dma=True)
        gain = sbs(esD, "d_gain", [128, 128], F32)
        P.op('sp', lambda e: e.dma_start(out=gain[:], in_=dn_gain[:, :]), w=['d_gain'], dma=True)
        tokS = sbs(esD, "tokS", [128, NT, 48], F32)
        one1 = sbs(esD, "one1", [128, 1], F32)
        P.op('dve', lambda e: e.memset(one1[:], 1.0), w=['one1'])
        eps_l2 = sbs(esD, "eps_l2", [128, 1], F32)
        P.op('dve', lambda e: e.memset(eps_l2[:], EPS), w=['eps_l2'])
        es1 = ExitStack()
        sc = sbs(es1, "d_sc", [8, 4], F32)
        nA = sbs(es1, "d_nA", [8, 2], F32)
        P.op('sp', lambda e: e.dma_start(out=sc[:], in_=dn_sc[:, :]), w=['d_sc'], dma=True)
        P.op('act', lambda e: e.activation(out=nA[:], in_=sc[:, 0:2], func=AF.Exp), r=['d_sc'], w=['d_nA'])
        P.op('dve', lambda e: e.tensor_scalar(out=nA[:], in0=nA[:], scalar1=-1.0, scalar2=None, op0=ALU.mult), r=['d_nA'], w=['d_nA'])
        rows = {}
        ra = sbs(es1, "d_ra", [8, S], F32)
        rb = sbs(es1, "d_rb", [8, S], F32)
        rc = sbs(es1, "d_rc", [8, S], F32)
        for d in range(2):
            beta = sbs(es1, f"d_beta{d}", [8, S], F32)
            nbeta = sbs(es1, f"d_nbeta{d}", [8, S], F32)
            gc = sbs(es1, f"d_gc{d}", [8, S], F32)
            rows[d] = (beta, nbeta, gc)
            P.op('sp', lambda e: e.dma_start(out=ra[:], in_=baT[d * 8:(d + 1) * 8, :]), r=[('scr', 'dst_ba')], w=['d_ra'], dma=True)
            P.op('act', lambda e: e.activation(out=beta[:], in_=ra[:], func=AF.Sigmoid), r=['d_ra'], w=[f'd_beta{d}'])
            P.op('dve', lambda e: e.tensor_scalar(out=nbeta[:], in0=beta[:], scalar1=-1.0, scalar2=None, op0=ALU.mult), r=[f'd_beta{d}'], w=[f'd_nbeta{d}'])
            P.op('sp', lambda e: e.dma_start(out=ra[:], in_=baT[16 + d * 8:16 + (d + 1) * 8, :]), r=[('scr', 'dst_ba')], w=['d_ra'], dma=True)
            P.op('dve', lambda e: e.tensor_scalar(out=ra[:], in0=ra[:], scalar1=sc[:, 2 + d:3 + d], scalar2=None, op0=ALU.add), r=['d_ra', 'd_sc'], w=['d_ra'])
            P.op('act', lambda e: e.activation(out=rb[:], in_=ra[:], func=AF.Abs), r=['d_ra'], w=['d_rb'])
            P.op('act', lambda e: e.activation(out=rb[:], in_=rb[:], func=AF.Exp, scale=-1.0), r=['d_rb'], w=['d_rb'])
            P.op('act', lambda e: e.activation(out=rb[:], in_=rb[:], func=AF.Ln, bias=one1[0:8, :], scale=1.0), r=['d_rb', 'one1'], w=['d_rb'])
            P.op('dve', lambda e: e.scalar_tensor_tensor(out=rc[:], in0=ra[:], scalar=0.0, in1=rb[:], op0=ALU.max, op1=ALU.add), r=['d_ra', 'd_rb'], w=['d_rc'])
            P.op('dve', lambda e: e.tensor_scalar(out=rc[:], in0=rc[:], scalar1=nA[:, d:d + 1], scalar2=None, op0=ALU.mult), r=['d_rc', 'd_nA'], w=['d_rc'])
            cur, curk, nxt, nxtk = rc, 'd_rc', gc, f'd_gc{d}'
            for sft in (1, 2, 4, 8, 16, 32, 64):
                c3 = cur[:].rearrange("p (t n) -> p t n", n=128)
                n3 = nxt[:].rearrange("p (t n) -> p t n", n=128)
                P.op('act', lambda e: e.copy(out=nxt[:], in_=cur[:]), r=[curk], w=[nxtk])
                if d == 0:
                    P.op('dve', lambda e: e.tensor_tensor(out=n3[:, :, sft:], in0=c3[:, :, sft:], in1=c3[:, :, :128 - sft], op=ALU.add), r=[curk], w=[nxtk])
                else:
                    P.op('dve', lambda e: e.tensor_tensor(out=n3[:, :, :128 - sft], in0=c3[:, :, :128 - sft], in1=c3[:, :, sft:], op=ALU.add), r=[curk], w=[nxtk])
                cur, curk, nxt, nxtk = nxt, nxtk, cur, curk
            if cur is not gc:
                P.op('act', lambda e: e.copy(out=gc[:], in_=cur[:]), r=[curk], w=[f'd_gc{d}'])
        for t in range(NT):
            for d in range(2):
                for j in range(3):
                    src = rows[d][j]
                    col = d * 24 + j * 8
                    P.op('pe', lambda e: e.transpose(out=pG[:, col:col + 8], in_=src[:, t * 128:(t + 1) * 128], identity=ident[0:8, 0:8]),
                         r=[f'd_beta{d}', f'd_nbeta{d}', f'd_gc{d}', 'ident'], w=[kG])
            P.op('act', lambda e: e.copy(out=tokS[:, t, :], in_=pG[:, 0:48]), r=[kG], w=['tokS'])
        P.barrier()
        stop(1)
        es1.close()
        lm = sbs(esD, "d_lm", [128, 5, 4 * 128], F32)
        for j5 in range(5):
            P.op('sp', lambda e: e.dma_start(out=lm[:, j5, :], in_=dn_lmask[:, j5, :]), w=['d_lm'], dma=True)
        QKV = [sbs(esD, f"d_qkv{i}", [128, S], BF16) for i in range(3)]
        Ust = sbs(esD, "d_U", [128, NT, 2, 128], BF16)
        WTst = sbs(esD, "d_WT", [128, NT, 2, 128], BF16)
        ITst = sbs(esD, "d_IT", [128, NT, 2, 128], BF16)
        QDst = sbs(esD, "d_QD", [128, NT, 2, 128], BF16)
        KSst = sbs(esD, "d_KS", [128, NT, 2, 128], BF16)
        egl = sbs(esD, "d_egl", [128, NT, 2], F32)
        Oacc = sbs(esD, "d_Oacc", [128, NT, 128], F32)
        zsh = sbs(esD, "d_zsh", [128, NT, 128], BF16)
        oTd = sbs(esD, "d_oTd", [128, S], BF16)
        S32 = [sbs(esD, f"d_S32{d}", [128, 128], F32) for d in range(2)]
        Sbf = [sbs(esD, f"d_Sbf{d}", [128, 128], BF16) for d in range(2)]
        Vn = [sbs(esD, f"d_Vn{d}", [128, 128], BF16) for d in range(2)]
        ost = sbs(esD, "d_ost", [128, 8], F32)
        on = sbs(esD, "d_on", [128, 128], F32)
        onb = sbs(esD, "d_onb", [128, 128], BF16)
        G = 4
        QSC = float(128 ** -0.5)
        for h in dn_heads:
            esC = ExitStack()
            xpad = sbs(esC, f"d_xpad_{h}", [128, S + 4], F32)
            acc = sbs(esC, f"d_acc_{h}", [128, S], F32)
            cw = sbs(esC, f"d_cw_{h}", [128, 5], F32)
            rst = sbs(esC, f"d_rst_{h}", [128, 512], F32)
            sqb = sbs(esC, f"d_sqb_{h}", [128, 512], BF16)
            P.op('dve', lambda e: e.memset(xpad[:, 0:2], 0.0), w=['d_xpad'])
            P.op('dve', lambda e: e.memset(xpad[:, S + 2:S + 4], 0.0), w=['d_xpad'])
            for ci in range(3):
                ch = ci * 8 + h
                P.op('sp', lambda e: e.dma_start(out=cw[:], in_=conv_wT[ch * 128:(ch + 1) * 128, :]), w=['d_cw'], dma=True)
                P.op('pool', lambda e: e.dma_start(out=xpad[:, 2:S + 2], in_=qkvT[ch, :, :]), r=[('scr', 'dst_qkv')], w=['d_xpad'], dma=True)
                eng = 'dve'
                P.op(eng, lambda e: e.tensor_scalar(out=acc[:], in0=xpad[:, 0:S], scalar1=cw[:, 0:1], scalar2=None, op0=ALU.mult), r=['d_xpad', 'd_cw'], w=['d_acc'])
                for j in range(1, 5):
                    P.op(eng, lambda e: e.scalar_tensor_tensor(out=acc[:], in0=xpad[:, j:j + S], scalar=cw[:, j:j + 1], in1=acc[:], op0=ALU.mult, op1=ALU.add),
                         r=['d_xpad', 'd_cw', 'd_acc'], w=['d_acc'])
                P.op('act', lambda e: e.activation(out=acc[:], in_=acc[:], func=AF.Silu), r=['d_acc'], w=['d_acc'])
                if ci == 2:
                    P.op('dve', lambda e: e.tensor_copy(out=QKV[2][:], in_=acc[:]), r=['d_acc'], w=['d_qkv2'])
                else:
                    for g in range(8):
                        gs_ = slice(g * 512, (g + 1) * 512)
                        P.op('act', lambda e: e.activation(out=sqb[:], in_=acc[:, gs_], func=AF.Square), r=['d_acc'], w=['d_sqb'])
                        P.op('pe', lambda e: e.matmul(pM[:], lhsT=onesb[:], rhs=sqb[:], start=True, stop=True), r=['d_onesb', 'd_sqb'], w=[kM])
                        P.op('act', lambda e: e.activation(out=rst[:], in_=pM[:], func=AF.Sqrt, bias=eps_l2[:], scale=1.0), r=[kM, 'eps_l2'], w=['d_rst'])
                        P.op('dve', lambda e: e.reciprocal(out=rst[:], in_=rst[:]), r=['d_rst'], w=['d_rst'])
                        P.op('dve', lambda e: e.scalar_tensor_tensor(out=QKV[ci][:, gs_], in0=acc[:, gs_], scalar=(QSC if ci == 0 else 1.0), in1=rst[:], op0=ALU.mult, op1=ALU.mult),
                             r=['d_acc', 'd_rst'], w=[f'd_qkv{ci}'])
            Qt, Kt, Vch = QKV
            stop(2)
            P.barrier()
            esC.close()
            esW = ExitStack()
            Ktok = sbs(esW, f"d_Ktok_{h}", [128, 2, 128], BF16)
            Vtok = sbs(esW, f"d_Vtok_{h}", [128, 2, 128], BF16)
            Dg = sbs(esW, f"d_Dg_{h}", [128, G, 128], F32)
            tA = sbs(esW, f"d_tA_{h}", [128, G, 128], F32)
            tI = sbs(esW, f"d_tI_{h}", [128, G, 128], F32)
            EGB = sbs(esW, f"d_EGB_{h}", [128, G, 128], F32)
            A32 = sbs(esW, f"d_A32_{h}", [128, G, 128], F32)
            AT32 = sbs(esW, f"d_AT32_{h}", [128, G, 128], F32)
            ZY = sbs(esW, f"d_ZY_{h}", [128, G, 2, 128], F32)
            ZTYT = sbs(esW, f"d_ZTYT_{h}", [128, G, 2, 128], F32)
            Lb = sbs(esW, f"d_Lb_{h}", [128, G, 128], BF16)
            LTb = sbs(esW, f"d_LTb_{h}", [128, G, 128], BF16)
            Qb = sbs(esW, f"d_Qb_{h}", [128, G, 128], BF16)
            Rb = sbs(esW, f"d_Rb_{h}", [128, G, 128], BF16)
            TT = sbs(esW, f"d_TT_{h}", [128, G, 128], BF16)
            Tb = sbs(esW, f"d_Tb_{h}", [128, G, 128], BF16)
            ZYb = sbs(esW, f"d_ZYb_{h}", [128, G, 2, 128], BF16)
            ZTYTb = sbs(esW, f"d_ZTYTb_{h}", [128, G, 2, 128], BF16)
            Kbe = sbs(esW, f"d_Kbe_{h}", [128, G, 128], BF16)
            Vb = sbs(esW, f"d_Vb_{h}", [128, G, 128], BF16)
            egc = sbs(esW, f"d_egc_{h}", [128, G], F32)
            ebh = sbs(esW, f"d_ebh_{h}", [128, NT, 2], F32)
            for d_ in range(2):
                P.op('act', lambda e: e.activation(out=ebh[:, :, d_], in_=tokS[:, :, d_ * 24 + 16 + h], func=AF.Exp), r=['tokS'], w=['d_ebh'])
                P.op('dve', lambda e: e.tensor_tensor(out=ebh[:, :, d_], in0=ebh[:, :, d_], in1=tokS[:, :, d_ * 24 + h], op=ALU.mult), r=['d_ebh', 'tokS'], w=['d_ebh'])
            zs_v = zs[:, h * 128:(h + 1) * 128].rearrange("(t p) c -> p t c", p=128)
            for q4 in range(8):
                P.op('sp', lambda e: e.dma_start(out=zsh[:, q4 * 4:(q4 + 1) * 4, :], in_=zs_v[:, q4 * 4:(q4 + 1) * 4, :]),
                     r=[('scr', 'zs', t_, b_) for t_ in range(q4 * 4, q4 * 4 + 4) for b_ in range(2)], w=['d_zsh'], dma=True)
            for t0 in range(0, NT, 2):
                units = [(ti, d) for ti in range(2) for d in range(2)]
                fl = []
                for ti in range(2):
                    ts_ = slice((t0 + ti) * 128, (t0 + ti + 1) * 128)
                    fl.append(lambda e, ti=ti, ts_=ts_: e.transpose(out=psT[0][:, ti * 256:ti * 256 + 128], in_=Kt[:, ts_], identity=identb[:]))
                    fl.append(lambda e, ti=ti, ts_=ts_: e.transpose(out=psT[0][:, ti * 256 + 128:ti * 256 + 256], in_=Vch[:, ts_], identity=identb[:]))
                    fl.append(lambda e, ti=ti, ts_=ts_: e.matmul(pM[:, ti * 256:ti * 256 + 128], lhsT=Kt[:, ts_], rhs=Kt[:, ts_], start=True, stop=True))
                    fl.append(lambda e, ti=ti, ts_=ts_: e.matmul(pM[:, ti * 256 + 128:ti * 256 + 256], lhsT=Kt[:, ts_], rhs=Qt[:, ts_], start=True, stop=True))
                P.pe_group(fl, r=['d_qkv0', 'd_qkv1', 'd_qkv2', 'identb'], w=['psT0', kM])
                pT4 = psT[0][:].rearrange("p (a b n) -> p a b n", a=2, b=2)
                P.op('act', lambda e: e.copy(out=Ktok[:], in_=pT4[:, :, 0, :]), r=['psT0'], w=['d_Ktok'])
                P.op('act', lambda e: e.copy(out=Vtok[:], in_=pT4[:, :, 1, :]), r=['psT0'], w=['d_Vtok'])
                stop(31)
                for u, (ti, d) in enumerate(units):
                    t = t0 + ti
                    gcol = tokS[:, t, d * 24 + 16 + h:d * 24 + 17 + h]
                    P.op('dve', lambda e: e.tensor_scalar(out=Dg[:, u, :], in0=ident[:], scalar1=gcol, scalar2=None, op0=ALU.mult), r=['ident', 'tokS'], w=[('d_Dg', u)])
                P.pe_group([(lambda e, u=u: e.matmul(pG[:, u * 128:(u + 1) * 128], lhsT=ones_f[:], rhs=Dg[:, u, :], start=True, stop=True)) for u in range(G)],
                           r=['ones_f'] + [('d_Dg', u) for u in range(G)], w=[kG])
                stop(32)
                P.op('act', lambda e: e.copy(out=EGB[:], in_=pG[:].rearrange("p (u n) -> p u n", u=G)), r=[kG], w=['d_GBs'])
                for u, (ti, d) in enumerate(units):
                    t = t0 + ti
                    gcol = tokS[:, t, d * 24 + 16 + h:d * 24 + 17 + h]
                    P.op('dve', lambda e: e.scalar_tensor_tensor(out=tA[:, u, :], in0=EGB[:, u, :], scalar=gcol, in1=msk[:, 2 * d, :], op0=ALU.subtract, op1=ALU.max),
                         r=['d_GBs', 'tokS', 'd_msk'], w=[('d_tA', u)])
                    P.op('dve', lambda e: e.scalar_tensor_tensor(out=tI[:, u, :], in0=EGB[:, u, :], scalar=gcol, in1=msk[:, 2 * d + 1, :], op0=ALU.subtract, op1=ALU.min),
                         r=['d_GBs', 'tokS', 'd_msk'], w=[('d_tI', u)])
                kTA = [('d_tA', u) for u in range(G)]
                kTI = [('d_tI', u) for u in range(G)]
                P.op('act', lambda e: e.activation(out=tA[:], in_=tA[:], func=AF.Exp, scale=-1.0), r=kTA, w=kTA)
                P.op('act', lambda e: e.activation(out=tI[:], in_=tI[:], func=AF.Exp), r=kTI, w=kTI)
                P.op('act', lambda e: e.activation(out=EGB[:], in_=EGB[:], func=AF.Exp), r=['d_GBs'] + kTA + kTI, w=['d_GBs'])
                stop(33)
                for u, (ti, d) in enumerate(units):
                    t = t0 + ti
                    ts_ = slice(t * 128, (t + 1) * 128)
                    bcol = tokS[:, t, d * 24 + h:d * 24 + h + 1]
                    lastc = 127 if d == 0 else 0
                    P.op('dve', lambda e: e.scalar_tensor_tensor(out=A32[:, u, :], in0=pM[:, ti * 256:ti * 256 + 128], scalar=bcol, in1=tA[:, u, :], op0=ALU.mult, op1=ALU.mult),
                         r=[kM, 'tokS', ('d_tA', u)], w=[('d_A32', u)])
                P.pe_group([(lambda e, u=u: e.transpose(out=pX0[:, u * 128:(u + 1) * 128], in_=A32[:, u, :], identity=ident[:])) for u in range(G)],
                           r=[('d_A32', u) for u in range(G)] + ['ident'], w=[kX0])
                for u, (ti, d) in enumerate(units):
                    t = t0 + ti
                    ts_ = slice(t * 128, (t + 1) * 128)
                    bcol = tokS[:, t, d * 24 + h:d * 24 + h + 1]
                    lastc = 127 if d == 0 else 0
                    P.op('dve', lambda e: e.tensor_tensor(out=ITst[:, t, d, :], in0=pM[:, ti * 256 + 128:ti * 256 + 256], in1=tI[:, u, :], op=ALU.mult),
                         r=[kM, ('d_tI', u)], w=[('d_IT', t, d)])
                    P.op('dve', lambda e: e.tensor_tensor(out=QDst[:, t, d, :], in0=Qt[:, ts_], in1=EGB[:, u, :], op=ALU.mult), r=['d_qkv0', 'd_GBs'], w=[('d_QD', t, d)])
                    P.op('act', lambda e: e.copy(out=egl[:, t, d:d + 1], in_=EGB[:, u, lastc:lastc + 1]), r=['d_GBs'], w=[('d_egl', t, d)])
                    P.op('act', lambda e: e.activation(out=KSst[:, t, d, :], in_=Ktok[:, ti, :], func=AF.Copy, scale=tI[:, u, lastc:lastc + 1]),
                         r=['d_Ktok', ('d_tI', u)], w=[('d_KS', t, d)])
                    P.op('act', lambda e: e.activation(out=Kbe[:, u, :], in_=Ktok[:, ti, :], func=AF.Copy, scale=ebh[:, t, d:d + 1]),
                         r=['d_Ktok', 'd_ebh'], w=[('d_Kbe', u)])
                    P.op('act', lambda e: e.activation(out=Vb[:, u, :], in_=Vtok[:, ti, :], func=AF.Copy, scale=bcol), r=['d_Vtok', 'tokS'], w=[('d_Vb', u)])
                stop(34)
                kA = [('d_A32', u) for u in range(G)]
                v3 = lambda p_: p_[:].rearrange("p (u n) -> p u n", u=G)
                lmv = lambda j_: lm[:, j_, :].rearrange("p (u n) -> p u n", u=G)
                P.op('act', lambda e: e.copy(out=AT32[:], in_=v3(pX0)), r=[kX0], w=['d_AT32'])
                P.op('dve', lambda e: e.scalar_tensor_tensor(out=ZY[:, :, 0, :], in0=A32[:], scalar=-1.0, in1=lmv(0), op0=ALU.mult, op1=ALU.mult), r=kA + ['d_lm'], w=['d_ZY'])
                P.op('dve', lambda e: e.scalar_tensor_tensor(out=ZTYT[:, :, 0, :], in0=AT32[:], scalar=-1.0, in1=lmv(0), op0=ALU.mult, op1=ALU.mult), r=['d_AT32', 'd_lm'], w=['d_ZTYT'])
                P.op('pool', lambda e: e.tensor_tensor(out=ZY[:, :, 1, :], in0=ZY[:, :, 0, :], in1=lmv(4), op=ALU.add), r=['d_ZY', 'd_lm'], w=['d_ZY'])
                P.op('dve', lambda e: e.tensor_tensor(out=ZTYT[:, :, 1, :], in0=ZTYT[:, :, 0, :], in1=lmv(4), op=ALU.add), r=['d_ZTYT', 'd_lm'], w=['d_ZTYT'])
                stop(35)
                P.op('act', lambda e: e.copy(out=ZYb[:], in_=ZY[:]), r=['d_ZY'], w=['d_ZYb'])
                P.op('dve', lambda e: e.tensor_copy(out=ZTYTb[:], in_=ZTYT[:]), r=['d_ZTYT'], w=['d_ZTYTb'])
                P.pe_group([f_ for u in range(G) for f_ in (
                    (lambda e, u=u: e.matmul(pX0[:, u * 128:(u + 1) * 128], lhsT=ZTYTb[:, u, 0, :], rhs=ZYb[:, u, 0, :], start=True, stop=True)),
                    (lambda e, u=u: e.matmul(pX1[:, u * 128:(u + 1) * 128], lhsT=ZYb[:, u, 0, :], rhs=ZTYTb[:, u, 0, :], start=True, stop=True)))],
                    r=['d_ZYb', 'd_ZTYTb'], w=[kX0, kX1])
                P.op('act', lambda e: e.copy(out=ZYb[:, :, 0, :], in_=v3(pX0)), r=[kX0], w=['d_ZYb'])
                P.op('dve', lambda e: e.tensor_copy(out=ZTYTb[:, :, 0, :], in_=v3(pX1)), r=[kX1], w=['d_ZTYTb'])
                for lvl in (1, 2):
                    P.pe_group([f_ for u in range(G) for f_ in (
                        (lambda e, u=u: e.matmul((pX0 if u < 2 else pX1)[:, (u % 2) * 256:(u % 2) * 256 + 256], lhsT=ZTYTb[:, u, 0, :], rhs=ZYb[:, u, :, :].rearrange("p c n -> p (c n)"), start=True, stop=True)),
                        (lambda e, u=u: e.matmul((pZT if u < 2 else pU)[:, (u % 2) * 256:(u % 2) * 256 + 256], lhsT=ZYb[:, u, 0, :], rhs=ZTYTb[:, u, :, :].rearrange("p c n -> p (c n)"), start=True, stop=True)))],
                        r=['d_ZYb', 'd_ZTYTb'], w=[kX0, kX1, kZT, kU])
                    for hf, (px, kx, pz, kz) in enumerate(((pX0, kX0, pZT, kZT), (pX1, kX1, pU, kU))):
                        p4 = px[:].rearrange("p (u c n) -> p u c n", u=2, c=2)
                        z4 = pz[:].rearrange("p (u c n) -> p u c n", u=2, c=2)
                        hs2 = slice(hf * 2, hf * 2 + 2)
                        P.op('act', lambda e: e.copy(out=ZYb[:, hs2, 0, :], in_=p4[:, :, 0, :]), r=[kx], w=['d_ZYb'])
                        P.op('dve', lambda e: e.tensor_tensor(out=ZY[:, hs2, 1, :], in0=p4[:, :, 1, :], in1=ZY[:, hs2, 1, :], op=ALU.add), r=[kx, 'd_ZY'], w=['d_ZY'])
                        P.op('act', lambda e: e.copy(out=ZTYTb[:, hs2, 0, :], in_=z4[:, :, 0, :]), r=[kz], w=['d_ZTYTb'])
                        P.op('dve', lambda e: e.tensor_tensor(out=ZTYT[:, hs2, 1, :], in0=z4[:, :, 1, :], in1=ZTYT[:, hs2, 1, :], op=ALU.add), r=[kz, 'd_ZTYT'], w=['d_ZTYT'])
                    P.op('act', lambda e: e.copy(out=ZYb[:, :, 1, :], in_=ZY[:, :, 1, :]), r=['d_ZY'], w=['d_ZYb'])
                    P.op('dve', lambda e: e.tensor_copy(out=ZTYTb[:, :, 1, :], in_=ZTYT[:, :, 1, :]), r=['d_ZTYT'], w=['d_ZTYTb'])
                P.pe_group([f_ for u in range(G) for f_ in (
                    (lambda e, u=u: e.matmul(pX0[:, u * 128:(u + 1) * 128], lhsT=ZTYTb[:, u, 0, :], rhs=ZYb[:, u, 1, :], start=True, stop=True)),
                    (lambda e, u=u: e.matmul(pX1[:, u * 128:(u + 1) * 128], lhsT=ZYb[:, u, 0, :], rhs=ZTYTb[:, u, 1, :], start=True, stop=True)))],
                    r=['d_ZYb', 'd_ZTYTb'], w=[kX0, kX1])
                P.op('dve', lambda e: e.tensor_tensor(out=ZY[:, :, 1, :], in0=v3(pX0), in1=ZY[:, :, 1, :], op=ALU.add), r=[kX0, 'd_ZY'], w=['d_ZY'])
                P.op('dve', lambda e: e.tensor_tensor(out=ZTYT[:, :, 1, :], in0=v3(pX1), in1=ZTYT[:, :, 1, :], op=ALU.add), r=[kX1, 'd_ZTYT'], w=['d_ZTYT'])
                P.op('act', lambda e: e.copy(out=TT[:], in_=ZTYT[:, :, 1, :]), r=['d_ZTYT'], w=['d_TT'])
                P.op('dve', lambda e: e.tensor_copy(out=Tb[:], in_=ZY[:, :, 1, :]), r=['d_ZY'], w=['d_Tb'])
                for li in range(3):
                    last = (li == 2)
                    P.op('pool', lambda e: e.tensor_tensor(out=Lb[:], in0=A32[:], in1=lmv(1 + li), op=ALU.mult), r=kA + ['d_lm'], w=['d_Lb'])
                    if not last:
                        P.op('dve', lambda e: e.tensor_tensor(out=LTb[:], in0=AT32[:], in1=lmv(1 + li), op=ALU.mult), r=['d_AT32', 'd_lm'], w=['d_LTb'])
                    fl = [(lambda e, u=u: e.matmul(pX1[:, u * 128:(u + 1) * 128], lhsT=Lb[:, u, :], rhs=TT[:, u, :], start=True, stop=True)) for u in range(G)]
                    if not last:
                        fl += [(lambda e, u=u: e.matmul(pX0[:, u * 128:(u + 1) * 128], lhsT=LTb[:, u, :], rhs=Tb[:, u, :], start=True, stop=True)) for u in range(G)]
                    P.pe_group(fl, r=['d_Lb', 'd_TT'] + ([] if last else ['d_LTb', 'd_Tb']), w=[kX1] + ([] if last else [kX0]))
                    P.op('act', lambda e: e.copy(out=Rb[:], in_=v3(pX1)), r=[kX1], w=['d_Rb'])
                    if not last:
                        P.op('dve', lambda e: e.tensor_copy(out=Qb[:], in_=v3(pX0)), r=[kX0], w=['d_Qb'])
                    fl = [(lambda e, u=u: e.matmul(pU[:, u * 128:(u + 1) * 128], lhsT=Tb[:, u, :], rhs=Rb[:, u, :], start=True, stop=True)) for u in range(G)]
                    if not last:
                        fl += [(lambda e, u=u: e.matmul(pZT[:, u * 128:(u + 1) * 128], lhsT=TT[:, u, :], rhs=Qb[:, u, :], start=True, stop=True)) for u in range(G)]
                    P.pe_group(fl, r=['d_Tb', 'd_Rb'] + ([] if last else ['d_TT', 'd_Qb']), w=[kU] + ([] if last else [kZT]))
                    if not last:
                        P.op('dve', lambda e: e.tensor_tensor(out=ZTYT[:, :, 1, :], in0=ZTYT[:, :, 1, :], in1=v3(pU), op=ALU.subtract), r=[kU, 'd_ZTYT'], w=['d_ZTYT'])
                        P.op('dve', lambda e: e.tensor_tensor(out=ZY[:, :, 1, :], in0=ZY[:, :, 1, :], in1=v3(pZT), op=ALU.subtract), r=[kZT, 'd_ZY'], w=['d_ZY'])
                        P.op('act', lambda e: e.copy(out=TT[:], in_=ZTYT[:, :, 1, :]), r=['d_ZTYT'], w=['d_TT'])
                        P.op('act', lambda e: e.copy(out=Tb[:], in_=ZY[:, :, 1, :]), r=['d_ZY'], w=['d_Tb'])
                    else:
                        P.op('dve', lambda e: e.tensor_tensor(out=TT[:], in0=ZTYT[:, :, 1, :], in1=v3(pU), op=ALU.subtract), r=[kU, 'd_ZTYT'], w=['d_TT'])
                stop(36)
                P.pe_group([f_ for u in range(G) for f_ in (
                    (lambda e, u=u: e.matmul(pU[:, u * 128:(u + 1) * 128], lhsT=TT[:, u, :], rhs=Vb[:, u, :], start=True, stop=True)),
                    (lambda e, u=u: e.matmul(pG[:, u * 128:(u + 1) * 128], lhsT=Kbe[:, u, :], rhs=TT[:, u, :], start=True, stop=True)))],
                    r=['d_TT'] + [('d_Vb', u) for u in range(G)] + [('d_Kbe', u) for u in range(G)], w=[kU, kG])
                P.op('act', lambda e: e.copy(out=Ust[:, t0:t0 + 2, :, :].rearrange("p a b n -> p (a b) n"), in_=pU[:].rearrange("p (u n) -> p u n", u=G)), r=[kU], w=[('d_U', t0)])
                P.op('dve', lambda e: e.tensor_copy(out=WTst[:, t0:t0 + 2, :, :].rearrange("p a b n -> p (a b) n"), in_=pG[:].rearrange("p (u n) -> p u n", u=G)), r=[kG], w=[('d_WT', t0)])
                stop(3)
            P.barrier()
            stop(4)
            for d in range(2):
                P.op('dve', lambda e: e.memset(S32[d][:], 0.0), w=[f'd_S32{d}'])
                P.op('dve', lambda e: e.memset(Sbf[d][:], 0.0), w=[f'd_Sbf{d}'])
            for step in range(NT):
                tt = [step, NT - 1 - step]
                bank = [((pM, kM), (pX0, kX0), (pZT, kZT)), ((pG, kG), (pX1, kX1), (pU, kU))]
                P.pe_group([(lambda e, d=d: e.matmul(bank[d][0][0][:, 0:128], lhsT=WTst[:, tt[d], d, :], rhs=Sbf[d][:], start=True, stop=True)) for d in range(2)],
                           r=[('d_WT', (tt[0] // 2) * 2), ('d_WT', (tt[1] // 2) * 2), 'd_Sbf0', 'd_Sbf1'], w=[bank[0][0][1], bank[1][0][1]])
                for d in range(2):
                    t = tt[d]; t0 = (t // 2) * 2
                    (pW, kW), (pO, kO) = bank[d][0], bank[d][1]
                    P.op('dve', lambda e: e.tensor_tensor(out=Vn[d][:], in0=Ust[:, t, d, :], in1=pW[:, 0:128], op=ALU.subtract), r=[('d_U', t0), kW], w=[f'd_Vn{d}'])
                    P.op('pe', lambda e: e.matmul(pO[:, 0:128], lhsT=QDst[:, t, d, :], rhs=Sbf[d][:], start=True, stop=False), r=[('d_QD', t, d), f'd_Sbf{d}'], w=[kO])
                for d in range(2):
                    t = tt[d]
                    (pO, kO), (pD, kD) = bank[d][1], bank[d][2]
                    P.pe_group([lambda e: e.matmul(pO[:, 0:128], lhsT=ITst[:, t, d, :], rhs=Vn[d][:], start=False, stop=True),
                                lambda e: e.matmul(pD[:, 0:128], lhsT=KSst[:, t, d, :], rhs=Vn[d][:], start=True, stop=True)],
                               r=[('d_IT', t, d), ('d_KS', t, d), f'd_Vn{d}'], w=[kO, kD])
                for d in range(2):
                    t = tt[d]
                    (pO, kO), (pD, kD) = bank[d][1], bank[d][2]
                    P.op('dve', lambda e: e.scalar_tensor_tensor(out=Sbf[d][:], in0=S32[d][:], scalar=egl[:, t, d:d + 1], in1=pD[:, 0:128], op0=ALU.mult, op1=ALU.add),
                         r=[f'd_S32{d}', ('d_egl', t, d), kD], w=[f'd_Sbf{d}'])
                    P.op('dve', lambda e: e.scalar_tensor_tensor(out=S32[d][:], in0=S32[d][:], scalar=egl[:, t, d:d + 1], in1=pD[:, 0:128], op0=ALU.mult, op1=ALU.add),
                         r=[f'd_S32{d}', ('d_egl', t, d), kD], w=[f'd_S32{d}'])
                    if step < NT // 2:
                        P.op('act', lambda e: e.copy(out=Oacc[:, t, :], in_=pO[:, 0:128]), r=[kO], w=[('d_Oacc', t)])
                    else:
                        P.op('pool', lambda e: e.tensor_tensor(out=Oacc[:, t, :], in0=Oacc[:, t, :], in1=Oacc[:, t, :], op=ALU.add), r=[('d_Oacc', t)], w=[('d_Oacc', t)]) if False else \
                            P.op('dve', lambda e: e.tensor_tensor(out=Oacc[:, t, :], in0=pO[:, 0:128], in1=Oacc[:, t, :], op=ALU.add), r=[kO, ('d_Oacc', t)], w=[('d_Oacc', t)])
            stop(5)
            osq = [sbs(esW, f"d_osq{i_}_{h}", [128, 128], F32) for i_ in range(2)]
            ors = sbs(esW, f"d_ors_{h}", [128, 2, NT], F32)
            ont = [sbs(esW, f"d_ont{i_}_{h}", [128, 128], F32) for i_ in range(4)]
            onbt = [sbs(esW, f"d_onbt{i_}_{h}", [128, 128], BF16) for i_ in range(4)]
            kO_all = [('d_Oacc', t_) for t_ in range(NT)]
            P.op('dve', lambda e: e.memset(ors[:], 0.0), w=['d_ors'])
            for t in range(NT):
                P.op('act', lambda e: e.activation(out=osq[t % 2][:], in_=Oacc[:, t, :], func=AF.Square, accum_out=ors[:, 0, t:t + 1]), r=[('d_Oacc', t), 'd_ors'], w=[f'd_osq{t % 2}', ('d_ors_acc', t)])
            P.op('act', lambda e: e.activation(out=ors[:, 1, :], in_=ors[:, 0, :], func=AF.Sqrt, scale=1.0 / 128, bias=eps_l2[:]), r=['d_ors', 'eps_l2'] + [('d_ors_acc', t_) for t_ in range(NT)], w=['d_ors'])
            P.op('dve', lambda e: e.reciprocal(out=ors[:, 1, :], in_=ors[:, 1, :]), r=['d_ors'], w=['d_ors'])
            for t4 in range(0, NT, 4):
                for j in range(4):
                    t = t4 + j
                    P.op('dve', lambda e: e.scalar_tensor_tensor(out=ont[j][:], in0=Oacc[:, t, :], scalar=ors[:, 1, t:t + 1], in1=gain[:], op0=ALU.mult, op1=ALU.mult),
                         r=[('d_Oacc', t), 'd_ors', 'd_gain'], w=[f'd_ont{j}'])
                    P.op('pool', lambda e: e.tensor_tensor(out=onbt[j][:], in0=ont[j][:], in1=zsh[:, t, :], op=ALU.mult), r=[f'd_ont{j}', 'd_zsh'], w=[f'd_onbt{j}'])
                P.pe_group([(lambda e, j=j: e.transpose(out=psT[1][:, j * 128:(j + 1) * 128], in_=onbt[j][:], identity=identb[:])) for j in range(4)],
                           r=[f'd_onbt{j}' for j in range(4)] + ['identb'], w=['psT1'])
                P.op('act', lambda e: e.copy(out=oTd[:, t4 * 128:(t4 + 4) * 128], in_=psT[1][:]), r=['psT1'], w=['d_oTd'])
            fin.append(P.op('sp', lambda e: e.dma_start(out=oT_dn[h, :, :], in_=oTd[:]), r=['d_oTd'], w=[('scr', 'oT_dn', h)], dma=True))
            P.barrier()
            esW.close()
        P.barrier()
        esD.close()

    def phase_MG_MOE():
        esP = ExitStack()
        affTM = sbs(esP, "affTM", [128, NT, 16], F32)
        posm = sbs(esP, "posm", [128, NT, 16], F32)
        iot = sbs(esP, "iot", [128, 512], F32)
        bcs = sbs(esP, "bcs", [128, 4, D], F32)
        gfin = sbs(esP, "gfin", [128, D], F32)
        onesb = sbs(esP, "m_onesb", [128, 128], BF16)
        trib = sbs(esP, "trib", [128, 128], BF16)
        st2 = sbs(esP, "st2", [128, 8], F32)
        P.op('dve', lambda e: e.memset(onesb[:], 1.0), w=['m_onesb'])
        P.op('sp', lambda e: e.dma_start(out=iot[:], in_=iota512[:, :]), w=['iot'], dma=True)
        P.op('sp', lambda e: e.dma_start(out=gfin[:], in_=g_final[:, :]), w=['gfin'], dma=True)
        esH = ExitStack()
        h2b = sbs(esH, "h2b", [128, NT, D], BF16)
        esT = ExitStack()
        affT = sbs(esT, "affT", [16, S], F32)
        esG = ExitStack()
        es1 = ExitStack()
        mrow = sbs(es1, "g_mrow", [1, 4 * D], F32)
        trif = sbs(es1, "g_trif", [128, 128], F32)
        gff = sbs(es1, "g_gff", [128, D], F32)
        P.op('sp', lambda e: e.dma_start(out=mrow[:], in_=modrow[:, 2 * D:6 * D]), r=['mod'], w=['g_mrow'], dma=True)
        P.op('sp', lambda e: e.dma_start(out=trif[:], in_=tri_in[:, :]), w=['g_trif'], dma=True)
        P.op('dve', lambda e: e.tensor_copy(out=trib[:], in_=trif[:]), r=['g_trif'], w=['trib'])
        P.op('sp', lambda e: e.dma_start(out=gff[:], in_=g_ffn[:, :]), w=['g_gff'], dma=True)
        for j in range(8):
            src = j // 2
            dsti = {0: 0, 1: 2, 2: 1, 3: 3}[src]
            pt, pk = next_psA()
            P.op('pe', lambda e: e.matmul(pt[:], lhsT=ones_f[0:1, :], rhs=mrow[:, j * 512:(j + 1) * 512], start=True, stop=True), r=['ones_f', 'g_mrow'], w=[pk])
            P.op('act', lambda e: e.copy(out=bcs[:, dsti, (j % 2) * 512:(j % 2 + 1) * 512], in_=pt[:]), r=[pk], w=['bcs'])
        P.op('dve', lambda e: e.scalar_tensor_tensor(out=bcs[:, 1, :], in0=bcs[:, 1, :], scalar=1.0, in1=gff[:], op0=ALU.add, op1=ALU.mult), r=['bcs', 'g_gff'], w=['bcs'])
        P.barrier()
        es1.close()
        wod = sbs(esG, "g_wod", [128, 8, D], BF16)
        wom = sbs(esG, "g_wom", [128, 8, D], BF16)
        wou = sbs(esG, "g_wou", [128, 8, D], BF16)
        wr = sbs(esG, "g_wr", [128, 8, 16], F32)
        for wt_, src_, k_ in ((wod, w_o_dn, 'g_wod'), (wom, w_o_mla, 'g_wom'), (wou, w_out, 'g_wou')):
            v = src_.rearrange("(k p) n -> p k n", p=128)
            for kk in range(0, 8, 2):
                P.op('pool', lambda e: e.dma_start(out=wt_[:, kk:kk + 2, :], in_=v[:, kk:kk + 2, :]), w=[k_], dma=True)
        P.op('sp', lambda e: e.dma_start(out=wr[:], in_=w_router.rearrange("(k p) n -> p k n", p=128)), w=['g_wr'], dma=True)
        odn = [sbs(esG, f"g_odn{i}", [128, 8, 128], BF16) for i in range(2)]
        oml = [sbs(esG, f"g_oml{i}", [128, 8, 128], BF16) for i in range(2)]
        gst = [sbs(esG, f"g_gst{i}", [128, 2 * D], BF16) for i in range(2)]
        xt0 = sbs(esG, "g_xt0", [128, D], F32)
        xt = [xt0, xt0]
        m1 = sbs(esG, "g_m1", [128, D], F32)
        m2 = sbs(esG, "g_m2", [128, D], F32)
        mb = sbs(esG, "g_mb", [128, D], BF16)
        mT = sbs(esG, "g_mT", [128, 8, 128], BF16)
        x1t = [sbs(esG, f"g_x1t{i}", [128, D], F32) for i in range(2)]
        h2f = sbs(esG, "g_h2f", [128, D], F32)
        h2T = sbs(esG, "g_h2T", [128, 8, 128], F32)
        lg = sbs(esG, "g_lg", [128, 16], F32)
        for t in range(NT):
            i = t % 2
            ts_ = slice(t * 128, (t + 1) * 128)
            P.op('sp', lambda e: e.dma_start(out=odn[i][:], in_=oT_dn[:, :, ts_].rearrange("h p n -> p h n")), r=[('scr', 'oT_dn', h_) for h_ in range(8)], w=[f'g_odn{i}'], dma=True)
            P.op('sp', lambda e: e.dma_start(out=oml[i][:], in_=oT_mla[:, :, ts_].rearrange("h p n -> p h n")), r=[('scr', 'oT_mla', h_) for h_ in range(8)], w=[f'g_oml{i}'], dma=True)
            P.op('sp', lambda e: e.dma_start(out=gst[i][:], in_=gs[ts_, :]), r=[('scr', 'gs', t, b_) for b_ in range(4)], w=[f'g_gst{i}'], dma=True)
            P.op('sp', lambda e: e.dma_start(out=xt[i][:], in_=x[ts_, :]), w=['g_xt0'], dma=True)
            for br, (o_, ok_, w_, wk_) in enumerate(((odn[i], f'g_odn{i}', wod, 'g_wod'), (oml[i], f'g_oml{i}', wom, 'g_wom'))):
                for hc in range(2):
                    pt, pk = psA[br * 2 + hc], f'psA{br * 2 + hc}'
                    P.pe_group([(lambda e, k=k: e.matmul(pt[:], lhsT=o_[:, k, :], rhs=w_[:, k, hc * 512:(hc + 1) * 512], start=(k == 0), stop=(k == 7))) for k in range(8)], r=[ok_, wk_], w=[pk])
            for hc in range(2):
                hs_ = slice(hc * 512, (hc + 1) * 512)
                P.op('dve', lambda e: e.tensor_tensor(out=m1[:, hs_], in0=psA[hc][:], in1=gst[i][:, hs_], op=ALU.mult), r=[f'psA{hc}', f'g_gst{i}'], w=['g_m1'])
                P.op('dve', lambda e: e.tensor_tensor(out=m2[:, hs_], in0=psA[2 + hc][:], in1=gst[i][:, D + hc * 512:D + (hc + 1) * 512], op=ALU.mult), r=[f'psA{2 + hc}', f'g_gst{i}'], w=['g_m2'])
            P.op('pool', lambda e: e.tensor_tensor(out=mb[:], in0=m1[:], in1=m2[:], op=ALU.add), r=['g_m1', 'g_m2'], w=['g_mb'])
            for half in range(2):
                P.pe_group([(lambda e, k4=k4: e.transpose(out=psT[half][:, k4 * 128:(k4 + 1) * 128], in_=mb[:, (half * 4 + k4) * 128:(half * 4 + k4 + 1) * 128], identity=identb[:])) for k4 in range(4)], r=['g_mb', 'identb'], w=[f'psT{half}'])
                P.op('act', lambda e: e.copy(out=mT[:, half * 4:(half + 1) * 4, :], in_=psT[half][:].rearrange("p (k n) -> p k n", k=4)), r=[f'psT{half}'], w=['g_mT'])
            for hc in range(2):
                hs_ = slice(hc * 512, (hc + 1) * 512)
                P.pe_group([(lambda e, k=k: e.matmul(psS[hc][:], lhsT=mT[:, k, :], rhs=wou[:, k, hs_], start=(k == 0), stop=(k == 7))) for k in range(8)], r=['g_mT', 'g_wou'], w=[f'psS{hc}'])
                P.op('dve', lambda e: e.tensor_tensor(out=x1t[i][:, hs_], in0=psS[hc][:], in1=bcs[:, 0, hs_], op=ALU.mult), r=[f'psS{hc}', 'bcs'], w=[f'g_x1t{i}'])
            P.op('pool', lambda e: e.tensor_tensor(out=x1t[i][:], in0=x1t[i][:], in1=xt[i][:], op=ALU.add), r=[f'g_x1t{i}', 'g_xt0'], w=[f'g_x1t{i}'])
            fin.append(P.op('sp', lambda e: e.dma_start(out=x1s[ts_, :], in_=x1t[i][:]), r=[f'g_x1t{i}'], w=[('scr', 'x1s', t)], dma=True))
            P.op('dve', lambda e: e.memset(st2[:], 0.0), w=['st2'])
            P.op('act', lambda e: e.activation(out=m1[:], in_=x1t[i][:], func=AF.Square, accum_out=st2[:, 0:1]), r=[f'g_x1t{i}'], w=['g_m1', 'st2'])
            P.op('act', lambda e: e.activation(out=st2[:, 1:2], in_=st2[:, 0:1], func=AF.Sqrt, scale=1.0 / D, bias=epsb[:]), r=['st2', 'epsb'], w=['st2'])
            P.op('dve', lambda e: e.reciprocal(out=st2[:, 2:3], in_=st2[:, 1:2]), r=['st2'], w=['st2'])
            P.op('dve', lambda e: e.scalar_tensor_tensor(out=h2f[:], in0=x1t[i][:], scalar=st2[:, 2:3], in1=bcs[:, 1, :], op0=ALU.mult, op1=ALU.mult), r=[f'g_x1t{i}', 'st2', 'bcs'], w=['g_h2f'])
            P.op('pool', lambda e: e.tensor_tensor(out=h2f[:], in0=h2f[:], in1=bcs[:, 2, :], op=ALU.add), r=['g_h2f', 'bcs'], w=['g_h2f'])
            P.op('act', lambda e: e.copy(out=h2b[:, t, :], in_=h2f[:]), r=['g_h2f'], w=[('h2b', t)])
            for half in range(2):
                P.pe_group([(lambda e, k4=k4: e.transpose(out=psA[half][:, k4 * 128:(k4 + 1) * 128], in_=h2f[:, (half * 4 + k4) * 128:(half * 4 + k4 + 1) * 128], identity=ident[:])) for k4 in range(4)], r=['g_h2f', 'ident'], w=[f'psA{half}'])
                P.op('act', lambda e: e.copy(out=h2T[:, half * 4:(half + 1) * 4, :], in_=psA[half][:].rearrange("p (k n) -> p k n", k=4)), r=[f'psA{half}'], w=['g_h2T'])
            P.pe_group([(lambda e, k=k: e.matmul(psA[2][:, 0:16], lhsT=h2T[:, k, :], rhs=wr[:, k, :], start=(k == 0), stop=(k == 7))) for k in range(8)], r=['g_h2T', 'g_wr'], w=['psA2'])
            P.op('dve', lambda e: e.tensor_reduce(out=st2[:, 3:4], in_=psA[2][:, 0:16], axis=AX.X, op=ALU.max), r=['psA2'], w=['st2'])
            P.op('dve', lambda e: e.tensor_scalar(out=st2[:, 4:5], in0=st2[:, 3:4], scalar1=-1.0, scalar2=None, op0=ALU.mult), r=['st2'], w=['st2'])
            P.op('dve', lambda e: e.memset(st2[:, 5:6], 0.0), r=['st2'], w=['st2'])
            P.op('act', lambda e: e.activation(out=lg[:], in_=psA[2][:, 0:16], func=AF.Exp, bias=st2[:, 4:5], scale=1.0, accum_out=st2[:, 5:6]), r=['psA2', 'st2'], w=['g_lg', 'st2'])
            P.op('dve', lambda e: e.reciprocal(out=st2[:, 6:7], in_=st2[:, 5:6]), r=['st2'], w=['st2'])
            P.op('dve', lambda e: e.tensor_scalar(out=affTM[:, t, :], in0=lg[:], scalar1=st2[:, 6:7], scalar2=None, op0=ALU.mult), r=['g_lg', 'st2'], w=[('affTM', t)])
            P.op('pe', lambda e: e.transpose(out=psA[3][0:16, 0:128], in_=affTM[:, t, :], identity=ident[:]), r=[('affTM', t), 'ident'], w=['psA3'])
            P.op('act', lambda e: e.copy(out=affT[:, ts_], in_=psA[3][0:16, 0:128]), r=['psA3'], w=['affT'])
        P.barrier()
        esG.close()
        esE = ExitStack()
        es1 = ExitStack()
        cmpb = sbs(es1, "e_cmp", [16, S], F32)
        bis = sbs(es1, "e_bis", [16, 8], F32)
        maskT = sbs(es1, "e_maskT", [16, S], F32)
        P.op('dve', lambda e: e.memset(bis[:], 0.0), w=['e_bis'])
        P.op('dve', lambda e: e.memset(bis[:, 1:2], 1.0), r=['e_bis'], w=['e_bis'])
        for it in range(32):
            P.op('dve', lambda e: e.tensor_tensor(out=bis[:, 2:3], in0=bis[:, 0:1], in1=bis[:, 1:2], op=ALU.add), r=['e_bis'], w=['e_bis'])
            P.op('dve', lambda e: e.tensor_scalar(out=bis[:, 2:3], in0=bis[:, 2:3], scalar1=0.5, scalar2=None, op0=ALU.mult), r=['e_bis'], w=['e_bis'])
            P.op('dve', lambda e: e.tensor_scalar(out=cmpb[:], in0=affT[:], scalar1=bis[:, 2:3], scalar2=None, op0=ALU.is_ge), r=['affT', 'e_bis'], w=['e_cmp'])
            P.op('dve', lambda e: e.tensor_reduce(out=bis[:, 3:4], in_=cmpb[:], axis=AX.X, op=ALU.add), r=['e_cmp'], w=['e_bis'])
            P.op('dve', lambda e: e.tensor_scalar(out=bis[:, 4:5], in0=bis[:, 3:4], scalar1=511.5, scalar2=None, op0=ALU.is_ge), r=['e_bis'], w=['e_bis'])
            P.op('dve', lambda e: e.tensor_tensor(out=bis[:, 5:6], in0=bis[:, 2:3], in1=bis[:, 0:1], op=ALU.subtract), r=['e_bis'], w=['e_bis'])
            P.op('dve', lambda e: e.tensor_tensor(out=bis[:, 6:7], in0=bis[:, 1:2], in1=bis[:, 2:3], op=ALU.subtract), r=['e_bis'], w=['e_bis'])
            P.op('dve', lambda e: e.scalar_tensor_tensor(out=bis[:, 0:1], in0=bis[:, 5:6], scalar=bis[:, 4:5], in1=bis[:, 0:1], op0=ALU.mult, op1=ALU.add), r=['e_bis'], w=['e_bis'])
            P.op('dve', lambda e: e.scalar_tensor_tensor(out=bis[:, 1:2], in0=bis[:, 6:7], scalar=bis[:, 4:5], in1=bis[:, 2:3], op0=ALU.mult, op1=ALU.add), r=['e_bis'], w=['e_bis'])
        P.op('dve', lambda e: e.tensor_scalar(out=maskT[:], in0=affT[:], scalar1=bis[:, 0:1], scalar2=None, op0=ALU.is_ge), r=['affT', 'e_bis'], w=['e_maskT'])
        mk32 = sbs(es1, "e_mk32", [128, 16], F32)
        mkb = sbs(es1, "e_mkb", [128, 16], BF16)
        base = sbs(es1, "e_base", [128, 16], F32)
        ptmp = sbs(es1, "e_ptmp", [128, 16], F32)
        P.op('dve', lambda e: e.memset(base[:], 0.0), w=['e_base'])
        for t in range(NT):
            ts_ = slice(t * 128, (t + 1) * 128)
            P.op('pe', lambda e: e.transpose(out=psA[0][:, 0:16], in_=maskT[:, ts_], identity=ident[0:16, 0:16]), r=['e_maskT', 'ident'], w=['psA0'])
            P.op('act', lambda e: e.copy(out=mk32[:], in_=psA[0][:, 0:16]), r=['psA0'], w=['e_mk32'])
            P.op('dve', lambda e: e.tensor_copy(out=mkb[:], in_=mk32[:]), r=['e_mk32'], w=['e_mkb'])
            P.op('pe', lambda e: e.matmul(psA[1][:, 0:16], lhsT=trib[:], rhs=mkb[:], start=True, stop=True), r=['trib', 'e_mkb'], w=['psA1'])
            P.op('pe', lambda e: e.matmul(psA[2][:, 0:16], lhsT=onesb[:], rhs=mkb[:], start=True, stop=True), r=['m_onesb', 'e_mkb'], w=['psA2'])
            P.op('dve', lambda e: e.tensor_tensor(out=ptmp[:], in0=psA[1][:, 0:16], in1=base[:], op=ALU.add), r=['psA1', 'e_base'], w=['e_ptmp'])
            P.op('dve', lambda e: e.scalar_tensor_tensor(out=ptmp[:], in0=ptmp[:], scalar=1.0, in1=mk32[:], op0=ALU.add, op1=ALU.mult), r=['e_ptmp', 'e_mk32'], w=['e_ptmp'])
            P.op('dve', lambda e: e.tensor_scalar(out=posm[:, t, :], in0=ptmp[:], scalar1=-1.0, scalar2=None, op0=ALU.add), r=['e_ptmp'], w=['posm'])
            P.op('dve', lambda e: e.tensor_tensor(out=base[:], in0=psA[2][:, 0:16], in1=base[:], op=ALU.add), r=['psA2', 'e_base'], w=['e_base'])
        P.barrier()
        es1.close()
        esT.close()
        wg = sbs(esE, "e_wg", [128, 8, D], BF16)
        wu = sbs(esE, "e_wu", [128, 8, D], BF16)
        wd = sbs(esE, "e_wd", [128, 8, D], BF16)
        Sel = sbs(esE, "e_Sel", [128, NT, 512], BF16)
        xeT = sbs(esE, "e_xeT", [128, 8, 512], BF16)
        hid = sbs(esE, "e_hid", [128, 8, 512], BF16)
        sg0 = sbs(esE, "e_sg0", [128, 512], F32)
        sg = [sg0, sg0]
        yeb = sbs(esE, "e_yeb", [128, 4, D], BF16)
        stgw = [sbs(esE, f"e_stg{i}", [128, D], F32) for i in range(2)]
        stg_n = [0]
        for ex in range(16):
            for wt_, src_, k_ in ((wg, w_gate, 'e_wg'), (wu, w_up, 'e_wu'), (wd, w_down, 'e_wd')):
                v = src_[ex].rearrange("(k p) n -> p k n", p=128)
                for kk in range(8):
                    if kk % 2 == 0:
                        P.op('pool', lambda e: e.dma_start(out=wt_[:, kk, :], in_=v[:, kk, :]), w=[k_], dma=True)
                    else:
                        j = stg_n[0] % 2
                        stg_n[0] += 1
                        P.op('sp', lambda e: e.dma_start(out=stgw[j][:], in_=v[:, kk, :]), w=[f'e_stg{j}'], dma=True)
                        P.op('act', lambda e: e.copy(out=wt_[:, kk, :], in_=stgw[j][:]), r=[f'e_stg{j}'], w=[k_])
            for t in range(NT):
                P.op('dve', lambda e: e.tensor_scalar(out=Sel[:, t, :], in0=iot[:], scalar1=posm[:, t, ex:ex + 1], scalar2=None, op0=ALU.is_equal), r=['iot', 'posm'], w=[('e_Sel', t)])
            for k in range(8):
                pt, pk = psA[k % 4], f'psA{k % 4}'
                P.pe_group([(lambda e, t=t: e.matmul(pt[:], lhsT=h2b[:, t, k * 128:(k + 1) * 128], rhs=Sel[:, t, :], start=(t == 0), stop=(t == NT - 1))) for t in range(NT)],
                           r=[('h2b', t) for t in range(NT)] + [('e_Sel', t) for t in range(NT)], w=[pk])
                if k % 2 == 0:
                    P.op('act', lambda e: e.copy(out=xeT[:, k, :], in_=pt[:]), r=[pk], w=['e_xeT'])
                else:
                    P.op('dve', lambda e: e.tensor_copy(out=xeT[:, k, :], in_=pt[:]), r=[pk], w=['e_xeT'])
            for f in range(8):
                j = f % 2
                pg, pgk = psA[j * 2], f'psA{j * 2}'
                pu, puk = psA[j * 2 + 1], f'psA{j * 2 + 1}'
                P.pe_group([(lambda e, k=k: e.matmul(pg[:], lhsT=wg[:, k, f * 128:(f + 1) * 128], rhs=xeT[:, k, :], start=(k == 0), stop=(k == 7))) for k in range(8)], r=['e_wg', 'e_xeT'], w=[pgk])
                P.pe_group([(lambda e, k=k: e.matmul(pu[:], lhsT=wu[:, k, f * 128:(f + 1) * 128], rhs=xeT[:, k, :], start=(k == 0), stop=(k == 7))) for k in range(8)], r=['e_wu', 'e_xeT'], w=[puk])
                P.op('act', lambda e: e.activation(out=sg[j][:], in_=pg[:], func=AF.Silu), r=[pgk], w=['e_sg0'])
                P.op('dve', lambda e: e.tensor_tensor(out=hid[:, f, :], in0=pu[:], in1=sg[j][:], op=ALU.mult), r=[puk, 'e_sg0'], w=['e_hid'])
            for q in range(4):
                for hc in range(2):
                    ps_y, pyk = psS[hc], f'psS{hc}'
                    P.pe_group([(lambda e, f=f: e.matmul(ps_y[:], lhsT=hid[:, f, q * 128:(q + 1) * 128], rhs=wd[:, f, hc * 512:(hc + 1) * 512], start=(f == 0), stop=(f == 7))) for f in range(8)], r=['e_hid', 'e_wd'], w=[pyk])
                    if hc == 0:
                        P.op('act', lambda e: e.copy(out=yeb[:, q, 0:512], in_=ps_y[:]), r=[pyk], w=['e_yeb'])
                    else:
                        P.op('dve', lambda e: e.tensor_copy(out=yeb[:, q, 512:1024], in_=ps_y[:]), r=[pyk], w=['e_yeb'])
            fin.append(P.op('sp', lambda e: e.dma_start(out=ye_all[ex].rearrange("(q p) d -> p q d", p=128), in_=yeb[:]), r=['e_yeb'], w=[('scr', 'ye', ex)], dma=True))
        P.barrier()
        esE.close()
        esH.close()
        esF = ExitStack()
        yeA = sbs(esF, "f_yeA", [128, 16, 4, D], BF16)
        for ex in range(16):
            P.op('sp', lambda e: e.dma_start(out=yeA[:, ex, :, :], in_=ye_all[ex].rearrange("(q p) d -> p q d", p=128)), r=[('scr', 'ye', ex)], w=['f_yeA'], dma=True)
        Sg = [sbs(esF, f"f_Sg{i}", [128, 512], BF16) for i in range(2)]
        SgT = sbs(esF, "f_SgT", [128, 16, 4, 128], BF16)
        x1l = [sbs(esF, f"f_x1l{i}", [128, D], F32) for i in range(2)]
        ft = sbs(esF, "f_ft", [128, D], F32)
        ot = [sbs(esF, f"f_ot{i}", [128, D], F32) for i in range(2)]
        junk2 = sbs(esF, "f_junk", [128, D], F32)
        for t in range(NT // 2):
            i = t % 2
            ts_ = slice(t * 128, (t + 1) * 128)
            P.op('sp', lambda e: e.dma_start(out=x1l[i][:], in_=x1s[ts_, :]), r=[('scr', 'x1s', t)], w=[f'f_x1l{i}'], dma=True)
            for ex in range(16):
                j = ex % 2
                P.op('dve', lambda e: e.tensor_scalar(out=Sg[j][:], in0=iot[:], scalar1=posm[:, t, ex:ex + 1], scalar2=affTM[:, t, ex:ex + 1], op0=ALU.is_equal, op1=ALU.mult),
                     r=['iot', 'posm', ('affTM', t)], w=[f'f_Sg{j}'])
                P.pe_group([(lambda e, q=q: e.transpose(out=psT[j][:, q * 128:(q + 1) * 128], in_=Sg[j][:, q * 128:(q + 1) * 128], identity=identb[:])) for q in range(4)], r=[f'f_Sg{j}', 'identb'], w=[f'psT{j}'])
                if j == 0:
                    P.op('act', lambda e: e.copy(out=SgT[:, ex, :, :], in_=psT[j][:].rearrange("p (q n) -> p q n", q=4)), r=[f'psT{j}'], w=[('f_SgT', ex)])
                else:
                    P.op('pool', lambda e: e.tensor_copy(out=SgT[:, ex, :, :], in_=psT[j][:].rearrange("p (q n) -> p q n", q=4)), r=[f'psT{j}'], w=[('f_SgT', ex)]) if False else \
                        P.op('act', lambda e: e.copy(out=SgT[:, ex, :, :], in_=psT[j][:].rearrange("p (q n) -> p q n", q=4)), r=[f'psT{j}'], w=[('f_SgT', ex)])
            for hc in range(2):
                hs_ = slice(hc * 512, (hc + 1) * 512)
                pt, pk = psA[(t % 2) * 2 + hc], f'psA{(t % 2) * 2 + hc}'
                P.pe_group([(lambda e, ex=ex, q=q: e.matmul(pt[:], lhsT=SgT[:, ex, q, :], rhs=yeA[:, ex, q, hs_], start=(ex == 0 and q == 0), stop=(ex == 15 and q == 3)))
                            for ex in range(16) for q in range(4)], r=[('f_SgT', ex) for ex in range(16)] + ['f_yeA'], w=[pk])
                P.op('dve', lambda e: e.tensor_tensor(out=ft[:, hs_], in0=pt[:], in1=bcs[:, 3, hs_], op=ALU.mult), r=[pk, 'bcs'], w=['f_ft'])
            P.op('pool', lambda e: e.tensor_tensor(out=ft[:], in0=ft[:], in1=x1l[i][:], op=ALU.add), r=['f_ft', f'f_x1l{i}'], w=['f_ft'])
            P.op('dve', lambda e: e.memset(st2[:], 0.0), w=['st2'])
            P.op('act', lambda e: e.activation(out=junk2[:], in_=ft[:], func=AF.Square, accum_out=st2[:, 0:1]), r=['f_ft'], w=['f_junk', 'st2'])
            P.op('act', lambda e: e.activation(out=st2[:, 1:2], in_=st2[:, 0:1], func=AF.Sqrt, scale=1.0 / D, bias=epsb[:]), r=['st2', 'epsb'], w=['st2'])
            P.op('dve', lambda e: e.reciprocal(out=st2[:, 2:3], in_=st2[:, 1:2]), r=['st2'], w=['st2'])
            P.op('dve', lambda e: e.scalar_tensor_tensor(out=ot[i][:], in0=ft[:], scalar=st2[:, 2:3], in1=gfin[:], op0=ALU.mult, op1=ALU.mult), r=['f_ft', 'st2', 'gfin'], w=[f'f_ot{i}'])
            fin.append(P.op('sp', lambda e: e.dma_start(out=out[ts_, :], in_=ot[i][:]), r=[f'f_ot{i}'], dma=True))
        P.barrier()
        esF.close()
        esP.close()

    if 'A' in stages:
        phase_A()
    if 'MLA' in stages:
        phase_MLA()
    if 'DN' in stages:
        try:
            phase_DN()
        except _Stop:
            P.barrier()
    if 'MG' in stages:
        phase_MG_MOE()
    P.finish(fin)
    print("instructions:", P.n)
    return nc


_invf = (10000.0 ** (-np.arange(32, dtype=np.float32) / np.float32(32))).astype(np.float32)
INVF2 = np.concatenate([_invf, _invf])[:, None].astype(np.float32)
SGN2 = np.concatenate([-np.ones(32), np.ones(32)])[:, None].astype(np.float32)
SEL64 = np.zeros((128, 65), np.float32)
SEL64[:, 64] = 1.0
_xi = np.arange(128)[:, None]
_yi = np.arange(128)[None, :]
BIG = 1.0e4
DMASKS = np.stack([np.where(_xi > _yi, 0.0, BIG), np.where(_yi >= _xi, 0.0, -BIG),
                   np.where(_xi < _yi, 0.0, BIG), np.where(_yi <= _xi, 0.0, -BIG)], axis=1).astype(np.float32)
def _bd(s_):
    return ((_xi // s_) == (_yi // s_)).astype(np.float32)
DN_LMASK = np.ascontiguousarray(np.stack([np.tile(m_, (1, 4)) for m_ in
                                          (_bd(16), _bd(32) - _bd(16), _bd(64) - _bd(32), 1.0 - _bd(64), np.eye(128, dtype=np.float32))], axis=1))
IOTA512 = np.ascontiguousarray(np.broadcast_to(np.arange(512, dtype=np.float32)[None, :], (128, 512)))
TRI = (_xi < _yi).astype(np.float32)
IN_SPL = np.cumsum([3072, 1024, 16, 16, 512, 256, 64, 2048])


def prep_inputs(inputs, core):
    b, half = core // 2, core % 2
    f = lambda a: np.ascontiguousarray(a, dtype=np.float32)
    w_in = inputs['w_in'][0]
    qkv, z, bb, aa, cq, ckv, kr, g = np.split(w_in, IN_SPL[:-1], axis=1)
    xb = inputs['x'][b]
    posb = inputs['positions'][b]
    conv_w = inputs['conv_w'][0]
    a_log, dt_bias = inputs['a_log'][0], inputs['dt_bias'][0]
    if half == 1:
        xb = xb[::-1]
        posb = posb[::-1]
        conv_w = conv_w[::-1]
        bb = np.concatenate([bb[:, 8:16], bb[:, 0:8]], axis=1)
        aa = np.concatenate([aa[:, 8:16], aa[:, 0:8]], axis=1)
        a_log, dt_bias = a_log[::-1], dt_bias[::-1]
    swap = np.concatenate([np.arange(32, 64), np.arange(0, 32)])
    wuq_ = inputs['w_uq'][0].reshape(512, 8, 192)
    wukv_ = inputs['w_ukv'][0].reshape(256, 8, 256)
    m = {
        'x': f(xb),
        'cT': f(inputs['c'][b].reshape(8, 128).T),
        'w_mod': f(inputs['w_mod'][0]),
        'b_mod': f(inputs['b_mod'][0][None, :]),
        'g_mix': f(np.broadcast_to(inputs['g_mix'][0][None, :], (128, D))),
        'w_qkv': f(qkv), 'w_z': f(z), 'w_g': f(g),
        'w_ba': f(np.concatenate([bb, aa], axis=1)),
        'w_cq': f(cq), 'w_ckv': f(ckv),
        'w_kr2': f(np.concatenate([kr, kr[:, swap]], axis=1)),
        'q_gain': f(np.broadcast_to(inputs['q_gain'][0][None, :], (128, 512))),
        'kv_gain': f(np.broadcast_to(inputs['kv_gain'][0][None, :], (128, 256))),
        'identf': np.eye(128, dtype=np.float32),
        'posr': np.ascontiguousarray(np.broadcast_to(posb[None, :], (64, S)).astype(np.int32)),
        'invf2': INVF2, 'sgn2': SGN2, 'sel64': SEL64,
        'w_uqh': f(np.stack([np.concatenate([wuq_[:, h, 0:128], wuq_[:, h, 128:192], wuq_[:, h, 128:192][:, swap]], axis=1) for h in range(8)])),
        'w_ukvh': f(np.stack([wukv_[:, h, :] for h in range(8)])),
        'conv_wT': f(conv_w.T),
        'dn_sc': f(np.stack([a_log[0], a_log[1], dt_bias[0], dt_bias[1]], axis=1)),
        'dn_gain': f(np.broadcast_to(inputs['dn_o_gain'][0][None, :], (128, 128))),
        'dmasks': DMASKS, 'dn_lmask': DN_LMASK,
        'w_o_dn': f(inputs['w_o_dn'][0]), 'w_o_mla': f(inputs['w_o_mla'][0]), 'w_out': f(inputs['w_out'][0]),
        'g_ffn': f(np.broadcast_to(inputs['g_ffn'][0][None, :], (128, D))),
        'g_final': f(np.broadcast_to(inputs['g_final'][None, :], (128, D))),
        'w_router': f(inputs['w_router'][0]),
        'w_gate': f(inputs['w_gate'][0]), 'w_up': f(inputs['w_up'][0]), 'w_down': f(inputs['w_down'][0]),
        'iota512': IOTA512, 'tri_in': TRI,
    }
    return m


def kernel(**inputs):
    inputs = {k: np.asarray(v) for k, v in inputs.items()}
    nc = build()
    in_maps = [prep_inputs(inputs, c) for c in range(8)]
    res = run_bass_kernel_spmd(nc, in_maps, core_ids=list(range(8)))
    outp = np.zeros((4, S, D), np.float32)
    for c in range(8):
        b, half = c // 2, c % 2
        o_ = res.results[c]["out"]
        if half == 0:
            outp[b, 0:2048] = o_
        else:
            outp[b, 2048:4096] = o_[::-1]
    return outp
```

```python
import numpy as np
import ml_dtypes
import concourse.bass as bass
import concourse.mybir as mybir
from concourse.bass_utils import run_bass_kernel_spmd
from contextlib import ExitStack

F32 = mybir.dt.float32
BF16 = mybir.dt.bfloat16
I32 = mybir.dt.int32
AF = mybir.ActivationFunctionType
ALU = mybir.AluOpType
AX = mybir.AxisListType

import os
DBGN = int(os.environ.get('DBGN', '99'))
S = 4096
D = 1024
NT = S // 128
EPS = 1e-6


class Prog:
    def __init__(self, nc, ndma=16):
        self.nc = nc
        self.eng = {'pe': nc.tensor, 'act': nc.scalar, 'dve': nc.vector,
                    'pool': nc.gpsimd, 'sp': nc.sync}
        self.sem = {k: nc.alloc_semaphore(name=f"s_{k}") for k in self.eng}
        self.cnt = {k: 0 for k in self.eng}
        self.waited = {k: {} for k in self.eng}
        self.dsem = {q: [nc.alloc_semaphore(name=f"s_dma_{q}{i}") for i in range(ndma)] for q in ('sp', 'pool')}
        self.dcnt = {q: [0] * ndma for q in ('sp', 'pool')}
        self.nd = {'sp': 0, 'pool': 0}
        self.lastw = {}
        self.readers = {}
        self.n = 0

    def _wait(self, e, tok):
        if tok is None:
            return
        sem, val, key = tok
        w = self.waited[e]
        if w.get(key, 0) >= val:
            return
        w[key] = val
        self.eng[e].wait_ge(sem, val)

    def op(self, e, fn, r=(), w=(), dma=False):
        w = list(w) + [t for t in r if isinstance(t, str) and t.startswith('ps') and t not in w]
        toks = []
        for t in r:
            toks.append(self.lastw.get(t))
        for t in w:
            toks.append(self.lastw.get(t))
            toks.extend(self.readers.get(t, ()))
        for tok in toks:
            self._wait(e, tok)
        if dma:
            i = self.nd[e] % len(self.dsem[e])
            if self.dcnt[e][i] > 0:
                self._wait(e, (self.dsem[e][i], self.dcnt[e][i], f"dma_{e}{i}"))
        ins = fn(self.eng[e])
        self.n += 1
        if dma:
            i = self.nd[e] % len(self.dsem[e])
            self.nd[e] += 1
            self.dcnt[e][i] += 16
            ins.then_inc(self.dsem[e][i], 16)
            tok = (self.dsem[e][i], self.dcnt[e][i], f"dma_{e}{i}")
        else:
            self.cnt[e] += 1
            ins.then_inc(self.sem[e], 1)
            tok = (self.sem[e], self.cnt[e], e)
        for t in r:
            self.readers.setdefault(t, []).append(tok)
        for t in w:
            self.lastw[t] = tok
            self.readers[t] = []
        return tok

    def pe_group(self, fns, r=(), w=()):
        e = 'pe'
        w = list(w) + [t for t in r if isinstance(t, str) and t.startswith('ps') and t not in w]
        toks = []
        for t in r:
            toks.append(self.lastw.get(t))
        for t in w:
            toks.append(self.lastw.get(t))
            toks.extend(self.readers.get(t, ()))
        for tok in toks:
            self._wait(e, tok)
        for fn in fns[:-1]:
            fn(self.eng[e])
            self.n += 1
        ins = fns[-1](self.eng[e])
        self.n += 1
        self.cnt[e] += 1
        ins.then_inc(self.sem[e], 1)
        tok = (self.sem[e], self.cnt[e], e)
        for t in r:
            self.readers.setdefault(t, []).append(tok)
        for t in w:
            self.lastw[t] = tok
            self.readers[t] = []
        return tok

    def barrier(self):
        toks = [(self.sem[k], self.cnt[k], k) for k in self.eng if self.cnt[k] > 0]
        toks += [(self.dsem[q][i], self.dcnt[q][i], f"dma_{q}{i}") for q in self.dsem for i in range(len(self.dsem[q])) if self.dcnt[q][i] > 0]
        for e in self.eng:
            for tok in toks:
                self._wait(e, tok)

    def finish(self, toks):
        for tok in toks:
            self._wait('sp', tok)


class _Stop(Exception):
    pass


def build(debug=None, stages=('A', 'MLA', 'DN', 'MG'), ext_in=(), dn_heads=range(8), dn_stop=0):
    nc = bass.Bass("TRN2", target_bir_lowering=False)
    P = Prog(nc)

    def din(name, shape, dt=F32):
        return nc.dram_tensor(name, list(shape), dt, kind="ExternalInput").ap()

    def dscr(name, shape, dt):
        kind = "ExternalOutput" if (debug and name in debug) else ("ExternalInput" if name in ext_in else "Internal")
        return nc.dram_tensor(name, list(shape), dt, kind=kind).ap()

    x = din("x", [S, D])
    cT = din("cT", [128, 8])
    w_mod = din("w_mod", [D, 6 * D])
    b_mod = din("b_mod", [1, 6 * D])
    g_mix = din("g_mix", [128, D])
    w_qkv = din("w_qkv", [D, 3072])
    w_z = din("w_z", [D, 1024])
    w_g = din("w_g", [D, 2048])
    w_ba = din("w_ba", [D, 32])
    w_cq = din("w_cq", [D, 512])
    w_ckv = din("w_ckv", [D, 256])
    w_kr2 = din("w_kr2", [D, 128])
    q_gain = din("q_gain", [128, 512])
    kv_gain = din("kv_gain", [128, 256])
    identf = din("identf", [128, 128])
    posr = din("posr", [64, S], I32)
    invf2 = din("invf2", [64, 1])
    sgn2 = din("sgn2", [64, 1])
    sel64 = din("sel64", [128, 65])
    w_uqh = din("w_uqh", [8, 512, 256])
    w_ukvh = din("w_ukvh", [8, 256, 256])
    conv_wT = din("conv_wT", [3072, 5])
    dn_sc = din("dn_sc", [8, 4])
    dn_gain = din("dn_gain", [128, 128])
    dmasks = din("dmasks", [128, 4, 128])
    dn_lmask = din("dn_lmask", [128, 5, 512])
    w_o_dn = din("w_o_dn", [D, D])
    w_o_mla = din("w_o_mla", [D, D])
    w_out = din("w_out", [D, D])
    g_ffn = din("g_ffn", [128, D])
    g_final = din("g_final", [128, D])
    w_router = din("w_router", [D, 16])
    w_gate = din("w_gate", [16, D, D])
    w_up = din("w_up", [16, D, D])
    w_down = din("w_down", [16, D, D])
    iota512 = din("iota512", [128, 512])
    tri_in = din("tri_in", [128, 128])
    out = nc.dram_tensor("out", [S // 2, D], F32, kind="ExternalOutput").ap()

    qkvT = dscr("qkvT", [24, 128, S], BF16)
    zs = dscr("zs", [S, 1024], BF16)
    gs = dscr("gs", [S, 2048], BF16)
    baT = dscr("baT", [32, S], F32)
    cqnT = dscr("cqnT", [4, 128, S], BF16)
    ckvnT = dscr("ckvnT", [2, 128, S], BF16)
    krT = dscr("krT", [2, 64, S], BF16)
    modrow = dscr("modrow", [1, 6 * D], F32)
    oT_mla = dscr("oT_mla", [8, 128, S], BF16)
    oT_dn = dscr("oT_dn", [8, 128, S], BF16)
    x1s = dscr("x1s", [S, D], F32)
    ye_all = dscr("ye_all", [16, 512, D], BF16)

    sb = lambda n, s, d: nc.alloc_sbuf_tensor(n, list(s), d)
    ps_ = lambda n, s, d=F32: nc.alloc_psum_tensor(n, list(s), d)

    ident = sb("ident", [128, 128], F32)
    identb = sb("identb", [128, 128], BF16)
    ones_f = sb("ones_f", [128, 128], F32)
    P.op('sp', lambda e: e.dma_start(out=ident[:], in_=identf[:, :]), w=['ident'], dma=True)
    P.op('dve', lambda e: e.tensor_copy(out=identb[:], in_=ident[:]), r=['ident'], w=['identb'])
    P.op('dve', lambda e: e.memset(ones_f[:], 1.0), w=['ones_f'])
    epsb = sb("epsb", [128, 1], F32)
    P.op('dve', lambda e: e.memset(epsb[:], EPS), w=['epsb'])

    psA = [ps_(f"psA{i}", [128, 512]) for i in range(4)]
    psT = [ps_(f"psT{i}", [128, 512], BF16) for i in range(2)]
    psS = [ps_(f"psS{i}", [128, 512]) for i in range(2)]
    pa_i = [0]

    def next_psA():
        i = pa_i[0] % 4
        pa_i[0] += 1
        return psA[i], f"psA{i}"

    sbs = lambda es, n, s_, d: es.enter_context(nc.sbuf_tensor(n, list(s_), d))
    fin = []

    def phase_A():
        cT_sb = sb("cT_sb", [128, 8], F32)
        scT = sb("scT", [128, 8], F32)
        P.op('sp', lambda e: e.dma_start(out=cT_sb[:], in_=cT[:, :]), w=['cT'], dma=True)
        P.op('act', lambda e: e.activation(out=scT[:], in_=cT_sb[:], func=AF.Silu), r=['cT'], w=['scT'])
        esA = ExitStack()
        modbc = sbs(esA, "modbc", [128, 6, D], F32)
        gmx = sbs(esA, "gmx", [128, D], F32)
        A1 = sbs(esA, "A1", [128, D], F32)
        es0 = ExitStack()
        mod_sb = sbs(es0, "mod_sb", [1, 6 * D], F32)
        bm_sb = sbs(es0, "bm_sb", [1, 6 * D], F32)
        P.op('sp', lambda e: e.dma_start(out=bm_sb[:], in_=b_mod[:, :]), w=['bm'], dma=True)
        wm = [sbs(es0, f"wm{i}", [128, 8, 512], F32) for i in range(2)]
        w_mod_v = w_mod.rearrange("(k p) n -> p k n", p=128)
        for j in range(12):
            wt, wk = wm[j % 2], f"wm{j % 2}"
            P.op('sp', lambda e: e.dma_start(out=wt[:], in_=w_mod_v[:, :, j * 512:(j + 1) * 512]), w=[wk], dma=True)
            pt, pk = next_psA()
            for k in range(8):
                P.op('pe', lambda e: e.matmul(pt[0:1, :], lhsT=scT[:, k:k + 1], rhs=wt[:, k, :], start=(k == 0), stop=(k == 7)),
                     r=[wk, 'scT'], w=[pk])
            P.op('dve', lambda e: e.tensor_tensor(out=mod_sb[:, j * 512:(j + 1) * 512], in0=pt[0:1, :], in1=bm_sb[:, j * 512:(j + 1) * 512], op=ALU.add),
                 r=[pk, 'bm'], w=['mod'])
        for j in range(12):
            pt, pk = next_psA()
            P.op('pe', lambda e: e.matmul(pt[:], lhsT=ones_f[0:1, :], rhs=mod_sb[:, j * 512:(j + 1) * 512], start=True, stop=True),
                 r=['ones_f', 'mod'], w=[pk])
            P.op('act', lambda e: e.copy(out=modbc[:, j // 2, (j % 2) * 512:(j % 2 + 1) * 512], in_=pt[:]), r=[pk], w=['modbc'])
        P.op('sp', lambda e: e.dma_start(out=gmx[:], in_=g_mix[:, :]), w=['gmx'], dma=True)
        P.op('dve', lambda e: e.scalar_tensor_tensor(out=A1[:], in0=modbc[:, 1, :], scalar=1.0, in1=gmx[:], op0=ALU.add, op1=ALU.mult),
             r=['modbc', 'gmx'], w=['A1'])

        fin.append(P.op('sp', lambda e: e.dma_start(out=modrow[:, :], in_=mod_sb[:]), r=['mod'], dma=True))
        P.barrier()
        es0.close()
        hT = sbs(esA, "hT", [128, 8, S], BF16)
        xt = [sbs(esA, f"xt{i}", [128, D], F32) for i in range(2)]
        xn = [sbs(esA, f"xn{i}", [128, D], F32) for i in range(2)]
        hb = [sbs(esA, f"hb{i}", [128, D], BF16) for i in range(2)]
        st = [sbs(esA, f"st{i}", [128, 4], F32) for i in range(2)]
        junk = sbs(esA, "junk", [128, D], F32)
        for t in range(NT):
            i = t % 2
            P.op('sp', lambda e: e.dma_start(out=xt[i][:], in_=x[t * 128:(t + 1) * 128, :]), w=[f'xt{i}'], dma=True)
            P.op('dve', lambda e: e.memset(st[i][:], 0.0), w=[f'st{i}'])
            P.op('act', lambda e: e.activation(out=junk[:], in_=xt[i][:], func=AF.Square, accum_out=st[i][:, 0:1]),
                 r=[f'xt{i}'], w=['junk', f'st{i}'])
            P.op('act', lambda e: e.activation(out=st[i][:, 1:2], in_=st[i][:, 0:1], func=AF.Sqrt, scale=1.0 / D, bias=epsb[:]),
                 r=[f'st{i}', 'epsb'], w=[f'st{i}'])
            P.op('dve', lambda e: e.reciprocal(out=st[i][:, 2:3], in_=st[i][:, 1:2]), r=[f'st{i}'], w=[f'st{i}'])
            P.op('dve', lambda e: e.scalar_tensor_tensor(out=xn[i][:], in0=xt[i][:], scalar=st[i][:, 2:3], in1=A1[:], op0=ALU.mult, op1=ALU.mult),
                 r=[f'xt{i}', f'st{i}', 'A1'], w=[f'xn{i}'])
            P.op('pool', lambda e: e.tensor_tensor(out=hb[i][:], in0=xn[i][:], in1=modbc[:, 0, :], op=ALU.add),
                 r=[f'xn{i}', 'modbc'], w=[f'hb{i}'])
            for half in range(2):
                pt, pk = psT[half], f'psT{half}'
                P.pe_group([(lambda e, k4=k4: e.transpose(out=pt[:, k4 * 128:(k4 + 1) * 128], in_=hb[i][:, (half * 4 + k4) * 128:(half * 4 + k4 + 1) * 128], identity=identb[:])) for k4 in range(4)],
                           r=[f'hb{i}', 'identb'], w=[pk])
                eng = 'act' if half == 0 else 'dve'
                if eng == 'act':
                    P.op('act', lambda e: e.copy(out=hT[:, half * 4:(half + 1) * 4, t * 128:(t + 1) * 128],
                                                 in_=pt[:].rearrange("p (k n) -> p k n", k=4)), r=[pk], w=[('hT', t)])
                else:
                    P.op('dve', lambda e: e.tensor_copy(out=hT[:, half * 4:(half + 1) * 4, t * 128:(t + 1) * 128],
                                                        in_=pt[:].rearrange("p (k n) -> p k n", k=4)), r=[pk], w=[('hT', t)])
        hT_all = [('hT', t) for t in range(NT)]

        wb = [sbs(esA, f"wb{i}", [128, 8, 512], BF16) for i in range(2)]
        wb_i = [0]

        def load_w(src, c0, ncols):
            i = wb_i[0] % 2
            wb_i[0] += 1
            v = src.rearrange("(k p) n -> p k n", p=128)
            P.op('pool', lambda e: e.dma_start(out=wb[i][:, :, 0:ncols], in_=v[:, :, c0:c0 + ncols]), w=[f'wb{i}'], dma=True)
            return wb[i], f'wb{i}'

        stg = [sbs(esA, f"stg{i}", [128, S], BF16) for i in range(2)]
        stg_i = [0]

        def chan_major(src, ncols_total, dst_fn, M, dt_out=BF16, stgs=stg):
            nblk = (ncols_total + 511) // 512
            for blk in range(nblk):
                nc_ = min(512, ncols_total - blk * 512)
                wt, wk = load_w(src, blk * 512, nc_)
                for c in range(nc_ // M):
                    si = stg_i[0] % 2
                    stg_i[0] += 1
                    sg, sk = stgs[si], f'{stgs[si].name}'
                    for g in range(8):
                        pt, pk = next_psA()
                        P.pe_group([(lambda e, k=k: e.matmul(pt[0:M, :], lhsT=wt[:, k, c * M:(c + 1) * M], rhs=hT[:, k, g * 512:(g + 1) * 512],
                                                            start=(k == 0), stop=(k == 7))) for k in range(8)],
                                   r=[wk] + hT_all[g * 4:(g + 1) * 4], w=[pk])
                        if g % 2 == 0:
                            P.op('act', lambda e: e.copy(out=sg[0:M, g * 512:(g + 1) * 512], in_=pt[0:M, :]), r=[pk], w=[sk])
                        else:
                            P.op('dve', lambda e: e.tensor_copy(out=sg[0:M, g * 512:(g + 1) * 512], in_=pt[0:M, :]), r=[pk], w=[sk])
                    fin.append(P.op('sp', lambda e: e.dma_start(out=dst_fn(blk * (512 // M) + c), in_=sg[0:M, :]), r=[sk], w=[('scr', dst_fn.__name__)], dma=True))

        def dst_qkv(c):
            return qkvT[c, :, :]
        chan_major(w_qkv, 3072, dst_qkv, 128)

        def dst_kr(c):
            return krT[c, :, :]
        chan_major(w_kr2, 128, dst_kr, 64)
        stgf0 = sbs(esA, "stgf0", [32, S], F32)
        stgf = [stgf0, stgf0]

        def dst_ba(c):
            return baT[:, :]
        chan_major(w_ba, 32, dst_ba, 32, F32, stgf)

        tst = [sbs(esA, f"tst{i}", [128, 512], BF16) for i in range(4)]
        tst_i = [0]

        def tok_major_act(src, ncols_total, dst, func):
            for blk in range(ncols_total // 512):
                wt, wk = load_w(src, blk * 512, 512)
                for t in range(NT):
                    pt, pk = next_psA()
                    P.pe_group([(lambda e, k=k: e.matmul(pt[:], lhsT=hT[:, k, t * 128:(t + 1) * 128], rhs=wt[:, k, :], start=(k == 0), stop=(k == 7))) for k in range(8)],
                               r=[wk, ('hT', t)], w=[pk])
                    si = tst_i[0] % 4
                    tst_i[0] += 1
                    P.op('act', lambda e: e.activation(out=tst[si][:], in_=pt[:], func=func), r=[pk], w=[f'tst{si}'])
                    fin.append(P.op('sp', lambda e: e.dma_start(out=dst[t * 128:(t + 1) * 128, blk * 512:(blk + 1) * 512], in_=tst[si][:]),
                                    r=[f'tst{si}'], w=[('scr', dst.name, t, blk)], dma=True))
        tok_major_act(w_z, 1024, zs, AF.Silu)
        tok_major_act(w_g, 2048, gs, AF.Sigmoid)

        def latent(src, ncols, gain_in, dstT, nm):
            gsb = sbs(esA, f"gain_{nm}", [128, ncols], F32)
            P.op('sp', lambda e: e.dma_start(out=gsb[:], in_=gain_in[:, :]), w=[f'gain_{nm}'], dma=True)
            lst = [sbs(esA, f"lst_{nm}{i_}", [128, ncols // 128, 128], BF16) for i_ in range(2)]
            wt, wk = load_w(src, 0, ncols)
            for t in range(NT):
                i = t % 2
                pt, pk = next_psA()
                P.pe_group([(lambda e, k=k: e.matmul(pt[:, 0:ncols], lhsT=hT[:, k, t * 128:(t + 1) * 128], rhs=wt[:, k, 0:ncols], start=(k == 0), stop=(k == 7))) for k in range(8)],
                           r=[wk, ('hT', t)], w=[pk])
                P.op('dve', lambda e: e.memset(st[i][:], 0.0), w=[f'st{i}'])
                P.op('act', lambda e: e.activation(out=junk[:, 0:ncols], in_=pt[:, 0:ncols], func=AF.Square, accum_out=st[i][:, 0:1]),
                     r=[pk], w=['junk', f'st{i}'])
                P.op('act', lambda e: e.activation(out=st[i][:, 1:2], in_=st[i][:, 0:1], func=AF.Sqrt, scale=1.0 / ncols, bias=epsb[:]),
                     r=[f'st{i}', 'epsb'], w=[f'st{i}'])
                P.op('dve', lambda e: e.reciprocal(out=st[i][:, 2:3], in_=st[i][:, 1:2]), r=[f'st{i}'], w=[f'st{i}'])
                P.op('dve', lambda e: e.scalar_tensor_tensor(out=hb[i][:, 0:ncols], in0=pt[:, 0:ncols], scalar=st[i][:, 2:3], in1=gsb[:], op0=ALU.mult, op1=ALU.mult),
                     r=[pk, f'st{i}', f'gain_{nm}'], w=[f'hb{i}'])
                tp, tk = psT[i], f'psT{i}'
                P.pe_group([(lambda e, c=c: e.transpose(out=tp[:, c * 128:(c + 1) * 128], in_=hb[i][:, c * 128:(c + 1) * 128], identity=identb[:])) for c in range(ncols // 128)],
                           r=[f'hb{i}', 'identb'], w=[tk])
                P.op('act', lambda e: e.copy(out=lst[i][:], in_=tp[:, 0:ncols].rearrange("p (k n) -> p k n", k=ncols // 128)),
                     r=[tk], w=[f'lst_{nm}{i}'])
                fin.append(P.op('sp', lambda e: e.dma_start(out=dstT[:, :, t * 128:(t + 1) * 128].rearrange("c p n -> p c n"), in_=lst[i][:]),
                                r=[f'lst_{nm}{i}'], w=[('scr', nm, t)], dma=True))
        latent(w_cq, 512, q_gain, cqnT, 'cq')
        latent(w_ckv, 256, kv_gain, ckvnT, 'ckv')


        P.barrier()
        esA.close()

    def phase_MLA():
        esM = ExitStack()
        TWO_PI = float(2 * np.pi)
        SCL = float(192 ** -0.5)
        cos2 = sbs(esM, "cos2", [64, S], F32)
        sin2 = sbs(esM, "sin2", [64, S], F32)
        krA = sbs(esM, "krA", [65, S], BF16)
        QrA = sbs(esM, "QrA", [65, S], BF16)
        onesb = sbs(esM, "onesb", [128, 128], BF16)
        sel_b = sbs(esM, "sel_b", [128, 65], BF16)
        if True:
            es1 = ExitStack()
            posi = sbs(es1, "posi", [64, S], I32)
            ang = sbs(es1, "ang", [64, S], F32)
            ti = sbs(es1, "ti", [64, S], I32)
            tf = sbs(es1, "tf", [64, S], F32)
            tg = sbs(es1, "tg", [64, S], F32)
            ivf = sbs(es1, "ivf", [64, 2], F32)
            kr0 = sbs(es1, "kr0", [64, S], BF16)
            kr1 = sbs(es1, "kr1", [64, S], BF16)
            self_f = sbs(es1, "self_f", [128, 65], F32)
            P.op('sp', lambda e: e.dma_start(out=posi[:], in_=posr[:, :]), w=['posi'], dma=True)
            P.op('sp', lambda e: e.dma_start(out=ivf[:, 0:1], in_=invf2[:, :]), w=['ivf'], dma=True)
            P.op('sp', lambda e: e.dma_start(out=ivf[:, 1:2], in_=sgn2[:, :]), w=['ivf'], dma=True)
            P.op('sp', lambda e: e.dma_start(out=self_f[:], in_=sel64[:, :]), w=['self_f'], dma=True)
            P.op('dve', lambda e: e.tensor_copy(out=sel_b[:], in_=self_f[:]), r=['self_f'], w=['sel_b'])
            P.op('dve', lambda e: e.memset(onesb[:], 1.0), w=['onesb'])
            P.op('dve', lambda e: e.tensor_copy(out=ang[:], in_=posi[:]), r=['posi'], w=['ang'])
            P.op('dve', lambda e: e.tensor_scalar(out=ang[:], in0=ang[:], scalar1=ivf[:, 0:1], scalar2=float(1.0 / TWO_PI), op0=ALU.mult, op1=ALU.mult),
                 r=['ang', 'ivf'], w=['ang'])
            for which, dst in ((0, sin2), (1, cos2)):
                dk_ = 'sin2' if which == 0 else 'cos2'
                P.op('dve', lambda e: e.tensor_scalar(out=tg[:], in0=ang[:], scalar1=0.25 * which, scalar2=None, op0=ALU.add), r=['ang'], w=['tg'])
                P.op('dve', lambda e: e.tensor_copy(out=ti[:], in_=tg[:]), r=['tg'], w=['ti'])
                P.op('dve', lambda e: e.tensor_copy(out=tf[:], in_=ti[:]), r=['ti'], w=['tf'])
                P.op('dve', lambda e: e.tensor_tensor(out=tg[:], in0=tg[:], in1=tf[:], op=ALU.subtract), r=['tg', 'tf'], w=['tg'])
                P.op('dve', lambda e: e.tensor_scalar(out=tf[:], in0=tg[:], scalar1=0.5, scalar2=None, op0=ALU.is_gt), r=['tg'], w=['tf'])
                P.op('dve', lambda e: e.tensor_tensor(out=tg[:], in0=tg[:], in1=tf[:], op=ALU.subtract), r=['tg', 'tf'], w=['tg'])
                P.op('dve', lambda e: e.tensor_scalar(out=tf[:], in0=tg[:], scalar1=-0.5, scalar2=None, op0=ALU.is_lt), r=['tg'], w=['tf'])
                P.op('dve', lambda e: e.tensor_tensor(out=tg[:], in0=tg[:], in1=tf[:], op=ALU.add), r=['tg', 'tf'], w=['tg'])
                P.op('act', lambda e: e.activation(out=dst[:], in_=tg[:], func=AF.Sin, scale=TWO_PI), r=['tg'], w=[dk_])
            P.op('dve', lambda e: e.tensor_scalar(out=sin2[:], in0=sin2[:], scalar1=ivf[:, 1:2], scalar2=None, op0=ALU.mult), r=['sin2', 'ivf'], w=['sin2'])
            P.op('sp', lambda e: e.dma_start(out=kr0[:], in_=krT[0, :, :]), r=[('scr', 'dst_kr')], w=['kr0'], dma=True)
            P.op('sp', lambda e: e.dma_start(out=kr1[:], in_=krT[1, :, :]), r=[('scr', 'dst_kr')], w=['kr1'], dma=True)
            P.op('dve', lambda e: e.tensor_tensor(out=tg[:], in0=kr0[:], in1=cos2[:], op=ALU.mult), r=['kr0', 'cos2'], w=['tg'])
            P.op('dve', lambda e: e.tensor_tensor(out=tf[:], in0=kr1[:], in1=sin2[:], op=ALU.mult), r=['kr1', 'sin2'], w=['tf'])
            P.op('dve', lambda e: e.memset(krA[:], 1.0), w=['krA'])
            P.op('dve', lambda e: e.tensor_tensor(out=krA[0:64, :], in0=tg[:], in1=tf[:], op=ALU.add), r=['tg', 'tf'], w=['krA'])
            P.op('dve', lambda e: e.memset(QrA[:], 0.0), w=['QrA'])
            P.barrier()
            es1.close()
        cqn = sbs(esM, "cqn", [128, 4, S], BF16)
        ckvn = sbs(esM, "ckvn", [128, 2, S], BF16)
        QnT = sbs(esM, "QnT", [128, S], BF16)
        KnT = sbs(esM, "KnT", [128, S], BF16)
        Vt = sbs(esM, "Vt", [128, NT, 128], BF16)
        oTs = sbs(esM, "oTs", [128, S], BF16)
        kmx = sbs(esM, "kmx", [65, 16], F32)
        wuq = sbs(esM, "wuq", [128, 4, 256], BF16)
        wukv = sbs(esM, "wukv", [128, 2, 256], BF16)
        sq = sbs(esM, "sq", [128, 512], BF16)
        t1 = sbs(esM, "t1", [64, 512], F32)
        t2 = sbs(esM, "t2", [64, 512], F32)
        rowt = sbs(esM, "rowt", [65, 512], F32)
        pT = [sbs(esM, f"pT{i}", [128, 512], BF16) for i in range(3)]
        rden = sbs(esM, "rden", [128, 512], F32)
        for c in range(4):
            P.op('sp', lambda e: e.dma_start(out=cqn[:, c, :], in_=cqnT[c, :, :]), r=[('scr', 'cq', t_) for t_ in range(NT)], w=['cqn'], dma=True)
        for c in range(2):
            P.op('sp', lambda e: e.dma_start(out=ckvn[:, c, :], in_=ckvnT[c, :, :]), r=[('scr', 'ckv', t_) for t_ in range(NT)], w=['ckvn'], dma=True)
        for h in range(8):
            P.op('pool', lambda e: e.dma_start(out=wuq[:], in_=w_uqh[h].rearrange("(k p) n -> p k n", p=128)), w=['wuq'], dma=True)
            P.op('pool', lambda e: e.dma_start(out=wukv[:], in_=w_ukvh[h].rearrange("(k p) n -> p k n", p=128)), w=['wukv'], dma=True)
            for g in range(8):
                gs_ = slice(g * 512, (g + 1) * 512)
                pt, pk = psA[2], 'psA2'
                P.pe_group([(lambda e, k=k: e.matmul(pt[:], lhsT=wuq[:, k, 0:128], rhs=cqn[:, k, gs_], start=(k == 0), stop=(k == 3))) for k in range(4)], r=['wuq', 'cqn'], w=[pk])
                P.op('act', lambda e: e.copy(out=QnT[:, gs_], in_=pt[:]), r=[pk], w=[('QnT', g)])
                pt, pk = psA[3], 'psA3'
                P.pe_group([(lambda e, k=k: e.matmul(pt[0:64, :], lhsT=wuq[:, k, 128:192], rhs=cqn[:, k, gs_], start=(k == 0), stop=(k == 3))) for k in range(4)], r=['wuq', 'cqn'], w=[pk])
                P.op('dve', lambda e: e.tensor_tensor(out=t1[:], in0=pt[0:64, :], in1=cos2[:, gs_], op=ALU.mult), r=[pk, 'cos2'], w=['t1'])
                P.pe_group([(lambda e, k=k: e.matmul(pt[0:64, :], lhsT=wuq[:, k, 192:256], rhs=cqn[:, k, gs_], start=(k == 0), stop=(k == 3))) for k in range(4)], r=['wuq', 'cqn'], w=[pk])
                P.op('dve', lambda e: e.tensor_tensor(out=t2[:], in0=pt[0:64, :], in1=sin2[:, gs_], op=ALU.mult), r=[pk, 'sin2'], w=['t2'])
                P.op('dve', lambda e: e.tensor_tensor(out=QrA[0:64, gs_], in0=t1[:], in1=t2[:], op=ALU.add), r=['t1', 't2'], w=[('QrA', g)])
                pt, pk = psA[2], 'psA2'
                P.pe_group([(lambda e, k=k: e.matmul(pt[:], lhsT=wukv[:, k, 0:128], rhs=ckvn[:, k, gs_], start=(k == 0), stop=(k == 1))) for k in range(2)], r=['wukv', 'ckvn'], w=[pk])
                P.op('act', lambda e: e.copy(out=KnT[:, gs_], in_=pt[:]), r=[pk], w=[('KnT', g)])
                pt, pk = psA[3], 'psA3'
                P.pe_group([(lambda e, j=j, k=k: e.matmul(pt[:, j * 128:(j + 1) * 128], lhsT=ckvn[:, k, (g * 4 + j) * 128:(g * 4 + j + 1) * 128], rhs=wukv[:, k, 128:256], start=(k == 0), stop=(k == 1)))
                            for j in range(4) for k in range(2)], r=['wukv', 'ckvn'], w=[pk])
                P.op('dve', lambda e: e.tensor_copy(out=Vt[:, g * 4:(g + 1) * 4, :], in_=pt[:].rearrange("p (j n) -> p j n", j=4)), r=[pk], w=[('Vt', g)])
                pt, pk = psA[2], 'psA2'
                P.op('act', lambda e: e.activation(out=sq[:], in_=KnT[:, gs_], func=AF.Square), r=[('KnT', g)], w=['sq'])
                P.op('pe', lambda e: e.matmul(pt[0:65, :], lhsT=sel_b[:, :], rhs=sq[:], start=True, stop=False), r=['sq', 'sel_b'], w=[pk])
                P.op('act', lambda e: e.activation(out=sq[0:64, :], in_=krA[0:64, gs_], func=AF.Square), r=['krA'], w=['sq'])
                P.op('pe', lambda e: e.matmul(pt[0:65, :], lhsT=sel_b[0:64, :], rhs=sq[0:64, :], start=False, stop=True), r=['sq', 'sel_b'], w=[pk])
                P.op('dve', lambda e: e.tensor_reduce(out=kmx[64:65, g:g + 1], in_=pt[64:65, :], axis=AX.X, op=ALU.max), r=[pk], w=['kmx'])
            P.op('dve', lambda e: e.tensor_reduce(out=kmx[64:65, 8:9], in_=kmx[64:65, 0:8], axis=AX.X, op=ALU.max), r=['kmx'], w=['kmx'])
            for g in range(8):
                gs_ = slice(g * 512, (g + 1) * 512)
                pt, pk = psA[2], 'psA2'
                P.op('act', lambda e: e.activation(out=sq[:], in_=QnT[:, gs_], func=AF.Square), r=[('QnT', g)], w=['sq'])
                P.op('pe', lambda e: e.matmul(pt[0:65, :], lhsT=sel_b[:, :], rhs=sq[:], start=True, stop=False), r=['sq', 'sel_b'], w=[pk])
                P.op('act', lambda e: e.activation(out=sq[0:64, :], in_=QrA[0:64, gs_], func=AF.Square), r=[('QrA', g)], w=['sq'])
                P.op('pe', lambda e: e.matmul(pt[0:65, :], lhsT=sel_b[0:64, :], rhs=sq[0:64, :], start=False, stop=True), r=['sq', 'sel_b'], w=[pk])
                P.op('act', lambda e: e.activation(out=rowt[64:65, :], in_=pt[64:65, :], func=AF.Sqrt, scale=kmx[64:65, 8:9]), r=[pk, 'kmx'], w=['rowt'])
                P.op('dve', lambda e: e.tensor_scalar(out=QrA[64:65, gs_], in0=rowt[64:65, :], scalar1=-1.0, scalar2=None, op0=ALU.mult), r=['rowt'], w=[('QrA', g)])
            for g in range(8):
                gs_ = slice(g * 512, (g + 1) * 512)
                po, pd = psA[(g % 2) * 2], psA[(g % 2) * 2 + 1]
                kpo, kpd = f'psA{(g % 2) * 2}', f'psA{(g % 2) * 2 + 1}'

                def scores(kt):
                    ks_ = slice(kt * 128, (kt + 1) * 128)
                    sc_, sck = psS[kt % 2], f'psS{kt % 2}'
                    P.pe_group([lambda e: e.matmul(sc_[:], lhsT=KnT[:, ks_], rhs=QnT[:, gs_], start=True, stop=False),
                                lambda e: e.matmul(sc_[:], lhsT=krA[:, ks_], rhs=QrA[:, gs_], start=False, stop=True)],
                               r=[('KnT', kt // 4), ('QnT', g), 'krA', ('QrA', g)], w=[sck])
                scores(0)
                for kt in range(NT):
                    sc_, sck = psS[kt % 2], f'psS{kt % 2}'
                    pi = kt % 3
                    P.op('act', lambda e: e.activation(out=pT[pi][:], in_=sc_[:], func=AF.Exp, scale=SCL), r=[sck], w=[f'pT{pi}'])
                    if kt + 1 < NT:
                        scores(kt + 1)
                    P.pe_group([lambda e: e.matmul(po[:], lhsT=Vt[:, kt, :], rhs=pT[pi][:], start=(kt == 0), stop=(kt == NT - 1)),
                                lambda e: e.matmul(pd[:], lhsT=onesb[:], rhs=pT[pi][:], start=(kt == 0), stop=(kt == NT - 1))],
                               r=[('Vt', kt // 4), f'pT{pi}', 'onesb'], w=[kpo, kpd])
                P.op('dve', lambda e: e.reciprocal(out=rden[:], in_=pd[:]), r=[kpd], w=['rden'])
                P.op('dve', lambda e: e.tensor_tensor(out=oTs[:, gs_], in0=po[:], in1=rden[:], op=ALU.mult), r=[kpo, 'rden'], w=['oTs'])
            fin.append(P.op('sp', lambda e: e.dma_start(out=oT_mla[h, :, :], in_=oTs[:]), r=['oTs'], w=[('scr', 'oT_mla', h)], dma=True))
        P.barrier()
        esM.close()


    def phase_DN():
        def stop(k):
            if dn_stop == k:
                raise _Stop()
        esD = ExitStack()
        pM, pG, pX0, pX1, pZT, pU = psA[0], psA[1], psA[2], psA[3], psS[0], psS[1]
        kM, kG, kX0, kX1, kZT, kU = 'psA0', 'psA1', 'psA2', 'psA3', 'psS0', 'psS1'
        onesb = sbs(esD, "d_onesb", [128, 128], BF16)
        P.op('dve', lambda e: e.memset(onesb[:], 1.0), w=['d_onesb'])
        msk = sbs(esD, "d_msk", [128, 4, 128], F32)
        P.op('sp', lambda e: e.dma_start(out=msk[:], in_=dmasks[:, :, :]), w=['d_msk'], dma=True)
        gain = sbs(esD, "d_gain", [128, 128], F32)
        P.op('sp', lambda e: e.dma_start(out=gain[:], in_=dn_gain[:, :]), w=['d_gain'], dma=True)
        tokS = sbs(esD, "tokS", [128, NT, 48], F32)
        one1 = sbs(esD, "one1", [128, 1], F32)
        P.op('dve', lambda e: e.memset(one1[:], 1.0), w=['one1'])
        eps_l2 = sbs(esD, "eps_l2", [128, 1], F32)
        P.op('dve', lambda e: e.memset(eps_l2[:], EPS), w=['eps_l2'])
        es1 = ExitStack()
        sc = sbs(es1, "d_sc", [8, 4], F32)
        nA = sbs(es1, "d_nA", [8, 2], F32)
        P.op('sp', lambda e: e.dma_start(out=sc[:], in_=dn_sc[:, :]), w=['d_sc'], dma=True)
        P.op('act', lambda e: e.activation(out=nA[:], in_=sc[:, 0:2], func=AF.Exp), r=['d_sc'], w=['d_nA'])
        P.op('dve', lambda e: e.tensor_scalar(out=nA[:], in0=nA[:], scalar1=-1.0, scalar2=None, op0=ALU.mult), r=['d_nA'], w=['d_nA'])
        rows = {}
        ra = sbs(es1, "d_ra", [8, S], F32)
        rb = sbs(es1, "d_rb", [8, S], F32)
        rc = sbs(es1, "d_rc", [8, S], F32)
        for d in range(2):
            beta = sbs(es1, f"d_beta{d}", [8, S], F32)
            nbeta = sbs(es1, f"d_nbeta{d}", [8, S], F32)
            gc = sbs(es1, f"d_gc{d}", [8, S], F32)
            rows[d] = (beta, nbeta, gc)
            P.op('sp', lambda e: e.dma_start(out=ra[:], in_=baT[d * 8:(d + 1) * 8, :]), r=[('scr', 'dst_ba')], w=['d_ra'], dma=True)
            P.op('act', lambda e: e.activation(out=beta[:], in_=ra[:], func=AF.Sigmoid), r=['d_ra'], w=[f'd_beta{d}'])
            P.op('dve', lambda e: e.tensor_scalar(out=nbeta[:], in0=beta[:], scalar1=-1.0, scalar2=None, op0=ALU.mult), r=[f'd_beta{d}'], w=[f'd_nbeta{d}'])
            P.op('sp', lambda e: e.dma_start(out=ra[:], in_=baT[16 + d * 8:16 + (d + 1) * 8, :]), r=[('scr', 'dst_ba')], w=['d_ra'], dma=True)
            P.op('dve', lambda e: e.tensor_scalar(out=ra[:], in0=ra[:], scalar1=sc[:, 2 + d:3 + d], scalar2=None, op0=ALU.add), r=['d_ra', 'd_sc'], w=['d_ra'])
            P.op('act', lambda e: e.activation(out=rb[:], in_=ra[:], func=AF.Abs), r=['d_ra'], w=['d_rb'])
            P.op('act', lambda e: e.activation(out=rb[:], in_=rb[:], func=AF.Exp, scale=-1.0), r=['d_rb'], w=['d_rb'])
            P.op('act', lambda e: e.activation(out=rb[:], in_=rb[:], func=AF.Ln, bias=one1[0:8, :], scale=1.0), r=['d_rb', 'one1'], w=['d_rb'])
            P.op('dve', lambda e: e.scalar_tensor_tensor(out=rc[:], in0=ra[:], scalar=0.0, in1=rb[:], op0=ALU.max, op1=ALU.add), r=['d_ra', 'd_rb'], w=['d_rc'])
            P.op('dve', lambda e: e.tensor_scalar(out=rc[:], in0=rc[:], scalar1=nA[:, d:d + 1], scalar2=None, op0=ALU.mult), r=['d_rc', 'd_nA'], w=['d_rc'])
            cur, curk, nxt, nxtk = rc, 'd_rc', gc, f'd_gc{d}'
            for sft in (1, 2, 4, 8, 16, 32, 64):
                c3 = cur[:].rearrange("p (t n) -> p t n", n=128)
                n3 = nxt[:].rearrange("p (t n) -> p t n", n=128)
                P.op('act', lambda e: e.copy(out=nxt[:], in_=cur[:]), r=[curk], w=[nxtk])
                if d == 0:
                    P.op('dve', lambda e: e.tensor_tensor(out=n3[:, :, sft:], in0=c3[:, :, sft:], in1=c3[:, :, :128 - sft], op=ALU.add), r=[curk], w=[nxtk])
                else:
                    P.op('dve', lambda e: e.tensor_tensor(out=n3[:, :, :128 - sft], in0=c3[:, :, :128 - sft], in1=c3[:, :, sft:], op=ALU.add), r=[curk], w=[nxtk])
                cur, curk, nxt, nxtk = nxt, nxtk, cur, curk
            if cur is not gc:
                P.op('act', lambda e: e.copy(out=gc[:], in_=cur[:]), r=[curk], w=[f'd_gc{d}'])
        for t in range(NT):
            for d in range(2):
                for j in range(3):
                    src = rows[d][j]
                    col = d * 24 + j * 8
                    P.op('pe', lambda e: e.transpose(out=pG[:, col:col + 8], in_=src[:, t * 128:(t + 1) * 128], identity=ident[0:8, 0:8]),
                         r=[f'd_beta{d}', f'd_nbeta{d}', f'd_gc{d}', 'ident'], w=[kG])
            P.op('act', lambda e: e.copy(out=tokS[:, t, :], in_=pG[:, 0:48]), r=[kG], w=['tokS'])
        P.barrier()
        stop(1)
        es1.close()
        lm = sbs(esD, "d_lm", [128, 5, 4 * 128], F32)
        for j5 in range(5):
            P.op('sp', lambda e: e.dma_start(out=lm[:, j5, :], in_=dn_lmask[:, j5, :]), w=['d_lm'], dma=True)
        QKV = [sbs(esD, f"d_qkv{i}", [128, S], BF16) for i in range(3)]
        Ust = sbs(esD, "d_U", [128, NT, 2, 128], BF16)
        WTst = sbs(esD, "d_WT", [128, NT, 2, 128], BF16)
        ITst = sbs(esD, "d_IT", [128, NT, 2, 128], BF16)
        QDst = sbs(esD, "d_QD", [128, NT, 2, 128], BF16)
        KSst = sbs(esD, "d_KS", [128, NT, 2, 128], BF16)
        egl = sbs(esD, "d_egl", [128, NT, 2], F32)
        Oacc = sbs(esD, "d_Oacc", [128, NT, 128], F32)
        zsh = sbs(esD, "d_zsh", [128, NT, 128], BF16)
        oTd = sbs(esD, "d_oTd", [128, S], BF16)
        S32 = [sbs(esD, f"d_S32{d}", [128, 128], F32) for d in range(2)]
        Sbf = [sbs(esD, f"d_Sbf{d}", [128, 128], BF16) for d in range(2)]
        Vn = [sbs(esD, f"d_Vn{d}", [128, 128], BF16) for d in range(2)]
        ost = sbs(esD, "d_ost", [128, 8], F32)
        on = sbs(esD, "d_on", [128, 128], F32)
        onb = sbs(esD, "d_onb", [128, 128], BF16)
        G = 4
        QSC = float(128 ** -0.5)
        for h in dn_heads:
            esC = ExitStack()
            xpad = sbs(esC, f"d_xpad_{h}", [128, S + 4], F32)
            acc = sbs(esC, f"d_acc_{h}", [128, S], F32)
            cw = sbs(esC, f"d_cw_{h}", [128, 5], F32)
            rst = sbs(esC, f"d_rst_{h}", [128, 512], F32)
            sqb = sbs(esC, f"d_sqb_{h}", [128, 512], BF16)
            P.op('dve', lambda e: e.memset(xpad[:, 0:2], 0.0), w=['d_xpad'])
            P.op('dve', lambda e: e.memset(xpad[:, S + 2:S + 4], 0.0), w=['d_xpad'])
            for ci in range(3):
                ch = ci * 8 + h
                P.op('sp', lambda e: e.dma_start(out=cw[:], in_=conv_wT[ch * 128:(ch + 1) * 128, :]), w=['d_cw'], dma=True)
                P.op('pool', lambda e: e.dma_start(out=xpad[:, 2:S + 2], in_=qkvT[ch, :, :]), r=[('scr', 'dst_qkv')], w=['d_xpad'], dma=True)
                eng = 'dve'
                P.op(eng, lambda e: e.tensor_scalar(out=acc[:], in0=xpad[:, 0:S], scalar1=cw[:, 0:1], scalar2=None, op0=ALU.mult), r=['d_xpad', 'd_cw'], w=['d_acc'])
                for j in range(1, 5):
                    P.op(eng, lambda e: e.scalar_tensor_tensor(out=acc[:], in0=xpad[:, j:j + S], scalar=cw[:, j:j + 1], in1=acc[:], op0=ALU.mult, op1=ALU.add),
                         r=['d_xpad', 'd_cw', 'd_acc'], w=['d_acc'])
                P.op('act', lambda e: e.activation(out=acc[:], in_=acc[:], func=AF.Silu), r=['d_acc'], w=['d_acc'])
                if ci == 2:
                    P.op('dve', lambda e: e.tensor_copy(out=QKV[2][:], in_=acc[:]), r=['d_acc'], w=['d_qkv2'])
                else:
                    for g in range(8):
                        gs_ = slice(g * 512, (g + 1) * 512)
                        P.op('act', lambda e: e.activation(out=sqb[:], in_=acc[:, gs_], func=AF.Square), r=['d_acc'], w=['d_sqb'])
                        P.op('pe', lambda e: e.matmul(pM[:], lhsT=onesb[:], rhs=sqb[:], start=True, stop=True), r=['d_onesb', 'd_sqb'], w=[kM])
                        P.op('act', lambda e: e.activation(out=rst[:], in_=pM[:], func=AF.Sqrt, bias=eps_l2[:], scale=1.0), r=[kM, 'eps_l2'], w=['d_rst'])
                        P.op('dve', lambda e: e.reciprocal(out=rst[:], in_=rst[:]), r=['d_rst'], w=['d_rst'])
                        P.op('dve', lambda e: e.scalar_tensor_tensor(out=QKV[ci][:, gs_], in0=acc[:, gs_], scalar=(QSC if ci == 0 else 1.0), in1=rst[:], op0=ALU.mult, op1=ALU.mult),
                             r=['d_acc', 'd_rst'], w=[f'd_qkv{ci}'])
            Qt, Kt, Vch = QKV
            stop(2)
            P.barrier()
            esC.close()
            esW = ExitStack()
            Ktok = sbs(esW, f"d_Ktok_{h}", [128, 2, 128], BF16)
            Vtok = sbs(esW, f"d_Vtok_{h}", [128, 2, 128], BF16)
            Dg = sbs(esW, f"d_Dg_{h}", [128, G, 128], F32)
            tA = sbs(esW, f"d_tA_{h}", [128, G, 128], F32)
            tI = sbs(esW, f"d_tI_{h}", [128, G, 128], F32)
            EGB = sbs(esW, f"d_EGB_{h}", [128, G, 128], F32)
            A32 = sbs(esW, f"d_A32_{h}", [128, G, 128], F32)
            AT32 = sbs(esW, f"d_AT32_{h}", [128, G, 128], F32)
            ZY = sbs(esW, f"d_ZY_{h}", [128, G, 2, 128], F32)
            ZTYT = sbs(esW, f"d_ZTYT_{h}", [128, G, 2, 128], F32)
            Lb = sbs(esW, f"d_Lb_{h}", [128, G, 128], BF16)
            LTb = sbs(esW, f"d_LTb_{h}", [128, G, 128], BF16)
            Qb = sbs(esW, f"d_Qb_{h}", [128, G, 128], BF16)
            Rb = sbs(esW, f"d_Rb_{h}", [128, G, 128], BF16)
            TT = sbs(esW, f"d_TT_{h}", [128, G, 128], BF16)
            Tb = sbs(esW, f"d_Tb_{h}", [128, G, 128], BF16)
            ZYb = sbs(esW, f"d_ZYb_{h}", [128, G, 2, 128], BF16)
            ZTYTb = sbs(esW, f"d_ZTYTb_{h}", [128, G, 2, 128], BF16)
            Kbe = sbs(esW, f"d_Kbe_{h}", [128, G, 128], BF16)
            Vb = sbs(esW, f"d_Vb_{h}", [128, G, 128], BF16)
            egc = sbs(esW, f"d_egc_{h}", [128, G], F32)
            ebh = sbs(esW, f"d_ebh_{h}", [128, NT, 2], F32)
            for d_ in range(2):
                P.op('act', lambda e: e.activation(out=ebh[:, :, d_], in_=tokS[:, :, d_ * 24 + 16 + h], func=AF.Exp), r=['tokS'], w=['d_ebh'])
                P.op('dve', lambda e: e.tensor_tensor(out=ebh[:, :, d_], in0=ebh[:, :, d_], in1=tokS[:, :, d_ * 24 + h], op=ALU.mult), r=['d_ebh', 'tokS'], w=['d_ebh'])
            zs_v = zs[:, h * 128:(h + 1) * 128].rearrange("(t p) c -> p t c", p=128)
            for q4 in range(8):
                P.op('sp', lambda e: e.dma_start(out=zsh[:, q4 * 4:(q4 + 1) * 4, :], in_=zs_v[:, q4 * 4:(q4 + 1) * 4, :]),
                     r=[('scr', 'zs', t_, b_) for t_ in range(q4 * 4, q4 * 4 + 4) for b_ in range(2)], w=['d_zsh'], dma=True)
            for t0 in range(0, NT, 2):
                units = [(ti, d) for ti in range(2) for d in range(2)]
                fl = []
                for ti in range(2):
                    ts_ = slice((t0 + ti) * 128, (t0 + ti + 1) * 128)
                    fl.append(lambda e, ti=ti, ts_=ts_: e.transpose(out=psT[0][:, ti * 256:ti * 256 + 128], in_=Kt[:, ts_], identity=identb[:]))
                    fl.append(lambda e, ti=ti, ts_=ts_: e.transpose(out=psT[0][:, ti * 256 + 128:ti * 256 + 256], in_=Vch[:, ts_], identity=identb[:]))
                    fl.append(lambda e, ti=ti, ts_=ts_: e.matmul(pM[:, ti * 256:ti * 256 + 128], lhsT=Kt[:, ts_], rhs=Kt[:, ts_], start=True, stop=True))
                    fl.append(lambda e, ti=ti, ts_=ts_: e.matmul(pM[:, ti * 256 + 128:ti * 256 + 256], lhsT=Kt[:, ts_], rhs=Qt[:, ts_], start=True, stop=True))
                P.pe_group(fl, r=['d_qkv0', 'd_qkv1', 'd_qkv2', 'identb'], w=['psT0', kM])
                pT4 = psT[0][:].rearrange("p (a b n) -> p a b n", a=2, b=2)
                P.op('act', lambda e: e.copy(out=Ktok[:], in_=pT4[:, :, 0, :]), r=['psT0'], w=['d_Ktok'])
                P.op('act', lambda e: e.copy(out=Vtok[:], in_=pT4[:, :, 1, :]), r=['psT0'], w=['d_Vtok'])
                stop(31)
                for u, (ti, d) in enumerate(units):
                    t = t0 + ti
                    gcol = tokS[:, t, d * 24 + 16 + h:d * 24 + 17 + h]
                    P.op('dve', lambda e: e.tensor_scalar(out=Dg[:, u, :], in0=ident[:], scalar1=gcol, scalar2=None, op0=ALU.mult), r=['ident', 'tokS'], w=[('d_Dg', u)])
                P.pe_group([(lambda e, u=u: e.matmul(pG[:, u * 128:(u + 1) * 128], lhsT=ones_f[:], rhs=Dg[:, u, :], start=True, stop=True)) for u in range(G)],
                           r=['ones_f'] + [('d_Dg', u) for u in range(G)], w=[kG])
                stop(32)
                P.op('act', lambda e: e.copy(out=EGB[:], in_=pG[:].rearrange("p (u n) -> p u n", u=G)), r=[kG], w=['d_GBs'])
                for u, (ti, d) in enumerate(units):
                    t = t0 + ti
                    gcol = tokS[:, t, d * 24 + 16 + h:d * 24 + 17 + h]
                    P.op('dve', lambda e: e.scalar_tensor_tensor(out=tA[:, u, :], in0=EGB[:, u, :], scalar=gcol, in1=msk[:, 2 * d, :], op0=ALU.subtract, op1=ALU.max),
                         r=['d_GBs', 'tokS', 'd_msk'], w=[('d_tA', u)])
                    P.op('dve', lambda e: e.scalar_tensor_tensor(out=tI[:, u, :], in0=EGB[:, u, :], scalar=gcol, in1=msk[:, 2 * d + 1, :], op0=ALU.subtract, op1=ALU.min),
                         r=['d_GBs', 'tokS', 'd_msk'], w=[('d_tI', u)])
                kTA = [('d_tA', u) for u in range(G)]
                kTI = [('d_tI', u) for u in range(G)]
                P.op('act', lambda e: e.activation(out=tA[:], in_=tA[:], func=AF.Exp, scale=-1.0), r=kTA, w=kTA)
                P.op('act', lambda e: e.activation(out=tI[:], in_=tI[:], func=AF.Exp), r=kTI, w=kTI)
                P.op('act', lambda e: e.activation(out=EGB[:], in_=EGB[:], func=AF.Exp), r=['d_GBs'] + kTA + kTI, w=['d_GBs'])
                stop(33)
                for u, (ti, d) in enumerate(units):
                    t = t0 + ti
                    ts_ = slice(t * 128, (t + 1) * 128)
                    bcol = tokS[:, t, d * 24 + h:d * 24 + h + 1]
                    lastc = 127 if d == 0 else 0
                    P.op('dve', lambda e: e.scalar_tensor_tensor(out=A32[:, u, :], in0=pM[:, ti * 256:ti * 256 + 128], scalar=bcol, in1=tA[:, u, :], op0=ALU.mult, op1=ALU.mult),
                         r=[kM, 'tokS', ('d_tA', u)], w=[('d_A32', u)])
                P.pe_group([(lambda e, u=u: e.transpose(out=pX0[:, u * 128:(u + 1) * 128], in_=A32[:, u, :], identity=ident[:])) for u in range(G)],
                           r=[('d_A32', u) for u in range(G)] + ['ident'], w=[kX0])
                for u, (ti, d) in enumerate(units):
                    t = t0 + ti
                    ts_ = slice(t * 128, (t + 1) * 128)
                    bcol = tokS[:, t, d * 24 + h:d * 24 + h + 1]
                    lastc = 127 if d == 0 else 0
                    P.op('dve', lambda e: e.tensor_tensor(out=ITst[:, t, d, :], in0=pM[:, ti * 256 + 128:ti * 256 + 256], in1=tI[:, u, :], op=ALU.mult),
                         r=[kM, ('d_tI', u)], w=[('d_IT', t, d)])
                    P.op('dve', lambda e: e.tensor_tensor(out=QDst[:, t, d, :], in0=Qt[:, ts_], in1=EGB[:, u, :], op=ALU.mult), r=['d_qkv0', 'd_GBs'], w=[('d_QD', t, d)])
                    P.op('act', lambda e: e.copy(out=egl[:, t, d:d + 1], in_=EGB[:, u, lastc:lastc + 1]), r=['d_GBs'], w=[('d_egl', t, d)])
                    P.op('act', lambda e: e.activation(out=KSst[:, t, d, :], in_=Ktok[:, ti, :], func=AF.Copy, scale=tI[:, u, lastc:lastc + 1]),
                         r=['d_Ktok', ('d_tI', u)], w=[('d_KS', t, d)])
                    P.op('act', lambda e: e.activation(out=Kbe[:, u, :], in_=Ktok[:, ti, :], func=AF.Copy, scale=ebh[:, t, d:d + 1]),
                         r=['d_Ktok', 'd_ebh'], w=[('d_Kbe', u)])
                    P.op('act', lambda e: e.activation(out=Vb[:, u, :], in_=Vtok[:, ti, :], func=AF.Copy, scale=bcol), r=['d_Vtok', 'tokS'], w=[('d_Vb', u)])
                stop(34)
                kA = [('d_A32', u) for u in range(G)]
                v3 = lambda p_: p_[:].rearrange("p (u n) -> p u n", u=G)
                lmv = lambda j_: lm[:, j_, :].rearrange("p (u n) -> p u n", u=G)
                P.op('act', lambda e: e.copy(out=AT32[:], in_=v3(pX0)), r=[kX0], w=['d_AT32'])
                P.op('dve', lambda e: e.scalar_tensor_tensor(out=ZY[:, :, 0, :], in0=A32[:], scalar=-1.0, in1=lmv(0), op0=ALU.mult, op1=ALU.mult), r=kA + ['d_lm'], w=['d_ZY'])
                P.op('dve', lambda e: e.scalar_tensor_tensor(out=ZTYT[:, :, 0, :], in0=AT32[:], scalar=-1.0, in1=lmv(0), op0=ALU.mult, op1=ALU.mult), r=['d_AT32', 'd_lm'], w=['d_ZTYT'])
                P.op('pool', lambda e: e.tensor_tensor(out=ZY[:, :, 1, :], in0=ZY[:, :, 0, :], in1=lmv(4), op=ALU.add), r=['d_ZY', 'd_lm'], w=['d_ZY'])
                P.op('dve', lambda e: e.tensor_tensor(out=ZTYT[:, :, 1, :], in0=ZTYT[:, :, 0, :], in1=lmv(4), op=ALU.add), r=['d_ZTYT', 'd_lm'], w=['d_ZTYT'])
                stop(35)
                P.op('act', lambda e: e.copy(out=ZYb[:], in_=ZY[:]), r=['d_ZY'], w=['d_ZYb'])
                P.op('dve', lambda e: e.tensor_copy(out=ZTYTb[:], in_=ZTYT[:]), r=['d_ZTYT'], w=['d_ZTYTb'])
                P.pe_group([f_ for u in range(G) for f_ in (
                    (lambda e, u=u: e.matmul(pX0[:, u * 128:(u + 1) * 128], lhsT=ZTYTb[:, u, 0, :], rhs=ZYb[:, u, 0, :], start=True, stop=True)),
                    (lambda e, u=u: e.matmul(pX1[:, u * 128:(u + 1) * 128], lhsT=ZYb[:, u, 0, :], rhs=ZTYTb[:, u, 0, :], start=True, stop=True)))],
                    r=['d_ZYb', 'd_ZTYTb'], w=[kX0, kX1])
                P.op('act', lambda e: e.copy(out=ZYb[:, :, 0, :], in_=v3(pX0)), r=[kX0], w=['d_ZYb'])
                P.op('dve', lambda e: e.tensor_copy(out=ZTYTb[:, :, 0, :], in_=v3(pX1)), r=[kX1], w=['d_ZTYTb'])
                for lvl in (1, 2):
                    P.pe_group([f_ for u in range(G) for f_ in (
                        (lambda e, u=u: e.matmul((pX0 if u < 2 else pX1)[:, (u % 2) * 256:(u % 2) * 256 + 256], lhsT=ZTYTb[:, u, 0, :], rhs=ZYb[:, u, :, :].rearrange("p c n -> p (c n)"), start=True, stop=True)),
                        (lambda e, u=u: e.matmul((pZT if u < 2 else pU)[:, (u % 2) * 256:(u % 2) * 256 + 256], lhsT=ZYb[:, u, 0, :], rhs=ZTYTb[:, u, :, :].rearrange("p c n -> p (c n)"), start=True, stop=True)))],
                        r=['d_ZYb', 'd_ZTYTb'], w=[kX0, kX1, kZT, kU])
                    for hf, (px, kx, pz, kz) in enumerate(((pX0, kX0, pZT, kZT), (pX1, kX1, pU, kU))):
                        p4 = px[:].rearrange("p (u c n) -> p u c n", u=2, c=2)
                        z4 = pz[:].rearrange("p (u c n) -> p u c n", u=2, c=2)
                        hs2 = slice(hf * 2, hf * 2 + 2)
                        P.op('act', lambda e: e.copy(out=ZYb[:, hs2, 0, :], in_=p4[:, :, 0, :]), r=[kx], w=['d_ZYb'])
                        P.op('dve', lambda e: e.tensor_tensor(out=ZY[:, hs2, 1, :], in0=p4[:, :, 1, :], in1=ZY[:, hs2, 1, :], op=ALU.add), r=[kx, 'd_ZY'], w=['d_ZY'])
                        P.op('act', lambda e: e.copy(out=ZTYTb[:, hs2, 0, :], in_=z4[:, :, 0, :]), r=[kz], w=['d_ZTYTb'])
                        P.op('dve', lambda e: e.tensor_tensor(out=ZTYT[:, hs2, 1, :], in0=z4[:, :, 1, :], in1=ZTYT[:, hs2, 1, :], op=ALU.add), r=[kz, 'd_ZTYT'], w=['d_ZTYT'])
                    P.op('act', lambda e: e.copy(out=ZYb[:, :, 1, :], in_=ZY[:, :, 1, :]), r=['d_ZY'], w=['d_ZYb'])
                    P.op('dve', lambda e: e.tensor_copy(out=ZTYTb[:, :, 1, :], in_=ZTYT[:, :, 1, :]), r=['d_ZTYT'], w=['d_ZTYTb'])
                P.pe_group([f_ for u in range(G) for f_ in (
                    (lambda e, u=u: e.matmul(pX0[:, u * 128:(u + 1) * 128], lhsT=ZTYTb[:, u, 0, :], rhs=ZYb[:, u, 1, :], start=True, stop=True)),
                    (lambda e, u=u: e.matmul(pX1[:, u * 128:(u + 1) * 128], lhsT=ZYb[:, u, 0, :], rhs=ZTYTb[:, u, 1, :], start=True, stop=True)))],
                    r=['d_ZYb', 'd_ZTYTb'], w=[kX0, kX1])
                P.op('dve', lambda e: e.tensor_tensor(out=ZY[:, :, 1, :], in0=v3(pX0), in1=ZY[:, :, 1, :], op=ALU.add), r=[kX0, 'd_ZY'], w=['d_ZY'])
                P.op('dve', lambda e: e.tensor_tensor(out=ZTYT[:, :, 1, :], in0=v3(pX1), in1=ZTYT[:, :, 1, :], op=ALU.add), r=[kX1, 'd_ZTYT'], w=['d_ZTYT'])
                P.op('act', lambda e: e.copy(out=TT[:], in_=ZTYT[:, :, 1, :]), r=['d_ZTYT'], w=['d_TT'])
                P.op('dve', lambda e: e.tensor_copy(out=Tb[:], in_=ZY[:, :, 1, :]), r=['d_ZY'], w=['d_Tb'])
                for li in range(3):
                    last = (li == 2)
                    P.op('pool', lambda e: e.tensor_tensor(out=Lb[:], in0=A32[:], in1=lmv(1 + li), op=ALU.mult), r=kA + ['d_lm'], w=['d_Lb'])
                    if not last:
                        P.op('dve', lambda e: e.tensor_tensor(out=LTb[:], in0=AT32[:], in1=lmv(1 + li), op=ALU.mult), r=['d_AT32', 'd_lm'], w=['d_LTb'])
                    fl = [(lambda e, u=u: e.matmul(pX1[:, u * 128:(u + 1) * 128], lhsT=Lb[:, u, :], rhs=TT[:, u, :], start=True, stop=True)) for u in range(G)]
                    if not last:
                        fl += [(lambda e, u=u: e.matmul(pX0[:, u * 128:(u + 1) * 128], lhsT=LTb[:, u, :], rhs=Tb[:, u, :], start=True, stop=True)) for u in range(G)]
                    P.pe_group(fl, r=['d_Lb', 'd_TT'] + ([] if last else ['d_LTb', 'd_Tb']), w=[kX1] + ([] if last else [kX0]))
                    P.op('act', lambda e: e.copy(out=Rb[:], in_=v3(pX1)), r=[kX1], w=['d_Rb'])
                    if not last:
                        P.op('dve', lambda e: e.tensor_copy(out=Qb[:], in_=v3(pX0)), r=[kX0], w=['d_Qb'])
                    fl = [(lambda e, u=u: e.matmul(pU[:, u * 128:(u + 1) * 128], lhsT=Tb[:, u, :], rhs=Rb[:, u, :], start=True, stop=True)) for u in range(G)]
                    if not last:
                        fl += [(lambda e, u=u: e.matmul(pZT[:, u * 128:(u + 1) * 128], lhsT=TT[:, u, :], rhs=Qb[:, u, :], start=True, stop=True)) for u in range(G)]
                    P.pe_group(fl, r=['d_Tb', 'd_Rb'] + ([] if last else ['d_TT', 'd_Qb']), w=[kU] + ([] if last else [kZT]))
                    if not last:
                        P.op('dve', lambda e: e.tensor_tensor(out=ZTYT[:, :, 1, :], in0=ZTYT[:, :, 1, :], in1=v3(pU), op=ALU.subtract), r=[kU, 'd_ZTYT'], w=['d_ZTYT'])
                        P.op('dve', lambda e: e.tensor_tensor(out=ZY[:, :, 1, :], in0=ZY[:, :, 1, :], in1=v3(pZT), op=ALU.subtract), r=[kZT, 'd_ZY'], w=['d_ZY'])
                        P.op('act', lambda e: e.copy(out=TT[:], in_=ZTYT[:, :, 1, :]), r=['d_ZTYT'], w=['d_TT'])
                        P.op('act', lambda e: e.copy(out=Tb[:], in_=ZY[:, :, 1, :]), r=['d_ZY'], w=['d_Tb'])
                    else:
                        P.op('dve', lambda e: e.tensor_tensor(out=TT[:], in0=ZTYT[:, :, 1, :], in1=v3(pU), op=ALU.subtract), r=[kU, 'd_ZTYT'], w=['d_TT'])
                stop(36)
                P.pe_group([f_ for u in range(G) for f_ in (
                    (lambda e, u=u: e.matmul(pU[:, u * 128:(u + 1) * 128], lhsT=TT[:, u, :], rhs=Vb[:, u, :], start=True, stop=True)),
                    (lambda e, u=u: e.matmul(pG[:, u * 128:(u + 1) * 128], lhsT=Kbe[:, u, :], rhs=TT[:, u, :], start=True, stop=True)))],
                    r=['d_TT'] + [('d_Vb', u) for u in range(G)] + [('d_Kbe', u) for u in range(G)], w=[kU, kG])
                P.op('act', lambda e: e.copy(out=Ust[:, t0:t0 + 2, :, :].rearrange("p a b n -> p (a b) n"), in_=pU[:].rearrange("p (u n) -> p u n", u=G)), r=[kU], w=[('d_U', t0)])
                P.op('dve', lambda e: e.tensor_copy(out=WTst[:, t0:t0 + 2, :, :].rearrange("p a b n -> p (a b) n"), in_=pG[:].rearrange("p (u n) -> p u n", u=G)), r=[kG], w=[('d_WT', t0)])
                stop(3)
            P.barrier()
            stop(4)
            for d in range(2):
                P.op('dve', lambda e: e.memset(S32[d][:], 0.0), w=[f'd_S32{d}'])
                P.op('dve', lambda e: e.memset(Sbf[d][:], 0.0), w=[f'd_Sbf{d}'])
            for step in range(NT):
                tt = [step, NT - 1 - step]
                bank = [((pM, kM), (pX0, kX0), (pZT, kZT)), ((pG, kG), (pX1, kX1), (pU, kU))]
                P.pe_group([(lambda e, d=d: e.matmul(bank[d][0][0][:, 0:128], lhsT=WTst[:, tt[d], d, :], rhs=Sbf[d][:], start=True, stop=True)) for d in range(2)],
                           r=[('d_WT', (tt[0] // 2) * 2), ('d_WT', (tt[1] // 2) * 2), 'd_Sbf0', 'd_Sbf1'], w=[bank[0][0][1], bank[1][0][1]])
                for d in range(2):
                    t = tt[d]; t0 = (t // 2) * 2
                    (pW, kW), (pO, kO) = bank[d][0], bank[d][1]
                    P.op('dve', lambda e: e.tensor_tensor(out=Vn[d][:], in0=Ust[:, t, d, :], in1=pW[:, 0:128], op=ALU.subtract), r=[('d_U', t0), kW], w=[f'd_Vn{d}'])
                    P.op('pe', lambda e: e.matmul(pO[:, 0:128], lhsT=QDst[:, t, d, :], rhs=Sbf[d][:], start=True, stop=False), r=[('d_QD', t, d), f'd_Sbf{d}'], w=[kO])
                for d in range(2):
                    t = tt[d]
                    (pO, kO), (pD, kD) = bank[d][1], bank[d][2]
                    P.pe_group([lambda e: e.matmul(pO[:, 0:128], lhsT=ITst[:, t, d, :], rhs=Vn[d][:], start=False, stop=True),
                                lambda e: e.matmul(pD[:, 0:128], lhsT=KSst[:, t, d, :], rhs=Vn[d][:], start=True, stop=True)],
                               r=[('d_IT', t, d), ('d_KS', t, d), f'd_Vn{d}'], w=[kO, kD])
                for d in range(2):
                    t = tt[d]
                    (pO, kO), (pD, kD) = bank[d][1], bank[d][2]
                    P.op('dve', lambda e: e.scalar_tensor_tensor(out=Sbf[d][:], in0=S32[d][:], scalar=egl[:, t, d:d + 1], in1=pD[:, 0:128], op0=ALU.mult, op1=ALU.add),
                         r=[f'd_S32{d}', ('d_egl', t, d), kD], w=[f'd_Sbf{d}'])
                    P.op('dve', lambda e: e.scalar_tensor_tensor(out=S32[d][:], in0=S32[d][:], scalar=egl[:, t, d:d + 1], in1=pD[:, 0:128], op0=ALU.mult, op1=ALU.add),
                         r=[f'd_S32{d}', ('d_egl', t, d), kD], w=[f'd_S32{d}'])
                    if step < NT // 2:
                        P.op('act', lambda e: e.copy(out=Oacc[:, t, :], in_=pO[:, 0:128]), r=[kO], w=[('d_Oacc', t)])
                    else:
                        P.op('pool', lambda e: e.tensor_tensor(out=Oacc[:, t, :], in0=Oacc[:, t, :], in1=Oacc[:, t, :], op=ALU.add), r=[('d_Oacc', t)], w=[('d_Oacc', t)]) if False else \
                            P.op('dve', lambda e: e.tensor_tensor(out=Oacc[:, t, :], in0=pO[:, 0:128], in1=Oacc[:, t, :], op=ALU.add), r=[kO, ('d_Oacc', t)], w=[('d_Oacc', t)])
            stop(5)
            osq = [sbs(esW, f"d_osq{i_}_{h}", [128, 128], F32) for i_ in range(2)]
            ors = sbs(esW, f"d_ors_{h}", [128, 2, NT], F32)
            ont = [sbs(esW, f"d_ont{i_}_{h}", [128, 128], F32) for i_ in range(4)]
            onbt = [sbs(esW, f"d_onbt{i_}_{h}", [128, 128], BF16) for i_ in range(4)]
            kO_all = [('d_Oacc', t_) for t_ in range(NT)]
            P.op('dve', lambda e: e.memset(ors[:], 0.0), w=['d_ors'])
            for t in range(NT):
                P.op('act', lambda e: e.activation(out=osq[t % 2][:], in_=Oacc[:, t, :], func=AF.Square, accum_out=ors[:, 0, t:t + 1]), r=[('d_Oacc', t), 'd_ors'], w=[f'd_osq{t % 2}', ('d_ors_acc', t)])
            P.op('act', lambda e: e.activation(out=ors[:, 1, :], in_=ors[:, 0, :], func=AF.Sqrt, scale=1.0 / 128, bias=eps_l2[:]), r=['d_ors', 'eps_l2'] + [('d_ors_acc', t_) for t_ in range(NT)], w=['d_ors'])
            P.op('dve', lambda e: e.reciprocal(out=ors[:, 1, :], in_=ors[:, 1, :]), r=['d_ors'], w=['d_ors'])
            for t4 in range(0, NT, 4):
                for j in range(4):
                    t = t4 + j
                    P.op('dve', lambda e: e.scalar_tensor_tensor(out=ont[j][:], in0=Oacc[:, t, :], scalar=ors[:, 1, t:t + 1], in1=gain[:], op0=ALU.mult, op1=ALU.mult),
                         r=[('d_Oacc', t), 'd_ors', 'd_gain'], w=[f'd_ont{j}'])
                    P.op('pool', lambda e: e.tensor_tensor(out=onbt[j][:], in0=ont[j][:], in1=zsh[:, t, :], op=ALU.mult), r=[f'd_ont{j}', 'd_zsh'], w=[f'd_onbt{j}'])
                P.pe_group([(lambda e, j=j: e.transpose(out=psT[1][:, j * 128:(j + 1) * 128], in_=onbt[j][:], identity=identb[:])) for j in range(4)],
                           r=[f'd_onbt{j}' for j in range(4)] + ['identb'], w=['psT1'])
                P.op('act', lambda e: e.copy(out=oTd[:, t4 * 128:(t4 + 4) * 128], in_=psT[1][:]), r=['psT1'], w=['d_oTd'])
            fin.append(P.op('sp', lambda e: e.dma_start(out=oT_dn[h, :, :], in_=oTd[:]), r=['d_oTd'], w=[('scr', 'oT_dn', h)], dma=True))
            P.barrier()
            esW.close()
        P.barrier()
        esD.close()

    def phase_MG_MOE():
        esP = ExitStack()
        affTM = sbs(esP, "affTM", [128, NT, 16], F32)
        posm = sbs(esP, "posm", [128, NT, 16], F32)
        iot = sbs(esP, "iot", [128, 512], F32)
        bcs = sbs(esP, "bcs", [128, 4, D], F32)
        gfin = sbs(esP, "gfin", [128, D], F32)
        onesb = sbs(esP, "m_onesb", [128, 128], BF16)
        trib = sbs(esP, "trib", [128, 128], BF16)
        st2 = sbs(esP, "st2", [128, 8], F32)
        P.op('dve', lambda e: e.memset(onesb[:], 1.0), w=['m_onesb'])
        P.op('sp', lambda e: e.dma_start(out=iot[:], in_=iota512[:, :]), w=['iot'], dma=True)
        P.op('sp', lambda e: e.dma_start(out=gfin[:], in_=g_final[:, :]), w=['gfin'], dma=True)
        esH = ExitStack()
        h2b = sbs(esH, "h2b", [128, NT, D], BF16)
        esT = ExitStack()
        affT = sbs(esT, "affT", [16, S], F32)
        esG = ExitStack()
        es1 = ExitStack()
        mrow = sbs(es1, "g_mrow", [1, 4 * D], F32)
        trif = sbs(es1, "g_trif", [128, 128], F32)
        gff = sbs(es1, "g_gff", [128, D], F32)
        P.op('sp', lambda e: e.dma_start(out=mrow[:], in_=modrow[:, 2 * D:6 * D]), r=['mod'], w=['g_mrow'], dma=True)
        P.op('sp', lambda e: e.dma_start(out=trif[:], in_=tri_in[:, :]), w=['g_trif'], dma=True)
        P.op('dve', lambda e: e.tensor_copy(out=trib[:], in_=trif[:]), r=['g_trif'], w=['trib'])
        P.op('sp', lambda e: e.dma_start(out=gff[:], in_=g_ffn[:, :]), w=['g_gff'], dma=True)
        for j in range(8):
            src = j // 2
            dsti = {0: 0, 1: 2, 2: 1, 3: 3}[src]
            pt, pk = next_psA()
            P.op('pe', lambda e: e.matmul(pt[:], lhsT=ones_f[0:1, :], rhs=mrow[:, j * 512:(j + 1) * 512], start=True, stop=True), r=['ones_f', 'g_mrow'], w=[pk])
            P.op('act', lambda e: e.copy(out=bcs[:, dsti, (j % 2) * 512:(j % 2 + 1) * 512], in_=pt[:]), r=[pk], w=['bcs'])
        P.op('dve', lambda e: e.scalar_tensor_tensor(out=bcs[:, 1, :], in0=bcs[:, 1, :], scalar=1.0, in1=gff[:], op0=ALU.add, op1=ALU.mult), r=['bcs', 'g_gff'], w=['bcs'])
        P.barrier()
        es1.close()
        wod = sbs(esG, "g_wod", [128, 8, D], BF16)
        wom = sbs(esG, "g_wom", [128, 8, D], BF16)
        wou = sbs(esG, "g_wou", [128, 8, D], BF16)
        wr = sbs(esG, "g_wr", [128, 8, 16], F32)
        for wt_, src_, k_ in ((wod, w_o_dn, 'g_wod'), (wom, w_o_mla, 'g_wom'), (wou, w_out, 'g_wou')):
            v = src_.rearrange("(k p) n -> p k n", p=128)
            for kk in range(0, 8, 2):
                P.op('pool', lambda e: e.dma_start(out=wt_[:, kk:kk + 2, :], in_=v[:, kk:kk + 2, :]), w=[k_], dma=True)
        P.op('sp', lambda e: e.dma_start(out=wr[:], in_=w_router.rearrange("(k p) n -> p k n", p=128)), w=['g_wr'], dma=True)
        odn = [sbs(esG, f"g_odn{i}", [128, 8, 128], BF16) for i in range(2)]
        oml = [sbs(esG, f"g_oml{i}", [128, 8, 128], BF16) for i in range(2)]
        gst = [sbs(esG, f"g_gst{i}", [128, 2 * D], BF16) for i in range(2)]
        xt0 = sbs(esG, "g_xt0", [128, D], F32)
        xt = [xt0, xt0]
        m1 = sbs(esG, "g_m1", [128, D], F32)
        m2 = sbs(esG, "g_m2", [128, D], F32)
        mb = sbs(esG, "g_mb", [128, D], BF16)
        mT = sbs(esG, "g_mT", [128, 8, 128], BF16)
        x1t = [sbs(esG, f"g_x1t{i}", [128, D], F32) for i in range(2)]
        h2f = sbs(esG, "g_h2f", [128, D], F32)
        h2T = sbs(esG, "g_h2T", [128, 8, 128], F32)
        lg = sbs(esG, "g_lg", [128, 16], F32)
        for t in range(NT):
            i = t % 2
            ts_ = slice(t * 128, (t + 1) * 128)
            P.op('sp', lambda e: e.dma_start(out=odn[i][:], in_=oT_dn[:, :, ts_].rearrange("h p n -> p h n")), r=[('scr', 'oT_dn', h_) for h_ in range(8)], w=[f'g_odn{i}'], dma=True)
            P.op('sp', lambda e: e.dma_start(out=oml[i][:], in_=oT_mla[:, :, ts_].rearrange("h p n -> p h n")), r=[('scr', 'oT_mla', h_) for h_ in range(8)], w=[f'g_oml{i}'], dma=True)
            P.op('sp', lambda e: e.dma_start(out=gst[i][:], in_=gs[ts_, :]), r=[('scr', 'gs', t, b_) for b_ in range(4)], w=[f'g_gst{i}'], dma=True)
            P.op('sp', lambda e: e.dma_start(out=xt[i][:], in_=x[ts_, :]), w=['g_xt0'], dma=True)
            for br, (o_, ok_, w_, wk_) in enumerate(((odn[i], f'g_odn{i}', wod, 'g_wod'), (oml[i], f'g_oml{i}', wom, 'g_wom'))):
                for hc in range(2):
                    pt, pk = psA[br * 2 + hc], f'psA{br * 2 + hc}'
                    P.pe_group([(lambda e, k=k: e.matmul(pt[:], lhsT=o_[:, k, :], rhs=w_[:, k, hc * 512:(hc + 1) * 512], start=(k == 0), stop=(k == 7))) for k in range(8)], r=[ok_, wk_], w=[pk])
            for hc in range(2):
                hs_ = slice(hc * 512, (hc + 1) * 512)
                P.op('dve', lambda e: e.tensor_tensor(out=m1[:, hs_], in0=psA[hc][:], in1=gst[i][:, hs_], op=ALU.mult), r=[f'psA{hc}', f'g_gst{i}'], w=['g_m1'])
                P.op('dve', lambda e: e.tensor_tensor(out=m2[:, hs_], in0=psA[2 + hc][:], in1=gst[i][:, D + hc * 512:D + (hc + 1) * 512], op=ALU.mult), r=[f'psA{2 + hc}', f'g_gst{i}'], w=['g_m2'])
            P.op('pool', lambda e: e.tensor_tensor(out=mb[:], in0=m1[:], in1=m2[:], op=ALU.add), r=['g_m1', 'g_m2'], w=['g_mb'])
            for half in range(2):
                P.pe_group([(lambda e, k4=k4: e.transpose(out=psT[half][:, k4 * 128:(k4 + 1) * 128], in_=mb[:, (half * 4 + k4) * 128:(half * 4 + k4 + 1) * 128], identity=identb[:])) for k4 in range(4)], r=['g_mb', 'identb'], w=[f'psT{half}'])
                P.op('act', lambda e: e.copy(out=mT[:, half * 4:(half + 1) * 4, :], in_=psT[half][:].rearrange("p (k n) -> p k n", k=4)), r=[f'psT{half}'], w=['g_mT'])
            for hc in range(2):
                hs_ = slice(hc * 512, (hc + 1) * 512)
                P.pe_group([(lambda e, k=k: e.matmul(psS[hc][:], lhsT=mT[:, k, :], rhs=wou[:, k, hs_], start=(k == 0), stop=(k == 7))) for k in range(8)], r=['g_mT', 'g_wou'], w=[f'psS{hc}'])
                P.op('dve', lambda e: e.tensor_tensor(out=x1t[i][:, hs_], in0=psS[hc][:], in1=bcs[:, 0, hs_], op=ALU.mult), r=[f'psS{hc}', 'bcs'], w=[f'g_x1t{i}'])
            P.op('pool', lambda e: e.tensor_tensor(out=x1t[i][:], in0=x1t[i][:], in1=xt[i][:], op=ALU.add), r=[f'g_x1t{i}', 'g_xt0'], w=[f'g_x1t{i}'])
            fin.append(P.op('sp', lambda e: e.dma_start(out=x1s[ts_, :], in_=x1t[i][:]), r=[f'g_x1t{i}'], w=[('scr', 'x1s', t)], dma=True))
            P.op('dve', lambda e: e.memset(st2[:], 0.0), w=['st2'])
            P.op('act', lambda e: e.activation(out=m1[:], in_=x1t[i][:], func=AF.Square, accum_out=st2[:, 0:1]), r=[f'g_x1t{i}'], w=['g_m1', 'st2'])
            P.op('act', lambda e: e.activation(out=st2[:, 1:2], in_=st2[:, 0:1], func=AF.Sqrt, scale=1.0 / D, bias=epsb[:]), r=['st2', 'epsb'], w=['st2'])
            P.op('dve', lambda e: e.reciprocal(out=st2[:, 2:3], in_=st2[:, 1:2]), r=['st2'], w=['st2'])
            P.op('dve', lambda e: e.scalar_tensor_tensor(out=h2f[:], in0=x1t[i][:], scalar=st2[:, 2:3], in1=bcs[:, 1, :], op0=ALU.mult, op1=ALU.mult), r=[f'g_x1t{i}', 'st2', 'bcs'], w=['g_h2f'])
            P.op('pool', lambda e: e.tensor_tensor(out=h2f[:], in0=h2f[:], in1=bcs[:, 2, :], op=ALU.add), r=['g_h2f', 'bcs'], w=['g_h2f'])
            P.op('act', lambda e: e.copy(out=h2b[:, t, :], in_=h2f[:]), r=['g_h2f'], w=[('h2b', t)])
            for half in range(2):
                P.pe_group([(lambda e, k4=k4: e.transpose(out=psA[half][:, k4 * 128:(k4 + 1) * 128], in_=h2f[:, (half * 4 + k4) * 128:(half * 4 + k4 + 1) * 128], identity=ident[:])) for k4 in range(4)], r=['g_h2f', 'ident'], w=[f'psA{half}'])
                P.op('act', lambda e: e.copy(out=h2T[:, half * 4:(half + 1) * 4, :], in_=psA[half][:].rearrange("p (k n) -> p k n", k=4)), r=[f'psA{half}'], w=['g_h2T'])
            P.pe_group([(lambda e, k=k: e.matmul(psA[2][:, 0:16], lhsT=h2T[:, k, :], rhs=wr[:, k, :], start=(k == 0), stop=(k == 7))) for k in range(8)], r=['g_h2T', 'g_wr'], w=['psA2'])
            P.op('dve', lambda e: e.tensor_reduce(out=st2[:, 3:4], in_=psA[2][:, 0:16], axis=AX.X, op=ALU.max), r=['psA2'], w=['st2'])
            P.op('dve', lambda e: e.tensor_scalar(out=st2[:, 4:5], in0=st2[:, 3:4], scalar1=-1.0, scalar2=None, op0=ALU.mult), r=['st2'], w=['st2'])
            P.op('dve', lambda e: e.memset(st2[:, 5:6], 0.0), r=['st2'], w=['st2'])
            P.op('act', lambda e: e.activation(out=lg[:], in_=psA[2][:, 0:16], func=AF.Exp, bias=st2[:, 4:5], scale=1.0, accum_out=st2[:, 5:6]), r=['psA2', 'st2'], w=['g_lg', 'st2'])
            P.op('dve', lambda e: e.reciprocal(out=st2[:, 6:7], in_=st2[:, 5:6]), r=['st2'], w=['st2'])
            P.op('dve', lambda e: e.tensor_scalar(out=affTM[:, t, :], in0=lg[:], scalar1=st2[:, 6:7], scalar2=None, op0=ALU.mult), r=['g_lg', 'st2'], w=[('affTM', t)])
            P.op('pe', lambda e: e.transpose(out=psA[3][0:16, 0:128], in_=affTM[:, t, :], identity=ident[:]), r=[('affTM', t), 'ident'], w=['psA3'])
            P.op('act', lambda e: e.copy(out=affT[:, ts_], in_=psA[3][0:16, 0:128]), r=['psA3'], w=['affT'])
        P.barrier()
        esG.close()
        esE = ExitStack()
        es1 = ExitStack()
        cmpb = sbs(es1, "e_cmp", [16, S], F32)
        bis = sbs(es1, "e_bis", [16, 8], F32)
        maskT = sbs(es1, "e_maskT", [16, S], F32)
        P.op('dve', lambda e: e.memset(bis[:], 0.0), w=['e_bis'])
        P.op('dve', lambda e: e.memset(bis[:, 1:2], 1.0), r=['e_bis'], w=['e_bis'])
        for it in range(32):
            P.op('dve', lambda e: e.tensor_tensor(out=bis[:, 2:3], in0=bis[:, 0:1], in1=bis[:, 1:2], op=ALU.add), r=['e_bis'], w=['e_bis'])
            P.op('dve', lambda e: e.tensor_scalar(out=bis[:, 2:3], in0=bis[:, 2:3], scalar1=0.5, scalar2=None, op0=ALU.mult), r=['e_bis'], w=['e_bis'])
            P.op('dve', lambda e: e.tensor_scalar(out=cmpb[:], in0=affT[:], scalar1=bis[:, 2:3], scalar2=None, op0=ALU.is_ge), r=['affT', 'e_bis'], w=['e_cmp'])
            P.op('dve', lambda e: e.tensor_reduce(out=bis[:, 3:4], in_=cmpb[:], axis=AX.X, op=ALU.add), r=['e_cmp'], w=['e_bis'])
            P.op('dve', lambda e: e.tensor_scalar(out=bis[:, 4:5], in0=bis[:, 3:4], scalar1=511.5, scalar2=None, op0=ALU.is_ge), r=['e_bis'], w=['e_bis'])
            P.op('dve', lambda e: e.tensor_tensor(out=bis[:, 5:6], in0=bis[:, 2:3], in1=bis[:, 0:1], op=ALU.subtract), r=['e_bis'], w=['e_bis'])
            P.op('dve', lambda e: e.tensor_tensor(out=bis[:, 6:7], in0=bis[:, 1:2], in1=bis[:, 2:3], op=ALU.subtract), r=['e_bis'], w=['e_bis'])
            P.op('dve', lambda e: e.scalar_tensor_tensor(out=bis[:, 0:1], in0=bis[:, 5:6], scalar=bis[:, 4:5], in1=bis[:, 0:1], op0=ALU.mult, op1=ALU.add), r=['e_bis'], w=['e_bis'])
            P.op('dve', lambda e: e.scalar_tensor_tensor(out=bis[:, 1:2], in0=bis[:, 6:7], scalar=bis[:, 4:5], in1=bis[:, 2:3], op0=ALU.mult, op1=ALU.add), r=['e_bis'], w=['e_bis'])
        P.op('dve', lambda e: e.tensor_scalar(out=maskT[:], in0=affT[:], scalar1=bis[:, 0:1], scalar2=None, op0=ALU.is_ge), r=['affT', 'e_bis'], w=['e_maskT'])
        mk32 = sbs(es1, "e_mk32", [128, 16], F32)
        mkb = sbs(es1, "e_mkb", [128, 16], BF16)
        base = sbs(es1, "e_base", [128, 16], F32)
        ptmp = sbs(es1, "e_ptmp", [128, 16], F32)
        P.op('dve', lambda e: e.memset(base[:], 0.0), w=['e_base'])
        for t in range(NT):
            ts_ = slice(t * 128, (t + 1) * 128)
            P.op('pe', lambda e: e.transpose(out=psA[0][:, 0:16], in_=maskT[:, ts_], identity=ident[0:16, 0:16]), r=['e_maskT', 'ident'], w=['psA0'])
            P.op('act', lambda e: e.copy(out=mk32[:], in_=psA[0][:, 0:16]), r=['psA0'], w=['e_mk32'])
            P.op('dve', lambda e: e.tensor_copy(out=mkb[:], in_=mk32[:]), r=['e_mk32'], w=['e_mkb'])
            P.op('pe', lambda e: e.matmul(psA[1][:, 0:16], lhsT=trib[:], rhs=mkb[:], start=True, stop=True), r=['trib', 'e_mkb'], w=['psA1'])
            P.op('pe', lambda e: e.matmul(psA[2][:, 0:16], lhsT=onesb[:], rhs=mkb[:], start=True, stop=True), r=['m_onesb', 'e_mkb'], w=['psA2'])
            P.op('dve', lambda e: e.tensor_tensor(out=ptmp[:], in0=psA[1][:, 0:16], in1=base[:], op=ALU.add), r=['psA1', 'e_base'], w=['e_ptmp'])
            P.op('dve', lambda e: e.scalar_tensor_tensor(out=ptmp[:], in0=ptmp[:], scalar=1.0, in1=mk32[:], op0=ALU.add, op1=ALU.mult), r=['e_ptmp', 'e_mk32'], w=['e_ptmp'])
            P.op('dve', lambda e: e.tensor_scalar(out=posm[:, t, :], in0=ptmp[:], scalar1=-1.0, scalar2=None, op0=ALU.add), r=['e_ptmp'], w=['posm'])
            P.op('dve', lambda e: e.tensor_tensor(out=base[:], in0=psA[2][:, 0:16], in1=base[:], op=ALU.add), r=['psA2', 'e_base'], w=['e_base'])
        P.barrier()
        es1.close()
        esT.close()
        wg = sbs(esE, "e_wg", [128, 8, D], BF16)
        wu = sbs(esE, "e_wu", [128, 8, D], BF16)
        wd = sbs(esE, "e_wd", [128, 8, D], BF16)
        Sel = sbs(esE, "e_Sel", [128, NT, 512], BF16)
        xeT = sbs(esE, "e_xeT", [128, 8, 512], BF16)
        hid = sbs(esE, "e_hid", [128, 8, 512], BF16)
        sg0 = sbs(esE, "e_sg0", [128, 512], F32)
        sg = [sg0, sg0]
        yeb = sbs(esE, "e_yeb", [128, 4, D], BF16)
        stgw = [sbs(esE, f"e_stg{i}", [128, D], F32) for i in range(2)]
        stg_n = [0]
        for ex in range(16):
            for wt_, src_, k_ in ((wg, w_gate, 'e_wg'), (wu, w_up, 'e_wu'), (wd, w_down, 'e_wd')):
                v = src_[ex].rearrange("(k p) n -> p k n", p=128)
                for kk in range(8):
                    if kk % 2 == 0:
                        P.op('pool', lambda e: e.dma_start(out=wt_[:, kk, :], in_=v[:, kk, :]), w=[k_], dma=True)
                    else:
                        j = stg_n[0] % 2
                        stg_n[0] += 1
                        P.op('sp', lambda e: e.dma_start(out=stgw[j][:], in_=v[:, kk, :]), w=[f'e_stg{j}'], dma=True)
                        P.op('act', lambda e: e.copy(out=wt_[:, kk, :], in_=stgw[j][:]), r=[f'e_stg{j}'], w=[k_])
            for t in range(NT):
                P.op('dve', lambda e: e.tensor_scalar(out=Sel[:, t, :], in0=iot[:], scalar1=posm[:, t, ex:ex + 1], scalar2=None, op0=ALU.is_equal), r=['iot', 'posm'], w=[('e_Sel', t)])
            for k in range(8):
                pt, pk = psA[k % 4], f'psA{k % 4}'
                P.pe_group([(lambda e, t=t: e.matmul(pt[:], lhsT=h2b[:, t, k * 128:(k + 1) * 128], rhs=Sel[:, t, :], start=(t == 0), stop=(t == NT - 1))) for t in range(NT)],
                           r=[('h2b', t) for t in range(NT)] + [('e_Sel', t) for t in range(NT)], w=[pk])
                if k % 2 == 0:
                    P.op('act', lambda e: e.copy(out=xeT[:, k, :], in_=pt[:]), r=[pk], w=['e_xeT'])
                else:
                    P.op('dve', lambda e: e.tensor_copy(out=xeT[:, k, :], in_=pt[:]), r=[pk], w=['e_xeT'])
            for f in range(8):
                j = f % 2
                pg, pgk = psA[j * 2], f'psA{j * 2}'
                pu, puk = psA[j * 2 + 1], f'psA{j * 2 + 1}'
                P.pe_group([(lambda e, k=k: e.matmul(pg[:], lhsT=wg[:, k, f * 128:(f + 1) * 128], rhs=xeT[:, k, :], start=(k == 0), stop=(k == 7))) for k in range(8)], r=['e_wg', 'e_xeT'], w=[pgk])
                P.pe_group([(lambda e, k=k: e.matmul(pu[:], lhsT=wu[:, k, f * 128:(f + 1) * 128], rhs=xeT[:, k, :], start=(k == 0), stop=(k == 7))) for k in range(8)], r=['e_wu', 'e_xeT'], w=[puk])
                P.op('act', lambda e: e.activation(out=sg[j][:], in_=pg[:], func=AF.Silu), r=[pgk], w=['e_sg0'])
                P.op('dve', lambda e: e.tensor_tensor(out=hid[:, f, :], in0=pu[:], in1=sg[j][:], op=ALU.mult), r=[puk, 'e_sg0'], w=['e_hid'])
            for q in range(4):
                for hc in range(2):
                    ps_y, pyk = psS[hc], f'psS{hc}'
                    P.pe_group([(lambda e, f=f: e.matmul(ps_y[:], lhsT=hid[:, f, q * 128:(q + 1) * 128], rhs=wd[:, f, hc * 512:(hc + 1) * 512], start=(f == 0), stop=(f == 7))) for f in range(8)], r=['e_hid', 'e_wd'], w=[pyk])
                    if hc == 0:
                        P.op('act', lambda e: e.copy(out=yeb[:, q, 0:512], in_=ps_y[:]), r=[pyk], w=['e_yeb'])
                    else:
                        P.op('dve', lambda e: e.tensor_copy(out=yeb[:, q, 512:1024], in_=ps_y[:]), r=[pyk], w=['e_yeb'])
            fin.append(P.op('sp', lambda e: e.dma_start(out=ye_all[ex].rearrange("(q p) d -> p q d", p=128), in_=yeb[:]), r=['e_yeb'], w=[('scr', 'ye', ex)], dma=True))
        P.barrier()
        esE.close()
        esH.close()
        esF = ExitStack()
        yeA = sbs(esF, "f_yeA", [128, 16, 4, D], BF16)
        for ex in range(16):
            P.op('sp', lambda e: e.dma_start(out=yeA[:, ex, :, :], in_=ye_all[ex].rearrange("(q p) d -> p q d", p=128)), r=[('scr', 'ye', ex)], w=['f_yeA'], dma=True)
        Sg = [sbs(esF, f"f_Sg{i}", [128, 512], BF16) for i in range(2)]
        SgT = sbs(esF, "f_SgT", [128, 16, 4, 128], BF16)
        x1l = [sbs(esF, f"f_x1l{i}", [128, D], F32) for i in range(2)]
        ft = sbs(esF, "f_ft", [128, D], F32)
        ot = [sbs(esF, f"f_ot{i}", [128, D], F32) for i in range(2)]
        junk2 = sbs(esF, "f_junk", [128, D], F32)
        for t in range(NT // 2):
            i = t % 2
            ts_ = slice(t * 128, (t + 1) * 128)
            P.op('sp', lambda e: e.dma_start(out=x1l[i][:], in_=x1s[ts_, :]), r=[('scr', 'x1s', t)], w=[f'f_x1l{i}'], dma=True)
            for ex in range(16):
                j = ex % 2
                P.op('dve', lambda e: e.tensor_scalar(out=Sg[j][:], in0=iot[:], scalar1=posm[:, t, ex:ex + 1], scalar2=affTM[:, t, ex:ex + 1], op0=ALU.is_equal, op1=ALU.mult),
                     r=['iot', 'posm', ('affTM', t)], w=[f'f_Sg{j}'])
                P.pe_group([(lambda e, q=q: e.transpose(out=psT[j][:, q * 128:(q + 1) * 128], in_=Sg[j][:, q * 128:(q + 1) * 128], identity=identb[:])) for q in range(4)], r=[f'f_Sg{j}', 'identb'], w=[f'psT{j}'])
                if j == 0:
                    P.op('act', lambda e: e.copy(out=SgT[:, ex, :, :], in_=psT[j][:].rearrange("p (q n) -> p q n", q=4)), r=[f'psT{j}'], w=[('f_SgT', ex)])
                else:
                    P.op('pool', lambda e: e.tensor_copy(out=SgT[:, ex, :, :], in_=psT[j][:].rearrange("p (q n) -> p q n", q=4)), r=[f'psT{j}'], w=[('f_SgT', ex)]) if False else \
                        P.op('act', lambda e: e.copy(out=SgT[:, ex, :, :], in_=psT[j][:].rearrange("p (q n) -> p q n", q=4)), r=[f'psT{j}'], w=[('f_SgT', ex)])
            for hc in range(2):
                hs_ = slice(hc * 512, (hc + 1) * 512)
                pt, pk = psA[(t % 2) * 2 + hc], f'psA{(t % 2) * 2 + hc}'
                P.pe_group([(lambda e, ex=ex, q=q: e.matmul(pt[:], lhsT=SgT[:, ex, q, :], rhs=yeA[:, ex, q, hs_], start=(ex == 0 and q == 0), stop=(ex == 15 and q == 3)))
                            for ex in range(16) for q in range(4)], r=[('f_SgT', ex) for ex in range(16)] + ['f_yeA'], w=[pk])
                P.op('dve', lambda e: e.tensor_tensor(out=ft[:, hs_], in0=pt[:], in1=bcs[:, 3, hs_], op=ALU.mult), r=[pk, 'bcs'], w=['f_ft'])
            P.op('pool', lambda e: e.tensor_tensor(out=ft[:], in0=ft[:], in1=x1l[i][:], op=ALU.add), r=['f_ft', f'f_x1l{i}'], w=['f_ft'])
            P.op('dve', lambda e: e.memset(st2[:], 0.0), w=['st2'])
            P.op('act', lambda e: e.activation(out=junk2[:], in_=ft[:], func=AF.Square, accum_out=st2[:, 0:1]), r=['f_ft'], w=['f_junk', 'st2'])
            P.op('act', lambda e: e.activation(out=st2[:, 1:2], in_=st2[:, 0:1], func=AF.Sqrt, scale=1.0 / D, bias=epsb[:]), r=['st2', 'epsb'], w=['st2'])
            P.op('dve', lambda e: e.reciprocal(out=st2[:, 2:3], in_=st2[:, 1:2]), r=['st2'], w=['st2'])
            P.op('dve', lambda e: e.scalar_tensor_tensor(out=ot[i][:], in0=ft[:], scalar=st2[:, 2:3], in1=gfin[:], op0=ALU.mult, op1=ALU.mult), r=['f_ft', 'st2', 'gfin'], w=[f'f_ot{i}'])
            fin.append(P.op('sp', lambda e: e.dma_start(out=out[ts_, :], in_=ot[i][:]), r=[f'f_ot{i}'], dma=True))
        P.barrier()
        esF.close()
        esP.close()

    if 'A' in stages:
        phase_A()
    if 'MLA' in stages:
        phase_MLA()
    if 'DN' in stages:
        try:
            phase_DN()
        except _Stop:
            P.barrier()
    if 'MG' in stages:
        phase_MG_MOE()
    P.finish(fin)
    print("instructions:", P.n)
    return nc


_invf = (10000.0 ** (-np.arange(32, dtype=np.float32) / np.float32(32))).astype(np.float32)
INVF2 = np.concatenate([_invf, _invf])[:, None].astype(np.float32)
SGN2 = np.concatenate([-np.ones(32), np.ones(32)])[:, None].astype(np.float32)
SEL64 = np.zeros((128, 65), np.float32)
SEL64[:, 64] = 1.0
_xi = np.arange(128)[:, None]
_yi = np.arange(128)[None, :]
BIG = 1.0e4
DMASKS = np.stack([np.where(_xi > _yi, 0.0, BIG), np.where(_yi >= _xi, 0.0, -BIG),
                   np.where(_xi < _yi, 0.0, BIG), np.where(_yi <= _xi, 0.0, -BIG)], axis=1).astype(np.float32)
def _bd(s_):
    return ((_xi // s_) == (_yi // s_)).astype(np.float32)
DN_LMASK = np.ascontiguousarray(np.stack([np.tile(m_, (1, 4)) for m_ in
                                          (_bd(16), _bd(32) - _bd(16), _bd(64) - _bd(32), 1.0 - _bd(64), np.eye(128, dtype=np.float32))], axis=1))
IOTA512 = np.ascontiguousarray(np.broadcast_to(np.arange(512, dtype=np.float32)[None, :], (128, 512)))
TRI = (_xi < _yi).astype(np.float32)
IN_SPL = np.cumsum([3072, 1024, 16, 16, 512, 256, 64, 2048])


def prep_inputs(inputs, core):
    b, half = core // 2, core % 2
    f = lambda a: np.ascontiguousarray(a, dtype=np.float32)
    w_in = inputs['w_in'][0]
    qkv, z, bb, aa, cq, ckv, kr, g = np.split(w_in, IN_SPL[:-1], axis=1)
    xb = inputs['x'][b]
    posb = inputs['positions'][b]
    conv_w = inputs['conv_w'][0]
    a_log, dt_bias = inputs['a_log'][0], inputs['dt_bias'][0]
    if half == 1:
        xb = xb[::-1]
        posb = posb[::-1]
        conv_w = conv_w[::-1]
        bb = np.concatenate([bb[:, 8:16], bb[:, 0:8]], axis=1)
        aa = np.concatenate([aa[:, 8:16], aa[:, 0:8]], axis=1)
        a_log, dt_bias = a_log[::-1], dt_bias[::-1]
    swap = np.concatenate([np.arange(32, 64), np.arange(0, 32)])
    wuq_ = inputs['w_uq'][0].reshape(512, 8, 192)
    wukv_ = inputs['w_ukv'][0].reshape(256, 8, 256)
    m = {
        'x': f(xb),
        'cT': f(inputs['c'][b].reshape(8, 128).T),
        'w_mod': f(inputs['w_mod'][0]),
        'b_mod': f(inputs['b_mod'][0][None, :]),
        'g_mix': f(np.broadcast_to(inputs['g_mix'][0][None, :], (128, D))),
        'w_qkv': f(qkv), 'w_z': f(z), 'w_g': f(g),
        'w_ba': f(np.concatenate([bb, aa], axis=1)),
        'w_cq': f(cq), 'w_ckv': f(ckv),
        'w_kr2': f(np.concatenate([kr, kr[:, swap]], axis=1)),
        'q_gain': f(np.broadcast_to(inputs['q_gain'][0][None, :], (128, 512))),
        'kv_gain': f(np.broadcast_to(inputs['kv_gain'][0][None, :], (128, 256))),
        'identf': np.eye(128, dtype=np.float32),
        'posr': np.ascontiguousarray(np.broadcast_to(posb[None, :], (64, S)).astype(np.int32)),
        'invf2': INVF2, 'sgn2': SGN2, 'sel64': SEL64,
        'w_uqh': f(np.stack([np.concatenate([wuq_[:, h, 0:128], wuq_[:, h, 128:192], wuq_[:, h, 128:192][:, swap]], axis=1) for h in range(8)])),
        'w_ukvh': f(np.stack([wukv_[:, h, :] for h in range(8)])),
        'conv_wT': f(conv_w.T),
        'dn_sc': f(np.stack([a_log[0], a_log[1], dt_bias[0], dt_bias[1]], axis=1)),
        'dn_gain': f(np.broadcast_to(inputs['dn_o_gain'][0][None, :], (128, 128))),
        'dmasks': DMASKS, 'dn_lmask': DN_LMASK,
        'w_o_dn': f(inputs['w_o_dn'][0]), 'w_o_mla': f(inputs['w_o_mla'][0]), 'w_out': f(inputs['w_out'][0]),
        'g_ffn': f(np.broadcast_to(inputs['g_ffn'][0][None, :], (128, D))),
        'g_final': f(np.broadcast_to(inputs['g_final'][None, :], (128, D))),
        'w_router': f(inputs['w_router'][0]),
        'w_gate': f(inputs['w_gate'][0]), 'w_up': f(inputs['w_up'][0]), 'w_down': f(inputs['w_down'][0]),
        'iota512': IOTA512, 'tri_in': TRI,
    }
    return m


def kernel(**inputs):
    inputs = {k: np.asarray(v) for k, v in inputs.items()}
    nc = build()
    in_maps = [prep_inputs(inputs, c) for c in range(8)]
    res = run_bass_kernel_spmd(nc, in_maps, core_ids=list(range(8)))
    outp = np.zeros((4, S, D), np.float32)
    for c in range(8):
        b, half = c // 2, c % 2
        o_ = res.results[c]["out"]
        if half == 0:
            outp[b, 0:2048] = o_
        else:
            outp[b, 2048:4096] = o_[::-1]
    return outp
```
